# Optimizing a Trainium2 kernel written in Bass

```python
import jax, jax.numpy as jnp
from jax import lax
import numpy as np

D_MODEL = 2048
BATCH = 8
SEQ = 2048
DEPTH = 2

MEM_LEN = 256
GLA_HEADS = 4
GLA_DK = 128
GLA_DV = 256
GLA_GATE_RANK = 16
GLA_TAU = 16.0
GLA_CHUNK = 64
NSA_HEADS = 16
NSA_GROUPS = 4
NSA_HPG = NSA_HEADS // NSA_GROUPS
NSA_DH = 64
CMP_LEN = 32
CMP_STRIDE = 16
CMP_HIDDEN = 256
SEL_LEN = 64
SEL_TOPN = 8
WINDOW = 512
Q_BLOCK = 128
SEL_Q_BLOCK = 64
XA_HEADS = 4
XA_DH = 128
N_EXPERTS = 16
N_GROUPS = 4
EXPERTS_PER_GROUP = N_EXPERTS // N_GROUPS
TOP_K = 2
D_FF = 1536
MOE_BLOCK = 256
DN_ALPHA = float((2 * DEPTH) ** 0.25)
DN_BETA = float((8 * DEPTH) ** -0.25)
LN_EPS = 1e-5
NEG = -1e30
FORCE_BONUS = 1e6

GLA_QK = GLA_HEADS * GLA_DK
GLA_V = GLA_HEADS * GLA_DV
NSA_Q = NSA_HEADS * NSA_DH
NSA_KV = NSA_GROUPS * NSA_DH
IN_SPLITS = (GLA_QK, GLA_QK, GLA_V, GLA_V, GLA_GATE_RANK, NSA_Q, 6 * NSA_KV, 3 * NSA_HEADS, 2 * D_MODEL)
D_IN = sum(IN_SPLITS)
SPLIT_IDX = tuple(int(v) for v in np.cumsum(IN_SPLITS)[:-1])

kernel_name = "hybrid_gla_nsa_moe_deepnorm"


def layer_norm(x, g, b):
    xf = x.astype(jnp.float32)
    mu = jnp.mean(xf, -1, keepdims=True)
    var = jnp.mean(jnp.square(xf - mu), -1, keepdims=True)
    return ((xf - mu) * lax.rsqrt(var + LN_EPS)).astype(x.dtype) * g + b


def alibi_slopes(n):
    return 2.0 ** (-8.0 * jnp.arange(1, n + 1, dtype=jnp.float32) / n)


def gla_mixer(q, k, v, r, a_low, w_a2, b_a, norm_g):
    B, T, _ = q.shape
    H, C = GLA_HEADS, GLA_CHUNK
    N = T // C
    f32 = jnp.float32
    log_a = jax.nn.log_sigmoid((a_low @ w_a2 + b_a).astype(f32)) / GLA_TAU

    def chunks(t, d):
        return t.astype(f32).reshape(B, N, C, H, d).transpose(1, 0, 3, 2, 4)

    qc = chunks(q, GLA_DK) * (GLA_DK ** -0.5)
    kc = chunks(k, GLA_DK)
    vc = chunks(v, GLA_DV)
    gc = chunks(log_a, GLA_DK)
    causal = jnp.tril(jnp.ones((C, C), dtype=bool))[:, :, None]

    def step(S, inp):
        qi, ki, vi, gi = inp
        b = jnp.cumsum(gi, axis=2)
        b_last = b[:, :, -1, :]
        o_inter = jnp.einsum('bhtd,bhdv->bhtv', qi * jnp.exp(b), S)
        rel = jnp.exp(jnp.where(causal, b[:, :, :, None, :] - b[:, :, None, :, :], -jnp.inf))
        att = jnp.einsum('bhtd,bhsd,bhtsd->bhts', qi, ki, rel)
        o_intra = jnp.einsum('bhts,bhsv->bhtv', att, vi)
        S = jnp.exp(b_last)[..., None] * S + jnp.einsum(
            'bhsd,bhsv->bhdv', ki * jnp.exp(b_last[:, :, None, :] - b), vi)
        return S, o_inter + o_intra

    S0 = jnp.zeros((B, H, GLA_DK, GLA_DV), f32)
    _, o = lax.scan(step, S0, (qc, kc, vc, gc))
    o = o.transpose(1, 0, 3, 2, 4).reshape(B, T, H, GLA_DV)
    mu = jnp.mean(o, -1, keepdims=True)
    var = jnp.mean(jnp.square(o - mu), -1, keepdims=True)
    o = (o - mu) * lax.rsqrt(var + LN_EPS) * norm_g.reshape(H, GLA_DV)
    return (o.reshape(B, T, GLA_V) * jax.nn.silu(r.astype(f32))).astype(q.dtype)


def nsa_mixer(q, kv, gate_logits, cmp_pe, cmp_w1, cmp_w2):
    B, T, _ = q.shape
    G, HPG, DH = NSA_GROUPS, NSA_HPG, NSA_DH
    f32 = jnp.float32
    q = q.astype(f32).reshape(B, T, G, HPG, DH).transpose(0, 2, 3, 1, 4) * (DH ** -0.5)
    kv = kv.astype(f32).reshape(B, T, 6, G, DH).transpose(2, 0, 3, 1, 4)
    k_c, v_c, k_s, v_s, k_w, v_w = kv[0], kv[1], kv[2], kv[3], kv[4], kv[5]
    slope = alibi_slopes(NSA_HEADS).reshape(G, HPG)[None, :, :, None, None]
    pos = jnp.arange(T)

    n_cmp = (T - CMP_LEN) // CMP_STRIDE + 1
    blk_start = jnp.arange(n_cmp) * CMP_STRIDE
    blk_idx = blk_start[:, None] + jnp.arange(CMP_LEN)[None, :]

    def compress(t, pe, w1, w2):
        blocks = t[:, :, blk_idx, :] + pe
        flat = blocks.reshape(B, G, n_cmp, CMP_LEN * DH)
        return jax.nn.gelu(flat @ w1) @ w2

    kc = compress(k_c, cmp_pe[0], cmp_w1[0], cmp_w2[0])
    vc = compress(v_c, cmp_pe[1], cmp_w1[1], cmp_w2[1])
    blk_end = blk_start + CMP_LEN - 1
    blk_center = blk_start.astype(f32) + 0.5 * (CMP_LEN - 1)
    s_c = jnp.einsum('bghtd,bgnd->bghtn', q, kc)
    s_c = s_c - slope * jnp.abs(pos[:, None].astype(f32) - blk_center[None, :])
    mask_c = blk_end[None, :] <= pos[:, None]
    p_c = jax.nn.softmax(jnp.where(mask_c, s_c, NEG), axis=-1) * mask_c
    o_cmp = jnp.einsum('bghtn,bgnd->bghtd', p_c, vc)

    n_sel = T // SEL_LEN
    sel_start = jnp.arange(n_sel) * SEL_LEN
    overlap = ((blk_start[:, None] < sel_start[None, :] + SEL_LEN)
               & (blk_start[:, None] + CMP_LEN > sel_start[None, :])).astype(f32)
    imp = jnp.einsum('bghtn,nj->bgtj', p_c, overlap)
    cur = pos // SEL_LEN
    jj = jnp.arange(n_sel)
    forced = (jj[None, :] == 0) | (jj[None, :] == cur[:, None]) | (jj[None, :] == cur[:, None] - 1)
    valid_blk = sel_start[None, :] <= pos[:, None]
    score = jnp.where(valid_blk, imp + jnp.where(forced, FORCE_BONUS, 0.0), NEG)
    top_n = min(SEL_TOPN, n_sel)
    top_val, sel_idx = lax.top_k(score, top_n)
    sel_ok = top_val > 0.5 * NEG
    ks_blk = k_s.reshape(B, G, n_sel, SEL_LEN, DH)
    vs_blk = v_s.reshape(B, G, n_sel, SEL_LEN, DH)
    b_ix = jnp.arange(B)[:, None, None, None]
    g_ix = jnp.arange(G)[None, :, None, None]

    def sel_block(i):
        t0 = i * SEL_Q_BLOCK
        qi = lax.dynamic_slice_in_dim(q, t0, SEL_Q_BLOCK, axis=3)
        idx_i = lax.dynamic_slice_in_dim(sel_idx, t0, SEL_Q_BLOCK, axis=2)
        ok_i = lax.dynamic_slice_in_dim(sel_ok, t0, SEL_Q_BLOCK, axis=2)
        kg = ks_blk[b_ix, g_ix, idx_i]
        vg = vs_blk[b_ix, g_ix, idx_i]
        tq = t0 + jnp.arange(SEL_Q_BLOCK)
        kpos = idx_i[..., None] * SEL_LEN + jnp.arange(SEL_LEN)
        dist = (tq[:, None, None] - kpos).astype(f32)
        s = jnp.einsum('bghqd,bgqnkd->bghqnk', qi, kg) - slope[..., None] * dist[:, :, None]
        mask = (ok_i[..., None] & (kpos <= tq[:, None, None]))[:, :, None]
        s = jnp.where(mask, s, NEG).reshape(B, G, HPG, SEL_Q_BLOCK, top_n * SEL_LEN)
        p = jax.nn.softmax(s, axis=-1)
        return jnp.einsum('bghqm,bgqmd->bghqd', p, vg.reshape(B, G, SEL_Q_BLOCK, top_n * SEL_LEN, DH))

    o_sel = lax.map(sel_block, jnp.arange(T // SEL_Q_BLOCK))
    o_sel = jnp.moveaxis(o_sel, 0, 3).reshape(B, G, HPG, T, DH)

    zpad = jnp.zeros((B, G, WINDOW, DH), f32)
    kw_pad = jnp.concatenate([zpad, k_w], axis=2)
    vw_pad = jnp.concatenate([zpad, v_w], axis=2)
    span = WINDOW + Q_BLOCK

    def win_block(i):
        t0 = i * Q_BLOCK
        qi = lax.dynamic_slice_in_dim(q, t0, Q_BLOCK, axis=3)
        kb = lax.dynamic_slice_in_dim(kw_pad, t0, span, axis=2)
        vb = lax.dynamic_slice_in_dim(vw_pad, t0, span, axis=2)
        tq = t0 + jnp.arange(Q_BLOCK)
        tk = t0 - WINDOW + jnp.arange(span)
        d = tq[:, None] - tk[None, :]
        mask = (d >= 0) & (d < WINDOW) & (tk[None, :] >= 0)
        s = jnp.einsum('bghqd,bgkd->bghqk', qi, kb) - slope * d.astype(f32)
        p = jax.nn.softmax(jnp.where(mask, s, NEG), axis=-1)
        return jnp.einsum('bghqk,bgkd->bghqd', p, vb)

    o_win = lax.map(win_block, jnp.arange(T // Q_BLOCK))
    o_win = jnp.moveaxis(o_win, 0, 3).reshape(B, G, HPG, T, DH)

    gt = jax.nn.sigmoid(gate_logits.astype(f32)).reshape(B, T, G, HPG, 3).transpose(0, 2, 3, 1, 4)
    o = gt[..., 0:1] * o_cmp + gt[..., 1:2] * o_sel + gt[..., 2:3] * o_win
    return o.transpose(0, 3, 1, 2, 4).reshape(B, T, NSA_Q).astype(gate_logits.dtype)


def memory_xattn(x, mem, wq, wkv, wo):
    B, T, _ = x.shape
    M = mem.shape[1]
    q = (x @ wq).reshape(B, T, XA_HEADS, XA_DH) * (XA_DH ** -0.5)
    kv = (mem @ wkv).reshape(B, M, 2, XA_HEADS, XA_DH)
    s = jnp.einsum('bthd,bmhd->bhtm', q, kv[:, :, 0]).astype(jnp.float32)
    p = jax.nn.softmax(s, axis=-1).astype(x.dtype)
    o = jnp.einsum('bhtm,bmhd->bthd', p, kv[:, :, 1]).reshape(B, T, XA_HEADS * XA_DH)
    return o @ wo


def moe_ffn(x, router_w, router_b, w_in, w_down):
    B, T, D = x.shape
    N = B * T
    xt = x.reshape(N, D)
    logits = (xt @ router_w).astype(jnp.float32)
    grp = (logits + router_b.astype(jnp.float32)).reshape(N, N_GROUPS, EXPERTS_PER_GROUP)
    grp_score = lax.top_k(grp, TOP_K)[0].sum(-1)
    g_best = jnp.argmax(grp_score, axis=-1)
    in_grp = grp[jnp.arange(N), g_best]
    _, local = lax.top_k(in_grp, TOP_K)
    expert = g_best[:, None] * EXPERTS_PER_GROUP + local
    gate = jax.nn.softmax(jnp.take_along_axis(logits, expert, axis=1), axis=-1)

    M = N * TOP_K
    e_flat = expert.reshape(M)
    tok = jnp.repeat(jnp.arange(N), TOP_K)
    order = jnp.argsort(e_flat)
    e_sorted = e_flat[order]
    tok_sorted = tok[order]
    w_sorted = gate.reshape(M)[order]
    counts = jnp.bincount(e_flat, length=N_EXPERTS)
    padded = (counts + MOE_BLOCK - 1) // MOE_BLOCK * MOE_BLOCK
    pad_end = jnp.cumsum(padded)
    pad_start = pad_end - padded
    raw_start = jnp.cumsum(counts) - counts
    dest = pad_start[e_sorted] + jnp.arange(M) - raw_start[e_sorted]
    n_blocks = -(-M // MOE_BLOCK) + N_EXPERTS
    P = n_blocks * MOE_BLOCK
    buf = jnp.zeros((P, D), x.dtype).at[dest].set(xt[tok_sorted])
    blk_expert = jnp.clip(jnp.searchsorted(pad_end, jnp.arange(n_blocks) * MOE_BLOCK, side='right'),
                          0, N_EXPERTS - 1)

    def expert_block(args):
        xb, e = args
        a, u = jnp.split(xb @ w_in[e], 2, axis=-1)
        return (jax.nn.silu(a) * u) @ w_down[e]

    y_buf = lax.map(expert_block, (buf.reshape(n_blocks, MOE_BLOCK, D), blk_expert)).reshape(P, D)
    y = y_buf[dest] * w_sorted[:, None].astype(x.dtype)
    return jnp.zeros((N, D), x.dtype).at[tok_sorted].add(y).reshape(B, T, D)


def setup_inputs(seed: int = 0) -> dict:
    key = jax.random.key(seed)
    ks = jax.random.split(key, 25)
    L, D, E = DEPTH, D_MODEL, N_EXPERTS
    f32 = jnp.float32

    def nrm(k, shape, fan_in, scale=1.0):
        return jax.random.normal(k, shape, f32) * (scale * fan_in ** -0.5)

    def gain(k, shape):
        return 1.0 + 0.02 * jax.random.normal(k, shape, f32)

    def small(k, shape, s=0.01):
        return s * jax.random.normal(k, shape, f32)

    return {
        "x": jax.random.normal(ks[0], (BATCH, SEQ, D), f32),
        "mem": jax.random.normal(ks[1], (BATCH, MEM_LEN, D), f32),
        "w_in": nrm(ks[2], (L, D, D_IN), D),
        "gla_w_a2": nrm(ks[3], (L, GLA_GATE_RANK, GLA_QK), GLA_GATE_RANK),
        "gla_b_a": small(ks[4], (L, GLA_QK), 0.1),
        "gla_norm_g": gain(ks[5], (L, GLA_V)),
        "nsa_cmp_pe": small(ks[6], (L, 2, CMP_LEN, NSA_DH), 0.02),
        "nsa_cmp_w1": nrm(ks[7], (L, 2, CMP_LEN * NSA_DH, CMP_HIDDEN), CMP_LEN * NSA_DH),
        "nsa_cmp_w2": nrm(ks[8], (L, 2, CMP_HIDDEN, NSA_DH), CMP_HIDDEN),
        "w_branch_gla": nrm(ks[9], (L, GLA_V, D), GLA_V),
        "w_branch_nsa": nrm(ks[10], (L, NSA_Q, D), NSA_Q),
        "w_out": nrm(ks[11], (L, D, D), D, DN_BETA),
        "ln_mix_g": gain(ks[12], (L, D)),
        "ln_mix_b": small(ks[13], (L, D)),
        "xa_wq": nrm(ks[14], (L, D, XA_HEADS * XA_DH), D),
        "xa_wkv": nrm(ks[15], (L, D, 2 * XA_HEADS * XA_DH), D),
        "xa_wo": nrm(ks[16], (L, XA_HEADS * XA_DH, D), XA_HEADS * XA_DH, DN_BETA),
        "ln_xa_g": gain(ks[17], (L, D)),
        "ln_xa_b": small(ks[18], (L, D)),
        "router_w": nrm(ks[19], (D, E), D),
        "router_b": small(ks[20], (E,)),
        "moe_w_in": nrm(ks[21], (L, E, D, 2 * D_FF), D),
        "moe_w_down": nrm(ks[22], (L, E, D_FF, D), D_FF, DN_BETA),
        "ln_ffn_g": gain(ks[23], (L, D)),
        "ln_ffn_b": small(ks[24], (L, D)),
    }


def reference(x, mem, w_in, gla_w_a2, gla_b_a, gla_norm_g, nsa_cmp_pe, nsa_cmp_w1, nsa_cmp_w2,
              w_branch_gla, w_branch_nsa, w_out, ln_mix_g, ln_mix_b, xa_wq, xa_wkv, xa_wo,
              ln_xa_g, ln_xa_b, router_w, router_b, moe_w_in, moe_w_down, ln_ffn_g, ln_ffn_b):
    D = D_MODEL
    for l in range(DEPTH):
        h = x @ w_in[l]
        g_q, g_k, g_v, g_r, g_a, n_q, n_kv, n_g, m_g = jnp.split(h, SPLIT_IDX, axis=-1)
        o_gla = gla_mixer(g_q, g_k, g_v, g_r, g_a, gla_w_a2[l], gla_b_a[l], gla_norm_g[l])
        o_nsa = nsa_mixer(n_q, n_kv, n_g, nsa_cmp_pe[l], nsa_cmp_w1[l], nsa_cmp_w2[l])
        gates = jax.nn.sigmoid(m_g)
        merged = gates[..., :D] * (o_gla @ w_branch_gla[l]) + gates[..., D:] * (o_nsa @ w_branch_nsa[l])
        x = layer_norm(DN_ALPHA * x + merged @ w_out[l], ln_mix_g[l], ln_mix_b[l])
        x = layer_norm(DN_ALPHA * x + memory_xattn(x, mem, xa_wq[l], xa_wkv[l], xa_wo[l]),
                       ln_xa_g[l], ln_xa_b[l])
        x = layer_norm(DN_ALPHA * x + moe_ffn(x, router_w, router_b, moe_w_in[l], moe_w_down[l]),
                       ln_ffn_g[l], ln_ffn_b[l])
    return x
```

```python
import numpy as np
from contextlib import ExitStack
import concourse.bass as bass
import concourse.mybir as mybir
from concourse.bass_utils import run_bass_kernel_spmd

F32 = mybir.dt.float32
BF16 = mybir.dt.bfloat16
AF = mybir.ActivationFunctionType
ALU = mybir.AluOpType
AX = mybir.AxisListType

T = 2048
D = 2048
NT = 16
DEPTH = 2
DN_ALPHA = float((2 * DEPTH) ** 0.25)
LN_EPS = 1e-5
D_IN = 9792
C_GQ, C_GK, C_GV, C_GR, C_GA, C_NQ, C_NKV, C_NG, C_MG = 0, 512, 1024, 2048, 3072, 3088, 4112, 5648, 5696
R_GQ, R_GK, R_GA, R_NQ, R_KC, R_VC, R_KS, R_KW, R_MG, NFM = 0, 512, 1024, 1056, 2080, 2336, 2592, 2848, 3104, 7200
TC_GK, TC_GV, TC_GR, TC_VS, TC_VW, TC_NG, NTM = 0, 512, 1536, 2560, 2816, 3072, 3120
NEGB = -30000.0


class Sch:
    def __init__(self, nc, es):
        self.nc = nc
        self.eng = {'pe': nc.tensor, 'act': nc.scalar, 'dve': nc.vector, 'pool': nc.gpsimd, 'sp': nc.sync}
        self.sem = {}
        self.cnt = {}
        for e in ('pe', 'act', 'dve', 'pool'):
            self.sem[e] = es.enter_context(nc.semaphore('s_' + e))
            self.cnt[e] = 0
        self.NS = 8
        for q in ('sp', 'pool'):
            for i in range(self.NS):
                k = ('dma', q, i)
                self.sem[k] = es.enter_context(nc.semaphore('d_%s%d' % (q, i)))
                self.cnt[k] = 0
        self.dma_i = {'sp': 0, 'pool': 0}
        self.waited = {e: {} for e in self.eng}
        self.lw = {}
        self.rd = {}
        self.nops = 0

    def _wait(self, e, tok):
        s, v = tok
        if self.waited[e].get(s, 0) >= v:
            return
        self.waited[e][s] = v
        self.eng[e].wait_ge(self.sem[s], v)

    def op(self, e, fn, r=(), w=(), dma=False, inc=True):
        deps = []
        for k in r:
            if k in self.lw:
                deps.append(self.lw[k])
        for k in w:
            if k in self.lw:
                deps.append(self.lw[k])
            deps.extend(self.rd.get(k, {}).values())
        if dma:
            i = self.dma_i[e]
            self.dma_i[e] += 1
            s = ('dma', e, i % self.NS)
            if self.cnt[s] > 0:
                deps.append((s, self.cnt[s]))
            self.cnt[s] += 16
            tok = (s, self.cnt[s])
        else:
            s = e
            if inc:
                self.cnt[s] += 1
                tok = (s, self.cnt[s])
            else:
                tok = (s, self.cnt[s] + 1)
        for d in deps:
            if d[0] == 'pe' and e == 'pe' and not dma:
                continue
            self._wait(e, d)
        ins = fn(self.eng[e])
        if dma:
            ins.then_inc(self.sem[s], 16)
        elif inc:
            ins.then_inc(self.sem[s], 1)
        for k in w:
            self.lw[k] = tok
            self.rd[k] = {}
        for k in r:
            self.rd.setdefault(k, {})[tok[0]] = tok
        self.nops += 1
        return tok

    def barrier(self):
        for e in self.eng:
            for s, c in self.cnt.items():
                if c > 0:
                    self._wait(e, (s, c))
        self.lw.clear()
        self.rd.clear()


def make_consts():
    c = {}
    i = np.arange(128)
    c['ident'] = np.eye(128, dtype=np.float32)
    c['um'] = (-(1.0 / 16.0) * (i[:, None] <= i[None, :])).astype(np.float32)
    c['um2'] = (-(1.0 / 16.0) * (i[:, None] > i[None, :])).astype(np.float32)
    c['caus4'] = np.tile((i[:, None] <= i[None, :]).astype(np.float32), (1, 4))
    slopes = 2.0 ** (-8.0 * np.arange(1, 17) / 16.0)
    rel = -(slopes[None, :, None]) * (i[None, None, :] - i[:, None, None]).astype(np.float64)
    c['rel_mid'] = rel.astype(np.float32).reshape(128, 16 * 128)
    dt = np.arange(17)
    c['dconst'] = np.broadcast_to((-slopes[:, None] * 128.0 * dt[None, :])[None], (128, 16, 17)).astype(np.float32).reshape(128, 16 * 17).copy()
    n = np.arange(128)
    cb = slopes[None, :, None] * (16.0 * n[:, None, None] + 15.5) - slopes[None, :, None] * 128.0 * np.arange(16)[None, None, :]
    cb = slopes[None, :, None] * (15.0 * n[:, None, None] + 15.5) - slopes[None, :, None] * 128.0 * np.arange(16)[None, None, :]
    c['cmp_pb'] = cb.astype(np.float32).reshape(128, 256)
    c['cdiag'] = np.where(i[None, :] >= i[:, None], 0.0, NEGB).astype(np.float32)
    c['cfar'] = np.where(i[None, :] < i[:, None], 0.0, NEGB).astype(np.float32)
    tq = (np.arange(16)[:, None] * 128 + i[None, :])
    valid = (16 * n[:, None, None] + 31) <= tq[None]
    valid[127] = False
    c['cmp_mask'] = np.where(valid, 0.0, NEGB).astype(np.float32).reshape(128, 2048)
    bs = 16 * n
    ss = 64 * np.arange(32)
    ov = ((bs[:, None] < ss[None, :] + 64) & (bs[:, None] + 32 > ss[None, :])).astype(np.float32)
    ov[127] = 0
    c['overlap'] = ov
    t = np.arange(T)
    cur = t // 64
    jj = np.arange(32)
    forced = (jj[None, :] == 0) | (jj[None, :] == cur[:, None]) | (jj[None, :] == cur[:, None] - 1)
    validb = (64 * jj[None, :]) <= t[:, None]
    c['selc'] = np.where(validb, np.where(forced, 1e6, 0.0), -1e30).astype(np.float32)
    ex = np.zeros((32, 16, 128), np.float32)
    for kt in range(16):
        ex[2 * kt, kt, :64] = 1
        ex[2 * kt + 1, kt, 64:] = 1
    c['expand'] = ex.reshape(32, 2048)
    return c


CONST_SHAPES = {k: v.shape for k, v in make_consts().items()}

WNAMES = ["w_in", "gla_w_a2", "gla_b_a", "gla_norm_g", "nsa_cmp_pe", "nsa_cmp_w1", "nsa_cmp_w2",
          "w_branch_gla", "w_branch_nsa", "w_out", "ln_mix_g", "ln_mix_b", "xa_wq", "xa_wkv", "xa_wo",
          "ln_xa_g", "ln_xa_b", "router_w", "router_b", "moe_w_in", "moe_w_down", "ln_ffn_g", "ln_ffn_b"]
WSHAPES = {
    "w_in": [2, 2048, 9792], "gla_w_a2": [2, 16, 512], "gla_b_a": [2, 512], "gla_norm_g": [2, 1024],
    "nsa_cmp_pe": [2, 2, 32, 64], "nsa_cmp_w1": [2, 2, 2048, 256], "nsa_cmp_w2": [2, 2, 256, 64],
    "w_branch_gla": [2, 1024, 2048], "w_branch_nsa": [2, 1024, 2048], "w_out": [2, 2048, 2048],
    "ln_mix_g": [2, 2048], "ln_mix_b": [2, 2048], "xa_wq": [2, 2048, 512], "xa_wkv": [2, 2048, 1024],
    "xa_wo": [2, 512, 2048], "ln_xa_g": [2, 2048], "ln_xa_b": [2, 2048], "router_w": [2048, 16],
    "router_b": [16], "moe_w_in": [2, 16, 2048, 3072], "moe_w_down": [2, 16, 1536, 2048],
    "ln_ffn_g": [2, 2048], "ln_ffn_b": [2, 2048],
}


def build(n_layers=DEPTH, stages=("mix", "xa", "moe"), debug=(), wshapes=None):
    WS = dict(WSHAPES)
    WS.update(wshapes or {})
    nc = bass.Bass("TRN2", target_bir_lowering=False)
    dr = {}
    dr["x"] = nc.dram_tensor("x", [T, D], F32, kind="ExternalInput").ap()
    dr["mem"] = nc.dram_tensor("mem", [256, D], F32, kind="ExternalInput").ap()
    for k in WNAMES:
        dr[k] = nc.dram_tensor(k, WS[k], F32, kind="ExternalInput").ap()
    cst = {k: nc.dram_tensor("c_" + k, list(s), F32, kind="ExternalInput").ap() for k, s in CONST_SHAPES.items()}
    out = nc.dram_tensor("out", [T, D], F32, kind="ExternalOutput").ap()
    dbg = {k: nc.dram_tensor("dbg_" + k, list(s), F32, kind="ExternalOutput").ap() for k, s in debug}
    xres = nc.dram_tensor("xres", [T, D], F32).ap()
    hT_d = nc.dram_tensor("hT_d", [NFM, T], BF16).ap()
    h_d = nc.dram_tensor("h_d", [T, NTM], BF16).ap()
    y_d = nc.dram_tensor("y_d", [T, D], F32).ap()

    with ExitStack() as es:
        block = es.enter_context(nc.Block())

        @block.gpsimd
        def _(_g):
            _emit(nc, dr, cst, out, dbg, xres, hT_d, h_d, y_d, n_layers, stages)
    return nc


def _emit(nc, dr, cst, out, dbg, xres, hT_d, h_d, y_d, n_layers, stages):
    es = ExitStack()
    S = Sch(nc, es)

    uid = [0]

    def sb(name, shape, dt, st=es):
        uid[0] += 1
        return st.enter_context(nc.sbuf_tensor(name + '_%d' % uid[0], shape, dt))

    def ps(name, shape, dt=F32, st=es):
        return st.enter_context(nc.psum_tensor(name, shape, dt))

    bufA = sb("bufA", [128, 16, T], BF16)
    bufB = None
    Wt = [sb("Wt%d" % i, [128, 16, 512], BF16) for i in range(2)]
    ident = sb("ident", [128, 128], F32)
    identb = sb("identb", [128, 128], BF16)
    PS = [ps("ps%d" % i, [128, 512]) for i in range(8)]
    S.op('sp', lambda e: e.dma_start(out=ident[:], in_=cst['ident'][:, :]), w=['ident'], dma=True)
    S.op('dve', lambda e: e.tensor_copy(out=identb[:], in_=ident[:]), r=['ident'], w=['identb'])

    wt_i = [0]
    ps_i = [0]

    def next_ps(lo=0, hi=4):
        i = lo + ps_i[0] % (hi - lo)
        ps_i[0] += 1
        return i

    def load_w(W2d, k0, KC, c0, nb):
        b = wt_i[0] % 2
        wt_i[0] += 1
        src = W2d[k0:k0 + KC * 128, c0:c0 + nb].rearrange("(kc p) n -> p kc n", p=128)
        S.op('pool', lambda e: e.dma_start(out=Wt[b][:, 0:KC, 0:nb], in_=src), w=[('Wt', b)], dma=True)
        return b

    def proj_tm(src, skey, KC, W2d, c0, nb, epi, k0=0, tts=range(NT)):
        b = load_w(W2d, k0, KC, c0, nb)
        for tt in tts:
            pi = next_ps()
            for kc in range(KC):
                S.op('pe', lambda e, kc=kc, tt=tt, pi=pi: e.matmul(PS[pi][:, 0:nb], lhsT=src[:, kc, tt * 128:(tt + 1) * 128],
                                                                    rhs=Wt[b][:, kc, 0:nb], start=(kc == 0), stop=(kc == KC - 1)),
                     r=[('Wt', b), skey], w=[('ps', pi)], inc=(kc == KC - 1))
            epi(tt, pi)

    def proj_fm(src, skey, KC, W2d, c0, nb, M, epi, k0=0):
        b = load_w(W2d, k0, KC, c0, nb)
        for m0 in range(0, nb, M):
            mm = min(M, nb - m0)
            for tb in range(4):
                pi = next_ps()
                for kc in range(KC):
                    S.op('pe', lambda e, kc=kc, tb=tb, pi=pi, m0=m0, mm=mm: e.matmul(
                        PS[pi][0:mm, :], lhsT=Wt[b][:, kc, m0:m0 + mm], rhs=src[:, kc, tb * 512:(tb + 1) * 512],
                        start=(kc == 0), stop=(kc == KC - 1)),
                         r=[('Wt', b), skey], w=[('ps', pi)], inc=(kc == KC - 1))
                epi(c0 + m0, mm, tb, pi)

    def ln_and_store(l, st, xt_tile, key, tt, g_bc, b_bc, dst_dram, small):
        stats, mv, rstd = small
        for c in range(4):
            S.op('dve', lambda e, c=c: e.bn_stats(out=stats[:, c, :], in_=xt_tile[:, c * 512:(c + 1) * 512]), r=[key], w=['stats'])
        S.op('dve', lambda e: e.bn_aggr(out=mv[:], in_=stats[:].rearrange("p a b -> p (a b)")), r=['stats'], w=['mv'])
        S.op('act', lambda e: e.activation(out=rstd[:], in_=mv[:, 1:2], func=AF.Sqrt, bias=LN_EPS, scale=1.0), r=['mv'], w=['rstd'])
        S.op('dve', lambda e: e.reciprocal(out=rstd[:], in_=rstd[:]), r=['rstd'], w=['rstd'])
        S.op('dve', lambda e: e.tensor_scalar(out=xt_tile[:], in0=xt_tile[:], scalar1=mv[:, 0:1], scalar2=rstd[:, 0:1],
                                              op0=ALU.subtract, op1=ALU.mult), r=[key, 'mv', 'rstd'], w=[key])
        S.op('pool', lambda e: e.tensor_tensor(out=xt_tile[:], in0=xt_tile[:], in1=g_bc[:], op=ALU.mult), r=[key, 'lng'], w=[key])
        S.op('pool', lambda e: e.tensor_tensor(out=xt_tile[:], in0=xt_tile[:], in1=b_bc[:], op=ALU.add), r=[key, 'lnb'], w=[key])
        S.op('sp', lambda e: e.dma_start(out=dst_dram[tt * 128:(tt + 1) * 128, :], in_=xt_tile[:]), r=[key], w=[('xres', tt)], dma=True)
        transpose_to(xt_tile, key, 16, bufA, 'bufA', tt, F32)

    def transpose_to(tile, key, nchunks, dst, dkey, tt, dt, c_off=0):
        idm = ident if dt == F32 else identb
        for c4 in range(0, nchunks, 4):
            pi = next_ps(4, 8)
            pst = PS[pi] if dt == F32 else PSB[pi - 4]
            for c in range(c4, min(c4 + 4, nchunks)):
                S.op('pe', lambda e, c=c, c4=c4, pst=pst: e.transpose(out=pst[:, (c - c4) * 128:(c - c4 + 1) * 128],
                                                                   in_=tile[:, c * 128:(c + 1) * 128], identity=idm[:]),
                     r=[key, 'ident', 'identb'], w=[('ps', pi)])
            n = min(4, nchunks - c4)
            S.op('act', lambda e, c4=c4, n=n, pst=pst: e.activation(
                out=dst[:, c_off + c4:c_off + c4 + n, tt * 128:(tt + 1) * 128],
                in_=pst[:, 0:n * 128].rearrange("p (a b) -> p a b", a=n), func=AF.Copy), r=[('ps', pi)], w=[dkey])

    PSB = [PS[i][:].bitcast(BF16)[:, 0:512] for i in range(4, 8)]

    with ExitStack() as st:
        xt = [sb("xt%d" % i, [128, D], F32, st) for i in range(2)]
        for tt in range(NT):
            b = tt % 2
            S.op('sp', lambda e, b=b, tt=tt: e.dma_start(out=xt[b][:], in_=dr["x"][tt * 128:(tt + 1) * 128, :]), w=[('xt', b)], dma=True)
            S.op('sp', lambda e, b=b, tt=tt: e.dma_start(out=xres[tt * 128:(tt + 1) * 128, :], in_=xt[b][:]), r=[('xt', b)], w=[('xres', tt)], dma=True)
            transpose_to(xt[b], ('xt', b), 16, bufA, 'bufA', tt, F32)
        S.barrier()

    for l in range(n_layers):
        _mixer(nc, S, sb, PS, PSB, dr, cst, l, bufA, bufB, hT_d, h_d, xres, proj_tm, proj_fm, transpose_to, ln_and_store, next_ps,
               ident, identb, dbg)
        if "mixonly" in stages:
            break
        with ExitStack() as lq:
            qx = sb("qxT", [128, 4, T], BF16, lq)
            with ExitStack() as lb:
                bB = _merge(nc, S, sb, PS, PSB, dr, cst, l, bufA, hT_d, xres, proj_fm, transpose_to, next_ps, lb)

                def epi_q(col, mm, tb, pi):
                    S.op('act', lambda e: e.activation(out=qx[:, col // 128, tb * 512:(tb + 1) * 512], in_=PS[pi][:, :], func=AF.Copy, scale=float(128 ** -0.5)),
                         r=[('ps', pi)], w=['qxT'])
                proj_fm(bB, 'bufBall', 16, dr["xa_wq"][l], 0, 512, 128, epi_q)
                S.barrier()
            _xattn(nc, S, sb, PS, PSB, dr, cst, l, bufA, qx, xres, proj_tm, proj_fm, transpose_to, ln_and_store, next_ps, ident, identb, dbg, load_w, Wt)
        _moe(nc, S, sb, PS, PSB, dr, cst, l, bufA, None, xres, y_d, out if l == n_layers - 1 else xres, proj_tm, proj_fm,
             transpose_to, ln_and_store, next_ps, ident, identb, dbg, load_w, Wt)
    S.barrier()
    es.close()


def _mixer(nc, S, sb, PS, PSB, dr, cst, l, bufA, bufB, hT_d, h_d, xres, proj_tm, proj_fm, transpose_to, ln_and_store, next_ps,
           ident, identb, dbg):
    W = dr["w_in"][l]
    aT_d = nc.dram_tensor("aT_d%d" % l, [16, T], F32).ap()
    with ExitStack() as st:
        stg = [sb("stg%d" % i, [128, 512], BF16, st) for i in range(4)]
        stgf = sb("stgf", [16, 512], F32, st)
        si = [0]

        def epi_fm(rbase, cbase, func, scale=1.0):
            def f(col, mm, tb, pi):
                i = si[0] % 4
                si[0] += 1
                S.op('act', lambda e: e.activation(out=stg[i][0:mm, :], in_=PS[pi][0:mm, :], func=func, scale=scale), r=[('ps', pi)], w=[('stg', i)])
                r0 = rbase + col - cbase
                S.op('sp', lambda e: e.dma_start(out=hT_d[r0:r0 + mm, tb * 512:(tb + 1) * 512], in_=stg[i][0:mm, :]), r=[('stg', i)],
                     w=[('hT', r0 // 64, tb)] + ([('hT', r0 // 64 + 1, tb)] if mm == 128 else []), dma=True)
            return f

        def epi_ga(col, mm, tb, pi):
            S.op('act', lambda e: e.activation(out=stgf[0:16, :], in_=PS[pi][0:16, :], func=AF.Copy), r=[('ps', pi)], w=['stgf'])
            S.op('sp', lambda e: e.dma_start(out=aT_d[:, tb * 512:(tb + 1) * 512], in_=stgf[0:16, :]), r=['stgf'], w=[('aT', tb)], dma=True)

        def epi_tm(tcbase, nb, func):
            def f(tt, pi):
                i = si[0] % 4
                si[0] += 1
                S.op('act', lambda e: e.activation(out=stg[i][:, 0:nb], in_=PS[pi][:, 0:nb], func=func), r=[('ps', pi)], w=[('stg', i)])
                S.op('sp', lambda e: e.dma_start(out=h_d[tt * 128:(tt + 1) * 128, tcbase:tcbase + nb], in_=stg[i][:, 0:nb]), r=[('stg', i)],
                     w=[('h', tt, tcbase)], dma=True)
            return f

        A = (bufA, 'bufA', 16, W)
        proj_fm(*A, C_GQ, 512, 128, epi_fm(R_GQ, C_GQ, AF.Copy))
        proj_fm(*A, C_GK, 512, 128, epi_fm(R_GK, C_GK, AF.Copy))
        proj_fm(*A, C_GA, 16, 16, epi_ga)
        proj_tm(*A, C_GK, 512, epi_tm(TC_GK, 512, AF.Copy))
        for j in range(2):
            proj_tm(*A, C_GV + 512 * j, 512, epi_tm(TC_GV + 512 * j, 512, AF.Copy))
            proj_tm(*A, C_GR + 512 * j, 512, epi_tm(TC_GR + 512 * j, 512, AF.Silu))
            proj_fm(*A, C_NQ + 512 * j, 512, 64, epi_fm(R_NQ + 512 * j, C_NQ + 512 * j, AF.Copy, 0.125))
        for (cc, rr) in ((0, R_KC), (256, R_VC), (512, R_KS), (1024, R_KW)):
            proj_fm(*A, C_NKV + cc, 256, 64, epi_fm(rr, C_NKV + cc, AF.Copy))
        proj_tm(*A, C_NKV + 768, 256, epi_tm(TC_VS, 256, AF.Copy))
        proj_tm(*A, C_NKV + 1280, 256, epi_tm(TC_VW, 256, AF.Copy))
        proj_tm(*A, C_NG, 48, epi_tm(TC_NG, 48, AF.Sigmoid))
        for j in range(8):
            proj_fm(*A, C_MG + 512 * j, 512, 128, epi_fm(R_MG + 512 * j, C_MG + 512 * j, AF.Sigmoid))
        S.barrier()

    _gla(nc, S, sb, PS, PSB, dr, cst, l, bufA, hT_d, h_d, aT_d, transpose_to, ident, identb, dbg)
    _nsa(nc, S, sb, PS, PSB, dr, cst, l, bufA, hT_d, h_d, transpose_to, next_ps, ident, identb, dbg)


def _gla(nc, S, sb, PS, PSB, dr, cst, l, bufA, hT_d, h_d, aT_d, transpose_to, ident, identb, dbg):
    with ExitStack() as st:
        um = sb("um", [128, 128], F32, st)
        um2 = sb("um2", [128, 128], F32, st)
        caus4 = sb("caus4", [128, 512], F32, st)
        gnbc = sb("gnbc", [128, 1024], F32, st)
        wa2 = sb("wa2", [32, 512], F32, st)
        aT = sb("aT", [32, T], F32, st)
        S.op('sp', lambda e: e.dma_start(out=um[:], in_=cst['um'][:, :]), w=['um'], dma=True)
        S.op('sp', lambda e: e.dma_start(out=um2[:], in_=cst['um2'][:, :]), w=['um2'], dma=True)
        S.op('sp', lambda e: e.dma_start(out=caus4[:], in_=cst['caus4'][:, :]), w=['caus4'], dma=True)
        S.op('sp', lambda e: e.dma_start(out=gnbc[:], in_=dr['gla_norm_g'][l, :].partition_broadcast(128)), w=['gnbc'], dma=True)
        S.op('sp', lambda e: e.dma_start(out=wa2[0:16, :], in_=dr['gla_w_a2'][l]), w=['wa2a'], dma=True)
        S.op('sp', lambda e: e.dma_start(out=wa2[16:17, :], in_=dr['gla_b_a'][l:l + 1, :]), w=['wa2b'], dma=True)
        S.op('dve', lambda e: e.memset(aT[:], 1.0), w=['aT'])
        S.op('sp', lambda e: e.dma_start(out=aT[0:16, :], in_=aT_d[:, :]), w=['aT'], dma=True)
        S32 = sb("S32", [128, 4, 256], F32, st)
        Sb = sb("Sb", [128, 4, 256], BF16, st)
        S.op('dve', lambda e: e.memset(S32[:], 0.0), w=['S32'])
        S.op('pool', lambda e: e.memset(Sb[:], 0.0), w=['Sb'])
        qT = [sb("gqT%d" % i, [128, 4, 128], BF16, st) for i in range(2)]
        kT = [sb("gkT%d" % i, [128, 4, 128], BF16, st) for i in range(2)]
        kk = [sb("gk%d" % i, [128, 512], BF16, st) for i in range(2)]
        vv = [sb("gv%d" % i, [128, 1024], BF16, st) for i in range(2)]
        rs = [sb("grs%d" % i, [128, 1024], BF16, st) for i in range(2)]
        le = sb("le", [128, 512], F32, st)
        ll = sb("ll", [128, 512], F32, st)
        Eq = sb("Eq", [128, 512], F32, st)
        Ek = sb("Ek", [128, 512], F32, st)
        Ekh = sb("Ekh", [128, 512], F32, st)
        qs = sb("qs", [128, 512], BF16, st)
        ks = sb("ks", [128, 512], BF16, st)
        kh = sb("kh", [128, 512], BF16, st)
        att = sb("att", [128, 512], BF16, st)
        og = sb("og", [128, 1024], F32, st)
        ogb = sb("ogb", [128, 1024], BF16, st)
        grs = sb("grsf", [128, 1024], F32, st)
        stats = sb("gstats", [128, 4, 6], F32, st)
        mv = sb("gmv", [128, 4, 2], F32, st)
        rstd = sb("grstd", [128, 4], F32, st)
        for c in range(NT):
            b = c % 2
            cs = slice(c * 128, (c + 1) * 128)
            S.op('sp', lambda e: e.dma_start(out=qT[b][:], in_=hT_d[R_GQ:R_GQ + 512, cs].rearrange("(h d) t -> d h t", d=128)), w=[('qT', b)], dma=True)
            S.op('sp', lambda e: e.dma_start(out=kT[b][:], in_=hT_d[R_GK:R_GK + 512, cs].rearrange("(h d) t -> d h t", d=128)), w=[('kT', b)], dma=True)
            S.op('sp', lambda e: e.dma_start(out=kk[b][:], in_=h_d[cs, TC_GK:TC_GK + 512]), w=[('kk', b)], dma=True)
            S.op('sp', lambda e: e.dma_start(out=vv[b][:], in_=h_d[cs, TC_GV:TC_GV + 1024]), w=[('vv', b)], dma=True)
            S.op('sp', lambda e: e.dma_start(out=rs[b][:], in_=h_d[cs, TC_GR:TC_GR + 1024]), w=[('rs', b)], dma=True)
            S.op('pe', lambda e: e.matmul(PS[0][:, :], lhsT=aT[0:17, cs], rhs=wa2[0:17, :], start=True, stop=True), r=['aT', 'wa2a', 'wa2b'], w=[('ps', 0)])
            S.op('act', lambda e: e.activation(out=le[:], in_=PS[0][:, :], func=AF.Exp, scale=-1.0), r=[('ps', 0)], w=['le'])
            S.op('act', lambda e: e.activation(out=ll[:], in_=le[:], func=AF.Ln, bias=1.0, scale=1.0), r=['le'], w=['ll'])
            for h in range(4):
                S.op('pe', lambda e, h=h: e.matmul(PS[1][:, h * 128:(h + 1) * 128], lhsT=ll[:, h * 128:(h + 1) * 128], rhs=um[:], start=True, stop=True),
                     r=['ll', 'um'], w=[('ps', 1)], inc=(h == 3))
            S.op('pe', lambda e: e.matmul(PS[2][:, :], lhsT=um2[:], rhs=ll[:], start=True, stop=True), r=['ll', 'um2'], w=[('ps', 2)])
            S.op('act', lambda e: e.activation(out=Eq[:], in_=PS[1][:, :], func=AF.Exp), r=[('ps', 1)], w=['Eq'])
            S.op('act', lambda e: e.activation(out=Ek[:], in_=PS[1][:, :], func=AF.Exp, scale=-1.0), r=[('ps', 1)], w=['Ek'])
            S.op('act', lambda e: e.activation(out=Ekh[:], in_=PS[2][:, :], func=AF.Exp), r=[('ps', 2)], w=['Ekh'])
            S.op('dve', lambda e: e.scalar_tensor_tensor(out=qs[:], in0=qT[b][:].rearrange("p h t -> p (h t)"), scalar=float(128 ** -0.5), in1=Eq[:],
                                                         op0=ALU.mult, op1=ALU.mult), r=[('qT', b), 'Eq'], w=['qs'])
            S.op('dve', lambda e: e.tensor_tensor(out=ks[:], in0=kT[b][:].rearrange("p h t -> p (h t)"), in1=Ek[:], op=ALU.mult), r=[('kT', b), 'Ek'], w=['ks'])
            S.op('pool', lambda e: e.tensor_tensor(out=kh[:], in0=kk[b][:], in1=Ekh[:], op=ALU.mult), r=[('kk', b), 'Ekh'], w=['kh'])
            for h in range(4):
                hs = slice(h * 128, (h + 1) * 128)
                S.op('pe', lambda e, hs=hs: e.matmul(PS[3][:, hs], lhsT=ks[:, hs], rhs=qs[:, hs], start=True, stop=True), r=['ks', 'qs'], w=[('ps', 3)], inc=(h == 3))
            S.op('dve', lambda e: e.tensor_tensor(out=att[:], in0=PS[3][:, :], in1=caus4[:], op=ALU.mult), r=[('ps', 3), 'caus4'], w=['att'])
            for h in range(4):
                hs = slice(h * 128, (h + 1) * 128)
                pb = 4 + h // 2
                po = slice((h % 2) * 256, (h % 2) * 256 + 256)
                S.op('pe', lambda e, hs=hs, pb=pb, po=po, h=h: e.matmul(PS[pb][:, po], lhsT=att[:, hs], rhs=vv[b][:, h * 256:(h + 1) * 256], start=True, stop=False),
                     r=['att', ('vv', b)], w=[('ps', pb)], inc=False)
                S.op('pe', lambda e, hs=hs, pb=pb, po=po, h=h: e.matmul(PS[pb][:, po], lhsT=qs[:, hs], rhs=Sb[:, h, :], start=False, stop=True),
                     r=['qs', 'Sb'], w=[('ps', pb)], inc=True)
            for h in range(4):
                hs = slice(h * 128, (h + 1) * 128)
                pb = 6 + h // 2
                po = slice((h % 2) * 256, (h % 2) * 256 + 256)
                S.op('pe', lambda e, hs=hs, pb=pb, po=po, h=h: e.matmul(PS[pb][:, po], lhsT=kh[:, hs], rhs=vv[b][:, h * 256:(h + 1) * 256], start=True, stop=True),
                     r=['kh', ('vv', b)], w=[('ps', pb)])
                S.op('dve', lambda e, pb=pb, po=po, h=h: e.scalar_tensor_tensor(out=S32[:, h, :], in0=S32[:, h, :], scalar=Eq[:, h * 128 + 127:h * 128 + 128],
                                                                              in1=PS[pb][:, po], op0=ALU.mult, op1=ALU.add), r=[('ps', pb), 'Eq', 'S32'], w=['S32'])
            S.op('act', lambda e: e.activation(out=Sb[:], in_=S32[:], func=AF.Copy), r=['S32'], w=['Sb'])
            for h in range(4):
                pb = 4 + h // 2
                po = slice((h % 2) * 256, (h % 2) * 256 + 256)
                S.op('dve', lambda e, pb=pb, po=po, h=h: e.bn_stats(out=stats[:, h, :], in_=PS[pb][:, po]), r=[('ps', pb)], w=['gstats'])
                S.op('dve', lambda e, h=h: e.bn_aggr(out=mv[:, h, :], in_=stats[:, h, :]), r=['gstats'], w=['gmv'])
            S.op('act', lambda e: e.activation(out=rstd[:], in_=mv[:, :, 1], func=AF.Sqrt, bias=LN_EPS, scale=1.0), r=['gmv'], w=['grstd'])
            S.op('dve', lambda e: e.reciprocal(out=rstd[:], in_=rstd[:]), r=['grstd'], w=['grstd'])
            for h in range(4):
                pb = 4 + h // 2
                po = slice((h % 2) * 256, (h % 2) * 256 + 256)
                S.op('dve', lambda e, pb=pb, po=po, h=h: e.tensor_scalar(out=og[:, h * 256:(h + 1) * 256], in0=PS[pb][:, po], scalar1=mv[:, h, 0:1], scalar2=rstd[:, h:h + 1],
                                                                       op0=ALU.subtract, op1=ALU.mult), r=[('ps', pb), 'gmv', 'grstd'], w=['og'])
            S.op('pool', lambda e: e.tensor_tensor(out=grs[:], in0=rs[b][:], in1=gnbc[:], op=ALU.mult), r=[('rs', b), 'gnbc'], w=['grsf'])
            S.op('pool', lambda e: e.tensor_tensor(out=ogb[:], in0=og[:], in1=grs[:], op=ALU.mult), r=['og', 'grsf'], w=['ogb'])
            if 'o_gla' in dbg:
                S.op('pool', lambda e: e.tensor_tensor(out=og[:], in0=og[:], in1=grs[:], op=ALU.mult), r=['og', 'grsf'], w=['og'])
                S.op('sp', lambda e: e.dma_start(out=dbg['o_gla'][cs, :], in_=og[:]), r=['og'], w=[('dbg', c)], dma=True)
            transpose_to(ogb, 'ogb', 8, bufA, 'bufA', c, BF16)
        S.barrier()


def _resid_ln_phase(nc, S, sb, PS, st, l, Wres, KC, src, skeyf, alpha_src, gname, bname, dr, dst_dram, dstT, dstkeyf, transpose_to, ln_keys):
    xt = [sb("rl_xt%d" % i, [128, D], F32, st) for i in range(1)]
    g_bc = sb("rl_g", [128, D], F32, st)
    b_bc = sb("rl_b", [128, D], F32, st)
    stats = sb("rl_stats", [128, 4, 6], F32, st)
    mv = sb("rl_mv", [128, 2], F32, st)
    rstd = sb("rl_rstd", [128, 1], F32, st)
    S.op('sp', lambda e: e.dma_start(out=g_bc[:], in_=dr[gname][l, :].partition_broadcast(128)), w=['lng'], dma=True)
    S.op('sp', lambda e: e.dma_start(out=b_bc[:], in_=dr[bname][l, :].partition_broadcast(128)), w=['lnb'], dma=True)
    for tt in range(NT):
        b = 0
        key = ('rlxt', b)
        S.op('sp', lambda e: e.dma_start(out=xt[b][:], in_=alpha_src[tt * 128:(tt + 1) * 128, :]), r=[('xres', tt)], w=[key], dma=True)
        for cb in range(4):
            if Wres is not None:
                for kc in range(KC):
                    S.op('pe', lambda e, kc=kc, cb=cb: e.matmul(PS[cb][:, :], lhsT=src[:, kc, tt * 128:(tt + 1) * 128], rhs=Wres[:, kc, cb * 512:(cb + 1) * 512],
                                                                start=(kc == 0), stop=(kc == KC - 1)), r=[skeyf(tt), 'Wres'], w=[('ps', cb)], inc=(kc == KC - 1))
                S.op('dve', lambda e, cb=cb: e.scalar_tensor_tensor(out=xt[b][:, cb * 512:(cb + 1) * 512], in0=xt[b][:, cb * 512:(cb + 1) * 512], scalar=DN_ALPHA,
                                                                   in1=PS[cb][:, :], op0=ALU.mult, op1=ALU.add), r=[('ps', cb), key], w=[key])
            else:
                ys = src
                S.op('sp', lambda e, cb=cb: e.dma_start(out=ys[1][:, cb * 512:(cb + 1) * 512], in_=ys[0][tt * 128:(tt + 1) * 128, cb * 512:(cb + 1) * 512]),
                     r=[('y_d', tt, cb)], w=[('ysb', cb)], dma=True)
                S.op('dve', lambda e, cb=cb: e.scalar_tensor_tensor(out=xt[b][:, cb * 512:(cb + 1) * 512], in0=xt[b][:, cb * 512:(cb + 1) * 512], scalar=DN_ALPHA,
                                                                   in1=ys[1][:, cb * 512:(cb + 1) * 512], op0=ALU.mult, op1=ALU.add), r=[('ysb', cb), key], w=[key])
        for c in range(4):
            S.op('dve', lambda e, c=c: e.bn_stats(out=stats[:, c, :], in_=xt[b][:, c * 512:(c + 1) * 512]), r=[key], w=['stats'])
        S.op('dve', lambda e: e.bn_aggr(out=mv[:], in_=stats[:].rearrange("p a b -> p (a b)")), r=['stats'], w=['mv'])
        S.op('act', lambda e: e.activation(out=rstd[:], in_=mv[:, 1:2], func=AF.Sqrt, bias=LN_EPS, scale=1.0), r=['mv'], w=['rstd'])
        S.op('dve', lambda e: e.reciprocal(out=rstd[:], in_=rstd[:]), r=['rstd'], w=['rstd'])
        S.op('dve', lambda e: e.tensor_scalar(out=xt[b][:], in0=xt[b][:], scalar1=mv[:, 0:1], scalar2=rstd[:, 0:1], op0=ALU.subtract, op1=ALU.mult),
             r=[key, 'mv', 'rstd'], w=[key])
        S.op('pool', lambda e: e.tensor_tensor(out=xt[b][:], in0=xt[b][:], in1=g_bc[:], op=ALU.mult), r=[key, 'lng'], w=[key])
        S.op('pool', lambda e: e.tensor_tensor(out=xt[b][:], in0=xt[b][:], in1=b_bc[:], op=ALU.add), r=[key, 'lnb'], w=[key])
        S.op('sp', lambda e: e.dma_start(out=dst_dram[tt * 128:(tt + 1) * 128, :], in_=xt[b][:]), r=[key], w=[('xres', tt)], dma=True)
        transpose_to(xt[b], key, 16, dstT, dstkeyf(tt), tt, F32)


def _load_resident(S, dst, dkey, W2d, KC):
    for kc in range(KC):
        for cb in range(4):
            S.op('pool', lambda e, kc=kc, cb=cb: e.dma_start(out=dst[:, kc, cb * 512:(cb + 1) * 512], in_=W2d[kc * 128:(kc + 1) * 128, cb * 512:(cb + 1) * 512]),
                 w=[dkey], dma=True)


def _merge(nc, S, sb, PS, PSB, dr, cst, l, bufA, hT_d, xres, proj_fm, transpose_to, next_ps, st):
    bufB = sb("bufB", [128, 16, T], BF16, st)
    with ExitStack() as s2:
        sg = [sb("sg%d" % i, [128, 512], BF16, s2) for i in range(2)]
        tmp = sb("mtmp", [128, 512], F32, s2)
        gi = [0]

        def epi(branch):
            def f(col, mm, tb, pi):
                i = gi[0] % 2
                gi[0] += 1
                r0 = R_MG + branch * 2048 + col
                S.op('sp', lambda e: e.dma_start(out=sg[i][:], in_=hT_d[r0:r0 + 128, tb * 512:(tb + 1) * 512]), w=[('sg', i)], dma=True)
                dsl = bufB[:, col // 128, tb * 512:(tb + 1) * 512]
                if branch == 0:
                    S.op('dve', lambda e: e.tensor_tensor(out=dsl, in0=PS[pi][:, :], in1=sg[i][:], op=ALU.mult), r=[('ps', pi), ('sg', i)], w=['bufB'])
                else:
                    S.op('dve', lambda e: e.tensor_tensor(out=tmp[:], in0=PS[pi][:, :], in1=sg[i][:], op=ALU.mult), r=[('ps', pi), ('sg', i)], w=['mtmp'])
                    S.op('pool', lambda e: e.tensor_tensor(out=dsl, in0=dsl, in1=tmp[:], op=ALU.add), r=['mtmp', 'bufB'], w=['bufB'])
            return f
        for j in range(4):
            proj_fm(bufA[:, 0:8, :], 'bufA', 8, dr["w_branch_gla"][l], 512 * j, 512, 128, epi(0))
        for j in range(4):
            proj_fm(bufA[:, 8:16, :], 'bufA', 8, dr["w_branch_nsa"][l], 512 * j, 512, 128, epi(1))
        S.barrier()
    with ExitStack() as s2:
        _load_resident(S, bufA, 'Wres', dr["w_out"][l], 16)
        _resid_ln_phase(nc, S, sb, PS, s2, l, bufA, 16, bufB, lambda tt: ('bufB', tt), xres, "ln_mix_g", "ln_mix_b", dr, xres, bufB,
                        lambda tt: ('bufB', tt), transpose_to, None)
        S.barrier()
    return bufB


def _xattn(nc, S, sb, PS, PSB, dr, cst, l, bufA, bufB, xres, proj_tm, proj_fm, transpose_to, ln_and_store, next_ps, ident, identb, dbg, load_w, Wt):
    with ExitStack() as st:
        memT = sb("memT", [128, 16, 256], BF16, st)
        mt_ = sb("memtile", [128, D], F32, st)
        kTs = sb("xkT", [128, 4, 256], BF16, st)
        vs = sb("xv", [128, 2, 4, 132], BF16, st)
        qx = bufB
        oT = sb("xoT", [128, 4, T], BF16, st)
        woT = sb("woT", [128, 4, D], BF16, st)
        eT = [sb("xeT%d" % i, [128, 512], BF16, st) for i in range(2)]
        oxa = sb("oxa", [128, 512], BF16, st)
        rden = sb("xrden", [128, 4], F32, st)
        for m in range(2):
            S.op('sp', lambda e: e.dma_start(out=mt_[:], in_=dr["mem"][m * 128:(m + 1) * 128, :]), w=['memtile'], dma=True)
            transpose_to(mt_, 'memtile', 16, memT, 'memT', m, F32)
        S.op('dve', lambda e: e.memset(vs[:], 1.0), w=['xv'])
        Wkv = dr["xa_wkv"][l]
        b = load_w(Wkv, 0, 16, 0, 512)
        for h in range(4):
            pi = next_ps()
            for kc in range(16):
                S.op('pe', lambda e, kc=kc: e.matmul(PS[pi][:, 0:256], lhsT=Wt[b][:, kc, h * 128:(h + 1) * 128], rhs=memT[:, kc, :], start=(kc == 0), stop=(kc == 15)),
                     r=[('Wt', b), 'memT'], w=[('ps', pi)], inc=(kc == 15))
            S.op('act', lambda e: e.activation(out=kTs[:, h, :], in_=PS[pi][:, 0:256], func=AF.Copy), r=[('ps', pi)], w=['xkT'])
        b = load_w(Wkv, 0, 16, 512, 512)
        for m in range(2):
            pi = next_ps()
            for kc in range(16):
                S.op('pe', lambda e, kc=kc: e.matmul(PS[pi][:, :], lhsT=memT[:, kc, m * 128:(m + 1) * 128], rhs=Wt[b][:, kc, :], start=(kc == 0), stop=(kc == 15)),
                     r=[('Wt', b), 'memT'], w=[('ps', pi)], inc=(kc == 15))
            S.op('act', lambda e: e.activation(out=vs[:, m, :, 0:128], in_=PS[pi][:, :].rearrange("p (h d) -> p h d", h=4), func=AF.Copy), r=[('ps', pi)], w=['xv'])

        for tt in range(NT):
            ts = slice(tt * 128, (tt + 1) * 128)
            for m in range(2):
                pi = next_ps()
                for h in range(4):
                    S.op('pe', lambda e, h=h: e.matmul(PS[pi][:, h * 128:(h + 1) * 128], lhsT=kTs[:, h, m * 128:(m + 1) * 128], rhs=qx[:, h, ts], start=True, stop=True),
                         r=['xkT', 'qxT'], w=[('ps', pi)], inc=(h == 3))
                S.op('act', lambda e: e.activation(out=eT[m][:], in_=PS[pi][:, :], func=AF.Exp), r=[('ps', pi)], w=[('xeT', m)])
            for h in range(4):
                pb = 4 + h // 2
                po = (h % 2) * 132
                for m in range(2):
                    S.op('pe', lambda e, m=m: e.matmul(PS[pb][:, po:po + 129], lhsT=eT[m][:, h * 128:(h + 1) * 128], rhs=vs[:, m, h, 0:129], start=(m == 0), stop=(m == 1)),
                         r=[('xeT', m), 'xv'], w=[('ps', pb)], inc=(m == 1))
                S.op('dve', lambda e: e.reciprocal(out=rden[:, h:h + 1], in_=PS[pb][:, po + 128:po + 129]), r=[('ps', pb)], w=['xrden'])
                S.op('dve', lambda e: e.tensor_scalar(out=oxa[:, h * 128:(h + 1) * 128], in0=PS[pb][:, po:po + 128], scalar1=rden[:, h:h + 1], scalar2=None, op0=ALU.mult),
                     r=[('ps', pb), 'xrden'], w=['oxa'])
            transpose_to(oxa, 'oxa', 4, oT, 'xoT', tt, BF16)
        S.barrier()
        for kc in range(4):
            for cb in range(4):
                S.op('pool', lambda e: e.dma_start(out=woT[:, kc, cb * 512:(cb + 1) * 512], in_=dr["xa_wo"][l][kc * 128:(kc + 1) * 128, cb * 512:(cb + 1) * 512]),
                     w=['Wres'], dma=True)
        _resid_ln_phase(nc, S, sb, PS, st, l, woT, 4, oT, lambda tt: 'xoT', xres, "ln_xa_g", "ln_xa_b", dr, xres, bufA, lambda tt: 'bufA', transpose_to, None)
        S.barrier()


def _moe(nc, S, sb, PS, PSB, dr, cst, l, bufA, bufB, xres, y_d, dst, proj_tm, proj_fm, transpose_to, ln_and_store, next_ps, ident, identb, dbg, load_w, Wt):
    with ExitStack() as st:
        gate = sb("gate", [128, NT, 16], F32, st)
        with ExitStack() as s2:
            actT = sb("actT", [128, 12, T], BF16, s2)
            rw = sb("rw", [128, 16, 16], BF16, s2)
            rb = sb("rb", [128, 16], F32, s2)
            lg = sb("lg", [128, 16], F32, s2)
            lb = sb("lb", [128, 4, 4], F32, s2)
            eq = sb("eq", [128, 4, 4], F32, s2)
            lb2 = sb("lb2", [128, 4, 4], F32, s2)
            m1 = sb("m1", [128, 4], F32, s2)
            m2 = sb("m2", [128, 4], F32, s2)
            gs = sb("gs", [128, 4], F32, s2)
            gm = sb("gm", [128, 1], F32, s2)
            ex = sb("ex", [128, 16], F32, s2)
            den = sb("den", [128, 1], F32, s2)
            ystg = [sb("ystg%d" % i, [128, 512], F32, s2) for i in range(3)]
            S.op('pool', lambda e: e.dma_start(out=rw[:], in_=dr["router_w"].rearrange("(kc p) e -> p kc e", p=128)), w=['rw'], dma=True)
            S.op('sp', lambda e: e.dma_start(out=rb[:], in_=dr["router_b"].partition_broadcast(128)), w=['rb'], dma=True)
            for tt in range(NT):
                ts = slice(tt * 128, (tt + 1) * 128)
                pi = next_ps()
                for kc in range(16):
                    S.op('pe', lambda e, kc=kc: e.matmul(PS[pi][:, 0:16], lhsT=bufA[:, kc, ts], rhs=rw[:, kc, :], start=(kc == 0), stop=(kc == 15)),
                         r=['bufA', 'rw'], w=[('ps', pi)], inc=(kc == 15))
                lbf = lb[:].rearrange("p a b -> p (a b)")
                S.op('dve', lambda e: e.tensor_copy(out=lg[:], in_=PS[pi][:, 0:16]), r=[('ps', pi)], w=['lg'])
                S.op('dve', lambda e: e.tensor_tensor(out=lbf, in0=lg[:], in1=rb[:], op=ALU.add), r=['lg', 'rb'], w=['lb'])
                S.op('dve', lambda e: e.tensor_reduce(out=m1[:], in_=lb[:], axis=AX.X, op=ALU.max), r=['lb'], w=['m1'])
                S.op('dve', lambda e: e.tensor_tensor(out=eq[:], in0=lb[:], in1=m1[:].unsqueeze(2).to_broadcast([128, 4, 4]), op=ALU.is_equal), r=['lb', 'm1'], w=['eq'])
                S.op('dve', lambda e: e.scalar_tensor_tensor(out=lb2[:], in0=eq[:], scalar=-1e30, in1=lb[:], op0=ALU.mult, op1=ALU.add), r=['eq', 'lb'], w=['lb2'])
                S.op('dve', lambda e: e.tensor_reduce(out=m2[:], in_=lb2[:], axis=AX.X, op=ALU.max), r=['lb2'], w=['m2'])
                S.op('dve', lambda e: e.tensor_tensor(out=gs[:], in0=m1[:], in1=m2[:], op=ALU.add), r=['m1', 'm2'], w=['gs'])
                S.op('dve', lambda e: e.tensor_reduce(out=gm[:], in_=gs[:], axis=AX.X, op=ALU.max), r=['gs'], w=['gm'])
                S.op('dve', lambda e: e.tensor_scalar(out=gs[:], in0=gs[:], scalar1=gm[:, 0:1], scalar2=None, op0=ALU.is_equal), r=['gs', 'gm'], w=['gs'])
                S.op('dve', lambda e: e.tensor_tensor(out=eq[:], in0=lb[:], in1=m2[:].unsqueeze(2).to_broadcast([128, 4, 4]), op=ALU.is_ge), r=['lb', 'm2'], w=['eq'])
                S.op('dve', lambda e: e.tensor_tensor(out=eq[:], in0=eq[:], in1=gs[:].unsqueeze(2).to_broadcast([128, 4, 4]), op=ALU.mult), r=['eq', 'gs'], w=['eq'])
                S.op('act', lambda e: e.activation(out=ex[:], in_=lg[:], func=AF.Exp), r=['lg'], w=['ex'])
                S.op('dve', lambda e: e.tensor_tensor(out=ex[:], in0=ex[:], in1=eq[:].rearrange("p a b -> p (a b)"), op=ALU.mult), r=['ex', 'eq'], w=['ex'])
                S.op('dve', lambda e: e.tensor_reduce(out=den[:], in_=ex[:], axis=AX.X, op=ALU.add), r=['ex'], w=['den'])
                S.op('dve', lambda e: e.reciprocal(out=den[:], in_=den[:]), r=['den'], w=['den'])
                S.op('dve', lambda e: e.tensor_scalar(out=gate[:, tt, :], in0=ex[:], scalar1=den[:, 0:1], scalar2=None, op0=ALU.mult), r=['ex', 'den'], w=['gate'])
            yi = [0]
            for ex_i in range(16):
                Wi = dr["moe_w_in"][l, ex_i]
                Wd = dr["moe_w_down"][l, ex_i]

                def epi_a(col, mm, tb, pi):
                    S.op('act', lambda e: e.activation(out=actT[:, col // 128, tb * 512:(tb + 1) * 512], in_=PS[pi][:, :], func=AF.Silu), r=[('ps', pi)], w=['actT'])

                def epi_u(col, mm, tb, pi):
                    dsl = actT[:, (col - 1536) // 128, tb * 512:(tb + 1) * 512]
                    S.op('dve', lambda e: e.tensor_tensor(out=dsl, in0=dsl, in1=PS[pi][:, :], op=ALU.mult), r=[('ps', pi), 'actT'], w=['actT'])
                for j in range(3):
                    proj_fm(bufA, 'bufA', 16, Wi, 512 * j, 512, 128, epi_a)
                for j in range(3, 6):
                    proj_fm(bufA, 'bufA', 16, Wi, 512 * j, 512, 128, epi_u)
                for cb in range(4):
                    def epi_y(tt, pi, cb=cb):
                        i = yi[0] % 3
                        yi[0] += 1
                        S.op('dve', lambda e: e.tensor_scalar(out=ystg[i][:], in0=PS[pi][:, :], scalar1=gate[:, tt, ex_i:ex_i + 1], scalar2=None, op0=ALU.mult),
                             r=[('ps', pi), 'gate'], w=[('ystg', i)])
                        dsl = y_d[tt * 128:(tt + 1) * 128, cb * 512:(cb + 1) * 512]
                        if ex_i == 0:
                            S.op('pool', lambda e: e.dma_start(out=dsl, in_=ystg[i][:]), r=[('ystg', i)], w=[('y_d', tt, cb)], dma=True)
                        else:
                            S.op('pool', lambda e: e.dma_start(out=dsl, in_=ystg[i][:], accum_op=ALU.add), r=[('ystg', i)], w=[('y_d', tt, cb)], dma=True)
                    proj_tm(actT, 'actT', 12, Wd, 512 * cb, 512, epi_y)
            S.barrier()
        with ExitStack() as s2:
            ysb = sb("ysb", [128, D], F32, s2)
            _resid_ln_phase(nc, S, sb, PS, s2, l, None, 0, (y_d, ysb), None, xres, "ln_ffn_g", "ln_ffn_b", dr, dst, bufA, lambda tt: 'bufA', transpose_to, None)
            S.barrier()


def _nsa(nc, S, sb, PS, PSB, dr, cst, l, bufA, hT_d, h_d, transpose_to, next_ps, ident, identb, dbg):
    slopes = [2.0 ** (-8.0 * (h + 1) / 16.0) for h in range(16)]
    with ExitStack() as st:
        rel = sb("rel", [128, 2048], F32, st)
        cdiag = sb("cdiag", [128, 128], F32, st)
        cfar = sb("cfar", [128, 128], F32, st)
        dcon = sb("dcon", [128, 272], F32, st)
        cpb = sb("cpb", [128, 256], F32, st)
        cmask = sb("cmask", [128, 2048], BF16, st)
        selc = sb("selc", [128, NT, 32], F32, st)
        expd = sb("expd", [32, 2048], BF16, st)
        ng = sb("ng", [128, NT, 48], BF16, st)
        kcT = sb("kcT", [64, 4, 128], BF16, st)
        vca = sb("vca", [128, 4, 97], BF16, st)
        S.op('sp', lambda e: e.dma_start(out=rel[:], in_=cst['rel_mid'][:, :]), w=['rel'], dma=True)
        S.op('sp', lambda e: e.dma_start(out=cdiag[:], in_=cst['cdiag'][:, :]), w=['cdiag'], dma=True)
        S.op('sp', lambda e: e.dma_start(out=cfar[:], in_=cst['cfar'][:, :]), w=['cfar'], dma=True)
        S.op('sp', lambda e: e.dma_start(out=dcon[:], in_=cst['dconst'][:, :]), w=['dcon'], dma=True)
        S.op('sp', lambda e: e.dma_start(out=cpb[:], in_=cst['cmp_pb'][:, :]), w=['cpb'], dma=True)
        S.op('pool', lambda e: e.dma_start(out=cmask[:], in_=cst['cmp_mask'][:, :]), w=['cmask'], dma=True)
        S.op('sp', lambda e: e.dma_start(out=selc[:], in_=cst['selc'].rearrange("(tt p) j -> p tt j", p=128)), w=['selc'], dma=True)
        S.op('pool', lambda e: e.dma_start(out=expd[:], in_=cst['expand'][:, :]), w=['expd'], dma=True)
        S.op('sp', lambda e: e.dma_start(out=ng[:], in_=h_d[:, TC_NG:TC_NG + 48].rearrange("(tt p) j -> p tt j", p=128)), w=['ng'], dma=True)
        S.op('dve', lambda e: e.memset(kcT[:], 0.0), w=['kcT'])
        S.op('dve', lambda e: e.memset(vca[:], 0.0), w=['vca'])
        with ExitStack() as s2:
            w1 = sb("w1", [64, 2, 32, 256], BF16, s2)
            w2 = sb("w2", [128, 2, 2, 64], BF16, s2)
            pes = sb("pes", [32, 2, 64], F32, s2)
            peT = sb("peT", [64, 2, 32], BF16, s2)
            c1 = sb("c1", [128, 2, 2], F32, s2)
            srcT = sb("csrcT", [64, T], BF16, s2)
            u = sb("cu", [128, 128], F32, s2)
            t1 = sb("ct1", [128, 128], F32, s2)
            gel = sb("cgel", [128, 2, 128], BF16, s2)
            ovl = sb("ovl", [128, 32], F32, s2)
            for kv in range(2):
                S.op('pool', lambda e: e.dma_start(out=w1[:, kv, :, :], in_=dr["nsa_cmp_w1"][l, kv].rearrange("(l d) h -> d l h", d=64)), w=['w1'], dma=True)
                S.op('pool', lambda e: e.dma_start(out=w2[:, kv, :, :], in_=dr["nsa_cmp_w2"][l, kv].rearrange("(hc p) d -> p hc d", p=128)), w=['w2'], dma=True)
            S.op('sp', lambda e: e.dma_start(out=pes[:], in_=dr["nsa_cmp_pe"][l].rearrange("k l d -> l k d")), w=['pes'], dma=True)
            S.op('sp', lambda e: e.dma_start(out=ovl[:], in_=cst['overlap'][:, :]), w=['ovl'], dma=True)
            for g in range(4):
                S.op('dve', lambda e: e.memset(vca[:, g, 64:65], 1.0), r=[], w=['vca'])
                S.op('dve', lambda e: e.tensor_copy(out=vca[:, g, 65:97], in_=ovl[:]), r=['ovl'], w=['vca'])
            for kv in range(2):
                S.op('pe', lambda e: e.transpose(out=PS[0][0:64, 0:32], in_=pes[0:32, kv, :], identity=ident[0:32, 0:32]), r=['pes', 'ident'], w=[('ps', 0)])
                S.op('act', lambda e: e.activation(out=peT[:, kv, :], in_=PS[0][0:64, 0:32], func=AF.Copy), r=[('ps', 0)], w=['peT'])
                for hc in range(2):
                    for li in range(32):
                        S.op('pe', lambda e, li=li: e.matmul(PS[1][:, 0:1], lhsT=w1[:, kv, li, hc * 128:(hc + 1) * 128], rhs=peT[:, kv, li:li + 1], start=(li == 0), stop=(li == 31)),
                             r=['w1', 'peT'], w=[('ps', 1)], inc=(li == 31))
                    S.op('act', lambda e: e.activation(out=c1[:, kv, hc:hc + 1], in_=PS[1][:, 0:1], func=AF.Copy), r=[('ps', 1)], w=['c1'])
            for g in range(4):
                for kv in range(2):
                    r0 = (R_KC if kv == 0 else R_VC) + g * 64
                    S.op('sp', lambda e: e.dma_start(out=srcT[:], in_=hT_d[r0:r0 + 64, :]), w=['csrcT'], dma=True)
                    for hc in range(2):
                        pi = next_ps()
                        for li in range(32):
                            S.op('pe', lambda e, li=li: e.matmul(PS[pi][:, 0:127], lhsT=w1[:, kv, li, hc * 128:(hc + 1) * 128], rhs=srcT[:, li:li + 16 * 126 + 1:16],
                                                                 start=(li == 0), stop=(li == 31)), r=['w1', 'csrcT'], w=[('ps', pi)], inc=(li == 31))
                        S.op('act', lambda e: e.activation(out=u[:, 0:127], in_=PS[pi][:, 0:127], func=AF.Identity, bias=c1[:, kv, hc:hc + 1], scale=1.0), r=[('ps', pi), 'c1'], w=['cu'])
                        S.op('dve', lambda e: e.tensor_tensor(out=t1[:, 0:127], in0=u[:, 0:127], in1=u[:, 0:127], op=ALU.mult), r=['cu'], w=['ct1'])
                        S.op('dve', lambda e: e.tensor_scalar(out=t1[:, 0:127], in0=t1[:, 0:127], scalar1=0.044715, scalar2=1.0, op0=ALU.mult, op1=ALU.add), r=['ct1'], w=['ct1'])
                        S.op('dve', lambda e: e.tensor_tensor(out=t1[:, 0:127], in0=t1[:, 0:127], in1=u[:, 0:127], op=ALU.mult), r=['ct1', 'cu'], w=['ct1'])
                        S.op('act', lambda e: e.activation(out=t1[:, 0:127], in_=t1[:, 0:127], func=AF.Sigmoid, scale=2.0 * 0.7978845608028654), r=['ct1'], w=['ct1'])
                        S.op('dve', lambda e: e.tensor_tensor(out=gel[:, hc, 0:127], in0=t1[:, 0:127], in1=u[:, 0:127], op=ALU.mult), r=['ct1', 'cu'], w=['cgel'])
                    pi = next_ps()
                    if kv == 0:
                        for hc in range(2):
                            S.op('pe', lambda e, hc=hc: e.matmul(PS[pi][0:64, 0:127], lhsT=w2[:, 0, hc, :], rhs=gel[:, hc, 0:127], start=(hc == 0), stop=(hc == 1)),
                                 r=['w2', 'cgel'], w=[('ps', pi)], inc=(hc == 1))
                        S.op('act', lambda e: e.activation(out=kcT[:, g, 0:127], in_=PS[pi][0:64, 0:127], func=AF.Copy), r=[('ps', pi)], w=['kcT'])
                    else:
                        for hc in range(2):
                            S.op('pe', lambda e, hc=hc: e.matmul(PS[pi][0:127, 0:64], lhsT=gel[:, hc, 0:127], rhs=w2[:, 1, hc, :], start=(hc == 0), stop=(hc == 1)),
                                 r=['w2', 'cgel'], w=[('ps', pi)], inc=(hc == 1))
                        S.op('act', lambda e: e.activation(out=vca[0:127, g, 0:64], in_=PS[pi][0:127, 0:64], func=AF.Copy), r=[('ps', pi)], w=['vca'])
            S.barrier()
        qT = sb("nqT", [64, 4, T], BF16, st)
        ksT = sb("nksT", [64, T], BF16, st)
        kwT = sb("nkwT", [64, T], BF16, st)
        vs = sb("nvs", [128, NT, 65], BF16, st)
        vw = sb("nvw", [128, NT, 65], BF16, st)
        sc = [sb("nsc%d" % i, [128, 512], F32, st) for i in range(2)]
        eTa = sb("neT", [128, NT + 1, 512], BF16, st)
        imp = sb("nimp", [128, 32], F32, st)
        mx8 = sb("nmx8", [128, 8], F32, st)
        selb = sb("nselb", [128, 32], F32, st)
        selbT = sb("nselbT", [32, 128], BF16, st)
        rd = sb("nrd", [128, 1], F32, st)
        oc = sb("noc", [128, 256], F32, st)
        ocb = sb("nocb", [128, 256], BF16, st)
        sci = [0]
        for g in range(4):
            S.op('sp', lambda e: e.dma_start(out=qT[:], in_=hT_d[R_NQ + g * 256:R_NQ + (g + 1) * 256, :].rearrange("(h d) t -> d h t", d=64)), w=['nqT'], dma=True)
            S.op('sp', lambda e: e.dma_start(out=ksT[:], in_=hT_d[R_KS + g * 64:R_KS + (g + 1) * 64, :]), w=['nksT'], dma=True)
            S.op('sp', lambda e: e.dma_start(out=kwT[:], in_=hT_d[R_KW + g * 64:R_KW + (g + 1) * 64, :]), w=['nkwT'], dma=True)
            S.op('dve', lambda e: e.memset(vs[:], 1.0), w=['nvs'])
            S.op('dve', lambda e: e.memset(vw[:], 1.0), w=['nvw'])
            S.op('sp', lambda e: e.dma_start(out=vs[:, :, 0:64], in_=h_d[:, TC_VS + g * 64:TC_VS + (g + 1) * 64].rearrange("(kt p) d -> p kt d", p=128)), w=['nvs'], dma=True)
            S.op('sp', lambda e: e.dma_start(out=vw[:, :, 0:64], in_=h_d[:, TC_VW + g * 64:TC_VW + (g + 1) * 64].rearrange("(kt p) d -> p kt d", p=128)), w=['nvw'], dma=True)
            for tt in range(NT):
                ts = slice(tt * 128, (tt + 1) * 128)

                def scores(kTsrc, kkey, kt, mode, use_sel, slot, extra_mask):
                    pi = next_ps(0, 4)
                    for h in range(4):
                        lhs = kcT[:, g, :] if mode == 'cmp' else kTsrc[:, kt * 128:(kt + 1) * 128]
                        S.op('pe', lambda e, h=h: e.matmul(PS[pi][:, h * 128:(h + 1) * 128], lhsT=lhs, rhs=qT[:, h, ts], start=True, stop=not use_sel),
                             r=[kkey, 'nqT'], w=[('ps', pi)], inc=(not use_sel) and h == 3)
                        if use_sel:
                            S.op('pe', lambda e, h=h: e.matmul(PS[pi][:, h * 128:(h + 1) * 128], lhsT=expd[:, kt * 128:(kt + 1) * 128], rhs=selbT[:, :], start=False, stop=True),
                                 r=['expd', 'nselbT'], w=[('ps', pi)], inc=(h == 3))
                    si = sci[0] % 2
                    sci[0] += 1
                    S.op('dve', lambda e: e.tensor_tensor(out=sc[si][:], in0=PS[pi][:, :], in1=rel[:, g * 512:(g + 1) * 512], op=ALU.add), r=[('ps', pi), 'rel'], w=[('nsc', si)])
                    if extra_mask is not None:
                        mk, mkey = extra_mask
                        S.op('pool', lambda e: e.tensor_tensor(out=sc[si][:].rearrange("p (h j) -> p h j", h=4), in0=sc[si][:].rearrange("p (h j) -> p h j", h=4),
                                                               in1=mk.unsqueeze(1).to_broadcast([128, 4, 128]), op=ALU.add), r=[('nsc', si), mkey], w=[('nsc', si)])
                    for h in range(4):
                        hh = 4 * g + h
                        if mode == 'cmp':
                            bia = cpb[:, hh * 16 + tt:hh * 16 + tt + 1]
                        else:
                            bia = dcon[:, hh * 17 + (tt - kt):hh * 17 + (tt - kt) + 1]
                        S.op('act', lambda e, h=h: e.activation(out=eTa[:, slot, h * 128:(h + 1) * 128], in_=sc[si][:, h * 128:(h + 1) * 128], func=AF.Exp, bias=bia, scale=1.0),
                             r=[('nsc', si), 'cpb', 'dcon'], w=[('neT', slot)], inc=True)

                def pv_combine(slots, vfn, ncol, br, first):
                    for h in range(4):
                        pb = 4 + h
                        for i, (slot, kt) in enumerate(slots):
                            S.op('pe', lambda e, i=i, slot=slot, kt=kt: e.matmul(PS[pb][:, 0:ncol], lhsT=eTa[:, slot, h * 128:(h + 1) * 128], rhs=vfn(kt),
                                                                              start=(i == 0), stop=(i == len(slots) - 1)),
                                 r=[('neT', slot), 'nvs', 'nvw', 'vca'], w=[('ps', pb)], inc=(i == len(slots) - 1))
                        S.op('dve', lambda e: e.tensor_scalar(out=rd[:], in0=PS[pb][:, 64:65], scalar1=1e-30, scalar2=None, op0=ALU.max), r=[('ps', pb)], w=['nrd'])
                        S.op('dve', lambda e: e.reciprocal(out=rd[:], in_=rd[:]), r=['nrd'], w=['nrd'])
                        if br == 0:
                            if h == 0:
                                S.op('dve', lambda e: e.tensor_scalar(out=imp[:], in0=PS[pb][:, 65:97], scalar1=rd[:, 0:1], scalar2=None, op0=ALU.mult), r=[('ps', pb), 'nrd'], w=['nimp'])
                            else:
                                S.op('dve', lambda e: e.scalar_tensor_tensor(out=imp[:], in0=PS[pb][:, 65:97], scalar=rd[:, 0:1], in1=imp[:], op0=ALU.mult, op1=ALU.add),
                                     r=[('ps', pb), 'nrd', 'nimp'], w=['nimp'])
                        gcol = (4 * g + h) * 3 + br
                        S.op('dve', lambda e: e.tensor_tensor(out=rd[:], in0=rd[:], in1=ng[:, tt, gcol:gcol + 1], op=ALU.mult), r=['nrd', 'ng'], w=['nrd'])
                        if first:
                            S.op('dve', lambda e: e.tensor_scalar(out=oc[:, h * 64:(h + 1) * 64], in0=PS[pb][:, 0:64], scalar1=rd[:, 0:1], scalar2=None, op0=ALU.mult),
                                 r=[('ps', pb), 'nrd'], w=['noc'])
                        else:
                            S.op('dve', lambda e: e.scalar_tensor_tensor(out=oc[:, h * 64:(h + 1) * 64], in0=PS[pb][:, 0:64], scalar=rd[:, 0:1], in1=oc[:, h * 64:(h + 1) * 64],
                                                                         op0=ALU.mult, op1=ALU.add), r=[('ps', pb), 'nrd', 'noc'], w=['noc'])

                scores(None, 'kcT', 0, 'cmp', False, 16, (cmask[:, ts], 'cmask'))
                pv_combine([(16, 0)], lambda kt: vca[:, g, 0:97], 97, 0, True)
                S.op('dve', lambda e: e.tensor_tensor(out=imp[:], in0=imp[:], in1=selc[:, tt, :], op=ALU.add), r=['nimp', 'selc'], w=['nimp'])
                S.op('dve', lambda e: e.max(out=mx8[:], in_=imp[:]), r=['nimp'], w=['nmx8'])
                S.op('dve', lambda e: e.tensor_scalar(out=mx8[:, 7:8], in0=mx8[:, 7:8], scalar1=-5e29, scalar2=None, op0=ALU.max), r=['nmx8'], w=['nmx8'])
                S.op('dve', lambda e: e.tensor_scalar(out=selb[:], in0=imp[:], scalar1=mx8[:, 7:8], scalar2=NEGB, op0=ALU.is_lt, op1=ALU.mult), r=['nimp', 'nmx8'], w=['nselb'])
                S.op('pe', lambda e: e.transpose(out=PS[3][0:32, 0:128], in_=selb[:, :], identity=ident[:]), r=['nselb', 'ident'], w=[('ps', 3)])
                S.op('act', lambda e: e.activation(out=selbT[:, :], in_=PS[3][0:32, 0:128], func=AF.Copy), r=[('ps', 3)], w=['nselbT'])
                for kt in range(tt + 1):
                    scores(ksT, 'nksT', kt, 'rel', True, kt, (cdiag[:], 'cdiag') if kt == tt else None)
                pv_combine([(kt, kt) for kt in range(tt + 1)], lambda kt: vs[:, kt, :], 65, 1, False)
                kts = list(range(max(0, tt - 4), tt + 1))
                for kt in kts:
                    em = (cdiag[:], 'cdiag') if kt == tt else ((cfar[:], 'cfar') if kt == tt - 4 else None)
                    scores(kwT, 'nkwT', kt, 'rel', False, kt, em)
                pv_combine([(kt, kt) for kt in kts], lambda kt: vw[:, kt, :], 65, 2, False)
                S.op('act', lambda e: e.activation(out=ocb[:], in_=oc[:], func=AF.Copy), r=['noc'], w=['nocb'])
                transpose_to(ocb, 'nocb', 2, bufA, 'bufA', tt, BF16, c_off=8 + 2 * g)
        S.barrier()


_NC_CACHE = {}


def kernel(**inputs):
    n = 8
    if "nc" not in _NC_CACHE:
        _NC_CACHE["nc"] = build()
    nc = _NC_CACHE["nc"]
    consts = make_consts()
    in_maps = []
    for c in range(n):
        m = {"x": np.ascontiguousarray(inputs["x"][c], dtype=np.float32), "mem": np.ascontiguousarray(inputs["mem"][c], dtype=np.float32)}
        for k in WNAMES:
            m[k] = np.ascontiguousarray(inputs[k], dtype=np.float32)
        for k, v in consts.items():
            m["c_" + k] = v
        in_maps.append(m)
    res = run_bass_kernel_spmd(nc, in_maps, core_ids=list(range(n)))
    return np.stack([res.results[c]["out"] for c in range(n)], axis=0).astype(np.float32)
```

```python
import numpy as np
from contextlib import ExitStack
import concourse.bass as bass
import concourse.mybir as mybir
from concourse.bass_utils import run_bass_kernel_spmd

F32 = mybir.dt.float32
BF16 = mybir.dt.bfloat16
AF = mybir.ActivationFunctionType
ALU = mybir.AluOpType
AX = mybir.AxisListType

T = 2048
D = 2048
NT = 16
DEPTH = 2
DN_ALPHA = float((2 * DEPTH) ** 0.25)
LN_EPS = 1e-5
D_IN = 9792
C_GQ, C_GK, C_GV, C_GR, C_GA, C_NQ, C_NKV, C_NG, C_MG = 0, 512, 1024, 2048, 3072, 3088, 4112, 5648, 5696
R_GQ, R_GK, R_GA, R_NQ, R_KC, R_VC, R_KS, R_KW, R_MG, NFM = 0, 512, 1024, 1056, 2080, 2336, 2592, 2848, 3104, 7200
TC_GK, TC_GV, TC_GR, TC_VS, TC_VW, TC_NG, NTM = 0, 512, 1536, 2560, 2816, 3072, 3120
NEGB = -30000.0


class Sch:
    def __init__(self, nc, es):
        self.nc = nc
        self.eng = {'pe': nc.tensor, 'act': nc.scalar, 'dve': nc.vector, 'pool': nc.gpsimd, 'sp': nc.sync}
        self.sem = {}
        self.cnt = {}
        for e in ('pe', 'act', 'dve', 'pool'):
            self.sem[e] = es.enter_context(nc.semaphore('s_' + e))
            self.cnt[e] = 0
        self.NS = 8
        for q in ('sp', 'pool'):
            for i in range(self.NS):
                k = ('dma', q, i)
                self.sem[k] = es.enter_context(nc.semaphore('d_%s%d' % (q, i)))
                self.cnt[k] = 0
        self.dma_i = {'sp': 0, 'pool': 0}
        self.waited = {e: {} for e in self.eng}
        self.lw = {}
        self.rd = {}
        self.nops = 0

    def _wait(self, e, tok):
        s, v = tok
        if self.waited[e].get(s, 0) >= v:
            return
        self.waited[e][s] = v
        self.eng[e].wait_ge(self.sem[s], v)

    def op(self, e, fn, r=(), w=(), dma=False, inc=True):
        deps = []
        for k in r:
            if k in self.lw:
                deps.append(self.lw[k])
        for k in w:
            if k in self.lw:
                deps.append(self.lw[k])
            deps.extend(self.rd.get(k, {}).values())
        if dma:
            i = self.dma_i[e]
            self.dma_i[e] += 1
            s = ('dma', e, i % self.NS)
            if self.cnt[s] > 0:
                deps.append((s, self.cnt[s]))
            self.cnt[s] += 16
            tok = (s, self.cnt[s])
        else:
            s = e
            if inc:
                self.cnt[s] += 1
                tok = (s, self.cnt[s])
            else:
                tok = (s, self.cnt[s] + 1)
        for d in deps:
            if d[0] == 'pe' and e == 'pe' and not dma:
                continue
            self._wait(e, d)
        ins = fn(self.eng[e])
        if dma:
            ins.then_inc(self.sem[s], 16)
        elif inc:
            ins.then_inc(self.sem[s], 1)
        for k in w:
            self.lw[k] = tok
            self.rd[k] = {}
        for k in r:
            self.rd.setdefault(k, {})[tok[0]] = tok
        self.nops += 1
        return tok

    def barrier(self):
        for e in self.eng:
            for s, c in self.cnt.items():
                if c > 0:
                    self._wait(e, (s, c))
        self.lw.clear()
        self.rd.clear()


def make_consts():
    c = {}
    i = np.arange(128)
    c['ident'] = np.eye(128, dtype=np.float32)
    c['um'] = (-(1.0 / 16.0) * (i[:, None] <= i[None, :])).astype(np.float32)
    c['um2'] = (-(1.0 / 16.0) * (i[:, None] > i[None, :])).astype(np.float32)
    c['caus4'] = np.tile((i[:, None] <= i[None, :]).astype(np.float32), (1, 4))
    slopes = 2.0 ** (-8.0 * np.arange(1, 17) / 16.0)
    rel = -(slopes[None, :, None]) * (i[None, None, :] - i[:, None, None]).astype(np.float64)
    c['rel_mid'] = rel.astype(np.float32).reshape(128, 16 * 128)
    dt = np.arange(17)
    c['dconst'] = np.broadcast_to((-slopes[:, None] * 128.0 * dt[None, :])[None], (128, 16, 17)).astype(np.float32).reshape(128, 16 * 17).copy()
    n = np.arange(128)
    cb = slopes[None, :, None] * (16.0 * n[:, None, None] + 15.5) - slopes[None, :, None] * 128.0 * np.arange(16)[None, None, :]
    cb = slopes[None, :, None] * (15.0 * n[:, None, None] + 15.5) - slopes[None, :, None] * 128.0 * np.arange(16)[None, None, :]
    c['cmp_pb'] = cb.astype(np.float32).reshape(128, 256)
    c['cdiag'] = np.where(i[None, :] >= i[:, None], 0.0, NEGB).astype(np.float32)
    c['cfar'] = np.where(i[None, :] < i[:, None], 0.0, NEGB).astype(np.float32)
    tq = (np.arange(16)[:, None] * 128 + i[None, :])
    valid = (16 * n[:, None, None] + 31) <= tq[None]
    valid[127] = False
    c['cmp_mask'] = np.where(valid, 0.0, NEGB).astype(np.float32).reshape(128, 2048)
    bs = 16 * n
    ss = 64 * np.arange(32)
    ov = ((bs[:, None] < ss[None, :] + 64) & (bs[:, None] + 32 > ss[None, :])).astype(np.float32)
    ov[127] = 0
    c['overlap'] = ov
    t = np.arange(T)
    cur = t // 64
    jj = np.arange(32)
    forced = (jj[None, :] == 0) | (jj[None, :] == cur[:, None]) | (jj[None, :] == cur[:, None] - 1)
    validb = (64 * jj[None, :]) <= t[:, None]
    c['selc'] = np.where(validb, np.where(forced, 1e6, 0.0), -1e30).astype(np.float32)
    ex = np.zeros((32, 16, 128), np.float32)
    for kt in range(16):
        ex[2 * kt, kt, :64] = 1
        ex[2 * kt + 1, kt, 64:] = 1
    c['expand'] = ex.reshape(32, 2048)
    c['lstr'] = (i[:, None] < i[None, :]).astype(np.float32)
    c['ebase'] = np.broadcast_to((np.arange(16) * 384 + 1.0e6)[None, :], (128, 16)).astype(np.float32).copy()
    return c


CONST_SHAPES = {k: v.shape for k, v in make_consts().items()}

WNAMES = ["w_in", "gla_w_a2", "gla_b_a", "gla_norm_g", "nsa_cmp_pe", "nsa_cmp_w1", "nsa_cmp_w2",
          "w_branch_gla", "w_branch_nsa", "w_out", "ln_mix_g", "ln_mix_b", "xa_wq", "xa_wkv", "xa_wo",
          "ln_xa_g", "ln_xa_b", "router_w", "router_b", "moe_w_in", "moe_w_down", "ln_ffn_g", "ln_ffn_b"]
WSHAPES = {
    "w_in": [2, 2048, 9792], "gla_w_a2": [2, 16, 512], "gla_b_a": [2, 512], "gla_norm_g": [2, 1024],
    "nsa_cmp_pe": [2, 2, 32, 64], "nsa_cmp_w1": [2, 2, 2048, 256], "nsa_cmp_w2": [2, 2, 256, 64],
    "w_branch_gla": [2, 1024, 2048], "w_branch_nsa": [2, 1024, 2048], "w_out": [2, 2048, 2048],
    "ln_mix_g": [2, 2048], "ln_mix_b": [2, 2048], "xa_wq": [2, 2048, 512], "xa_wkv": [2, 2048, 1024],
    "xa_wo": [2, 512, 2048], "ln_xa_g": [2, 2048], "ln_xa_b": [2, 2048], "router_w": [2048, 16],
    "router_b": [16], "moe_w_in": [2, 16, 2048, 3072], "moe_w_down": [2, 16, 1536, 2048],
    "ln_ffn_g": [2, 2048], "ln_ffn_b": [2, 2048],
}


def build(n_layers=DEPTH, stages=("mix", "xa", "moe"), debug=(), wshapes=None):
    WS = dict(WSHAPES)
    WS.update(wshapes or {})
    nc = bass.Bass("TRN2", target_bir_lowering=False)
    dr = {}
    dr["x"] = nc.dram_tensor("x", [T, D], F32, kind="ExternalInput").ap()
    dr["mem"] = nc.dram_tensor("mem", [256, D], F32, kind="ExternalInput").ap()
    for k in WNAMES:
        dr[k] = nc.dram_tensor(k, WS[k], F32, kind="ExternalInput").ap()
    cst = {k: nc.dram_tensor("c_" + k, list(s), F32, kind="ExternalInput").ap() for k, s in CONST_SHAPES.items()}
    out = nc.dram_tensor("out", [T, D], F32, kind="ExternalOutput").ap()
    dbg = {k: nc.dram_tensor("dbg_" + k, list(s), F32, kind="ExternalOutput").ap() for k, s in debug}
    xres = nc.dram_tensor("xres", [T, D], F32).ap()
    hT_d = nc.dram_tensor("hT_d", [NFM, T], BF16).ap()
    h_d = nc.dram_tensor("h_d", [T, NTM], BF16).ap()
    y_d = nc.dram_tensor("y_d", [T, D], F32).ap()

    with ExitStack() as es:
        block = es.enter_context(nc.Block())

        @block.gpsimd
        def _(_g):
            _emit(nc, dr, cst, out, dbg, xres, hT_d, h_d, y_d, n_layers, stages)
    return nc


def _emit(nc, dr, cst, out, dbg, xres, hT_d, h_d, y_d, n_layers, stages):
    es = ExitStack()
    S = Sch(nc, es)

    uid = [0]

    def sb(name, shape, dt, st=es):
        uid[0] += 1
        return st.enter_context(nc.sbuf_tensor(name + '_%d' % uid[0], shape, dt))

    def ps(name, shape, dt=F32, st=es):
        return st.enter_context(nc.psum_tensor(name, shape, dt))

    bufA = sb("bufA", [128, 16, T], BF16)
    bufB = None
    Wt = [sb("Wt%d" % i, [128, 16, 512], BF16) for i in range(2)]
    ident = sb("ident", [128, 128], F32)
    identb = sb("identb", [128, 128], BF16)
    PS = [ps("ps%d" % i, [128, 512]) for i in range(8)]
    S.op('sp', lambda e: e.dma_start(out=ident[:], in_=cst['ident'][:, :]), w=['ident'], dma=True)
    S.op('dve', lambda e: e.tensor_copy(out=identb[:], in_=ident[:]), r=['ident'], w=['identb'])

    wt_i = [0]
    ps_i = [0]

    def next_ps(lo=0, hi=4):
        i = lo + ps_i[0] % (hi - lo)
        ps_i[0] += 1
        return i

    def load_w(W2d, k0, KC, c0, nb):
        b = wt_i[0] % 2
        wt_i[0] += 1
        src = W2d[k0:k0 + KC * 128, c0:c0 + nb].rearrange("(kc p) n -> p kc n", p=128)
        S.op('pool', lambda e: e.dma_start(out=Wt[b][:, 0:KC, 0:nb], in_=src), w=[('Wt', b)], dma=True)
        return b

    def proj_tm(src, skey, KC, W2d, c0, nb, epi, k0=0, tts=range(NT)):
        b = load_w(W2d, k0, KC, c0, nb)
        for tt in tts:
            pi = next_ps()
            for kc in range(KC):
                S.op('pe', lambda e, kc=kc, tt=tt, pi=pi: e.matmul(PS[pi][:, 0:nb], lhsT=src[:, kc, tt * 128:(tt + 1) * 128],
                                                                    rhs=Wt[b][:, kc, 0:nb], start=(kc == 0), stop=(kc == KC - 1)),
                     r=[('Wt', b), skey], w=[('ps', pi)], inc=(kc == KC - 1))
            epi(tt, pi)

    def proj_fm(src, skey, KC, W2d, c0, nb, M, epi, k0=0):
        b = load_w(W2d, k0, KC, c0, nb)
        for m0 in range(0, nb, M):
            mm = min(M, nb - m0)
            for tb in range(4):
                pi = next_ps()
                for kc in range(KC):
                    S.op('pe', lambda e, kc=kc, tb=tb, pi=pi, m0=m0, mm=mm: e.matmul(
                        PS[pi][0:mm, :], lhsT=Wt[b][:, kc, m0:m0 + mm], rhs=src[:, kc, tb * 512:(tb + 1) * 512],
                        start=(kc == 0), stop=(kc == KC - 1)),
                         r=[('Wt', b), skey], w=[('ps', pi)], inc=(kc == KC - 1))
                epi(c0 + m0, mm, tb, pi)

    def ln_and_store(l, st, xt_tile, key, tt, g_bc, b_bc, dst_dram, small):
        stats, mv, rstd = small
        for c in range(4):
            S.op('dve', lambda e, c=c: e.bn_stats(out=stats[:, c, :], in_=xt_tile[:, c * 512:(c + 1) * 512]), r=[key], w=['stats'])
        S.op('dve', lambda e: e.bn_aggr(out=mv[:], in_=stats[:].rearrange("p a b -> p (a b)")), r=['stats'], w=['mv'])
        S.op('act', lambda e: e.activation(out=rstd[:], in_=mv[:, 1:2], func=AF.Sqrt, bias=LN_EPS, scale=1.0), r=['mv'], w=['rstd'])
        S.op('dve', lambda e: e.reciprocal(out=rstd[:], in_=rstd[:]), r=['rstd'], w=['rstd'])
        S.op('dve', lambda e: e.tensor_scalar(out=xt_tile[:], in0=xt_tile[:], scalar1=mv[:, 0:1], scalar2=rstd[:, 0:1],
                                              op0=ALU.subtract, op1=ALU.mult), r=[key, 'mv', 'rstd'], w=[key])
        S.op('pool', lambda e: e.tensor_tensor(out=xt_tile[:], in0=xt_tile[:], in1=g_bc[:], op=ALU.mult), r=[key, 'lng'], w=[key])
        S.op('pool', lambda e: e.tensor_tensor(out=xt_tile[:], in0=xt_tile[:], in1=b_bc[:], op=ALU.add), r=[key, 'lnb'], w=[key])
        S.op('sp', lambda e: e.dma_start(out=dst_dram[tt * 128:(tt + 1) * 128, :], in_=xt_tile[:]), r=[key], w=[('xres', tt)], dma=True)
        transpose_to(xt_tile, key, 16, bufA, 'bufA', tt, F32)

    def transpose_to(tile, key, nchunks, dst, dkey, tt, dt, c_off=0):
        idm = ident if dt == F32 else identb
        for c4 in range(0, nchunks, 4):
            pi = next_ps(4, 8)
            pst = PS[pi] if dt == F32 else PSB[pi - 4]
            for c in range(c4, min(c4 + 4, nchunks)):
                S.op('pe', lambda e, c=c, c4=c4, pst=pst: e.transpose(out=pst[:, (c - c4) * 128:(c - c4 + 1) * 128],
                                                                   in_=tile[:, c * 128:(c + 1) * 128], identity=idm[:]),
                     r=[key, 'ident', 'identb'], w=[('ps', pi)])
            n = min(4, nchunks - c4)
            S.op('act', lambda e, c4=c4, n=n, pst=pst: e.activation(
                out=dst[:, c_off + c4:c_off + c4 + n, tt * 128:(tt + 1) * 128],
                in_=pst[:, 0:n * 128].rearrange("p (a b) -> p a b", a=n), func=AF.Copy), r=[('ps', pi)], w=[dkey])

    PSB = [PS[i][:].bitcast(BF16)[:, 0:512] for i in range(4, 8)]

    with ExitStack() as st:
        xt = [sb("xt%d" % i, [128, D], F32, st) for i in range(2)]
        for tt in range(NT):
            b = tt % 2
            S.op('sp', lambda e, b=b, tt=tt: e.dma_start(out=xt[b][:], in_=dr["x"][tt * 128:(tt + 1) * 128, :]), w=[('xt', b)], dma=True)
            S.op('sp', lambda e, b=b, tt=tt: e.dma_start(out=xres[tt * 128:(tt + 1) * 128, :], in_=xt[b][:]), r=[('xt', b)], w=[('xres', tt)], dma=True)
            transpose_to(xt[b], ('xt', b), 16, bufA, 'bufA', tt, F32)
        S.barrier()

    for l in range(n_layers):
        _mixer(nc, S, sb, PS, PSB, dr, cst, l, bufA, bufB, hT_d, h_d, xres, proj_tm, proj_fm, transpose_to, ln_and_store, next_ps,
               ident, identb, dbg)
        if "mixonly" in stages:
            break
        with ExitStack() as lq:
            qx = sb("qxT", [128, 4, T], BF16, lq)
            with ExitStack() as lb:
                bB = _merge(nc, S, sb, PS, PSB, dr, cst, l, bufA, hT_d, xres, proj_fm, transpose_to, next_ps, lb)

                def epi_q(col, mm, tb, pi):
                    S.op('act', lambda e: e.activation(out=qx[:, col // 128, tb * 512:(tb + 1) * 512], in_=PS[pi][:, :], func=AF.Copy, scale=float(128 ** -0.5)),
                         r=[('ps', pi)], w=['qxT'])
                proj_fm(bB, 'bufBall', 16, dr["xa_wq"][l], 0, 512, 128, epi_q)
                S.barrier()
            _xattn(nc, S, sb, PS, PSB, dr, cst, l, bufA, qx, xres, proj_tm, proj_fm, transpose_to, ln_and_store, next_ps, ident, identb, dbg, load_w, Wt)
        _moe(nc, S, sb, PS, PSB, dr, cst, l, bufA, None, xres, y_d, out if l == n_layers - 1 else xres, proj_tm, proj_fm,
             transpose_to, ln_and_store, next_ps, ident, identb, dbg, load_w, Wt)
    S.barrier()
    es.close()


def _mixer(nc, S, sb, PS, PSB, dr, cst, l, bufA, bufB, hT_d, h_d, xres, proj_tm, proj_fm, transpose_to, ln_and_store, next_ps,
           ident, identb, dbg):
    W = dr["w_in"][l]
    aT_d = nc.dram_tensor("aT_d%d" % l, [16, T], F32).ap()
    with ExitStack() as st:
        stg = [sb("stg%d" % i, [128, 512], BF16, st) for i in range(4)]
        stgf = sb("stgf", [16, 512], F32, st)
        si = [0]

        def epi_fm(rbase, cbase, func, scale=1.0):
            def f(col, mm, tb, pi):
                i = si[0] % 4
                si[0] += 1
                S.op('act', lambda e: e.activation(out=stg[i][0:mm, :], in_=PS[pi][0:mm, :], func=func, scale=scale), r=[('ps', pi)], w=[('stg', i)])
                r0 = rbase + col - cbase
                S.op('sp', lambda e: e.dma_start(out=hT_d[r0:r0 + mm, tb * 512:(tb + 1) * 512], in_=stg[i][0:mm, :]), r=[('stg', i)],
                     w=[('hT', r0 // 64, tb)] + ([('hT', r0 // 64 + 1, tb)] if mm == 128 else []), dma=True)
            return f

        def epi_ga(col, mm, tb, pi):
            S.op('act', lambda e: e.activation(out=stgf[0:16, :], in_=PS[pi][0:16, :], func=AF.Copy), r=[('ps', pi)], w=['stgf'])
            S.op('sp', lambda e: e.dma_start(out=aT_d[:, tb * 512:(tb + 1) * 512], in_=stgf[0:16, :]), r=['stgf'], w=[('aT', tb)], dma=True)

        def epi_tm(tcbase, nb, func):
            def f(tt, pi):
                i = si[0] % 4
                si[0] += 1
                S.op('act', lambda e: e.activation(out=stg[i][:, 0:nb], in_=PS[pi][:, 0:nb], func=func), r=[('ps', pi)], w=[('stg', i)])
                S.op('sp', lambda e: e.dma_start(out=h_d[tt * 128:(tt + 1) * 128, tcbase:tcbase + nb], in_=stg[i][:, 0:nb]), r=[('stg', i)],
                     w=[('h', tt, tcbase)], dma=True)
            return f

        A = (bufA, 'bufA', 16, W)
        proj_fm(*A, C_GQ, 512, 128, epi_fm(R_GQ, C_GQ, AF.Copy))
        proj_fm(*A, C_GK, 512, 128, epi_fm(R_GK, C_GK, AF.Copy))
        proj_fm(*A, C_GA, 16, 16, epi_ga)
        proj_tm(*A, C_GK, 512, epi_tm(TC_GK, 512, AF.Copy))
        for j in range(2):
            proj_tm(*A, C_GV + 512 * j, 512, epi_tm(TC_GV + 512 * j, 512, AF.Copy))
            proj_tm(*A, C_GR + 512 * j, 512, epi_tm(TC_GR + 512 * j, 512, AF.Silu))
            proj_fm(*A, C_NQ + 512 * j, 512, 64, epi_fm(R_NQ + 512 * j, C_NQ + 512 * j, AF.Copy, 0.125))
        for (cc, rr) in ((0, R_KC), (256, R_VC), (512, R_KS), (1024, R_KW)):
            proj_fm(*A, C_NKV + cc, 256, 64, epi_fm(rr, C_NKV + cc, AF.Copy))
        proj_tm(*A, C_NKV + 768, 256, epi_tm(TC_VS, 256, AF.Copy))
        proj_tm(*A, C_NKV + 1280, 256, epi_tm(TC_VW, 256, AF.Copy))
        proj_tm(*A, C_NG, 48, epi_tm(TC_NG, 48, AF.Sigmoid))
        for j in range(8):
            proj_fm(*A, C_MG + 512 * j, 512, 128, epi_fm(R_MG + 512 * j, C_MG + 512 * j, AF.Sigmoid))
        S.barrier()

    _gla(nc, S, sb, PS, PSB, dr, cst, l, bufA, hT_d, h_d, aT_d, transpose_to, ident, identb, dbg)
    _nsa(nc, S, sb, PS, PSB, dr, cst, l, bufA, hT_d, h_d, transpose_to, next_ps, ident, identb, dbg)


def _gla(nc, S, sb, PS, PSB, dr, cst, l, bufA, hT_d, h_d, aT_d, transpose_to, ident, identb, dbg):
    with ExitStack() as st:
        um = sb("um", [128, 128], F32, st)
        um2 = sb("um2", [128, 128], F32, st)
        caus4 = sb("caus4", [128, 512], F32, st)
        gnbc = sb("gnbc", [128, 1024], F32, st)
        wa2 = sb("wa2", [32, 512], F32, st)
        aT = sb("aT", [32, T], F32, st)
        S.op('sp', lambda e: e.dma_start(out=um[:], in_=cst['um'][:, :]), w=['um'], dma=True)
        S.op('sp', lambda e: e.dma_start(out=um2[:], in_=cst['um2'][:, :]), w=['um2'], dma=True)
        S.op('sp', lambda e: e.dma_start(out=caus4[:], in_=cst['caus4'][:, :]), w=['caus4'], dma=True)
        S.op('sp', lambda e: e.dma_start(out=gnbc[:], in_=dr['gla_norm_g'][l, :].partition_broadcast(128)), w=['gnbc'], dma=True)
        S.op('sp', lambda e: e.dma_start(out=wa2[0:16, :], in_=dr['gla_w_a2'][l]), w=['wa2a'], dma=True)
        S.op('sp', lambda e: e.dma_start(out=wa2[16:17, :], in_=dr['gla_b_a'][l:l + 1, :]), w=['wa2b'], dma=True)
        S.op('dve', lambda e: e.memset(aT[:], 1.0), w=['aT'])
        S.op('sp', lambda e: e.dma_start(out=aT[0:16, :], in_=aT_d[:, :]), w=['aT'], dma=True)
        S32 = sb("S32", [128, 4, 256], F32, st)
        Sb = sb("Sb", [128, 4, 256], BF16, st)
        S.op('dve', lambda e: e.memset(S32[:], 0.0), w=['S32'])
        S.op('pool', lambda e: e.memset(Sb[:], 0.0), w=['Sb'])
        qT = [sb("gqT%d" % i, [128, 4, 128], BF16, st) for i in range(2)]
        kT = [sb("gkT%d" % i, [128, 4, 128], BF16, st) for i in range(2)]
        kk = [sb("gk%d" % i, [128, 512], BF16, st) for i in range(2)]
        vv = [sb("gv%d" % i, [128, 1024], BF16, st) for i in range(2)]
        rs = [sb("grs%d" % i, [128, 1024], BF16, st) for i in range(2)]
        le = sb("le", [128, 512], F32, st)
        ll = sb("ll", [128, 512], F32, st)
        Eq = sb("Eq", [128, 512], F32, st)
        Ek = sb("Ek", [128, 512], F32, st)
        Ekh = sb("Ekh", [128, 512], F32, st)
        qs = sb("qs", [128, 512], BF16, st)
        ks = sb("ks", [128, 512], BF16, st)
        kh = sb("kh", [128, 512], BF16, st)
        att = sb("att", [128, 512], BF16, st)
        og = sb("og", [128, 1024], F32, st)
        ogb = sb("ogb", [128, 1024], BF16, st)
        grs = sb("grsf", [128, 1024], F32, st)
        stats = sb("gstats", [128, 4, 6], F32, st)
        mv = sb("gmv", [128, 4, 2], F32, st)
        rstd = sb("grstd", [128, 4], F32, st)
        for c in range(NT):
            b = c % 2
            cs = slice(c * 128, (c + 1) * 128)
            S.op('sp', lambda e: e.dma_start(out=qT[b][:], in_=hT_d[R_GQ:R_GQ + 512, cs].rearrange("(h d) t -> d h t", d=128)), w=[('qT', b)], dma=True)
            S.op('sp', lambda e: e.dma_start(out=kT[b][:], in_=hT_d[R_GK:R_GK + 512, cs].rearrange("(h d) t -> d h t", d=128)), w=[('kT', b)], dma=True)
            S.op('sp', lambda e: e.dma_start(out=kk[b][:], in_=h_d[cs, TC_GK:TC_GK + 512]), w=[('kk', b)], dma=True)
            S.op('sp', lambda e: e.dma_start(out=vv[b][:], in_=h_d[cs, TC_GV:TC_GV + 1024]), w=[('vv', b)], dma=True)
            S.op('sp', lambda e: e.dma_start(out=rs[b][:], in_=h_d[cs, TC_GR:TC_GR + 1024]), w=[('rs', b)], dma=True)
            S.op('pe', lambda e: e.matmul(PS[0][:, :], lhsT=aT[0:17, cs], rhs=wa2[0:17, :], start=True, stop=True), r=['aT', 'wa2a', 'wa2b'], w=[('ps', 0)])
            S.op('act', lambda e: e.activation(out=le[:], in_=PS[0][:, :], func=AF.Exp, scale=-1.0), r=[('ps', 0)], w=['le'])
            S.op('act', lambda e: e.activation(out=ll[:], in_=le[:], func=AF.Ln, bias=1.0, scale=1.0), r=['le'], w=['ll'])
            for h in range(4):
                S.op('pe', lambda e, h=h: e.matmul(PS[1][:, h * 128:(h + 1) * 128], lhsT=ll[:, h * 128:(h + 1) * 128], rhs=um[:], start=True, stop=True),
                     r=['ll', 'um'], w=[('ps', 1)], inc=(h == 3))
            S.op('pe', lambda e: e.matmul(PS[2][:, :], lhsT=um2[:], rhs=ll[:], start=True, stop=True), r=['ll', 'um2'], w=[('ps', 2)])
            S.op('act', lambda e: e.activation(out=Eq[:], in_=PS[1][:, :], func=AF.Exp), r=[('ps', 1)], w=['Eq'])
            S.op('act', lambda e: e.activation(out=Ek[:], in_=PS[1][:, :], func=AF.Exp, scale=-1.0), r=[('ps', 1)], w=['Ek'])
            S.op('act', lambda e: e.activation(out=Ekh[:], in_=PS[2][:, :], func=AF.Exp), r=[('ps', 2)], w=['Ekh'])
            S.op('dve', lambda e: e.scalar_tensor_tensor(out=qs[:], in0=qT[b][:].rearrange("p h t -> p (h t)"), scalar=float(128 ** -0.5), in1=Eq[:],
                                                         op0=ALU.mult, op1=ALU.mult), r=[('qT', b), 'Eq'], w=['qs'])
            S.op('dve', lambda e: e.tensor_tensor(out=ks[:], in0=kT[b][:].rearrange("p h t -> p (h t)"), in1=Ek[:], op=ALU.mult), r=[('kT', b), 'Ek'], w=['ks'])
            S.op('pool', lambda e: e.tensor_tensor(out=kh[:], in0=kk[b][:], in1=Ekh[:], op=ALU.mult), r=[('kk', b), 'Ekh'], w=['kh'])
            for h in range(4):
                hs = slice(h * 128, (h + 1) * 128)
                S.op('pe', lambda e, hs=hs: e.matmul(PS[3][:, hs], lhsT=ks[:, hs], rhs=qs[:, hs], start=True, stop=True), r=['ks', 'qs'], w=[('ps', 3)], inc=(h == 3))
            S.op('dve', lambda e: e.tensor_tensor(out=att[:], in0=PS[3][:, :], in1=caus4[:], op=ALU.mult), r=[('ps', 3), 'caus4'], w=['att'])
            for h in range(4):
                hs = slice(h * 128, (h + 1) * 128)
                pb = 4 + h // 2
                po = slice((h % 2) * 256, (h % 2) * 256 + 256)
                S.op('pe', lambda e, hs=hs, pb=pb, po=po, h=h: e.matmul(PS[pb][:, po], lhsT=att[:, hs], rhs=vv[b][:, h * 256:(h + 1) * 256], start=True, stop=False),
                     r=['att', ('vv', b)], w=[('ps', pb)], inc=False)
                S.op('pe', lambda e, hs=hs, pb=pb, po=po, h=h: e.matmul(PS[pb][:, po], lhsT=qs[:, hs], rhs=Sb[:, h, :], start=False, stop=True),
                     r=['qs', 'Sb'], w=[('ps', pb)], inc=True)
            for h in range(4):
                hs = slice(h * 128, (h + 1) * 128)
                pb = 6 + h // 2
                po = slice((h % 2) * 256, (h % 2) * 256 + 256)
                S.op('pe', lambda e, hs=hs, pb=pb, po=po, h=h: e.matmul(PS[pb][:, po], lhsT=kh[:, hs], rhs=vv[b][:, h * 256:(h + 1) * 256], start=True, stop=True),
                     r=['kh', ('vv', b)], w=[('ps', pb)])
                S.op('dve', lambda e, pb=pb, po=po, h=h: e.scalar_tensor_tensor(out=S32[:, h, :], in0=S32[:, h, :], scalar=Eq[:, h * 128 + 127:h * 128 + 128],
                                                                              in1=PS[pb][:, po], op0=ALU.mult, op1=ALU.add), r=[('ps', pb), 'Eq', 'S32'], w=['S32'])
            S.op('act', lambda e: e.activation(out=Sb[:], in_=S32[:], func=AF.Copy), r=['S32'], w=['Sb'])
            for h in range(4):
                pb = 4 + h // 2
                po = slice((h % 2) * 256, (h % 2) * 256 + 256)
                S.op('dve', lambda e, pb=pb, po=po, h=h: e.bn_stats(out=stats[:, h, :], in_=PS[pb][:, po]), r=[('ps', pb)], w=['gstats'])
                S.op('dve', lambda e, h=h: e.bn_aggr(out=mv[:, h, :], in_=stats[:, h, :]), r=['gstats'], w=['gmv'])
            S.op('act', lambda e: e.activation(out=rstd[:], in_=mv[:, :, 1], func=AF.Sqrt, bias=LN_EPS, scale=1.0), r=['gmv'], w=['grstd'])
            S.op('dve', lambda e: e.reciprocal(out=rstd[:], in_=rstd[:]), r=['grstd'], w=['grstd'])
            for h in range(4):
                pb = 4 + h // 2
                po = slice((h % 2) * 256, (h % 2) * 256 + 256)
                S.op('dve', lambda e, pb=pb, po=po, h=h: e.tensor_scalar(out=og[:, h * 256:(h + 1) * 256], in0=PS[pb][:, po], scalar1=mv[:, h, 0:1], scalar2=rstd[:, h:h + 1],
                                                                       op0=ALU.subtract, op1=ALU.mult), r=[('ps', pb), 'gmv', 'grstd'], w=['og'])
            S.op('pool', lambda e: e.tensor_tensor(out=grs[:], in0=rs[b][:], in1=gnbc[:], op=ALU.mult), r=[('rs', b), 'gnbc'], w=['grsf'])
            S.op('pool', lambda e: e.tensor_tensor(out=ogb[:], in0=og[:], in1=grs[:], op=ALU.mult), r=['og', 'grsf'], w=['ogb'])
            if 'o_gla' in dbg:
                S.op('pool', lambda e: e.tensor_tensor(out=og[:], in0=og[:], in1=grs[:], op=ALU.mult), r=['og', 'grsf'], w=['og'])
                S.op('sp', lambda e: e.dma_start(out=dbg['o_gla'][cs, :], in_=og[:]), r=['og'], w=[('dbg', c)], dma=True)
            transpose_to(ogb, 'ogb', 8, bufA, 'bufA', c, BF16)
        S.barrier()


def _resid_ln_phase(nc, S, sb, PS, st, l, Wres, KC, src, skeyf, alpha_src, gname, bname, dr, dst_dram, dstT, dstkeyf, transpose_to, ln_keys):
    xt = [sb("rl_xt%d" % i, [128, D], F32, st) for i in range(1)]
    g_bc = sb("rl_g", [128, D], F32, st)
    b_bc = sb("rl_b", [128, D], F32, st)
    stats = sb("rl_stats", [128, 4, 6], F32, st)
    mv = sb("rl_mv", [128, 2], F32, st)
    rstd = sb("rl_rstd", [128, 1], F32, st)
    S.op('sp', lambda e: e.dma_start(out=g_bc[:], in_=dr[gname][l, :].partition_broadcast(128)), w=['lng'], dma=True)
    S.op('sp', lambda e: e.dma_start(out=b_bc[:], in_=dr[bname][l, :].partition_broadcast(128)), w=['lnb'], dma=True)
    for tt in range(NT):
        b = 0
        key = ('rlxt', b)
        S.op('sp', lambda e: e.dma_start(out=xt[b][:], in_=alpha_src[tt * 128:(tt + 1) * 128, :]), r=[('xres', tt)], w=[key], dma=True)
        for cb in range(4):
            if Wres is not None:
                for kc in range(KC):
                    S.op('pe', lambda e, kc=kc, cb=cb: e.matmul(PS[cb][:, :], lhsT=src[:, kc, tt * 128:(tt + 1) * 128], rhs=Wres[:, kc, cb * 512:(cb + 1) * 512],
                                                                start=(kc == 0), stop=(kc == KC - 1)), r=[skeyf(tt), 'Wres'], w=[('ps', cb)], inc=(kc == KC - 1))
                S.op('dve', lambda e, cb=cb: e.scalar_tensor_tensor(out=xt[b][:, cb * 512:(cb + 1) * 512], in0=xt[b][:, cb * 512:(cb + 1) * 512], scalar=DN_ALPHA,
                                                                   in1=PS[cb][:, :], op0=ALU.mult, op1=ALU.add), r=[('ps', cb), key], w=[key])
            else:
                if cb == 0:
                    ytile, ykey = src(tt)
                S.op('dve', lambda e, cb=cb: e.scalar_tensor_tensor(out=xt[b][:, cb * 512:(cb + 1) * 512], in0=xt[b][:, cb * 512:(cb + 1) * 512], scalar=DN_ALPHA,
                                                                   in1=ytile[:, cb * 512:(cb + 1) * 512], op0=ALU.mult, op1=ALU.add), r=[ykey, key], w=[key])
        for c in range(4):
            S.op('dve', lambda e, c=c: e.bn_stats(out=stats[:, c, :], in_=xt[b][:, c * 512:(c + 1) * 512]), r=[key], w=['stats'])
        S.op('dve', lambda e: e.bn_aggr(out=mv[:], in_=stats[:].rearrange("p a b -> p (a b)")), r=['stats'], w=['mv'])
        S.op('act', lambda e: e.activation(out=rstd[:], in_=mv[:, 1:2], func=AF.Sqrt, bias=LN_EPS, scale=1.0), r=['mv'], w=['rstd'])
        S.op('dve', lambda e: e.reciprocal(out=rstd[:], in_=rstd[:]), r=['rstd'], w=['rstd'])
        S.op('dve', lambda e: e.tensor_scalar(out=xt[b][:], in0=xt[b][:], scalar1=mv[:, 0:1], scalar2=rstd[:, 0:1], op0=ALU.subtract, op1=ALU.mult),
             r=[key, 'mv', 'rstd'], w=[key])
        S.op('pool', lambda e: e.tensor_tensor(out=xt[b][:], in0=xt[b][:], in1=g_bc[:], op=ALU.mult), r=[key, 'lng'], w=[key])
        S.op('pool', lambda e: e.tensor_tensor(out=xt[b][:], in0=xt[b][:], in1=b_bc[:], op=ALU.add), r=[key, 'lnb'], w=[key])
        S.op('sp', lambda e: e.dma_start(out=dst_dram[tt * 128:(tt + 1) * 128, :], in_=xt[b][:]), r=[key], w=[('xres', tt)], dma=True)
        transpose_to(xt[b], key, 16, dstT, dstkeyf(tt), tt, F32)


def _load_resident(S, dst, dkey, W2d, KC):
    for kc in range(KC):
        for cb in range(4):
            S.op('pool', lambda e, kc=kc, cb=cb: e.dma_start(out=dst[:, kc, cb * 512:(cb + 1) * 512], in_=W2d[kc * 128:(kc + 1) * 128, cb * 512:(cb + 1) * 512]),
                 w=[dkey], dma=True)


def _merge(nc, S, sb, PS, PSB, dr, cst, l, bufA, hT_d, xres, proj_fm, transpose_to, next_ps, st):
    bufB = sb("bufB", [128, 16, T], BF16, st)
    with ExitStack() as s2:
        sg = [sb("sg%d" % i, [128, 512], BF16, s2) for i in range(2)]
        tmp = sb("mtmp", [128, 512], F32, s2)
        gi = [0]

        def epi(branch):
            def f(col, mm, tb, pi):
                i = gi[0] % 2
                gi[0] += 1
                r0 = R_MG + branch * 2048 + col
                S.op('sp', lambda e: e.dma_start(out=sg[i][:], in_=hT_d[r0:r0 + 128, tb * 512:(tb + 1) * 512]), w=[('sg', i)], dma=True)
                dsl = bufB[:, col // 128, tb * 512:(tb + 1) * 512]
                if branch == 0:
                    S.op('dve', lambda e: e.tensor_tensor(out=dsl, in0=PS[pi][:, :], in1=sg[i][:], op=ALU.mult), r=[('ps', pi), ('sg', i)], w=['bufB'])
                else:
                    S.op('dve', lambda e: e.tensor_tensor(out=tmp[:], in0=PS[pi][:, :], in1=sg[i][:], op=ALU.mult), r=[('ps', pi), ('sg', i)], w=['mtmp'])
                    S.op('pool', lambda e: e.tensor_tensor(out=dsl, in0=dsl, in1=tmp[:], op=ALU.add), r=['mtmp', 'bufB'], w=['bufB'])
            return f
        for j in range(4):
            proj_fm(bufA[:, 0:8, :], 'bufA', 8, dr["w_branch_gla"][l], 512 * j, 512, 128, epi(0))
        for j in range(4):
            proj_fm(bufA[:, 8:16, :], 'bufA', 8, dr["w_branch_nsa"][l], 512 * j, 512, 128, epi(1))
        S.barrier()
    with ExitStack() as s2:
        _load_resident(S, bufA, 'Wres', dr["w_out"][l], 16)
        _resid_ln_phase(nc, S, sb, PS, s2, l, bufA, 16, bufB, lambda tt: ('bufB', tt), xres, "ln_mix_g", "ln_mix_b", dr, xres, bufB,
                        lambda tt: ('bufB', tt), transpose_to, None)
        S.barrier()
    return bufB


def _xattn(nc, S, sb, PS, PSB, dr, cst, l, bufA, bufB, xres, proj_tm, proj_fm, transpose_to, ln_and_store, next_ps, ident, identb, dbg, load_w, Wt):
    with ExitStack() as st:
        memT = sb("memT", [128, 16, 256], BF16, st)
        mt_ = sb("memtile", [128, D], F32, st)
        kTs = sb("xkT", [128, 4, 256], BF16, st)
        vs = sb("xv", [128, 2, 4, 132], BF16, st)
        qx = bufB
        oT = sb("xoT", [128, 4, T], BF16, st)
        woT = sb("woT", [128, 4, D], BF16, st)
        eT = [sb("xeT%d" % i, [128, 512], BF16, st) for i in range(2)]
        oxa = sb("oxa", [128, 512], BF16, st)
        rden = sb("xrden", [128, 4], F32, st)
        for m in range(2):
            S.op('sp', lambda e: e.dma_start(out=mt_[:], in_=dr["mem"][m * 128:(m + 1) * 128, :]), w=['memtile'], dma=True)
            transpose_to(mt_, 'memtile', 16, memT, 'memT', m, F32)
        S.op('dve', lambda e: e.memset(vs[:], 1.0), w=['xv'])
        Wkv = dr["xa_wkv"][l]
        b = load_w(Wkv, 0, 16, 0, 512)
        for h in range(4):
            pi = next_ps()
            for kc in range(16):
                S.op('pe', lambda e, kc=kc: e.matmul(PS[pi][:, 0:256], lhsT=Wt[b][:, kc, h * 128:(h + 1) * 128], rhs=memT[:, kc, :], start=(kc == 0), stop=(kc == 15)),
                     r=[('Wt', b), 'memT'], w=[('ps', pi)], inc=(kc == 15))
            S.op('act', lambda e: e.activation(out=kTs[:, h, :], in_=PS[pi][:, 0:256], func=AF.Copy), r=[('ps', pi)], w=['xkT'])
        b = load_w(Wkv, 0, 16, 512, 512)
        for m in range(2):
            pi = next_ps()
            for kc in range(16):
                S.op('pe', lambda e, kc=kc: e.matmul(PS[pi][:, :], lhsT=memT[:, kc, m * 128:(m + 1) * 128], rhs=Wt[b][:, kc, :], start=(kc == 0), stop=(kc == 15)),
                     r=[('Wt', b), 'memT'], w=[('ps', pi)], inc=(kc == 15))
            S.op('act', lambda e: e.activation(out=vs[:, m, :, 0:128], in_=PS[pi][:, :].rearrange("p (h d) -> p h d", h=4), func=AF.Copy), r=[('ps', pi)], w=['xv'])

        for tt in range(NT):
            ts = slice(tt * 128, (tt + 1) * 128)
            for m in range(2):
                pi = next_ps()
                for h in range(4):
                    S.op('pe', lambda e, h=h: e.matmul(PS[pi][:, h * 128:(h + 1) * 128], lhsT=kTs[:, h, m * 128:(m + 1) * 128], rhs=qx[:, h, ts], start=True, stop=True),
                         r=['xkT', 'qxT'], w=[('ps', pi)], inc=(h == 3))
                S.op('act', lambda e: e.activation(out=eT[m][:], in_=PS[pi][:, :], func=AF.Exp), r=[('ps', pi)], w=[('xeT', m)])
            for h in range(4):
                pb = 4 + h // 2
                po = (h % 2) * 132
                for m in range(2):
                    S.op('pe', lambda e, m=m: e.matmul(PS[pb][:, po:po + 129], lhsT=eT[m][:, h * 128:(h + 1) * 128], rhs=vs[:, m, h, 0:129], start=(m == 0), stop=(m == 1)),
                         r=[('xeT', m), 'xv'], w=[('ps', pb)], inc=(m == 1))
                S.op('dve', lambda e: e.reciprocal(out=rden[:, h:h + 1], in_=PS[pb][:, po + 128:po + 129]), r=[('ps', pb)], w=['xrden'])
                S.op('dve', lambda e: e.tensor_scalar(out=oxa[:, h * 128:(h + 1) * 128], in0=PS[pb][:, po:po + 128], scalar1=rden[:, h:h + 1], scalar2=None, op0=ALU.mult),
                     r=[('ps', pb), 'xrden'], w=['oxa'])
            transpose_to(oxa, 'oxa', 4, oT, 'xoT', tt, BF16)
        S.barrier()
        for kc in range(4):
            for cb in range(4):
                S.op('pool', lambda e: e.dma_start(out=woT[:, kc, cb * 512:(cb + 1) * 512], in_=dr["xa_wo"][l][kc * 128:(kc + 1) * 128, cb * 512:(cb + 1) * 512]),
                     w=['Wres'], dma=True)
        _resid_ln_phase(nc, S, sb, PS, st, l, woT, 4, oT, lambda tt: 'xoT', xres, "ln_xa_g", "ln_xa_b", dr, xres, bufA, lambda tt: 'bufA', transpose_to, None)
        S.barrier()


MOE_C = 384
MOE_BIG = 1.0e6


def _moe(nc, S, sb, PS, PSB, dr, cst, l, bufA, bufB, xres, y_d, dst, proj_tm, proj_fm, transpose_to, ln_and_store, next_ps, ident, identb, dbg, load_w, Wt):
    C = MOE_C
    CT = C // 128
    NSL = 16 * C
    xbuf = nc.dram_tensor("moe_xbuf%d" % l, [NSL, D], BF16).ap()
    ybuf = nc.dram_tensor("moe_ybuf%d" % l, [NSL, D], F32).ap()
    I32 = mybir.dt.int32
    breg = nc.gpsimd.to_reg(NSL - 1)
    with ExitStack() as st:
        gateA = sb("gateA", [128, NT], F32, st)
        slotAi = sb("slotAi", [128, NT], I32, st)
        slotBi = sb("slotBi", [128, NT], I32, st)
        with ExitStack() as s2:
            rw = sb("rw", [128, 16, 16], BF16, s2)
            rb = sb("rb", [128, 16], F32, s2)
            lg = sb("lg", [128, 16], F32, s2)
            lb = sb("lb", [128, 4, 4], F32, s2)
            eq = sb("eq", [128, 4, 4], F32, s2)
            lb2 = sb("lb2", [128, 4, 4], F32, s2)
            m1 = sb("m1", [128, 4], F32, s2)
            m2 = sb("m2", [128, 4], F32, s2)
            gs = sb("gs", [128, 4], F32, s2)
            gm = sb("gm", [128, 1], F32, s2)
            ex = sb("ex", [128, 16], F32, s2)
            den = sb("den", [128, 1], F32, s2)
            gate = sb("gate", [128, 16], F32, s2)
            maskall = sb("maskall", [128, NT, 16], BF16, s2)
            lstr = sb("lstr", [128, 128], BF16, s2)
            onesb = sb("onesb", [128, 128], BF16, s2)
            ebase = sb("ebase", [128, 16], F32, s2)
            smat = sb("smat", [128, 16], F32, s2)
            tA = sb("tA", [128, 16], F32, s2)
            tB = sb("tB", [128, 16], F32, s2)
            sA = sb("sA", [128, 1], F32, s2)
            sB = sb("sB", [128, 1], F32, s2)
            zt = sb("zt", [128, D], BF16, s2)
            xtf = [sb("mxtf%d" % i, [128, D], F32, s2) for i in range(2)]
            xtb = [sb("mxtb%d" % i, [128, D], BF16, s2) for i in range(2)]
            S.op('pool', lambda e: e.dma_start(out=rw[:], in_=dr["router_w"].rearrange("(kc p) e -> p kc e", p=128)), w=['rw'], dma=True)
            S.op('sp', lambda e: e.dma_start(out=rb[:], in_=dr["router_b"].partition_broadcast(128)), w=['rb'], dma=True)
            S.op('pool', lambda e: e.dma_start(out=lstr[:], in_=cst['lstr'][:, :]), w=['lstr'], dma=True)
            S.op('sp', lambda e: e.dma_start(out=ebase[:], in_=cst['ebase'][:, :]), w=['ebase'], dma=True)
            S.op('dve', lambda e: e.memset(onesb[:], 1.0), w=['onesb'])
            S.op('dve', lambda e: e.memset(zt[:], 0.0), w=['zt'])
            for ex_i in range(16):
                S.op('sp', lambda e: e.dma_start(out=xbuf[ex_i * C:(ex_i + 1) * C, :].rearrange("(a p) d -> p a d", p=128),
                                                 in_=zt[:].unsqueeze(1).to_broadcast([128, CT, D])), r=['zt'], w=['xbuf'], dma=True)
            for tt in range(NT):
                ts = slice(tt * 128, (tt + 1) * 128)
                b = tt % 2
                S.op('sp', lambda e: e.dma_start(out=xtf[b][:], in_=xres[ts, :]), r=[('xres', tt)], w=[('mxtf', b)], dma=True)
                S.op('act', lambda e: e.activation(out=xtb[b][:], in_=xtf[b][:], func=AF.Copy), r=[('mxtf', b)], w=[('mxtb', b)])
                pi = next_ps()
                for kc in range(16):
                    S.op('pe', lambda e, kc=kc: e.matmul(PS[pi][:, 0:16], lhsT=bufA[:, kc, ts], rhs=rw[:, kc, :], start=(kc == 0), stop=(kc == 15)),
                         r=['bufA', 'rw'], w=[('ps', pi)], inc=(kc == 15))
                lbf = lb[:].rearrange("p a b -> p (a b)")
                eqf = eq[:].rearrange("p a b -> p (a b)")
                S.op('dve', lambda e: e.tensor_copy(out=lg[:], in_=PS[pi][:, 0:16]), r=[('ps', pi)], w=['lg'])
                S.op('dve', lambda e: e.tensor_tensor(out=lbf, in0=lg[:], in1=rb[:], op=ALU.add), r=['lg', 'rb'], w=['lb'])
                S.op('dve', lambda e: e.tensor_reduce(out=m1[:], in_=lb[:], axis=AX.X, op=ALU.max), r=['lb'], w=['m1'])
                S.op('dve', lambda e: e.tensor_tensor(out=eq[:], in0=lb[:], in1=m1[:].unsqueeze(2).to_broadcast([128, 4, 4]), op=ALU.is_equal), r=['lb', 'm1'], w=['eq'])
                S.op('dve', lambda e: e.scalar_tensor_tensor(out=lb2[:], in0=eq[:], scalar=-1e30, in1=lb[:], op0=ALU.mult, op1=ALU.add), r=['eq', 'lb'], w=['lb2'])
                S.op('dve', lambda e: e.tensor_reduce(out=m2[:], in_=lb2[:], axis=AX.X, op=ALU.max), r=['lb2'], w=['m2'])
                S.op('dve', lambda e: e.tensor_tensor(out=gs[:], in0=m1[:], in1=m2[:], op=ALU.add), r=['m1', 'm2'], w=['gs'])
                S.op('dve', lambda e: e.tensor_reduce(out=gm[:], in_=gs[:], axis=AX.X, op=ALU.max), r=['gs'], w=['gm'])
                S.op('dve', lambda e: e.tensor_scalar(out=gs[:], in0=gs[:], scalar1=gm[:, 0:1], scalar2=None, op0=ALU.is_equal), r=['gs', 'gm'], w=['gs'])
                S.op('dve', lambda e: e.tensor_tensor(out=eq[:], in0=lb[:], in1=m2[:].unsqueeze(2).to_broadcast([128, 4, 4]), op=ALU.is_ge), r=['lb', 'm2'], w=['eq'])
                S.op('dve', lambda e: e.tensor_tensor(out=eq[:], in0=eq[:], in1=gs[:].unsqueeze(2).to_broadcast([128, 4, 4]), op=ALU.mult), r=['eq', 'gs'], w=['eq'])
                S.op('act', lambda e: e.activation(out=ex[:], in_=lg[:], func=AF.Exp), r=['lg'], w=['ex'])
                S.op('dve', lambda e: e.tensor_tensor(out=ex[:], in0=ex[:], in1=eqf, op=ALU.mult), r=['ex', 'eq'], w=['ex'])
                S.op('dve', lambda e: e.tensor_reduce(out=den[:], in_=ex[:], axis=AX.X, op=ALU.add), r=['ex'], w=['den'])
                S.op('dve', lambda e: e.reciprocal(out=den[:], in_=den[:]), r=['den'], w=['den'])
                S.op('dve', lambda e: e.tensor_scalar(out=gate[:], in0=ex[:], scalar1=den[:, 0:1], scalar2=None, op0=ALU.mult), r=['ex', 'den'], w=['gate'])
                S.op('act', lambda e: e.activation(out=maskall[:, tt, :], in_=eqf, func=AF.Copy), r=['eq'], w=[('maskall', tt)])
                pj = next_ps()
                for t2 in range(tt):
                    S.op('pe', lambda e, t2=t2: e.matmul(PS[pj][:, 0:16], lhsT=onesb[:], rhs=maskall[:, t2, :], start=(t2 == 0), stop=False),
                         r=['onesb', ('maskall', t2)], w=[('ps', pj)], inc=False)
                S.op('pe', lambda e: e.matmul(PS[pj][:, 0:16], lhsT=lstr[:], rhs=maskall[:, tt, :], start=(tt == 0), stop=True),
                     r=['lstr', ('maskall', tt)], w=[('ps', pj)], inc=True)
                S.op('dve', lambda e: e.tensor_tensor(out=smat[:], in0=PS[pj][:, 0:16], in1=ebase[:], op=ALU.add), r=[('ps', pj), 'ebase'], w=['smat'])
                S.op('dve', lambda e: e.scalar_tensor_tensor(out=tA[:], in0=eqf, scalar=-MOE_BIG, in1=smat[:], op0=ALU.mult, op1=ALU.add), r=['eq', 'smat'], w=['tA'])
                S.op('dve', lambda e: e.tensor_reduce(out=sA[:], in_=tA[:], axis=AX.X, op=ALU.min), r=['tA'], w=['sA'])
                S.op('dve', lambda e: e.tensor_tensor(out=tB[:], in0=tA[:], in1=eqf, op=ALU.mult), r=['tA', 'eq'], w=['tB'])
                S.op('dve', lambda e: e.tensor_reduce(out=sB[:], in_=tB[:], axis=AX.X, op=ALU.max), r=['tB'], w=['sB'])
                S.op('dve', lambda e: e.tensor_scalar(out=tB[:], in0=tA[:], scalar1=sA[:, 0:1], scalar2=None, op0=ALU.is_equal), r=['tA', 'sA'], w=['tB'])
                S.op('dve', lambda e: e.tensor_tensor(out=tB[:], in0=tB[:], in1=gate[:], op=ALU.mult), r=['tB', 'gate'], w=['tB'])
                S.op('dve', lambda e: e.tensor_reduce(out=gateA[:, tt:tt + 1], in_=tB[:], axis=AX.X, op=ALU.add), r=['tB'], w=['gateA'])
                S.op('dve', lambda e: e.tensor_copy(out=slotAi[:, tt:tt + 1], in_=sA[:]), r=['sA'], w=['slotAi'])
                S.op('dve', lambda e: e.tensor_copy(out=slotBi[:, tt:tt + 1], in_=sB[:]), r=['sB'], w=['slotBi'])
                for sl, sk in ((slotAi, 'slotAi'), (slotBi, 'slotBi')):
                    S.op('pool', lambda e: e.indirect_dma_start(out=xbuf[:, :], out_offset=bass.IndirectOffsetOnAxis(ap=sl[:, tt:tt + 1], axis=0),
                                                                in_=xtb[b][:, :], in_offset=None, bounds_check=breg, oob_is_err=False),
                         r=[('mxtb', b), sk], w=['xbuf'], dma=True)
            S.barrier()
        with ExitStack() as s2:
            xe = [sb("xe%d" % i, [128, D], BF16, s2) for i in range(2)]
            xeT = sb("xeT", [128, 16, C], BF16, s2)
            actT = sb("actT", [128, 12, C], BF16, s2)
            ystg = [sb("ystg%d" % i, [128, 512], F32, s2) for i in range(3)]
            yi = [0]
            xi = [0]
            for ex_i in range(16):
                Wi = dr["moe_w_in"][l, ex_i]
                Wd = dr["moe_w_down"][l, ex_i]
                for sti in range(CT):
                    b = xi[0] % 2
                    xi[0] += 1
                    r0 = ex_i * C + sti * 128
                    S.op('sp', lambda e: e.dma_start(out=xe[b][:], in_=xbuf[r0:r0 + 128, :]), w=[('xe', b)], dma=True)
                    transpose_to(xe[b], ('xe', b), 16, xeT, 'xeT', sti, BF16)
                for j in range(6):
                    wb = load_w(Wi, 0, 16, 512 * j, 512)
                    for m in range(4):
                        pi = next_ps()
                        for kc in range(16):
                            S.op('pe', lambda e, kc=kc: e.matmul(PS[pi][:, 0:C], lhsT=Wt[wb][:, kc, m * 128:(m + 1) * 128], rhs=xeT[:, kc, :], start=(kc == 0), stop=(kc == 15)),
                                 r=[('Wt', wb), 'xeT'], w=[('ps', pi)], inc=(kc == 15))
                        fc = (j * 4 + m) % 12
                        if j < 3:
                            S.op('act', lambda e: e.activation(out=actT[:, fc, :], in_=PS[pi][:, 0:C], func=AF.Silu), r=[('ps', pi)], w=[('actT', fc)])
                        else:
                            S.op('dve', lambda e: e.tensor_tensor(out=actT[:, fc, :], in0=actT[:, fc, :], in1=PS[pi][:, 0:C], op=ALU.mult), r=[('ps', pi), ('actT', fc)], w=[('actT', fc)])
                for cb in range(4):
                    wb = load_w(Wd, 0, 12, 512 * cb, 512)
                    for sti in range(CT):
                        pi = next_ps()
                        for fc in range(12):
                            S.op('pe', lambda e, fc=fc: e.matmul(PS[pi][:, :], lhsT=actT[:, fc, sti * 128:(sti + 1) * 128], rhs=Wt[wb][:, fc, :], start=(fc == 0), stop=(fc == 11)),
                                 r=[('Wt', wb), ('actT', fc)], w=[('ps', pi)], inc=(fc == 11))
                        i = yi[0] % 3
                        yi[0] += 1
                        S.op('act', lambda e: e.activation(out=ystg[i][:], in_=PS[pi][:, :], func=AF.Copy), r=[('ps', pi)], w=[('ystg', i)])
                        r0 = ex_i * C + sti * 128
                        S.op('sp', lambda e: e.dma_start(out=ybuf[r0:r0 + 128, cb * 512:(cb + 1) * 512], in_=ystg[i][:]), r=[('ystg', i)], w=['ybuf'], dma=True)
            S.barrier()
        with ExitStack() as s2:
            yA = sb("yA", [128, D], F32, s2)
            yB = sb("yB", [128, D], F32, s2)

            def yfn(tt):
                S.op('dve', lambda e: e.memset(yA[:], 0.0), w=['yA'])
                S.op('dve', lambda e: e.memset(yB[:], 0.0), w=['yB'])
                S.op('pool', lambda e: e.indirect_dma_start(out=yA[:, :], out_offset=None, in_=ybuf[:, :],
                                                            in_offset=bass.IndirectOffsetOnAxis(ap=slotAi[:, tt:tt + 1], axis=0), bounds_check=breg, oob_is_err=False),
                     r=['slotAi'], w=['yA', 'gth'], dma=True)
                S.op('pool', lambda e: e.indirect_dma_start(out=yB[:, :], out_offset=None, in_=ybuf[:, :],
                                                            in_offset=bass.IndirectOffsetOnAxis(ap=slotBi[:, tt:tt + 1], axis=0), bounds_check=breg, oob_is_err=False),
                     r=['slotBi'], w=['yB', 'gth'], dma=True)
                S.op('dve', lambda e: e.tensor_tensor(out=yA[:], in0=yA[:], in1=yB[:], op=ALU.subtract), r=['yA', 'yB'], w=['yA'])
                S.op('dve', lambda e: e.scalar_tensor_tensor(out=yA[:], in0=yA[:], scalar=gateA[:, tt:tt + 1], in1=yB[:], op0=ALU.mult, op1=ALU.add),
                     r=['yA', 'yB', 'gateA'], w=['yA'])
                return yA, 'yA'
            _resid_ln_phase(nc, S, sb, PS, s2, l, None, 0, yfn, None, xres, "ln_ffn_g", "ln_ffn_b", dr, dst, bufA, lambda tt: 'bufA', transpose_to, None)
            S.barrier()


def _nsa(nc, S, sb, PS, PSB, dr, cst, l, bufA, hT_d, h_d, transpose_to, next_ps, ident, identb, dbg):
    slopes = [2.0 ** (-8.0 * (h + 1) / 16.0) for h in range(16)]
    with ExitStack() as st:
        rel = sb("rel", [128, 2048], F32, st)
        cdiag = sb("cdiag", [128, 128], F32, st)
        cfar = sb("cfar", [128, 128], F32, st)
        dcon = sb("dcon", [128, 272], F32, st)
        cpb = sb("cpb", [128, 256], F32, st)
        cmask = sb("cmask", [128, 2048], BF16, st)
        selc = sb("selc", [128, NT, 32], F32, st)
        expd = sb("expd", [32, 2048], BF16, st)
        ng = sb("ng", [128, NT, 48], BF16, st)
        kcT = sb("kcT", [64, 4, 128], BF16, st)
        vca = sb("vca", [128, 4, 97], BF16, st)
        S.op('sp', lambda e: e.dma_start(out=rel[:], in_=cst['rel_mid'][:, :]), w=['rel'], dma=True)
        S.op('sp', lambda e: e.dma_start(out=cdiag[:], in_=cst['cdiag'][:, :]), w=['cdiag'], dma=True)
        S.op('sp', lambda e: e.dma_start(out=cfar[:], in_=cst['cfar'][:, :]), w=['cfar'], dma=True)
        S.op('sp', lambda e: e.dma_start(out=dcon[:], in_=cst['dconst'][:, :]), w=['dcon'], dma=True)
        S.op('sp', lambda e: e.dma_start(out=cpb[:], in_=cst['cmp_pb'][:, :]), w=['cpb'], dma=True)
        S.op('pool', lambda e: e.dma_start(out=cmask[:], in_=cst['cmp_mask'][:, :]), w=['cmask'], dma=True)
        S.op('sp', lambda e: e.dma_start(out=selc[:], in_=cst['selc'].rearrange("(tt p) j -> p tt j", p=128)), w=['selc'], dma=True)
        S.op('pool', lambda e: e.dma_start(out=expd[:], in_=cst['expand'][:, :]), w=['expd'], dma=True)
        S.op('sp', lambda e: e.dma_start(out=ng[:], in_=h_d[:, TC_NG:TC_NG + 48].rearrange("(tt p) j -> p tt j", p=128)), w=['ng'], dma=True)
        S.op('dve', lambda e: e.memset(kcT[:], 0.0), w=['kcT'])
        S.op('dve', lambda e: e.memset(vca[:], 0.0), w=['vca'])
        with ExitStack() as s2:
            w1 = sb("w1", [64, 2, 32, 256], BF16, s2)
            w2 = sb("w2", [128, 2, 2, 64], BF16, s2)
            pes = sb("pes", [32, 2, 64], F32, s2)
            peT = sb("peT", [64, 2, 32], BF16, s2)
            c1 = sb("c1", [128, 2, 2], F32, s2)
            srcT = sb("csrcT", [64, T], BF16, s2)
            u = sb("cu", [128, 128], F32, s2)
            t1 = sb("ct1", [128, 128], F32, s2)
            gel = sb("cgel", [128, 2, 128], BF16, s2)
            ovl = sb("ovl", [128, 32], F32, s2)
            for kv in range(2):
                S.op('pool', lambda e: e.dma_start(out=w1[:, kv, :, :], in_=dr["nsa_cmp_w1"][l, kv].rearrange("(l d) h -> d l h", d=64)), w=['w1'], dma=True)
                S.op('pool', lambda e: e.dma_start(out=w2[:, kv, :, :], in_=dr["nsa_cmp_w2"][l, kv].rearrange("(hc p) d -> p hc d", p=128)), w=['w2'], dma=True)
            S.op('sp', lambda e: e.dma_start(out=pes[:], in_=dr["nsa_cmp_pe"][l].rearrange("k l d -> l k d")), w=['pes'], dma=True)
            S.op('sp', lambda e: e.dma_start(out=ovl[:], in_=cst['overlap'][:, :]), w=['ovl'], dma=True)
            for g in range(4):
                S.op('dve', lambda e: e.memset(vca[:, g, 64:65], 1.0), r=[], w=['vca'])
                S.op('dve', lambda e: e.tensor_copy(out=vca[:, g, 65:97], in_=ovl[:]), r=['ovl'], w=['vca'])
            for kv in range(2):
                S.op('pe', lambda e: e.transpose(out=PS[0][0:64, 0:32], in_=pes[0:32, kv, :], identity=ident[0:32, 0:32]), r=['pes', 'ident'], w=[('ps', 0)])
                S.op('act', lambda e: e.activation(out=peT[:, kv, :], in_=PS[0][0:64, 0:32], func=AF.Copy), r=[('ps', 0)], w=['peT'])
                for hc in range(2):
                    for li in range(32):
                        S.op('pe', lambda e, li=li: e.matmul(PS[1][:, 0:1], lhsT=w1[:, kv, li, hc * 128:(hc + 1) * 128], rhs=peT[:, kv, li:li + 1], start=(li == 0), stop=(li == 31)),
                             r=['w1', 'peT'], w=[('ps', 1)], inc=(li == 31))
                    S.op('act', lambda e: e.activation(out=c1[:, kv, hc:hc + 1], in_=PS[1][:, 0:1], func=AF.Copy), r=[('ps', 1)], w=['c1'])
            for g in range(4):
                for kv in range(2):
                    r0 = (R_KC if kv == 0 else R_VC) + g * 64
                    S.op('sp', lambda e: e.dma_start(out=srcT[:], in_=hT_d[r0:r0 + 64, :]), w=['csrcT'], dma=True)
                    for hc in range(2):
                        pi = next_ps()
                        for li in range(32):
                            S.op('pe', lambda e, li=li: e.matmul(PS[pi][:, 0:127], lhsT=w1[:, kv, li, hc * 128:(hc + 1) * 128], rhs=srcT[:, li:li + 16 * 126 + 1:16],
                                                                 start=(li == 0), stop=(li == 31)), r=['w1', 'csrcT'], w=[('ps', pi)], inc=(li == 31))
                        S.op('act', lambda e: e.activation(out=u[:, 0:127], in_=PS[pi][:, 0:127], func=AF.Identity, bias=c1[:, kv, hc:hc + 1], scale=1.0), r=[('ps', pi), 'c1'], w=['cu'])
                        S.op('dve', lambda e: e.tensor_tensor(out=t1[:, 0:127], in0=u[:, 0:127], in1=u[:, 0:127], op=ALU.mult), r=['cu'], w=['ct1'])
                        S.op('dve', lambda e: e.tensor_scalar(out=t1[:, 0:127], in0=t1[:, 0:127], scalar1=0.044715, scalar2=1.0, op0=ALU.mult, op1=ALU.add), r=['ct1'], w=['ct1'])
                        S.op('dve', lambda e: e.tensor_tensor(out=t1[:, 0:127], in0=t1[:, 0:127], in1=u[:, 0:127], op=ALU.mult), r=['ct1', 'cu'], w=['ct1'])
                        S.op('act', lambda e: e.activation(out=t1[:, 0:127], in_=t1[:, 0:127], func=AF.Sigmoid, scale=2.0 * 0.7978845608028654), r=['ct1'], w=['ct1'])
                        S.op('dve', lambda e: e.tensor_tensor(out=gel[:, hc, 0:127], in0=t1[:, 0:127], in1=u[:, 0:127], op=ALU.mult), r=['ct1', 'cu'], w=['cgel'])
                    pi = next_ps()
                    if kv == 0:
                        for hc in range(2):
                            S.op('pe', lambda e, hc=hc: e.matmul(PS[pi][0:64, 0:127], lhsT=w2[:, 0, hc, :], rhs=gel[:, hc, 0:127], start=(hc == 0), stop=(hc == 1)),
                                 r=['w2', 'cgel'], w=[('ps', pi)], inc=(hc == 1))
                        S.op('act', lambda e: e.activation(out=kcT[:, g, 0:127], in_=PS[pi][0:64, 0:127], func=AF.Copy), r=[('ps', pi)], w=['kcT'])
                    else:
                        for hc in range(2):
                            S.op('pe', lambda e, hc=hc: e.matmul(PS[pi][0:127, 0:64], lhsT=gel[:, hc, 0:127], rhs=w2[:, 1, hc, :], start=(hc == 0), stop=(hc == 1)),
                                 r=['w2', 'cgel'], w=[('ps', pi)], inc=(hc == 1))
                        S.op('act', lambda e: e.activation(out=vca[0:127, g, 0:64], in_=PS[pi][0:127, 0:64], func=AF.Copy), r=[('ps', pi)], w=['vca'])
            S.barrier()
        qT = sb("nqT", [64, 4, T], BF16, st)
        ksT = sb("nksT", [64, T], BF16, st)
        kwT = sb("nkwT", [64, T], BF16, st)
        vs = sb("nvs", [128, NT, 65], BF16, st)
        vw = sb("nvw", [128, NT, 65], BF16, st)
        sc = [sb("nsc%d" % i, [128, 512], F32, st) for i in range(2)]
        eTa = sb("neT", [128, NT + 1, 512], BF16, st)
        imp = sb("nimp", [128, 32], F32, st)
        mx8 = sb("nmx8", [128, 8], F32, st)
        selb = sb("nselb", [128, 32], F32, st)
        selbT = sb("nselbT", [32, 128], BF16, st)
        rd = sb("nrd", [128, 1], F32, st)
        oc = sb("noc", [128, 256], F32, st)
        ocb = sb("nocb", [128, 256], BF16, st)
        sci = [0]
        for g in range(4):
            S.op('sp', lambda e: e.dma_start(out=qT[:], in_=hT_d[R_NQ + g * 256:R_NQ + (g + 1) * 256, :].rearrange("(h d) t -> d h t", d=64)), w=['nqT'], dma=True)
            S.op('sp', lambda e: e.dma_start(out=ksT[:], in_=hT_d[R_KS + g * 64:R_KS + (g + 1) * 64, :]), w=['nksT'], dma=True)
            S.op('sp', lambda e: e.dma_start(out=kwT[:], in_=hT_d[R_KW + g * 64:R_KW + (g + 1) * 64, :]), w=['nkwT'], dma=True)
            S.op('dve', lambda e: e.memset(vs[:], 1.0), w=['nvs'])
            S.op('dve', lambda e: e.memset(vw[:], 1.0), w=['nvw'])
            S.op('sp', lambda e: e.dma_start(out=vs[:, :, 0:64], in_=h_d[:, TC_VS + g * 64:TC_VS + (g + 1) * 64].rearrange("(kt p) d -> p kt d", p=128)), w=['nvs'], dma=True)
            S.op('sp', lambda e: e.dma_start(out=vw[:, :, 0:64], in_=h_d[:, TC_VW + g * 64:TC_VW + (g + 1) * 64].rearrange("(kt p) d -> p kt d", p=128)), w=['nvw'], dma=True)
            for tt in range(NT):
                ts = slice(tt * 128, (tt + 1) * 128)

                def scores(kTsrc, kkey, kt, mode, use_sel, slot, extra_mask):
                    pi = next_ps(0, 4)
                    for h in range(4):
                        lhs = kcT[:, g, :] if mode == 'cmp' else kTsrc[:, kt * 128:(kt + 1) * 128]
                        S.op('pe', lambda e, h=h: e.matmul(PS[pi][:, h * 128:(h + 1) * 128], lhsT=lhs, rhs=qT[:, h, ts], start=True, stop=not use_sel),
                             r=[kkey, 'nqT'], w=[('ps', pi)], inc=(not use_sel) and h == 3)
                        if use_sel:
                            S.op('pe', lambda e, h=h: e.matmul(PS[pi][:, h * 128:(h + 1) * 128], lhsT=expd[:, kt * 128:(kt + 1) * 128], rhs=selbT[:, :], start=False, stop=True),
                                 r=['expd', 'nselbT'], w=[('ps', pi)], inc=(h == 3))
                    si = sci[0] % 2
                    sci[0] += 1
                    S.op('dve', lambda e: e.tensor_tensor(out=sc[si][:], in0=PS[pi][:, :], in1=rel[:, g * 512:(g + 1) * 512], op=ALU.add), r=[('ps', pi), 'rel'], w=[('nsc', si)])
                    if extra_mask is not None:
                        mk, mkey = extra_mask
                        S.op('pool', lambda e: e.tensor_tensor(out=sc[si][:].rearrange("p (h j) -> p h j", h=4), in0=sc[si][:].rearrange("p (h j) -> p h j", h=4),
                                                               in1=mk.unsqueeze(1).to_broadcast([128, 4, 128]), op=ALU.add), r=[('nsc', si), mkey], w=[('nsc', si)])
                    for h in range(4):
                        hh = 4 * g + h
                        if mode == 'cmp':
                            bia = cpb[:, hh * 16 + tt:hh * 16 + tt + 1]
                        else:
                            bia = dcon[:, hh * 17 + (tt - kt):hh * 17 + (tt - kt) + 1]
                        S.op('act', lambda e, h=h: e.activation(out=eTa[:, slot, h * 128:(h + 1) * 128], in_=sc[si][:, h * 128:(h + 1) * 128], func=AF.Exp, bias=bia, scale=1.0),
                             r=[('nsc', si), 'cpb', 'dcon'], w=[('neT', slot)], inc=True)

                def pv_combine(slots, vfn, ncol, br, first):
                    for h in range(4):
                        pb = 4 + h
                        for i, (slot, kt) in enumerate(slots):
                            S.op('pe', lambda e, i=i, slot=slot, kt=kt: e.matmul(PS[pb][:, 0:ncol], lhsT=eTa[:, slot, h * 128:(h + 1) * 128], rhs=vfn(kt),
                                                                              start=(i == 0), stop=(i == len(slots) - 1)),
                                 r=[('neT', slot), 'nvs', 'nvw', 'vca'], w=[('ps', pb)], inc=(i == len(slots) - 1))
                        S.op('dve', lambda e: e.tensor_scalar(out=rd[:], in0=PS[pb][:, 64:65], scalar1=1e-30, scalar2=None, op0=ALU.max), r=[('ps', pb)], w=['nrd'])
                        S.op('dve', lambda e: e.reciprocal(out=rd[:], in_=rd[:]), r=['nrd'], w=['nrd'])
                        if br == 0:
                            if h == 0:
                                S.op('dve', lambda e: e.tensor_scalar(out=imp[:], in0=PS[pb][:, 65:97], scalar1=rd[:, 0:1], scalar2=None, op0=ALU.mult), r=[('ps', pb), 'nrd'], w=['nimp'])
                            else:
                                S.op('dve', lambda e: e.scalar_tensor_tensor(out=imp[:], in0=PS[pb][:, 65:97], scalar=rd[:, 0:1], in1=imp[:], op0=ALU.mult, op1=ALU.add),
                                     r=[('ps', pb), 'nrd', 'nimp'], w=['nimp'])
                        gcol = (4 * g + h) * 3 + br
                        S.op('dve', lambda e: e.tensor_tensor(out=rd[:], in0=rd[:], in1=ng[:, tt, gcol:gcol + 1], op=ALU.mult), r=['nrd', 'ng'], w=['nrd'])
                        if first:
                            S.op('dve', lambda e: e.tensor_scalar(out=oc[:, h * 64:(h + 1) * 64], in0=PS[pb][:, 0:64], scalar1=rd[:, 0:1], scalar2=None, op0=ALU.mult),
                                 r=[('ps', pb), 'nrd'], w=['noc'])
                        else:
                            S.op('dve', lambda e: e.scalar_tensor_tensor(out=oc[:, h * 64:(h + 1) * 64], in0=PS[pb][:, 0:64], scalar=rd[:, 0:1], in1=oc[:, h * 64:(h + 1) * 64],
                                                                         op0=ALU.mult, op1=ALU.add), r=[('ps', pb), 'nrd', 'noc'], w=['noc'])

                scores(None, 'kcT', 0, 'cmp', False, 16, (cmask[:, ts], 'cmask'))
                pv_combine([(16, 0)], lambda kt: vca[:, g, 0:97], 97, 0, True)
                S.op('dve', lambda e: e.tensor_tensor(out=imp[:], in0=imp[:], in1=selc[:, tt, :], op=ALU.add), r=['nimp', 'selc'], w=['nimp'])
                S.op('dve', lambda e: e.max(out=mx8[:], in_=imp[:]), r=['nimp'], w=['nmx8'])
                S.op('dve', lambda e: e.tensor_scalar(out=mx8[:, 7:8], in0=mx8[:, 7:8], scalar1=-5e29, scalar2=None, op0=ALU.max), r=['nmx8'], w=['nmx8'])
                S.op('dve', lambda e: e.tensor_scalar(out=selb[:], in0=imp[:], scalar1=mx8[:, 7:8], scalar2=NEGB, op0=ALU.is_lt, op1=ALU.mult), r=['nimp', 'nmx8'], w=['nselb'])
                S.op('pe', lambda e: e.transpose(out=PS[3][0:32, 0:128], in_=selb[:, :], identity=ident[:]), r=['nselb', 'ident'], w=[('ps', 3)])
                S.op('act', lambda e: e.activation(out=selbT[:, :], in_=PS[3][0:32, 0:128], func=AF.Copy), r=[('ps', 3)], w=['nselbT'])
                for kt in range(tt + 1):
                    scores(ksT, 'nksT', kt, 'rel', True, kt, (cdiag[:], 'cdiag') if kt == tt else None)
                pv_combine([(kt, kt) for kt in range(tt + 1)], lambda kt: vs[:, kt, :], 65, 1, False)
                kts = list(range(max(0, tt - 4), tt + 1))
                for kt in kts:
                    em = (cdiag[:], 'cdiag') if kt == tt else ((cfar[:], 'cfar') if kt == tt - 4 else None)
                    scores(kwT, 'nkwT', kt, 'rel', False, kt, em)
                pv_combine([(kt, kt) for kt in kts], lambda kt: vw[:, kt, :], 65, 2, False)
                S.op('act', lambda e: e.activation(out=ocb[:], in_=oc[:], func=AF.Copy), r=['noc'], w=['nocb'])
                transpose_to(ocb, 'nocb', 2, bufA, 'bufA', tt, BF16, c_off=8 + 2 * g)
        S.barrier()


_NC_CACHE = {}


def kernel(**inputs):
    n = 8
    if "nc" not in _NC_CACHE:
        _NC_CACHE["nc"] = build()
    nc = _NC_CACHE["nc"]
    consts = make_consts()
    in_maps = []
    for c in range(n):
        m = {"x": np.ascontiguousarray(inputs["x"][c], dtype=np.float32), "mem": np.ascontiguousarray(inputs["mem"][c], dtype=np.float32)}
        for k in WNAMES:
            m[k] = np.ascontiguousarray(inputs[k], dtype=np.float32)
        for k, v in consts.items():
            m["c_" + k] = v
        in_maps.append(m)
    res = run_bass_kernel_spmd(nc, in_maps, core_ids=list(range(n)))
    return np.stack([res.results[c]["out"] for c in range(n)], axis=0).astype(np.float32)
```

```python
import numpy as np
from contextlib import ExitStack
import concourse.bass as bass
import concourse.mybir as mybir
from concourse.bass_utils import run_bass_kernel_spmd

F32 = mybir.dt.float32
BF16 = mybir.dt.bfloat16
AF = mybir.ActivationFunctionType
ALU = mybir.AluOpType
AX = mybir.AxisListType

T = 2048
D = 2048
NT = 16
DEPTH = 2
DN_ALPHA = float((2 * DEPTH) ** 0.25)
LN_EPS = 1e-5
D_IN = 9792
C_GQ, C_GK, C_GV, C_GR, C_GA, C_NQ, C_NKV, C_NG, C_MG = 0, 512, 1024, 2048, 3072, 3088, 4112, 5648, 5696
R_GQ, R_GK, R_GA, R_NQ, R_KC, R_VC, R_KS, R_KW, R_MG, NFM = 0, 512, 1024, 1056, 2080, 2336, 2592, 2848, 3104, 7200
TC_GK, TC_GV, TC_GR, TC_VS, TC_VW, TC_NG, NTM = 0, 512, 1536, 2560, 2816, 3072, 3120
NEGB = -30000.0


class Sch:
    def __init__(self, nc, es):
        self.nc = nc
        self.eng = {'pe': nc.tensor, 'act': nc.scalar, 'dve': nc.vector, 'pool': nc.gpsimd, 'sp': nc.sync}
        self.sem = {}
        self.cnt = {}
        for e in ('pe', 'act', 'dve', 'pool'):
            self.sem[e] = es.enter_context(nc.semaphore('s_' + e))
            self.cnt[e] = 0
        self.NS = 8
        for q in ('sp', 'pool'):
            for i in range(self.NS):
                k = ('dma', q, i)
                self.sem[k] = es.enter_context(nc.semaphore('d_%s%d' % (q, i)))
                self.cnt[k] = 0
        self.dma_i = {'sp': 0, 'pool': 0}
        self.waited = {e: {} for e in self.eng}
        self.lw = {}
        self.rd = {}
        self.nops = 0

    def _wait(self, e, tok):
        s, v = tok
        if self.waited[e].get(s, 0) >= v:
            return
        self.waited[e][s] = v
        self.eng[e].wait_ge(self.sem[s], v)

    def op(self, e, fn, r=(), w=(), dma=False, inc=True):
        deps = []
        for k in r:
            if k in self.lw:
                deps.append(self.lw[k])
        for k in w:
            if k in self.lw:
                deps.append(self.lw[k])
            deps.extend(self.rd.get(k, {}).values())
        if dma:
            i = self.dma_i[e]
            self.dma_i[e] += 1
            s = ('dma', e, i % self.NS)
            if self.cnt[s] > 0:
                deps.append((s, self.cnt[s]))
            self.cnt[s] += 16
            tok = (s, self.cnt[s])
        else:
            s = e
            if inc:
                self.cnt[s] += 1
                tok = (s, self.cnt[s])
            else:
                tok = (s, self.cnt[s] + 1)
        for d in deps:
            if d[0] == 'pe' and e == 'pe' and not dma:
                continue
            self._wait(e, d)
        ins = fn(self.eng[e])
        if dma:
            ins.then_inc(self.sem[s], 16)
        elif inc:
            ins.then_inc(self.sem[s], 1)
        for k in w:
            self.lw[k] = tok
            self.rd[k] = {}
        for k in r:
            self.rd.setdefault(k, {})[tok[0]] = tok
        self.nops += 1
        return tok

    def barrier(self):
        for e in self.eng:
            for s, c in self.cnt.items():
                if c > 0:
                    self._wait(e, (s, c))
        self.lw.clear()
        self.rd.clear()


def make_consts():
    c = {}
    i = np.arange(128)
    c['ident'] = np.eye(128, dtype=np.float32)
    c['um'] = (-(1.0 / 16.0) * (i[:, None] <= i[None, :])).astype(np.float32)
    c['um2'] = (-(1.0 / 16.0) * (i[:, None] > i[None, :])).astype(np.float32)
    c['caus4'] = np.tile((i[:, None] <= i[None, :]).astype(np.float32), (1, 4))
    slopes = 2.0 ** (-8.0 * np.arange(1, 17) / 16.0)
    rel = -(slopes[None, :, None]) * (i[None, None, :] - i[:, None, None]).astype(np.float64)
    c['rel_mid'] = rel.astype(np.float32).reshape(128, 16 * 128)
    dt = np.arange(17)
    c['dconst'] = np.broadcast_to((-slopes[:, None] * 128.0 * dt[None, :])[None], (128, 16, 17)).astype(np.float32).reshape(128, 16 * 17).copy()
    n = np.arange(128)
    cb = slopes[None, :, None] * (16.0 * n[:, None, None] + 15.5) - slopes[None, :, None] * 128.0 * np.arange(16)[None, None, :]
    cb = slopes[None, :, None] * (15.0 * n[:, None, None] + 15.5) - slopes[None, :, None] * 128.0 * np.arange(16)[None, None, :]
    c['cmp_pb'] = cb.astype(np.float32).reshape(128, 256)
    c['cdiag'] = np.where(i[None, :] >= i[:, None], 0.0, NEGB).astype(np.float32)
    c['cfar'] = np.where(i[None, :] < i[:, None], 0.0, NEGB).astype(np.float32)
    tq = (np.arange(16)[:, None] * 128 + i[None, :])
    valid = (16 * n[:, None, None] + 31) <= tq[None]
    valid[127] = False
    c['cmp_mask'] = np.where(valid, 0.0, NEGB).astype(np.float32).reshape(128, 2048)
    bs = 16 * n
    ss = 64 * np.arange(32)
    ov = ((bs[:, None] < ss[None, :] + 64) & (bs[:, None] + 32 > ss[None, :])).astype(np.float32)
    ov[127] = 0
    c['overlap'] = ov
    t = np.arange(T)
    cur = t // 64
    jj = np.arange(32)
    forced = (jj[None, :] == 0) | (jj[None, :] == cur[:, None]) | (jj[None, :] == cur[:, None] - 1)
    validb = (64 * jj[None, :]) <= t[:, None]
    c['selc'] = np.where(validb, np.where(forced, 1e6, 0.0), -1e30).astype(np.float32)
    ex = np.zeros((32, 16, 128), np.float32)
    for kt in range(16):
        ex[2 * kt, kt, :64] = 1
        ex[2 * kt + 1, kt, 64:] = 1
    c['expand'] = ex.reshape(32, 2048)
    c['lstr'] = (i[:, None] < i[None, :]).astype(np.float32)
    c['ebase'] = np.broadcast_to((np.arange(16) * 384 + 1.0e6)[None, :], (128, 16)).astype(np.float32).copy()
    return c


CONST_SHAPES = {k: v.shape for k, v in make_consts().items()}

WNAMES = ["w_in", "gla_w_a2", "gla_b_a", "gla_norm_g", "nsa_cmp_pe", "nsa_cmp_w1", "nsa_cmp_w2",
          "w_branch_gla", "w_branch_nsa", "w_out", "ln_mix_g", "ln_mix_b", "xa_wq", "xa_wkv", "xa_wo",
          "ln_xa_g", "ln_xa_b", "router_w", "router_b", "moe_w_in", "moe_w_down", "ln_ffn_g", "ln_ffn_b"]
WSHAPES = {
    "w_in": [2, 2048, 9792], "gla_w_a2": [2, 16, 512], "gla_b_a": [2, 512], "gla_norm_g": [2, 1024],
    "nsa_cmp_pe": [2, 2, 32, 64], "nsa_cmp_w1": [2, 2, 2048, 256], "nsa_cmp_w2": [2, 2, 256, 64],
    "w_branch_gla": [2, 1024, 2048], "w_branch_nsa": [2, 1024, 2048], "w_out": [2, 2048, 2048],
    "ln_mix_g": [2, 2048], "ln_mix_b": [2, 2048], "xa_wq": [2, 2048, 512], "xa_wkv": [2, 2048, 1024],
    "xa_wo": [2, 512, 2048], "ln_xa_g": [2, 2048], "ln_xa_b": [2, 2048], "router_w": [2048, 16],
    "router_b": [16], "moe_w_in": [2, 16, 2048, 3072], "moe_w_down": [2, 16, 1536, 2048],
    "ln_ffn_g": [2, 2048], "ln_ffn_b": [2, 2048],
}


def build(n_layers=DEPTH, stages=("mix", "xa", "moe"), debug=(), wshapes=None):
    WS = dict(WSHAPES)
    WS.update(wshapes or {})
    nc = bass.Bass("TRN2", target_bir_lowering=False)
    dr = {}
    dr["x"] = nc.dram_tensor("x", [T, D], F32, kind="ExternalInput").ap()
    dr["mem"] = nc.dram_tensor("mem", [256, D], F32, kind="ExternalInput").ap()
    for k in WNAMES:
        dr[k] = nc.dram_tensor(k, WS[k], F32, kind="ExternalInput").ap()
    cst = {k: nc.dram_tensor("c_" + k, list(s), F32, kind="ExternalInput").ap() for k, s in CONST_SHAPES.items()}
    out = nc.dram_tensor("out", [T, D], F32, kind="ExternalOutput").ap()
    dbg = {k: nc.dram_tensor("dbg_" + k, list(s), F32, kind="ExternalOutput").ap() for k, s in debug}
    xres = nc.dram_tensor("xres", [T, D], F32).ap()
    hT_d = nc.dram_tensor("hT_d", [NFM, T], BF16).ap()
    h_d = nc.dram_tensor("h_d", [T, NTM], BF16).ap()
    y_d = nc.dram_tensor("y_d", [T, D], F32).ap()

    with ExitStack() as es:
        block = es.enter_context(nc.Block())

        @block.gpsimd
        def _(_g):
            _emit(nc, dr, cst, out, dbg, xres, hT_d, h_d, y_d, n_layers, stages)
    return nc


def _emit(nc, dr, cst, out, dbg, xres, hT_d, h_d, y_d, n_layers, stages):
    es = ExitStack()
    S = Sch(nc, es)

    uid = [0]

    def sb(name, shape, dt, st=es):
        uid[0] += 1
        return st.enter_context(nc.sbuf_tensor(name + '_%d' % uid[0], shape, dt))

    def ps(name, shape, dt=F32, st=es):
        return st.enter_context(nc.psum_tensor(name, shape, dt))

    bufA = sb("bufA", [128, 16, T], BF16)
    bufB = None
    Wt = [sb("Wt%d" % i, [128, 16, 512], BF16) for i in range(2)]
    ident = sb("ident", [128, 128], F32)
    identb = sb("identb", [128, 128], BF16)
    PS = [ps("ps%d" % i, [128, 512]) for i in range(8)]
    S.op('sp', lambda e: e.dma_start(out=ident[:], in_=cst['ident'][:, :]), w=['ident'], dma=True)
    S.op('dve', lambda e: e.tensor_copy(out=identb[:], in_=ident[:]), r=['ident'], w=['identb'])

    wt_i = [0]
    ps_i = [0]

    def next_ps(lo=0, hi=4):
        i = lo + ps_i[0] % (hi - lo)
        ps_i[0] += 1
        return i

    def load_w(W2d, k0, KC, c0, nb):
        b = wt_i[0] % 2
        wt_i[0] += 1
        src = W2d[k0:k0 + KC * 128, c0:c0 + nb].rearrange("(kc p) n -> p kc n", p=128)
        S.op('pool', lambda e: e.dma_start(out=Wt[b][:, 0:KC, 0:nb], in_=src), w=[('Wt', b)], dma=True)
        return b

    def proj_tm(src, skey, KC, W2d, c0, nb, epi, k0=0, tts=range(NT)):
        b = load_w(W2d, k0, KC, c0, nb)
        for tt in tts:
            pi = next_ps()
            for kc in range(KC):
                S.op('pe', lambda e, kc=kc, tt=tt, pi=pi: e.matmul(PS[pi][:, 0:nb], lhsT=src[:, kc, tt * 128:(tt + 1) * 128],
                                                                    rhs=Wt[b][:, kc, 0:nb], start=(kc == 0), stop=(kc == KC - 1)),
                     r=[('Wt', b), skey], w=[('ps', pi)], inc=(kc == KC - 1))
            epi(tt, pi)

    def proj_fm(src, skey, KC, W2d, c0, nb, M, epi, k0=0):
        b = load_w(W2d, k0, KC, c0, nb)
        for m0 in range(0, nb, M):
            mm = min(M, nb - m0)
            for tb in range(4):
                pi = next_ps()
                for kc in range(KC):
                    S.op('pe', lambda e, kc=kc, tb=tb, pi=pi, m0=m0, mm=mm: e.matmul(
                        PS[pi][0:mm, :], lhsT=Wt[b][:, kc, m0:m0 + mm], rhs=src[:, kc, tb * 512:(tb + 1) * 512],
                        start=(kc == 0), stop=(kc == KC - 1)),
                         r=[('Wt', b), skey], w=[('ps', pi)], inc=(kc == KC - 1))
                epi(c0 + m0, mm, tb, pi)

    def ln_and_store(l, st, xt_tile, key, tt, g_bc, b_bc, dst_dram, small):
        stats, mv, rstd = small
        for c in range(4):
            S.op('dve', lambda e, c=c: e.bn_stats(out=stats[:, c, :], in_=xt_tile[:, c * 512:(c + 1) * 512]), r=[key], w=['stats'])
        S.op('dve', lambda e: e.bn_aggr(out=mv[:], in_=stats[:].rearrange("p a b -> p (a b)")), r=['stats'], w=['mv'])
        S.op('act', lambda e: e.activation(out=rstd[:], in_=mv[:, 1:2], func=AF.Sqrt, bias=LN_EPS, scale=1.0), r=['mv'], w=['rstd'])
        S.op('dve', lambda e: e.reciprocal(out=rstd[:], in_=rstd[:]), r=['rstd'], w=['rstd'])
        S.op('dve', lambda e: e.tensor_scalar(out=xt_tile[:], in0=xt_tile[:], scalar1=mv[:, 0:1], scalar2=rstd[:, 0:1],
                                              op0=ALU.subtract, op1=ALU.mult), r=[key, 'mv', 'rstd'], w=[key])
        S.op('pool', lambda e: e.tensor_tensor(out=xt_tile[:], in0=xt_tile[:], in1=g_bc[:], op=ALU.mult), r=[key, 'lng'], w=[key])
        S.op('pool', lambda e: e.tensor_tensor(out=xt_tile[:], in0=xt_tile[:], in1=b_bc[:], op=ALU.add), r=[key, 'lnb'], w=[key])
        S.op('sp', lambda e: e.dma_start(out=dst_dram[tt * 128:(tt + 1) * 128, :], in_=xt_tile[:]), r=[key], w=[('xres', tt)], dma=True)
        transpose_to(xt_tile, key, 16, bufA, 'bufA', tt, F32)

    def transpose_to(tile, key, nchunks, dst, dkey, tt, dt, c_off=0, banks=(4, 8)):
        idm = ident if dt == F32 else identb
        for c4 in range(0, nchunks, 4):
            pi = next_ps(*banks)
            pst = PS[pi] if dt == F32 else PSB[pi - 4]
            for c in range(c4, min(c4 + 4, nchunks)):
                S.op('pe', lambda e, c=c, c4=c4, pst=pst: e.transpose(out=pst[:, (c - c4) * 128:(c - c4 + 1) * 128],
                                                                   in_=tile[:, c * 128:(c + 1) * 128], identity=idm[:]),
                     r=[key, 'ident', 'identb'], w=[('ps', pi)])
            n = min(4, nchunks - c4)
            S.op('act', lambda e, c4=c4, n=n, pst=pst: e.activation(
                out=dst[:, c_off + c4:c_off + c4 + n, tt * 128:(tt + 1) * 128],
                in_=pst[:, 0:n * 128].rearrange("p (a b) -> p a b", a=n), func=AF.Copy), r=[('ps', pi)], w=[dkey])

    PSB = [PS[i][:].bitcast(BF16)[:, 0:512] for i in range(4, 8)]

    with ExitStack() as st:
        xt = [sb("xt%d" % i, [128, D], F32, st) for i in range(2)]
        for tt in range(NT):
            b = tt % 2
            S.op('sp', lambda e, b=b, tt=tt: e.dma_start(out=xt[b][:], in_=dr["x"][tt * 128:(tt + 1) * 128, :]), w=[('xt', b)], dma=True)
            S.op('sp', lambda e, b=b, tt=tt: e.dma_start(out=xres[tt * 128:(tt + 1) * 128, :], in_=xt[b][:]), r=[('xt', b)], w=[('xres', tt)], dma=True)
            transpose_to(xt[b], ('xt', b), 16, bufA, 'bufA', tt, F32)
        S.barrier()

    for l in range(n_layers):
        _mixer(nc, S, sb, PS, PSB, dr, cst, l, bufA, bufB, hT_d, h_d, xres, proj_tm, proj_fm, transpose_to, ln_and_store, next_ps,
               ident, identb, dbg)
        if "mixonly" in stages:
            break
        with ExitStack() as lq:
            qx = sb("qxT", [128, 4, T], BF16, lq)
            with ExitStack() as lb:
                bB = _merge(nc, S, sb, PS, PSB, dr, cst, l, bufA, hT_d, xres, proj_fm, transpose_to, next_ps, lb)

                def epi_q(col, mm, tb, pi):
                    S.op('act', lambda e: e.activation(out=qx[:, col // 128, tb * 512:(tb + 1) * 512], in_=PS[pi][:, :], func=AF.Copy, scale=float(128 ** -0.5)),
                         r=[('ps', pi)], w=['qxT'])
                proj_fm(bB, 'bufBall', 16, dr["xa_wq"][l], 0, 512, 128, epi_q)
                S.barrier()
            _xattn(nc, S, sb, PS, PSB, dr, cst, l, bufA, qx, xres, proj_tm, proj_fm, transpose_to, ln_and_store, next_ps, ident, identb, dbg, load_w, Wt)
        _moe(nc, S, sb, PS, PSB, dr, cst, l, bufA, None, xres, y_d, out if l == n_layers - 1 else xres, proj_tm, proj_fm,
             transpose_to, ln_and_store, next_ps, ident, identb, dbg, load_w, Wt)
    S.barrier()
    es.close()


def _mixer(nc, S, sb, PS, PSB, dr, cst, l, bufA, bufB, hT_d, h_d, xres, proj_tm, proj_fm, transpose_to, ln_and_store, next_ps,
           ident, identb, dbg):
    W = dr["w_in"][l]
    aT_d = nc.dram_tensor("aT_d%d" % l, [16, T], F32).ap()
    with ExitStack() as st:
        stg = [sb("stg%d" % i, [128, 512], BF16, st) for i in range(4)]
        stgf = sb("stgf", [16, 512], F32, st)
        si = [0]

        def epi_fm(rbase, cbase, func, scale=1.0):
            def f(col, mm, tb, pi):
                i = si[0] % 4
                si[0] += 1
                S.op('act', lambda e: e.activation(out=stg[i][0:mm, :], in_=PS[pi][0:mm, :], func=func, scale=scale), r=[('ps', pi)], w=[('stg', i)])
                r0 = rbase + col - cbase
                S.op('sp', lambda e: e.dma_start(out=hT_d[r0:r0 + mm, tb * 512:(tb + 1) * 512], in_=stg[i][0:mm, :]), r=[('stg', i)],
                     w=[('hT', r0 // 64, tb)] + ([('hT', r0 // 64 + 1, tb)] if mm == 128 else []), dma=True)
            return f

        def epi_ga(col, mm, tb, pi):
            S.op('act', lambda e: e.activation(out=stgf[0:16, :], in_=PS[pi][0:16, :], func=AF.Copy), r=[('ps', pi)], w=['stgf'])
            S.op('sp', lambda e: e.dma_start(out=aT_d[:, tb * 512:(tb + 1) * 512], in_=stgf[0:16, :]), r=['stgf'], w=[('aT', tb)], dma=True)

        def epi_tm(tcbase, nb, func):
            def f(tt, pi):
                i = si[0] % 4
                si[0] += 1
                S.op('act', lambda e: e.activation(out=stg[i][:, 0:nb], in_=PS[pi][:, 0:nb], func=func), r=[('ps', pi)], w=[('stg', i)])
                S.op('sp', lambda e: e.dma_start(out=h_d[tt * 128:(tt + 1) * 128, tcbase:tcbase + nb], in_=stg[i][:, 0:nb]), r=[('stg', i)],
                     w=[('h', tt, tcbase)], dma=True)
            return f

        A = (bufA, 'bufA', 16, W)
        proj_fm(*A, C_GQ, 512, 128, epi_fm(R_GQ, C_GQ, AF.Copy))
        proj_fm(*A, C_GK, 512, 128, epi_fm(R_GK, C_GK, AF.Copy))
        proj_fm(*A, C_GA, 16, 16, epi_ga)
        proj_tm(*A, C_GK, 512, epi_tm(TC_GK, 512, AF.Copy))
        for j in range(2):
            proj_tm(*A, C_GV + 512 * j, 512, epi_tm(TC_GV + 512 * j, 512, AF.Copy))
            proj_tm(*A, C_GR + 512 * j, 512, epi_tm(TC_GR + 512 * j, 512, AF.Silu))
            proj_fm(*A, C_NQ + 512 * j, 512, 64, epi_fm(R_NQ + 512 * j, C_NQ + 512 * j, AF.Copy, 0.125))
        for (cc, rr) in ((0, R_KC), (256, R_VC), (512, R_KS), (1024, R_KW)):
            proj_fm(*A, C_NKV + cc, 256, 64, epi_fm(rr, C_NKV + cc, AF.Copy))
        proj_tm(*A, C_NKV + 768, 256, epi_tm(TC_VS, 256, AF.Copy))
        proj_tm(*A, C_NKV + 1280, 256, epi_tm(TC_VW, 256, AF.Copy))
        proj_tm(*A, C_NG, 48, epi_tm(TC_NG, 48, AF.Sigmoid))
        for j in range(8):
            proj_fm(*A, C_MG + 512 * j, 512, 128, epi_fm(R_MG + 512 * j, C_MG + 512 * j, AF.Sigmoid))
        S.barrier()

    if 'gla' not in SKIP:
        _gla(nc, S, sb, PS, PSB, dr, cst, l, bufA, hT_d, h_d, aT_d, transpose_to, ident, identb, dbg)
    if 'nsa' not in SKIP:
        _nsa(nc, S, sb, PS, PSB, dr, cst, l, bufA, hT_d, h_d, transpose_to, next_ps, ident, identb, dbg)


def _gla(nc, S, sb, PS, PSB, dr, cst, l, bufA, hT_d, h_d, aT_d, transpose_to, ident, identb, dbg):
    with ExitStack() as st:
        um = sb("um", [128, 128], F32, st)
        um2 = sb("um2", [128, 128], F32, st)
        caus4 = sb("caus4", [128, 512], F32, st)
        gnbc = sb("gnbc", [128, 1024], F32, st)
        wa2 = sb("wa2", [32, 512], F32, st)
        aT = sb("aT", [32, T], F32, st)
        S.op('sp', lambda e: e.dma_start(out=um[:], in_=cst['um'][:, :]), w=['um'], dma=True)
        S.op('sp', lambda e: e.dma_start(out=um2[:], in_=cst['um2'][:, :]), w=['um2'], dma=True)
        S.op('sp', lambda e: e.dma_start(out=caus4[:], in_=cst['caus4'][:, :]), w=['caus4'], dma=True)
        S.op('sp', lambda e: e.dma_start(out=gnbc[:], in_=dr['gla_norm_g'][l, :].partition_broadcast(128)), w=['gnbc'], dma=True)
        S.op('sp', lambda e: e.dma_start(out=wa2[0:16, :], in_=dr['gla_w_a2'][l]), w=['wa2a'], dma=True)
        S.op('sp', lambda e: e.dma_start(out=wa2[16:17, :], in_=dr['gla_b_a'][l:l + 1, :]), w=['wa2b'], dma=True)
        S.op('dve', lambda e: e.memset(aT[:], 1.0), w=['aT'])
        S.op('sp', lambda e: e.dma_start(out=aT[0:16, :], in_=aT_d[:, :]), w=['aT'], dma=True)
        S32 = sb("S32", [128, 4, 256], F32, st)
        Sb = sb("Sb", [128, 4, 256], BF16, st)
        S.op('dve', lambda e: e.memset(S32[:], 0.0), w=['S32'])
        S.op('pool', lambda e: e.memset(Sb[:], 0.0), w=['Sb'])
        qT = [sb("gqT%d" % i, [128, 4, 128], BF16, st) for i in range(2)]
        kT = [sb("gkT%d" % i, [128, 4, 128], BF16, st) for i in range(2)]
        kk = [sb("gk%d" % i, [128, 512], BF16, st) for i in range(2)]
        vv = [sb("gv%d" % i, [128, 1024], BF16, st) for i in range(2)]
        rs = [sb("grs%d" % i, [128, 1024], BF16, st) for i in range(2)]
        le = sb("le", [128, 512], F32, st)
        ll = sb("ll", [128, 512], F32, st)
        Eq = sb("Eq", [128, 512], F32, st)
        Ek = sb("Ek", [128, 512], F32, st)
        Ekh = sb("Ekh", [128, 512], F32, st)
        qs = sb("qs", [128, 512], BF16, st)
        ks = sb("ks", [128, 512], BF16, st)
        kh = sb("kh", [128, 512], BF16, st)
        att = sb("att", [128, 512], BF16, st)
        og = sb("og", [128, 1024], F32, st)
        ogb = sb("ogb", [128, 1024], BF16, st)
        grs = sb("grsf", [128, 1024], F32, st)
        stats = sb("gstats", [128, 4, 6], F32, st)
        mv = sb("gmv", [128, 4, 2], F32, st)
        rstd = sb("grstd", [128, 4], F32, st)
        for c in range(NT):
            b = c % 2
            cs = slice(c * 128, (c + 1) * 128)
            S.op('sp', lambda e: e.dma_start(out=qT[b][:], in_=hT_d[R_GQ:R_GQ + 512, cs].rearrange("(h d) t -> d h t", d=128)), w=[('qT', b)], dma=True)
            S.op('sp', lambda e: e.dma_start(out=kT[b][:], in_=hT_d[R_GK:R_GK + 512, cs].rearrange("(h d) t -> d h t", d=128)), w=[('kT', b)], dma=True)
            S.op('sp', lambda e: e.dma_start(out=kk[b][:], in_=h_d[cs, TC_GK:TC_GK + 512]), w=[('kk', b)], dma=True)
            S.op('sp', lambda e: e.dma_start(out=vv[b][:], in_=h_d[cs, TC_GV:TC_GV + 1024]), w=[('vv', b)], dma=True)
            S.op('sp', lambda e: e.dma_start(out=rs[b][:], in_=h_d[cs, TC_GR:TC_GR + 1024]), w=[('rs', b)], dma=True)
            S.op('pe', lambda e: e.matmul(PS[0][:, :], lhsT=aT[0:17, cs], rhs=wa2[0:17, :], start=True, stop=True), r=['aT', 'wa2a', 'wa2b'], w=[('ps', 0)])
            S.op('act', lambda e: e.activation(out=le[:], in_=PS[0][:, :], func=AF.Exp, scale=-1.0), r=[('ps', 0)], w=['le'])
            S.op('act', lambda e: e.activation(out=ll[:], in_=le[:], func=AF.Ln, bias=1.0, scale=1.0), r=['le'], w=['ll'])
            for h in range(4):
                S.op('pe', lambda e, h=h: e.matmul(PS[1][:, h * 128:(h + 1) * 128], lhsT=ll[:, h * 128:(h + 1) * 128], rhs=um[:], start=True, stop=True),
                     r=['ll', 'um'], w=[('ps', 1)], inc=(h == 3))
            S.op('pe', lambda e: e.matmul(PS[2][:, :], lhsT=um2[:], rhs=ll[:], start=True, stop=True), r=['ll', 'um2'], w=[('ps', 2)])
            S.op('act', lambda e: e.activation(out=Eq[:], in_=PS[1][:, :], func=AF.Exp), r=[('ps', 1)], w=['Eq'])
            S.op('act', lambda e: e.activation(out=Ek[:], in_=PS[1][:, :], func=AF.Exp, scale=-1.0), r=[('ps', 1)], w=['Ek'])
            S.op('act', lambda e: e.activation(out=Ekh[:], in_=PS[2][:, :], func=AF.Exp), r=[('ps', 2)], w=['Ekh'])
            S.op('dve', lambda e: e.scalar_tensor_tensor(out=qs[:], in0=qT[b][:].rearrange("p h t -> p (h t)"), scalar=float(128 ** -0.5), in1=Eq[:],
                                                         op0=ALU.mult, op1=ALU.mult), r=[('qT', b), 'Eq'], w=['qs'])
            S.op('dve', lambda e: e.tensor_tensor(out=ks[:], in0=kT[b][:].rearrange("p h t -> p (h t)"), in1=Ek[:], op=ALU.mult), r=[('kT', b), 'Ek'], w=['ks'])
            S.op('pool', lambda e: e.tensor_tensor(out=kh[:], in0=kk[b][:], in1=Ekh[:], op=ALU.mult), r=[('kk', b), 'Ekh'], w=['kh'])
            for h in range(4):
                hs = slice(h * 128, (h + 1) * 128)
                S.op('pe', lambda e, hs=hs: e.matmul(PS[3][:, hs], lhsT=ks[:, hs], rhs=qs[:, hs], start=True, stop=True), r=['ks', 'qs'], w=[('ps', 3)], inc=(h == 3))
            S.op('dve', lambda e: e.tensor_tensor(out=att[:], in0=PS[3][:, :], in1=caus4[:], op=ALU.mult), r=[('ps', 3), 'caus4'], w=['att'])
            for h in range(4):
                hs = slice(h * 128, (h + 1) * 128)
                pb = 4 + h // 2
                po = slice((h % 2) * 256, (h % 2) * 256 + 256)
                S.op('pe', lambda e, hs=hs, pb=pb, po=po, h=h: e.matmul(PS[pb][:, po], lhsT=att[:, hs], rhs=vv[b][:, h * 256:(h + 1) * 256], start=True, stop=False),
                     r=['att', ('vv', b)], w=[('ps', pb)], inc=False)
                S.op('pe', lambda e, hs=hs, pb=pb, po=po, h=h: e.matmul(PS[pb][:, po], lhsT=qs[:, hs], rhs=Sb[:, h, :], start=False, stop=True),
                     r=['qs', 'Sb'], w=[('ps', pb)], inc=True)
            for h in range(4):
                hs = slice(h * 128, (h + 1) * 128)
                pb = 6 + h // 2
                po = slice((h % 2) * 256, (h % 2) * 256 + 256)
                S.op('pe', lambda e, hs=hs, pb=pb, po=po, h=h: e.matmul(PS[pb][:, po], lhsT=kh[:, hs], rhs=vv[b][:, h * 256:(h + 1) * 256], start=True, stop=True),
                     r=['kh', ('vv', b)], w=[('ps', pb)])
                S.op('dve', lambda e, pb=pb, po=po, h=h: e.scalar_tensor_tensor(out=S32[:, h, :], in0=S32[:, h, :], scalar=Eq[:, h * 128 + 127:h * 128 + 128],
                                                                              in1=PS[pb][:, po], op0=ALU.mult, op1=ALU.add), r=[('ps', pb), 'Eq', 'S32'], w=['S32'])
            S.op('act', lambda e: e.activation(out=Sb[:], in_=S32[:], func=AF.Copy), r=['S32'], w=['Sb'])
            for h in range(4):
                pb = 4 + h // 2
                po = slice((h % 2) * 256, (h % 2) * 256 + 256)
                S.op('dve', lambda e, pb=pb, po=po, h=h: e.bn_stats(out=stats[:, h, :], in_=PS[pb][:, po]), r=[('ps', pb)], w=['gstats'])
                S.op('dve', lambda e, h=h: e.bn_aggr(out=mv[:, h, :], in_=stats[:, h, :]), r=['gstats'], w=['gmv'])
            S.op('act', lambda e: e.activation(out=rstd[:], in_=mv[:, :, 1], func=AF.Sqrt, bias=LN_EPS, scale=1.0), r=['gmv'], w=['grstd'])
            S.op('dve', lambda e: e.reciprocal(out=rstd[:], in_=rstd[:]), r=['grstd'], w=['grstd'])
            for h in range(4):
                pb = 4 + h // 2
                po = slice((h % 2) * 256, (h % 2) * 256 + 256)
                S.op('dve', lambda e, pb=pb, po=po, h=h: e.tensor_scalar(out=og[:, h * 256:(h + 1) * 256], in0=PS[pb][:, po], scalar1=mv[:, h, 0:1], scalar2=rstd[:, h:h + 1],
                                                                       op0=ALU.subtract, op1=ALU.mult), r=[('ps', pb), 'gmv', 'grstd'], w=['og'])
            S.op('pool', lambda e: e.tensor_tensor(out=grs[:], in0=rs[b][:], in1=gnbc[:], op=ALU.mult), r=[('rs', b), 'gnbc'], w=['grsf'])
            S.op('pool', lambda e: e.tensor_tensor(out=ogb[:], in0=og[:], in1=grs[:], op=ALU.mult), r=['og', 'grsf'], w=['ogb'])
            if 'o_gla' in dbg:
                S.op('pool', lambda e: e.tensor_tensor(out=og[:], in0=og[:], in1=grs[:], op=ALU.mult), r=['og', 'grsf'], w=['og'])
                S.op('sp', lambda e: e.dma_start(out=dbg['o_gla'][cs, :], in_=og[:]), r=['og'], w=[('dbg', c)], dma=True)
            transpose_to(ogb, 'ogb', 8, bufA, 'bufA', c, BF16)
        S.barrier()


def _resid_ln_phase(nc, S, sb, PS, st, l, Wres, KC, src, skeyf, alpha_src, gname, bname, dr, dst_dram, dstT, dstkeyf, transpose_to, ln_keys):
    xt = [sb("rl_xt%d" % i, [128, D], F32, st) for i in range(1)]
    g_bc = sb("rl_g", [128, D], F32, st)
    b_bc = sb("rl_b", [128, D], F32, st)
    stats = sb("rl_stats", [128, 4, 6], F32, st)
    mv = sb("rl_mv", [128, 2], F32, st)
    rstd = sb("rl_rstd", [128, 1], F32, st)
    S.op('sp', lambda e: e.dma_start(out=g_bc[:], in_=dr[gname][l, :].partition_broadcast(128)), w=['lng'], dma=True)
    S.op('sp', lambda e: e.dma_start(out=b_bc[:], in_=dr[bname][l, :].partition_broadcast(128)), w=['lnb'], dma=True)
    for tt in range(NT):
        b = 0
        key = ('rlxt', b)
        S.op('sp', lambda e: e.dma_start(out=xt[b][:], in_=alpha_src[tt * 128:(tt + 1) * 128, :]), r=[('xres', tt)], w=[key], dma=True)
        for cb in range(4):
            if Wres is not None:
                for kc in range(KC):
                    S.op('pe', lambda e, kc=kc, cb=cb: e.matmul(PS[cb][:, :], lhsT=src[:, kc, tt * 128:(tt + 1) * 128], rhs=Wres[:, kc, cb * 512:(cb + 1) * 512],
                                                                start=(kc == 0), stop=(kc == KC - 1)), r=[skeyf(tt), 'Wres'], w=[('ps', cb)], inc=(kc == KC - 1))
                S.op('dve', lambda e, cb=cb: e.scalar_tensor_tensor(out=xt[b][:, cb * 512:(cb + 1) * 512], in0=xt[b][:, cb * 512:(cb + 1) * 512], scalar=DN_ALPHA,
                                                                   in1=PS[cb][:, :], op0=ALU.mult, op1=ALU.add), r=[('ps', cb), key], w=[key])
            else:
                if cb == 0:
                    ytile, ykey = src(tt)
                S.op('dve', lambda e, cb=cb: e.scalar_tensor_tensor(out=xt[b][:, cb * 512:(cb + 1) * 512], in0=xt[b][:, cb * 512:(cb + 1) * 512], scalar=DN_ALPHA,
                                                                   in1=ytile[:, cb * 512:(cb + 1) * 512], op0=ALU.mult, op1=ALU.add), r=[ykey, key], w=[key])
        for c in range(4):
            S.op('dve', lambda e, c=c: e.bn_stats(out=stats[:, c, :], in_=xt[b][:, c * 512:(c + 1) * 512]), r=[key], w=['stats'])
        S.op('dve', lambda e: e.bn_aggr(out=mv[:], in_=stats[:].rearrange("p a b -> p (a b)")), r=['stats'], w=['mv'])
        S.op('act', lambda e: e.activation(out=rstd[:], in_=mv[:, 1:2], func=AF.Sqrt, bias=LN_EPS, scale=1.0), r=['mv'], w=['rstd'])
        S.op('dve', lambda e: e.reciprocal(out=rstd[:], in_=rstd[:]), r=['rstd'], w=['rstd'])
        S.op('dve', lambda e: e.tensor_scalar(out=xt[b][:], in0=xt[b][:], scalar1=mv[:, 0:1], scalar2=rstd[:, 0:1], op0=ALU.subtract, op1=ALU.mult),
             r=[key, 'mv', 'rstd'], w=[key])
        S.op('pool', lambda e: e.tensor_tensor(out=xt[b][:], in0=xt[b][:], in1=g_bc[:], op=ALU.mult), r=[key, 'lng'], w=[key])
        S.op('pool', lambda e: e.tensor_tensor(out=xt[b][:], in0=xt[b][:], in1=b_bc[:], op=ALU.add), r=[key, 'lnb'], w=[key])
        S.op('sp', lambda e: e.dma_start(out=dst_dram[tt * 128:(tt + 1) * 128, :], in_=xt[b][:]), r=[key], w=[('xres', tt)], dma=True)
        transpose_to(xt[b], key, 16, dstT, dstkeyf(tt), tt, F32)


def _load_resident(S, dst, dkey, W2d, KC):
    for kc in range(KC):
        for cb in range(4):
            S.op('pool', lambda e, kc=kc, cb=cb: e.dma_start(out=dst[:, kc, cb * 512:(cb + 1) * 512], in_=W2d[kc * 128:(kc + 1) * 128, cb * 512:(cb + 1) * 512]),
                 w=[dkey], dma=True)


def _merge(nc, S, sb, PS, PSB, dr, cst, l, bufA, hT_d, xres, proj_fm, transpose_to, next_ps, st):
    bufB = sb("bufB", [128, 16, T], BF16, st)
    with ExitStack() as s2:
        sg = [sb("sg%d" % i, [128, 512], BF16, s2) for i in range(2)]
        tmp = sb("mtmp", [128, 512], F32, s2)
        gi = [0]

        def epi(branch):
            def f(col, mm, tb, pi):
                i = gi[0] % 2
                gi[0] += 1
                r0 = R_MG + branch * 2048 + col
                S.op('sp', lambda e: e.dma_start(out=sg[i][:], in_=hT_d[r0:r0 + 128, tb * 512:(tb + 1) * 512]), w=[('sg', i)], dma=True)
                dsl = bufB[:, col // 128, tb * 512:(tb + 1) * 512]
                if branch == 0:
                    S.op('dve', lambda e: e.tensor_tensor(out=dsl, in0=PS[pi][:, :], in1=sg[i][:], op=ALU.mult), r=[('ps', pi), ('sg', i)], w=['bufB'])
                else:
                    S.op('dve', lambda e: e.tensor_tensor(out=tmp[:], in0=PS[pi][:, :], in1=sg[i][:], op=ALU.mult), r=[('ps', pi), ('sg', i)], w=['mtmp'])
                    S.op('pool', lambda e: e.tensor_tensor(out=dsl, in0=dsl, in1=tmp[:], op=ALU.add), r=['mtmp', 'bufB'], w=['bufB'])
            return f
        for j in range(4):
            proj_fm(bufA[:, 0:8, :], 'bufA', 8, dr["w_branch_gla"][l], 512 * j, 512, 128, epi(0))
        for j in range(4):
            proj_fm(bufA[:, 8:16, :], 'bufA', 8, dr["w_branch_nsa"][l], 512 * j, 512, 128, epi(1))
        S.barrier()
    with ExitStack() as s2:
        _load_resident(S, bufA, 'Wres', dr["w_out"][l], 16)
        _resid_ln_phase(nc, S, sb, PS, s2, l, bufA, 16, bufB, lambda tt: ('bufB', tt), xres, "ln_mix_g", "ln_mix_b", dr, xres, bufB,
                        lambda tt: ('bufB', tt), transpose_to, None)
        S.barrier()
    return bufB


def _xattn(nc, S, sb, PS, PSB, dr, cst, l, bufA, bufB, xres, proj_tm, proj_fm, transpose_to, ln_and_store, next_ps, ident, identb, dbg, load_w, Wt):
    with ExitStack() as st:
        memT = sb("memT", [128, 16, 256], BF16, st)
        mt_ = sb("memtile", [128, D], F32, st)
        kTs = sb("xkT", [128, 4, 256], BF16, st)
        vs = sb("xv", [128, 2, 4, 132], BF16, st)
        qx = bufB
        oT = sb("xoT", [128, 4, T], BF16, st)
        woT = sb("woT", [128, 4, D], BF16, st)
        eT = [sb("xeT%d" % i, [128, 512], BF16, st) for i in range(2)]
        oxa = sb("oxa", [128, 512], BF16, st)
        rden = sb("xrden", [128, 4], F32, st)
        for m in range(2):
            S.op('sp', lambda e: e.dma_start(out=mt_[:], in_=dr["mem"][m * 128:(m + 1) * 128, :]), w=['memtile'], dma=True)
            transpose_to(mt_, 'memtile', 16, memT, 'memT', m, F32)
        S.op('dve', lambda e: e.memset(vs[:], 1.0), w=['xv'])
        Wkv = dr["xa_wkv"][l]
        b = load_w(Wkv, 0, 16, 0, 512)
        for h in range(4):
            pi = next_ps()
            for kc in range(16):
                S.op('pe', lambda e, kc=kc: e.matmul(PS[pi][:, 0:256], lhsT=Wt[b][:, kc, h * 128:(h + 1) * 128], rhs=memT[:, kc, :], start=(kc == 0), stop=(kc == 15)),
                     r=[('Wt', b), 'memT'], w=[('ps', pi)], inc=(kc == 15))
            S.op('act', lambda e: e.activation(out=kTs[:, h, :], in_=PS[pi][:, 0:256], func=AF.Copy), r=[('ps', pi)], w=['xkT'])
        b = load_w(Wkv, 0, 16, 512, 512)
        for m in range(2):
            pi = next_ps()
            for kc in range(16):
                S.op('pe', lambda e, kc=kc: e.matmul(PS[pi][:, :], lhsT=memT[:, kc, m * 128:(m + 1) * 128], rhs=Wt[b][:, kc, :], start=(kc == 0), stop=(kc == 15)),
                     r=[('Wt', b), 'memT'], w=[('ps', pi)], inc=(kc == 15))
            S.op('act', lambda e: e.activation(out=vs[:, m, :, 0:128], in_=PS[pi][:, :].rearrange("p (h d) -> p h d", h=4), func=AF.Copy), r=[('ps', pi)], w=['xv'])

        for tt in range(NT):
            ts = slice(tt * 128, (tt + 1) * 128)
            for m in range(2):
                pi = next_ps()
                for h in range(4):
                    S.op('pe', lambda e, h=h: e.matmul(PS[pi][:, h * 128:(h + 1) * 128], lhsT=kTs[:, h, m * 128:(m + 1) * 128], rhs=qx[:, h, ts], start=True, stop=True),
                         r=['xkT', 'qxT'], w=[('ps', pi)], inc=(h == 3))
                S.op('act', lambda e: e.activation(out=eT[m][:], in_=PS[pi][:, :], func=AF.Exp), r=[('ps', pi)], w=[('xeT', m)])
            for h in range(4):
                pb = 4 + h // 2
                po = (h % 2) * 132
                for m in range(2):
                    S.op('pe', lambda e, m=m: e.matmul(PS[pb][:, po:po + 129], lhsT=eT[m][:, h * 128:(h + 1) * 128], rhs=vs[:, m, h, 0:129], start=(m == 0), stop=(m == 1)),
                         r=[('xeT', m), 'xv'], w=[('ps', pb)], inc=(m == 1))
                S.op('dve', lambda e: e.reciprocal(out=rden[:, h:h + 1], in_=PS[pb][:, po + 128:po + 129]), r=[('ps', pb)], w=['xrden'])
                S.op('dve', lambda e: e.tensor_scalar(out=oxa[:, h * 128:(h + 1) * 128], in0=PS[pb][:, po:po + 128], scalar1=rden[:, h:h + 1], scalar2=None, op0=ALU.mult),
                     r=[('ps', pb), 'xrden'], w=['oxa'])
            transpose_to(oxa, 'oxa', 4, oT, 'xoT', tt, BF16)
        S.barrier()
        for kc in range(4):
            for cb in range(4):
                S.op('pool', lambda e: e.dma_start(out=woT[:, kc, cb * 512:(cb + 1) * 512], in_=dr["xa_wo"][l][kc * 128:(kc + 1) * 128, cb * 512:(cb + 1) * 512]),
                     w=['Wres'], dma=True)
        _resid_ln_phase(nc, S, sb, PS, st, l, woT, 4, oT, lambda tt: 'xoT', xres, "ln_xa_g", "ln_xa_b", dr, xres, bufA, lambda tt: 'bufA', transpose_to, None)
        S.barrier()


SKIP = set()
MOE_C = 384
MOE_BIG = 1.0e6


def _moe(nc, S, sb, PS, PSB, dr, cst, l, bufA, bufB, xres, y_d, dst, proj_tm, proj_fm, transpose_to, ln_and_store, next_ps, ident, identb, dbg, load_w, Wt):
    C = MOE_C
    CT = C // 128
    NSL = 16 * C
    xbuf = nc.dram_tensor("moe_xbuf%d" % l, [NSL, D], BF16).ap()
    ybuf = nc.dram_tensor("moe_ybuf%d" % l, [NSL, D], F32).ap()
    I32 = mybir.dt.int32
    breg = nc.gpsimd.to_reg(NSL - 1)
    with ExitStack() as st:
        gateA = sb("gateA", [128, NT], F32, st)
        slotAi = sb("slotAi", [128, NT], I32, st)
        slotBi = sb("slotBi", [128, NT], I32, st)
        with ExitStack() as s2:
            rw = sb("rw", [128, 16, 16], BF16, s2)
            rb = sb("rb", [128, 16], F32, s2)
            lg = sb("lg", [128, 16], F32, s2)
            lb = sb("lb", [128, 4, 4], F32, s2)
            eq = sb("eq", [128, 4, 4], F32, s2)
            lb2 = sb("lb2", [128, 4, 4], F32, s2)
            m1 = sb("m1", [128, 4], F32, s2)
            m2 = sb("m2", [128, 4], F32, s2)
            gs = sb("gs", [128, 4], F32, s2)
            gm = sb("gm", [128, 1], F32, s2)
            ex = sb("ex", [128, 16], F32, s2)
            den = sb("den", [128, 1], F32, s2)
            gate = sb("gate", [128, 16], F32, s2)
            maskall = sb("maskall", [128, NT, 16], BF16, s2)
            lstr = sb("lstr", [128, 128], BF16, s2)
            onesb = sb("onesb", [128, 128], BF16, s2)
            ebase = sb("ebase", [128, 16], F32, s2)
            smat = sb("smat", [128, 16], F32, s2)
            tA = sb("tA", [128, 16], F32, s2)
            tB = sb("tB", [128, 16], F32, s2)
            sA = sb("sA", [128, 1], F32, s2)
            sB = sb("sB", [128, 1], F32, s2)
            zt = sb("zt", [128, D], BF16, s2)
            xtf = [sb("mxtf%d" % i, [128, D], F32, s2) for i in range(2)]
            xtb = [sb("mxtb%d" % i, [128, D], BF16, s2) for i in range(2)]
            S.op('pool', lambda e: e.dma_start(out=rw[:], in_=dr["router_w"].rearrange("(kc p) e -> p kc e", p=128)), w=['rw'], dma=True)
            S.op('sp', lambda e: e.dma_start(out=rb[:], in_=dr["router_b"].partition_broadcast(128)), w=['rb'], dma=True)
            S.op('pool', lambda e: e.dma_start(out=lstr[:], in_=cst['lstr'][:, :]), w=['lstr'], dma=True)
            S.op('sp', lambda e: e.dma_start(out=ebase[:], in_=cst['ebase'][:, :]), w=['ebase'], dma=True)
            S.op('dve', lambda e: e.memset(onesb[:], 1.0), w=['onesb'])
            S.op('dve', lambda e: e.memset(zt[:], 0.0), w=['zt'])
            for ex_i in range(16):
                S.op('sp', lambda e: e.dma_start(out=xbuf[ex_i * C:(ex_i + 1) * C, :].rearrange("(a p) d -> p a d", p=128),
                                                 in_=zt[:].unsqueeze(1).to_broadcast([128, CT, D])), r=['zt'], w=['xbuf'], dma=True)
            for tt in range(NT):
                ts = slice(tt * 128, (tt + 1) * 128)
                b = tt % 2
                S.op('sp', lambda e: e.dma_start(out=xtf[b][:], in_=xres[ts, :]), r=[('xres', tt)], w=[('mxtf', b)], dma=True)
                S.op('act', lambda e: e.activation(out=xtb[b][:], in_=xtf[b][:], func=AF.Copy), r=[('mxtf', b)], w=[('mxtb', b)])
                pi = next_ps()
                for kc in range(16):
                    S.op('pe', lambda e, kc=kc: e.matmul(PS[pi][:, 0:16], lhsT=bufA[:, kc, ts], rhs=rw[:, kc, :], start=(kc == 0), stop=(kc == 15)),
                         r=['bufA', 'rw'], w=[('ps', pi)], inc=(kc == 15))
                lbf = lb[:].rearrange("p a b -> p (a b)")
                eqf = eq[:].rearrange("p a b -> p (a b)")
                S.op('dve', lambda e: e.tensor_copy(out=lg[:], in_=PS[pi][:, 0:16]), r=[('ps', pi)], w=['lg'])
                S.op('dve', lambda e: e.tensor_tensor(out=lbf, in0=lg[:], in1=rb[:], op=ALU.add), r=['lg', 'rb'], w=['lb'])
                S.op('dve', lambda e: e.tensor_reduce(out=m1[:], in_=lb[:], axis=AX.X, op=ALU.max), r=['lb'], w=['m1'])
                S.op('dve', lambda e: e.tensor_tensor(out=eq[:], in0=lb[:], in1=m1[:].unsqueeze(2).to_broadcast([128, 4, 4]), op=ALU.is_equal), r=['lb', 'm1'], w=['eq'])
                S.op('dve', lambda e: e.scalar_tensor_tensor(out=lb2[:], in0=eq[:], scalar=-1e30, in1=lb[:], op0=ALU.mult, op1=ALU.add), r=['eq', 'lb'], w=['lb2'])
                S.op('dve', lambda e: e.tensor_reduce(out=m2[:], in_=lb2[:], axis=AX.X, op=ALU.max), r=['lb2'], w=['m2'])
                S.op('dve', lambda e: e.tensor_tensor(out=gs[:], in0=m1[:], in1=m2[:], op=ALU.add), r=['m1', 'm2'], w=['gs'])
                S.op('dve', lambda e: e.tensor_reduce(out=gm[:], in_=gs[:], axis=AX.X, op=ALU.max), r=['gs'], w=['gm'])
                S.op('dve', lambda e: e.tensor_scalar(out=gs[:], in0=gs[:], scalar1=gm[:, 0:1], scalar2=None, op0=ALU.is_equal), r=['gs', 'gm'], w=['gs'])
                S.op('dve', lambda e: e.tensor_tensor(out=eq[:], in0=lb[:], in1=m2[:].unsqueeze(2).to_broadcast([128, 4, 4]), op=ALU.is_ge), r=['lb', 'm2'], w=['eq'])
                S.op('dve', lambda e: e.tensor_tensor(out=eq[:], in0=eq[:], in1=gs[:].unsqueeze(2).to_broadcast([128, 4, 4]), op=ALU.mult), r=['eq', 'gs'], w=['eq'])
                S.op('act', lambda e: e.activation(out=ex[:], in_=lg[:], func=AF.Exp), r=['lg'], w=['ex'])
                S.op('dve', lambda e: e.tensor_tensor(out=ex[:], in0=ex[:], in1=eqf, op=ALU.mult), r=['ex', 'eq'], w=['ex'])
                S.op('dve', lambda e: e.tensor_reduce(out=den[:], in_=ex[:], axis=AX.X, op=ALU.add), r=['ex'], w=['den'])
                S.op('dve', lambda e: e.reciprocal(out=den[:], in_=den[:]), r=['den'], w=['den'])
                S.op('dve', lambda e: e.tensor_scalar(out=gate[:], in0=ex[:], scalar1=den[:, 0:1], scalar2=None, op0=ALU.mult), r=['ex', 'den'], w=['gate'])
                S.op('act', lambda e: e.activation(out=maskall[:, tt, :], in_=eqf, func=AF.Copy), r=['eq'], w=[('maskall', tt)])
                pj = next_ps()
                for t2 in range(tt):
                    S.op('pe', lambda e, t2=t2: e.matmul(PS[pj][:, 0:16], lhsT=onesb[:], rhs=maskall[:, t2, :], start=(t2 == 0), stop=False),
                         r=['onesb', ('maskall', t2)], w=[('ps', pj)], inc=False)
                S.op('pe', lambda e: e.matmul(PS[pj][:, 0:16], lhsT=lstr[:], rhs=maskall[:, tt, :], start=(tt == 0), stop=True),
                     r=['lstr', ('maskall', tt)], w=[('ps', pj)], inc=True)
                S.op('dve', lambda e: e.tensor_tensor(out=smat[:], in0=PS[pj][:, 0:16], in1=ebase[:], op=ALU.add), r=[('ps', pj), 'ebase'], w=['smat'])
                S.op('dve', lambda e: e.scalar_tensor_tensor(out=tA[:], in0=eqf, scalar=-MOE_BIG, in1=smat[:], op0=ALU.mult, op1=ALU.add), r=['eq', 'smat'], w=['tA'])
                S.op('dve', lambda e: e.tensor_reduce(out=sA[:], in_=tA[:], axis=AX.X, op=ALU.min), r=['tA'], w=['sA'])
                S.op('dve', lambda e: e.tensor_tensor(out=tB[:], in0=tA[:], in1=eqf, op=ALU.mult), r=['tA', 'eq'], w=['tB'])
                S.op('dve', lambda e: e.tensor_reduce(out=sB[:], in_=tB[:], axis=AX.X, op=ALU.max), r=['tB'], w=['sB'])
                S.op('dve', lambda e: e.tensor_scalar(out=tB[:], in0=tA[:], scalar1=sA[:, 0:1], scalar2=None, op0=ALU.is_equal), r=['tA', 'sA'], w=['tB'])
                S.op('dve', lambda e: e.tensor_tensor(out=tB[:], in0=tB[:], in1=gate[:], op=ALU.mult), r=['tB', 'gate'], w=['tB'])
                S.op('dve', lambda e: e.tensor_reduce(out=gateA[:, tt:tt + 1], in_=tB[:], axis=AX.X, op=ALU.add), r=['tB'], w=['gateA'])
                S.op('dve', lambda e: e.tensor_copy(out=slotAi[:, tt:tt + 1], in_=sA[:]), r=['sA'], w=['slotAi'])
                S.op('dve', lambda e: e.tensor_copy(out=slotBi[:, tt:tt + 1], in_=sB[:]), r=['sB'], w=['slotBi'])
                for sl, sk in ((slotAi, 'slotAi'), (slotBi, 'slotBi')):
                    S.op('pool', lambda e: e.indirect_dma_start(out=xbuf[:, :], out_offset=bass.IndirectOffsetOnAxis(ap=sl[:, tt:tt + 1], axis=0),
                                                                in_=xtb[b][:, :], in_offset=None, bounds_check=breg, oob_is_err=False),
                         r=[('mxtb', b), sk], w=['xbuf'], dma=True)
            S.barrier()
        with ExitStack() as s2:
            xe = [sb("xe%d" % i, [128, D], BF16, s2) for i in range(2)]
            xeT = sb("xeT", [128, 16, C], BF16, s2)
            actT = sb("actT", [128, 12, C], BF16, s2)
            ystg = [sb("ystg%d" % i, [128, 512], F32, s2) for i in range(3)]
            yi = [0]
            xi = [0]
            for ex_i in range(16):
                Wi = dr["moe_w_in"][l, ex_i]
                Wd = dr["moe_w_down"][l, ex_i]
                for sti in range(CT):
                    b = xi[0] % 2
                    xi[0] += 1
                    r0 = ex_i * C + sti * 128
                    S.op('sp', lambda e: e.dma_start(out=xe[b][:], in_=xbuf[r0:r0 + 128, :]), w=[('xe', b)], dma=True)
                    transpose_to(xe[b], ('xe', b), 16, xeT, 'xeT', sti, BF16)
                for j in range(6):
                    wb = load_w(Wi, 0, 16, 512 * j, 512)
                    for m in range(4):
                        pi = next_ps()
                        for kc in range(16):
                            S.op('pe', lambda e, kc=kc: e.matmul(PS[pi][:, 0:C], lhsT=Wt[wb][:, kc, m * 128:(m + 1) * 128], rhs=xeT[:, kc, :], start=(kc == 0), stop=(kc == 15)),
                                 r=[('Wt', wb), 'xeT'], w=[('ps', pi)], inc=(kc == 15))
                        fc = (j * 4 + m) % 12
                        if j < 3:
                            S.op('act', lambda e: e.activation(out=actT[:, fc, :], in_=PS[pi][:, 0:C], func=AF.Silu), r=[('ps', pi)], w=[('actT', fc)])
                        else:
                            S.op('dve', lambda e: e.tensor_tensor(out=actT[:, fc, :], in0=actT[:, fc, :], in1=PS[pi][:, 0:C], op=ALU.mult), r=[('ps', pi), ('actT', fc)], w=[('actT', fc)])
                for cb in range(4):
                    wb = load_w(Wd, 0, 12, 512 * cb, 512)
                    for sti in range(CT):
                        pi = next_ps()
                        for fc in range(12):
                            S.op('pe', lambda e, fc=fc: e.matmul(PS[pi][:, :], lhsT=actT[:, fc, sti * 128:(sti + 1) * 128], rhs=Wt[wb][:, fc, :], start=(fc == 0), stop=(fc == 11)),
                                 r=[('Wt', wb), ('actT', fc)], w=[('ps', pi)], inc=(fc == 11))
                        i = yi[0] % 3
                        yi[0] += 1
                        S.op('act', lambda e: e.activation(out=ystg[i][:], in_=PS[pi][:, :], func=AF.Copy), r=[('ps', pi)], w=[('ystg', i)])
                        r0 = ex_i * C + sti * 128
                        S.op('sp', lambda e: e.dma_start(out=ybuf[r0:r0 + 128, cb * 512:(cb + 1) * 512], in_=ystg[i][:]), r=[('ystg', i)], w=['ybuf'], dma=True)
            S.barrier()
        with ExitStack() as s2:
            yA = sb("yA", [128, D], F32, s2)
            yB = sb("yB", [128, D], F32, s2)

            def yfn(tt):
                S.op('dve', lambda e: e.memset(yA[:], 0.0), w=['yA'])
                S.op('dve', lambda e: e.memset(yB[:], 0.0), w=['yB'])
                S.op('pool', lambda e: e.indirect_dma_start(out=yA[:, :], out_offset=None, in_=ybuf[:, :],
                                                            in_offset=bass.IndirectOffsetOnAxis(ap=slotAi[:, tt:tt + 1], axis=0), bounds_check=breg, oob_is_err=False),
                     r=['slotAi'], w=['yA', 'gth'], dma=True)
                S.op('pool', lambda e: e.indirect_dma_start(out=yB[:, :], out_offset=None, in_=ybuf[:, :],
                                                            in_offset=bass.IndirectOffsetOnAxis(ap=slotBi[:, tt:tt + 1], axis=0), bounds_check=breg, oob_is_err=False),
                     r=['slotBi'], w=['yB', 'gth'], dma=True)
                S.op('dve', lambda e: e.tensor_tensor(out=yA[:], in0=yA[:], in1=yB[:], op=ALU.subtract), r=['yA', 'yB'], w=['yA'])
                S.op('dve', lambda e: e.scalar_tensor_tensor(out=yA[:], in0=yA[:], scalar=gateA[:, tt:tt + 1], in1=yB[:], op0=ALU.mult, op1=ALU.add),
                     r=['yA', 'yB', 'gateA'], w=['yA'])
                return yA, 'yA'
            _resid_ln_phase(nc, S, sb, PS, s2, l, None, 0, yfn, None, xres, "ln_ffn_g", "ln_ffn_b", dr, dst, bufA, lambda tt: 'bufA', transpose_to, None)
            S.barrier()


def _nsa(nc, S, sb, PS, PSB, dr, cst, l, bufA, hT_d, h_d, transpose_to, next_ps, ident, identb, dbg):
    slopes = [2.0 ** (-8.0 * (h + 1) / 16.0) for h in range(16)]
    with ExitStack() as st:
        rel = sb("rel", [128, 2048], F32, st)
        cdiag = sb("cdiag", [128, 128], F32, st)
        cfar = sb("cfar", [128, 128], F32, st)
        dcon = sb("dcon", [128, 272], F32, st)
        cpb = sb("cpb", [128, 256], F32, st)
        cmask = sb("cmask", [128, 2048], BF16, st)
        selc = sb("selc", [128, NT, 32], F32, st)
        expd = sb("expd", [32, 2048], BF16, st)
        ng = sb("ng", [128, NT, 48], BF16, st)
        kcT = sb("kcT", [64, 4, 128], BF16, st)
        vca = sb("vca", [128, 4, 97], BF16, st)
        S.op('sp', lambda e: e.dma_start(out=rel[:], in_=cst['rel_mid'][:, :]), w=['rel'], dma=True)
        S.op('sp', lambda e: e.dma_start(out=cdiag[:], in_=cst['cdiag'][:, :]), w=['cdiag'], dma=True)
        S.op('sp', lambda e: e.dma_start(out=cfar[:], in_=cst['cfar'][:, :]), w=['cfar'], dma=True)
        S.op('sp', lambda e: e.dma_start(out=dcon[:], in_=cst['dconst'][:, :]), w=['dcon'], dma=True)
        S.op('sp', lambda e: e.dma_start(out=cpb[:], in_=cst['cmp_pb'][:, :]), w=['cpb'], dma=True)
        S.op('pool', lambda e: e.dma_start(out=cmask[:], in_=cst['cmp_mask'][:, :]), w=['cmask'], dma=True)
        S.op('sp', lambda e: e.dma_start(out=selc[:], in_=cst['selc'].rearrange("(tt p) j -> p tt j", p=128)), w=['selc'], dma=True)
        S.op('pool', lambda e: e.dma_start(out=expd[:], in_=cst['expand'][:, :]), w=['expd'], dma=True)
        S.op('sp', lambda e: e.dma_start(out=ng[:], in_=h_d[:, TC_NG:TC_NG + 48].rearrange("(tt p) j -> p tt j", p=128)), w=['ng'], dma=True)
        S.op('dve', lambda e: e.memset(kcT[:], 0.0), w=['kcT'])
        S.op('dve', lambda e: e.memset(vca[:], 0.0), w=['vca'])
        with ExitStack() as s2:
            w1 = sb("w1", [64, 2, 32, 256], BF16, s2)
            w2 = sb("w2", [128, 2, 2, 64], BF16, s2)
            pes = sb("pes", [32, 2, 64], F32, s2)
            peT = sb("peT", [64, 2, 32], BF16, s2)
            c1 = sb("c1", [128, 2, 2], F32, s2)
            srcT = sb("csrcT", [64, T], BF16, s2)
            u = sb("cu", [128, 128], F32, s2)
            t1 = sb("ct1", [128, 128], F32, s2)
            gel = sb("cgel", [128, 2, 128], BF16, s2)
            ovl = sb("ovl", [128, 32], F32, s2)
            for kv in range(2):
                S.op('pool', lambda e: e.dma_start(out=w1[:, kv, :, :], in_=dr["nsa_cmp_w1"][l, kv].rearrange("(l d) h -> d l h", d=64)), w=['w1'], dma=True)
                S.op('pool', lambda e: e.dma_start(out=w2[:, kv, :, :], in_=dr["nsa_cmp_w2"][l, kv].rearrange("(hc p) d -> p hc d", p=128)), w=['w2'], dma=True)
            S.op('sp', lambda e: e.dma_start(out=pes[:], in_=dr["nsa_cmp_pe"][l].rearrange("k l d -> l k d")), w=['pes'], dma=True)
            S.op('sp', lambda e: e.dma_start(out=ovl[:], in_=cst['overlap'][:, :]), w=['ovl'], dma=True)
            for g in range(4):
                S.op('dve', lambda e: e.memset(vca[:, g, 64:65], 1.0), r=[], w=['vca'])
                S.op('dve', lambda e: e.tensor_copy(out=vca[:, g, 65:97], in_=ovl[:]), r=['ovl'], w=['vca'])
            for kv in range(2):
                S.op('pe', lambda e: e.transpose(out=PS[0][0:64, 0:32], in_=pes[0:32, kv, :], identity=ident[0:32, 0:32]), r=['pes', 'ident'], w=[('ps', 0)])
                S.op('act', lambda e: e.activation(out=peT[:, kv, :], in_=PS[0][0:64, 0:32], func=AF.Copy), r=[('ps', 0)], w=['peT'])
                for hc in range(2):
                    for li in range(32):
                        S.op('pe', lambda e, li=li: e.matmul(PS[1][:, 0:1], lhsT=w1[:, kv, li, hc * 128:(hc + 1) * 128], rhs=peT[:, kv, li:li + 1], start=(li == 0), stop=(li == 31)),
                             r=['w1', 'peT'], w=[('ps', 1)], inc=(li == 31))
                    S.op('act', lambda e: e.activation(out=c1[:, kv, hc:hc + 1], in_=PS[1][:, 0:1], func=AF.Copy), r=[('ps', 1)], w=['c1'])
            for g in range(4):
                for kv in range(2):
                    r0 = (R_KC if kv == 0 else R_VC) + g * 64
                    S.op('sp', lambda e: e.dma_start(out=srcT[:], in_=hT_d[r0:r0 + 64, :]), w=['csrcT'], dma=True)
                    for hc in range(2):
                        pi = next_ps()
                        for li in range(32):
                            S.op('pe', lambda e, li=li: e.matmul(PS[pi][:, 0:127], lhsT=w1[:, kv, li, hc * 128:(hc + 1) * 128], rhs=srcT[:, li:li + 16 * 126 + 1:16],
                                                                 start=(li == 0), stop=(li == 31)), r=['w1', 'csrcT'], w=[('ps', pi)], inc=(li == 31))
                        S.op('act', lambda e: e.activation(out=u[:, 0:127], in_=PS[pi][:, 0:127], func=AF.Identity, bias=c1[:, kv, hc:hc + 1], scale=1.0), r=[('ps', pi), 'c1'], w=['cu'])
                        S.op('dve', lambda e: e.tensor_tensor(out=t1[:, 0:127], in0=u[:, 0:127], in1=u[:, 0:127], op=ALU.mult), r=['cu'], w=['ct1'])
                        S.op('dve', lambda e: e.tensor_scalar(out=t1[:, 0:127], in0=t1[:, 0:127], scalar1=0.044715, scalar2=1.0, op0=ALU.mult, op1=ALU.add), r=['ct1'], w=['ct1'])
                        S.op('dve', lambda e: e.tensor_tensor(out=t1[:, 0:127], in0=t1[:, 0:127], in1=u[:, 0:127], op=ALU.mult), r=['ct1', 'cu'], w=['ct1'])
                        S.op('act', lambda e: e.activation(out=t1[:, 0:127], in_=t1[:, 0:127], func=AF.Sigmoid, scale=2.0 * 0.7978845608028654), r=['ct1'], w=['ct1'])
                        S.op('dve', lambda e: e.tensor_tensor(out=gel[:, hc, 0:127], in0=t1[:, 0:127], in1=u[:, 0:127], op=ALU.mult), r=['ct1', 'cu'], w=['cgel'])
                    pi = next_ps()
                    if kv == 0:
                        for hc in range(2):
                            S.op('pe', lambda e, hc=hc: e.matmul(PS[pi][0:64, 0:127], lhsT=w2[:, 0, hc, :], rhs=gel[:, hc, 0:127], start=(hc == 0), stop=(hc == 1)),
                                 r=['w2', 'cgel'], w=[('ps', pi)], inc=(hc == 1))
                        S.op('act', lambda e: e.activation(out=kcT[:, g, 0:127], in_=PS[pi][0:64, 0:127], func=AF.Copy), r=[('ps', pi)], w=['kcT'])
                    else:
                        for hc in range(2):
                            S.op('pe', lambda e, hc=hc: e.matmul(PS[pi][0:127, 0:64], lhsT=gel[:, hc, 0:127], rhs=w2[:, 1, hc, :], start=(hc == 0), stop=(hc == 1)),
                                 r=['w2', 'cgel'], w=[('ps', pi)], inc=(hc == 1))
                        S.op('act', lambda e: e.activation(out=vca[0:127, g, 0:64], in_=PS[pi][0:127, 0:64], func=AF.Copy), r=[('ps', pi)], w=['vca'])
            S.barrier()
        dtmax = []
        for h in range(16):
            d = 1
            while d < 15 and slopes[h] * (128 * (d + 1) - 127) <= 40.0:
                d += 1
            dtmax.append(d)
        qT = sb("nqT", [64, 4, T], BF16, st)
        ksT = sb("nksT", [64, T], BF16, st)
        kwT = sb("nkwT", [64, T], BF16, st)
        vs = sb("nvs", [128, NT, 65], BF16, st)
        vw = sb("nvw", [128, NT, 65], BF16, st)
        NSC = 4
        sc = [sb("nsc%d" % i, [128, 512], F32, st) for i in range(NSC)]
        eTs = sb("neTs", [128, NT, 512], BF16, st)
        eTw = sb("neTw", [128, 5, 512], BF16, st)
        eTc = [sb("neTc%d" % i, [128, 512], BF16, st) for i in range(2)]
        imp = [sb("nimp%d" % i, [128, 32], F32, st) for i in range(2)]
        mx8 = [sb("nmx8%d" % i, [128, 8], F32, st) for i in range(2)]
        selb = [sb("nselb%d" % i, [128, 32], F32, st) for i in range(2)]
        selbT = sb("nselbT", [32, NT, 128], BF16, st)
        rd4 = [sb("nrd%d" % i, [128, 4], F32, st) for i in range(3)]
        oc = sb("noc", [128, NT, 256], F32, st)
        octmp = [sb("noctmp%d" % i, [128, 256], F32, st) for i in range(2)]
        ocb = [sb("nocb%d" % i, [128, 256], BF16, st) for i in range(2)]
        sci = [0]
        PB_SEL, PB_WIN, PB_CMP, PB_TR = 4, 5, 6, 7

        for g in range(4):
            S.op('sp', lambda e: e.dma_start(out=qT[:], in_=hT_d[R_NQ + g * 256:R_NQ + (g + 1) * 256, :].rearrange("(h d) t -> d h t", d=64)), w=['nqT'], dma=True)
            S.op('sp', lambda e: e.dma_start(out=ksT[:], in_=hT_d[R_KS + g * 64:R_KS + (g + 1) * 64, :]), w=['nksT'], dma=True)
            S.op('sp', lambda e: e.dma_start(out=kwT[:], in_=hT_d[R_KW + g * 64:R_KW + (g + 1) * 64, :]), w=['nkwT'], dma=True)
            S.op('dve', lambda e: e.memset(vs[:], 1.0), w=['nvs'])
            S.op('dve', lambda e: e.memset(vw[:], 1.0), w=['nvw'])
            S.op('sp', lambda e: e.dma_start(out=vs[:, :, 0:64], in_=h_d[:, TC_VS + g * 64:TC_VS + (g + 1) * 64].rearrange("(kt p) d -> p kt d", p=128)), w=['nvs'], dma=True)
            S.op('sp', lambda e: e.dma_start(out=vw[:, :, 0:64], in_=h_d[:, TC_VW + g * 64:TC_VW + (g + 1) * 64].rearrange("(kt p) d -> p kt d", p=128)), w=['nvw'], dma=True)

            def scores(tt, lhs, kkey, kt, mode, use_sel, edst, ekey, extra_mask, heads):
                ts = slice(tt * 128, (tt + 1) * 128)
                pi = next_ps(0, 4)
                pv = PS[pi][:, :].rearrange("p (h j) -> p h j", h=4)
                S.op('pe', lambda e: e.matmul(pv, lhsT=lhs, rhs=qT[:, :, ts], start=True, stop=not use_sel), r=[kkey, 'nqT'], w=[('ps', pi)], inc=not use_sel)
                if use_sel:
                    S.op('pe', lambda e: e.matmul(pv, lhsT=expd[:, kt * 128:(kt + 1) * 128], rhs=selbT[:, tt, :].unsqueeze(1).to_broadcast([32, 4, 128]), start=False, stop=True),
                         r=['expd', ('nselbT', tt)], w=[('ps', pi)], inc=True)
                si = sci[0] % NSC
                sci[0] += 1
                S.op('dve', lambda e: e.tensor_tensor(out=sc[si][:], in0=PS[pi][:, :], in1=rel[:, g * 512:(g + 1) * 512], op=ALU.add), r=[('ps', pi), 'rel'], w=[('nsc', si)])
                if extra_mask is not None:
                    mk, mkey = extra_mask
                    S.op('pool', lambda e: e.tensor_tensor(out=sc[si][:].rearrange("p (h j) -> p h j", h=4), in0=sc[si][:].rearrange("p (h j) -> p h j", h=4),
                                                           in1=mk.unsqueeze(1).to_broadcast([128, 4, 128]), op=ALU.add), r=[('nsc', si), mkey], w=[('nsc', si)])
                for h in heads:
                    hh = 4 * g + h
                    if mode == 'cmp':
                        bia = cpb[:, hh * 16 + tt:hh * 16 + tt + 1]
                    else:
                        bia = dcon[:, hh * 17 + (tt - kt):hh * 17 + (tt - kt) + 1]
                    S.op('act', lambda e, h=h: e.activation(out=edst[:, h * 128:(h + 1) * 128], in_=sc[si][:, h * 128:(h + 1) * 128], func=AF.Exp, bias=bia, scale=1.0),
                         r=[('nsc', si), 'cpb', 'dcon'], w=[(ekey, h)], inc=True)

            def pv_combine(tt, pb, per_head, ncol, br, first, ri):
                for h in range(4):
                    lst = per_head[h]
                    for i, (lt, rt, rk) in enumerate(lst):
                        S.op('pe', lambda e, lt=lt, rt=rt, i=i: e.matmul(PS[pb][:, h * 128:h * 128 + ncol], lhsT=lt, rhs=rt, start=(i == 0), stop=(i == len(lst) - 1)),
                             r=rk, w=[('ps', pb)], inc=(i == len(lst) - 1))
                rd = rd4[ri]
                rk_ = ('nrd', ri)
                S.op('dve', lambda e: e.tensor_scalar(out=rd[:], in0=PS[pb][:, 64::128], scalar1=1e-30, scalar2=None, op0=ALU.max), r=[('ps', pb)], w=[rk_])
                S.op('dve', lambda e: e.reciprocal(out=rd[:], in_=rd[:]), r=[rk_], w=[rk_])
                if br == 0:
                    ib = tt % 2
                    for h in range(4):
                        if h == 0:
                            S.op('dve', lambda e: e.tensor_scalar(out=imp[ib][:], in0=PS[pb][:, 65:97], scalar1=rd[:, 0:1], scalar2=None, op0=ALU.mult), r=[('ps', pb), rk_], w=[('nimp', ib)])
                        else:
                            S.op('dve', lambda e, h=h: e.scalar_tensor_tensor(out=imp[ib][:], in0=PS[pb][:, h * 128 + 65:h * 128 + 97], scalar=rd[:, h:h + 1], in1=imp[ib][:],
                                                                              op0=ALU.mult, op1=ALU.add), r=[('ps', pb), rk_, ('nimp', ib)], w=[('nimp', ib)])
                c0 = 12 * g + br
                S.op('dve', lambda e: e.tensor_tensor(out=rd[:], in0=rd[:], in1=ng[:, tt, c0:c0 + 10:3], op=ALU.mult), r=[rk_, 'ng'], w=[rk_])
                psv = PS[pb][:, :].rearrange("p (h c) -> p h c", h=4)[:, :, 0:64]
                wbc = rd[:].unsqueeze(2).to_broadcast([128, 4, 64])
                ocv = oc[:, tt, :].rearrange("p (h c) -> p h c", h=4)
                if first:
                    S.op('dve', lambda e: e.tensor_tensor(out=ocv, in0=psv, in1=wbc, op=ALU.mult), r=[('ps', pb), rk_], w=[('noc', tt)])
                else:
                    ti = (tt + br) % 2
                    S.op('dve', lambda e: e.tensor_tensor(out=octmp[ti][:].rearrange("p (h c) -> p h c", h=4), in0=psv, in1=wbc, op=ALU.mult), r=[('ps', pb), rk_], w=[('noctmp', ti)])
                    S.op('pool', lambda e: e.tensor_tensor(out=oc[:, tt, :], in0=oc[:, tt, :], in1=octmp[ti][:], op=ALU.add), r=[('noctmp', ti), ('noc', tt)], w=[('noc', tt)])

            for tt in range(NT):
                ts = slice(tt * 128, (tt + 1) * 128)
                cb_ = tt % 2
                scores(tt, kcT[:, g, :], 'kcT', 0, 'cmp', False, eTc[cb_], ('neTc', cb_), (cmask[:, ts], 'cmask'), range(4))
                ph = [[(eTc[cb_][:, h * 128:(h + 1) * 128], vca[:, g, 0:97], [(('neTc', cb_), h), 'vca'])] for h in range(4)]
                pv_combine(tt, PB_CMP, ph, 97, 0, True, 0)
                ib = tt % 2
                S.op('dve', lambda e: e.tensor_tensor(out=imp[ib][:], in0=imp[ib][:], in1=selc[:, tt, :], op=ALU.add), r=[('nimp', ib), 'selc'], w=[('nimp', ib)])
                S.op('dve', lambda e: e.max(out=mx8[ib][:], in_=imp[ib][:]), r=[('nimp', ib)], w=[('nmx8', ib)])
                S.op('dve', lambda e: e.tensor_scalar(out=mx8[ib][:, 7:8], in0=mx8[ib][:, 7:8], scalar1=-5e29, scalar2=None, op0=ALU.max), r=[('nmx8', ib)], w=[('nmx8', ib)])
                S.op('dve', lambda e: e.tensor_scalar(out=selb[ib][:], in0=imp[ib][:], scalar1=mx8[ib][:, 7:8], scalar2=NEGB, op0=ALU.is_lt, op1=ALU.mult),
                     r=[('nimp', ib), ('nmx8', ib)], w=[('nselb', ib)])
                S.op('pe', lambda e: e.transpose(out=PS[PB_TR][0:32, 0:128], in_=selb[ib][:, :], identity=ident[:]), r=[('nselb', ib), 'ident'], w=[('ps', PB_TR)])
                S.op('act', lambda e: e.activation(out=selbT[:, tt, :], in_=PS[PB_TR][0:32, 0:128], func=AF.Copy), r=[('ps', PB_TR)], w=[('nselbT', tt)])
            gd = max(dtmax[4 * g:4 * g + 4])
            for tt in range(NT):
                kts = list(range(max(0, tt - 4), tt + 1))
                for kt in kts:
                    em = (cdiag[:], 'cdiag') if kt == tt else ((cfar[:], 'cfar') if kt == tt - 4 else None)
                    hs = [h for h in range(4) if tt - kt <= dtmax[4 * g + h]]
                    scores(tt, kwT[:, kt * 128:(kt + 1) * 128], 'nkwT', kt, 'rel', False, eTw[:, tt - kt, :], ('neTw', tt - kt), em, hs)
                ph = [[(eTw[:, tt - kt, h * 128:(h + 1) * 128], vw[:, kt, :], [(('neTw', tt - kt), h), 'nvw']) for kt in kts if tt - kt <= dtmax[4 * g + h]] for h in range(4)]
                pv_combine(tt, PB_WIN, ph, 65, 2, False, 1)
                kts = list(range(max(0, tt - gd), tt + 1))
                for kt in kts:
                    hs = [h for h in range(4) if tt - kt <= dtmax[4 * g + h]]
                    scores(tt, ksT[:, kt * 128:(kt + 1) * 128], 'nksT', kt, 'rel', True, eTs[:, kt, :], ('neTs', kt), (cdiag[:], 'cdiag') if kt == tt else None, hs)
                ph = [[(eTs[:, kt, h * 128:(h + 1) * 128], vs[:, kt, :], [(('neTs', kt), h), 'nvs']) for kt in kts if tt - kt <= dtmax[4 * g + h]] for h in range(4)]
                pv_combine(tt, PB_SEL, ph, 65, 1, False, 2)
                ob = tt % 2
                S.op('act', lambda e: e.activation(out=ocb[ob][:], in_=oc[:, tt, :], func=AF.Copy), r=[('noc', tt)], w=[('nocb', ob)])
                transpose_to(ocb[ob], ('nocb', ob), 2, bufA, 'bufA', tt, BF16, c_off=8 + 2 * g, banks=(PB_TR, PB_TR + 1))
        S.barrier()


_NC_CACHE = {}


def kernel(**inputs):
    n = 8
    if "nc" not in _NC_CACHE:
        _NC_CACHE["nc"] = build()
    nc = _NC_CACHE["nc"]
    consts = make_consts()
    in_maps = []
    for c in range(n):
        m = {"x": np.ascontiguousarray(inputs["x"][c], dtype=np.float32), "mem": np.ascontiguousarray(inputs["mem"][c], dtype=np.float32)}
        for k in WNAMES:
            m[k] = np.ascontiguousarray(inputs[k], dtype=np.float32)
        for k, v in consts.items():
            m["c_" + k] = v
        in_maps.append(m)
    res = run_bass_kernel_spmd(nc, in_maps, core_ids=list(range(n)))
    return np.stack([res.results[c]["out"] for c in range(n)], axis=0).astype(np.float32)
```

```python
import numpy as np
from contextlib import ExitStack
import concourse.bass as bass
import concourse.mybir as mybir
from concourse.bass_utils import run_bass_kernel_spmd

F32 = mybir.dt.float32
BF16 = mybir.dt.bfloat16
AF = mybir.ActivationFunctionType
ALU = mybir.AluOpType
AX = mybir.AxisListType

T = 2048
D = 2048
NT = 16
DEPTH = 2
DN_ALPHA = float((2 * DEPTH) ** 0.25)
LN_EPS = 1e-5
D_IN = 9792
C_GQ, C_GK, C_GV, C_GR, C_GA, C_NQ, C_NKV, C_NG, C_MG = 0, 512, 1024, 2048, 3072, 3088, 4112, 5648, 5696
R_GQ, R_GK, R_GA, R_NQ, R_KC, R_VC, R_KS, R_KW, R_MG, NFM = 0, 512, 1024, 1056, 2080, 2336, 2592, 2848, 3104, 7200
TC_GK, TC_GV, TC_GR, TC_VS, TC_VW, TC_NG, NTM = 0, 512, 1536, 2560, 2816, 3072, 3120
NEGB = -30000.0


class Sch:
    def __init__(self, nc, es):
        self.nc = nc
        self.eng = {'pe': nc.tensor, 'act': nc.scalar, 'dve': nc.vector, 'pool': nc.gpsimd, 'sp': nc.sync}
        self.sem = {}
        self.cnt = {}
        for e in ('pe', 'act', 'dve', 'pool'):
            self.sem[e] = es.enter_context(nc.semaphore('s_' + e))
            self.cnt[e] = 0
        self.NS = 8
        for q in ('sp', 'pool'):
            for i in range(self.NS):
                k = ('dma', q, i)
                self.sem[k] = es.enter_context(nc.semaphore('d_%s%d' % (q, i)))
                self.cnt[k] = 0
        self.dma_i = {'sp': 0, 'pool': 0}
        self.waited = {e: {} for e in self.eng}
        self.lw = {}
        self.rd = {}
        self.nops = 0

    def _wait(self, e, tok):
        s, v = tok
        if self.waited[e].get(s, 0) >= v:
            return
        self.waited[e][s] = v
        self.eng[e].wait_ge(self.sem[s], v)

    def op(self, e, fn, r=(), w=(), dma=False, inc=True):
        deps = []
        for k in r:
            if k in self.lw:
                deps.append(self.lw[k])
        for k in w:
            if k in self.lw:
                deps.append(self.lw[k])
            deps.extend(self.rd.get(k, {}).values())
        if dma:
            i = self.dma_i[e]
            self.dma_i[e] += 1
            s = ('dma', e, i % self.NS)
            if self.cnt[s] > 0:
                deps.append((s, self.cnt[s]))
            self.cnt[s] += 16
            tok = (s, self.cnt[s])
        else:
            s = e
            if inc:
                self.cnt[s] += 1
                tok = (s, self.cnt[s])
            else:
                tok = (s, self.cnt[s] + 1)
        for d in deps:
            if d[0] == 'pe' and e == 'pe' and not dma:
                continue
            self._wait(e, d)
        ins = fn(self.eng[e])
        if dma:
            ins.then_inc(self.sem[s], 16)
        elif inc:
            ins.then_inc(self.sem[s], 1)
        for k in w:
            self.lw[k] = tok
            self.rd[k] = {}
        for k in r:
            self.rd.setdefault(k, {})[tok[0]] = tok
        self.nops += 1
        return tok

    def barrier(self):
        for e in self.eng:
            for s, c in self.cnt.items():
                if c > 0:
                    self._wait(e, (s, c))
        self.lw.clear()
        self.rd.clear()


def make_consts():
    c = {}
    i = np.arange(128)
    c['ident'] = np.eye(128, dtype=np.float32)
    c['um'] = (-(1.0 / 16.0) * (i[:, None] <= i[None, :])).astype(np.float32)
    c['um2'] = (-(1.0 / 16.0) * (i[:, None] > i[None, :])).astype(np.float32)
    c['caus4'] = np.tile((i[:, None] <= i[None, :]).astype(np.float32), (1, 4))
    slopes = 2.0 ** (-8.0 * np.arange(1, 17) / 16.0)
    rel = -(slopes[None, :, None]) * (i[None, None, :] - i[:, None, None]).astype(np.float64)
    c['rel_mid'] = rel.astype(np.float32).reshape(128, 16 * 128)
    dt = np.arange(17)
    c['dconst'] = np.broadcast_to((-slopes[:, None] * 128.0 * dt[None, :])[None], (128, 16, 17)).astype(np.float32).reshape(128, 16 * 17).copy()
    n = np.arange(128)
    cb = slopes[None, :, None] * (16.0 * n[:, None, None] + 15.5) - slopes[None, :, None] * 128.0 * np.arange(16)[None, None, :]
    cb = slopes[None, :, None] * (15.0 * n[:, None, None] + 15.5) - slopes[None, :, None] * 128.0 * np.arange(16)[None, None, :]
    c['cmp_pb'] = cb.astype(np.float32).reshape(128, 256)
    c['cdiag'] = np.where(i[None, :] >= i[:, None], 0.0, NEGB).astype(np.float32)
    c['cfar'] = np.where(i[None, :] < i[:, None], 0.0, NEGB).astype(np.float32)
    tq = (np.arange(16)[:, None] * 128 + i[None, :])
    valid = (16 * n[:, None, None] + 31) <= tq[None]
    valid[127] = False
    c['cmp_mask'] = np.where(valid, 0.0, NEGB).astype(np.float32).reshape(128, 2048)
    bs = 16 * n
    ss = 64 * np.arange(32)
    ov = ((bs[:, None] < ss[None, :] + 64) & (bs[:, None] + 32 > ss[None, :])).astype(np.float32)
    ov[127] = 0
    c['overlap'] = ov
    t = np.arange(T)
    cur = t // 64
    jj = np.arange(32)
    forced = (jj[None, :] == 0) | (jj[None, :] == cur[:, None]) | (jj[None, :] == cur[:, None] - 1)
    validb = (64 * jj[None, :]) <= t[:, None]
    c['selc'] = np.where(validb, np.where(forced, 1e6, 0.0), -1e30).astype(np.float32)
    ex = np.zeros((32, 16, 128), np.float32)
    for kt in range(16):
        ex[2 * kt, kt, :64] = 1
        ex[2 * kt + 1, kt, 64:] = 1
    c['expand'] = ex.reshape(32, 2048)
    c['lstr'] = (i[:, None] < i[None, :]).astype(np.float32)
    c['ebase'] = np.broadcast_to((np.arange(16) * 384 + 1.0e6)[None, :], (128, 16)).astype(np.float32).copy()
    return c


CONST_SHAPES = {k: v.shape for k, v in make_consts().items()}

WNAMES = ["w_in", "gla_w_a2", "gla_b_a", "gla_norm_g", "nsa_cmp_pe", "nsa_cmp_w1", "nsa_cmp_w2",
          "w_branch_gla", "w_branch_nsa", "w_out", "ln_mix_g", "ln_mix_b", "xa_wq", "xa_wkv", "xa_wo",
          "ln_xa_g", "ln_xa_b", "router_w", "router_b", "moe_w_in", "moe_w_down", "ln_ffn_g", "ln_ffn_b"]
WSHAPES = {
    "w_in": [2, 2048, 9792], "gla_w_a2": [2, 16, 512], "gla_b_a": [2, 512], "gla_norm_g": [2, 1024],
    "nsa_cmp_pe": [2, 2, 32, 64], "nsa_cmp_w1": [2, 2, 2048, 256], "nsa_cmp_w2": [2, 2, 256, 64],
    "w_branch_gla": [2, 1024, 2048], "w_branch_nsa": [2, 1024, 2048], "w_out": [2, 2048, 2048],
    "ln_mix_g": [2, 2048], "ln_mix_b": [2, 2048], "xa_wq": [2, 2048, 512], "xa_wkv": [2, 2048, 1024],
    "xa_wo": [2, 512, 2048], "ln_xa_g": [2, 2048], "ln_xa_b": [2, 2048], "router_w": [2048, 16],
    "router_b": [16], "moe_w_in": [2, 16, 2048, 3072], "moe_w_down": [2, 16, 1536, 2048],
    "ln_ffn_g": [2, 2048], "ln_ffn_b": [2, 2048],
}


def build(n_layers=DEPTH, stages=("mix", "xa", "moe"), debug=(), wshapes=None):
    WS = dict(WSHAPES)
    WS.update(wshapes or {})
    nc = bass.Bass("TRN2", target_bir_lowering=False)
    dr = {}
    dr["x"] = nc.dram_tensor("x", [T, D], F32, kind="ExternalInput").ap()
    dr["mem"] = nc.dram_tensor("mem", [256, D], F32, kind="ExternalInput").ap()
    for k in WNAMES:
        dr[k] = nc.dram_tensor(k, WS[k], F32, kind="ExternalInput").ap()
    cst = {k: nc.dram_tensor("c_" + k, list(s), F32, kind="ExternalInput").ap() for k, s in CONST_SHAPES.items()}
    out = nc.dram_tensor("out", [T, D], F32, kind="ExternalOutput").ap()
    dbg = {k: nc.dram_tensor("dbg_" + k, list(s), F32, kind="ExternalOutput").ap() for k, s in debug}
    xres = nc.dram_tensor("xres", [T, D], F32).ap()
    hT_d = nc.dram_tensor("hT_d", [NFM, T], BF16).ap()
    h_d = nc.dram_tensor("h_d", [T, NTM], BF16).ap()
    y_d = nc.dram_tensor("y_d", [T, D], F32).ap()

    with ExitStack() as es:
        block = es.enter_context(nc.Block())

        @block.gpsimd
        def _(_g):
            _emit(nc, dr, cst, out, dbg, xres, hT_d, h_d, y_d, n_layers, stages)
    return nc


def _emit(nc, dr, cst, out, dbg, xres, hT_d, h_d, y_d, n_layers, stages):
    es = ExitStack()
    S = Sch(nc, es)

    uid = [0]

    def sb(name, shape, dt, st=es):
        uid[0] += 1
        return st.enter_context(nc.sbuf_tensor(name + '_%d' % uid[0], shape, dt))

    def ps(name, shape, dt=F32, st=es):
        return st.enter_context(nc.psum_tensor(name, shape, dt))

    bufA = sb("bufA", [128, 16, T], BF16)
    bufB = None
    wt_i = [0]
    Wt = []

    def alloc_wt(st, n):
        del Wt[:]
        for i in range(n):
            Wt.append(sb("Wt%d" % i, [128, 16, 512], BF16, st))
        wt_i[0] = 0
    ident = sb("ident", [128, 128], F32)
    identb = sb("identb", [128, 128], BF16)
    PS = [ps("ps%d" % i, [128, 512]) for i in range(8)]
    S.op('sp', lambda e: e.dma_start(out=ident[:], in_=cst['ident'][:, :]), w=['ident'], dma=True)
    S.op('dve', lambda e: e.tensor_copy(out=identb[:], in_=ident[:]), r=['ident'], w=['identb'])

    ps_i = [0]

    def next_ps(lo=0, hi=4):
        i = lo + ps_i[0] % (hi - lo)
        ps_i[0] += 1
        return i

    def load_w(W2d, k0, KC, c0, nb):
        b = wt_i[0] % len(Wt)
        wt_i[0] += 1
        src = W2d[k0:k0 + KC * 128, c0:c0 + nb].rearrange("(kc p) n -> p kc n", p=128)
        S.op('pool', lambda e: e.dma_start(out=Wt[b][:, 0:KC, 0:nb], in_=src), w=[('Wt', b)], dma=True)
        return b

    def proj_tm(src, skey, KC, W2d, c0, nb, epi, k0=0, tts=range(NT)):
        b = load_w(W2d, k0, KC, c0, nb)
        for tt in tts:
            pi = next_ps()
            for kc in range(KC):
                S.op('pe', lambda e, kc=kc, tt=tt, pi=pi: e.matmul(PS[pi][:, 0:nb], lhsT=src[:, kc, tt * 128:(tt + 1) * 128],
                                                                    rhs=Wt[b][:, kc, 0:nb], start=(kc == 0), stop=(kc == KC - 1)),
                     r=[('Wt', b), skey], w=[('ps', pi)], inc=(kc == KC - 1))
            epi(tt, pi)

    def proj_fm(src, skey, KC, W2d, c0, nb, M, epi, k0=0):
        b = load_w(W2d, k0, KC, c0, nb)
        for m0 in range(0, nb, M):
            mm = min(M, nb - m0)
            for tb in range(4):
                pi = next_ps()
                for kc in range(KC):
                    S.op('pe', lambda e, kc=kc, tb=tb, pi=pi, m0=m0, mm=mm: e.matmul(
                        PS[pi][0:mm, :], lhsT=Wt[b][:, kc, m0:m0 + mm], rhs=src[:, kc, tb * 512:(tb + 1) * 512],
                        start=(kc == 0), stop=(kc == KC - 1)),
                         r=[('Wt', b), skey], w=[('ps', pi)], inc=(kc == KC - 1))
                epi(c0 + m0, mm, tb, pi)

    def ln_and_store(l, st, xt_tile, key, tt, g_bc, b_bc, dst_dram, small):
        stats, mv, rstd = small
        for c in range(4):
            S.op('dve', lambda e, c=c: e.bn_stats(out=stats[:, c, :], in_=xt_tile[:, c * 512:(c + 1) * 512]), r=[key], w=['stats'])
        S.op('dve', lambda e: e.bn_aggr(out=mv[:], in_=stats[:].rearrange("p a b -> p (a b)")), r=['stats'], w=['mv'])
        S.op('act', lambda e: e.activation(out=rstd[:], in_=mv[:, 1:2], func=AF.Sqrt, bias=LN_EPS, scale=1.0), r=['mv'], w=['rstd'])
        S.op('dve', lambda e: e.reciprocal(out=rstd[:], in_=rstd[:]), r=['rstd'], w=['rstd'])
        S.op('dve', lambda e: e.tensor_scalar(out=xt_tile[:], in0=xt_tile[:], scalar1=mv[:, 0:1], scalar2=rstd[:, 0:1],
                                              op0=ALU.subtract, op1=ALU.mult), r=[key, 'mv', 'rstd'], w=[key])
        S.op('pool', lambda e: e.tensor_tensor(out=xt_tile[:], in0=xt_tile[:], in1=g_bc[:], op=ALU.mult), r=[key, 'lng'], w=[key])
        S.op('pool', lambda e: e.tensor_tensor(out=xt_tile[:], in0=xt_tile[:], in1=b_bc[:], op=ALU.add), r=[key, 'lnb'], w=[key])
        S.op('sp', lambda e: e.dma_start(out=dst_dram[tt * 128:(tt + 1) * 128, :], in_=xt_tile[:]), r=[key], w=[('xres', tt)], dma=True)
        transpose_to(xt_tile, key, 16, bufA, 'bufA', tt, F32)

    def transpose_to(tile, key, nchunks, dst, dkey, tt, dt, c_off=0, banks=(4, 8)):
        idm = ident if dt == F32 else identb
        for c4 in range(0, nchunks, 4):
            pi = next_ps(*banks)
            pst = PS[pi] if dt == F32 else PSB[pi - 4]
            for c in range(c4, min(c4 + 4, nchunks)):
                S.op('pe', lambda e, c=c, c4=c4, pst=pst: e.transpose(out=pst[:, (c - c4) * 128:(c - c4 + 1) * 128],
                                                                   in_=tile[:, c * 128:(c + 1) * 128], identity=idm[:]),
                     r=[key, 'ident', 'identb'], w=[('ps', pi)])
            n = min(4, nchunks - c4)
            S.op('act', lambda e, c4=c4, n=n, pst=pst: e.activation(
                out=dst[:, c_off + c4:c_off + c4 + n, tt * 128:(tt + 1) * 128],
                in_=pst[:, 0:n * 128].rearrange("p (a b) -> p a b", a=n), func=AF.Copy), r=[('ps', pi)], w=[dkey])

    PSB = [PS[i][:].bitcast(BF16)[:, 0:512] for i in range(4, 8)]

    with ExitStack() as st:
        xt = [sb("xt%d" % i, [128, D], F32, st) for i in range(2)]
        for tt in range(NT):
            b = tt % 2
            S.op('sp', lambda e, b=b, tt=tt: e.dma_start(out=xt[b][:], in_=dr["x"][tt * 128:(tt + 1) * 128, :]), w=[('xt', b)], dma=True)
            S.op('sp', lambda e, b=b, tt=tt: e.dma_start(out=xres[tt * 128:(tt + 1) * 128, :], in_=xt[b][:]), r=[('xt', b)], w=[('xres', tt)], dma=True)
            transpose_to(xt[b], ('xt', b), 16, bufA, 'bufA', tt, F32)
        S.barrier()

    _H['alloc_wt'] = alloc_wt
    for l in range(n_layers):
        _mixer(nc, S, sb, PS, PSB, dr, cst, l, bufA, bufB, hT_d, h_d, xres, proj_tm, proj_fm, transpose_to, ln_and_store, next_ps,
               ident, identb, dbg)
        if "mixonly" in stages:
            break
        with ExitStack() as lq:
            qx = sb("qxT", [128, 4, T], BF16, lq)
            with ExitStack() as lb:
                bB = _merge(nc, S, sb, PS, PSB, dr, cst, l, bufA, hT_d, xres, proj_fm, transpose_to, next_ps, lb)

                def epi_q(col, mm, tb, pi):
                    S.op('act', lambda e: e.activation(out=qx[:, col // 128, tb * 512:(tb + 1) * 512], in_=PS[pi][:, :], func=AF.Copy, scale=float(128 ** -0.5)),
                         r=[('ps', pi)], w=['qxT'])
                with ExitStack() as lw:
                    alloc_wt(lw, 2)
                    proj_fm(bB, 'bufBall', 16, dr["xa_wq"][l], 0, 512, 128, epi_q)
                    S.barrier()
            _xattn(nc, S, sb, PS, PSB, dr, cst, l, bufA, qx, xres, proj_tm, proj_fm, transpose_to, ln_and_store, next_ps, ident, identb, dbg, load_w, Wt)
        if 'moe' not in SKIP:
          _moe(nc, S, sb, PS, PSB, dr, cst, l, bufA, None, xres, y_d, out if l == n_layers - 1 else xres, proj_tm, proj_fm,
               transpose_to, ln_and_store, next_ps, ident, identb, dbg, load_w, Wt)
    S.barrier()
    es.close()


def _mixer(nc, S, sb, PS, PSB, dr, cst, l, bufA, bufB, hT_d, h_d, xres, proj_tm, proj_fm, transpose_to, ln_and_store, next_ps,
           ident, identb, dbg):
    W = dr["w_in"][l]
    aT_d = nc.dram_tensor("aT_d%d" % l, [16, T], F32).ap()
    with ExitStack() as st:
        _H['alloc_wt'](st, 3)
        stg = [sb("stg%d" % i, [128, 512], BF16, st) for i in range(4)]
        stgf = sb("stgf", [16, 512], F32, st)
        si = [0]

        def epi_fm(rbase, cbase, func, scale=1.0):
            def f(col, mm, tb, pi):
                i = si[0] % 4
                si[0] += 1
                S.op('act', lambda e: e.activation(out=stg[i][0:mm, :], in_=PS[pi][0:mm, :], func=func, scale=scale), r=[('ps', pi)], w=[('stg', i)])
                r0 = rbase + col - cbase
                S.op('sp', lambda e: e.dma_start(out=hT_d[r0:r0 + mm, tb * 512:(tb + 1) * 512], in_=stg[i][0:mm, :]), r=[('stg', i)],
                     w=[('hT', r0 // 64, tb)] + ([('hT', r0 // 64 + 1, tb)] if mm == 128 else []), dma=True)
            return f

        def epi_ga(col, mm, tb, pi):
            S.op('act', lambda e: e.activation(out=stgf[0:16, :], in_=PS[pi][0:16, :], func=AF.Copy), r=[('ps', pi)], w=['stgf'])
            S.op('sp', lambda e: e.dma_start(out=aT_d[:, tb * 512:(tb + 1) * 512], in_=stgf[0:16, :]), r=['stgf'], w=[('aT', tb)], dma=True)

        def epi_tm(tcbase, nb, func):
            def f(tt, pi):
                i = si[0] % 4
                si[0] += 1
                S.op('act', lambda e: e.activation(out=stg[i][:, 0:nb], in_=PS[pi][:, 0:nb], func=func), r=[('ps', pi)], w=[('stg', i)])
                S.op('sp', lambda e: e.dma_start(out=h_d[tt * 128:(tt + 1) * 128, tcbase:tcbase + nb], in_=stg[i][:, 0:nb]), r=[('stg', i)],
                     w=[('h', tt, tcbase)], dma=True)
            return f

        A = (bufA, 'bufA', 16, W)
        proj_fm(*A, C_GQ, 512, 128, epi_fm(R_GQ, C_GQ, AF.Copy))
        proj_fm(*A, C_GK, 512, 128, epi_fm(R_GK, C_GK, AF.Copy))
        proj_fm(*A, C_GA, 16, 16, epi_ga)
        proj_tm(*A, C_GK, 512, epi_tm(TC_GK, 512, AF.Copy))
        for j in range(2):
            proj_tm(*A, C_GV + 512 * j, 512, epi_tm(TC_GV + 512 * j, 512, AF.Copy))
            proj_tm(*A, C_GR + 512 * j, 512, epi_tm(TC_GR + 512 * j, 512, AF.Silu))
            proj_fm(*A, C_NQ + 512 * j, 512, 64, epi_fm(R_NQ + 512 * j, C_NQ + 512 * j, AF.Copy, 0.125))
        for (cc, rr) in ((0, R_KC), (256, R_VC), (512, R_KS), (1024, R_KW)):
            proj_fm(*A, C_NKV + cc, 256, 64, epi_fm(rr, C_NKV + cc, AF.Copy))
        proj_tm(*A, C_NKV + 768, 256, epi_tm(TC_VS, 256, AF.Copy))
        proj_tm(*A, C_NKV + 1280, 256, epi_tm(TC_VW, 256, AF.Copy))
        proj_tm(*A, C_NG, 48, epi_tm(TC_NG, 48, AF.Sigmoid))
        for j in range(8):
            proj_fm(*A, C_MG + 512 * j, 512, 128, epi_fm(R_MG + 512 * j, C_MG + 512 * j, AF.Sigmoid))
        S.barrier()

    if 'gla' not in SKIP:
        _gla(nc, S, sb, PS, PSB, dr, cst, l, bufA, hT_d, h_d, aT_d, transpose_to, ident, identb, dbg)
    if 'nsa' not in SKIP:
        _nsa(nc, S, sb, PS, PSB, dr, cst, l, bufA, hT_d, h_d, transpose_to, next_ps, ident, identb, dbg)


def _gla(nc, S, sb, PS, PSB, dr, cst, l, bufA, hT_d, h_d, aT_d, transpose_to, ident, identb, dbg):
    with ExitStack() as st:
        um = sb("um", [128, 128], F32, st)
        um2 = sb("um2", [128, 128], F32, st)
        caus4 = sb("caus4", [128, 512], F32, st)
        gnbc = sb("gnbc", [128, 1024], F32, st)
        wa2 = sb("wa2", [32, 512], F32, st)
        aT = sb("aT", [32, T], F32, st)
        S.op('sp', lambda e: e.dma_start(out=um[:], in_=cst['um'][:, :]), w=['um'], dma=True)
        S.op('sp', lambda e: e.dma_start(out=um2[:], in_=cst['um2'][:, :]), w=['um2'], dma=True)
        S.op('sp', lambda e: e.dma_start(out=caus4[:], in_=cst['caus4'][:, :]), w=['caus4'], dma=True)
        S.op('sp', lambda e: e.dma_start(out=gnbc[:], in_=dr['gla_norm_g'][l, :].partition_broadcast(128)), w=['gnbc'], dma=True)
        S.op('sp', lambda e: e.dma_start(out=wa2[0:16, :], in_=dr['gla_w_a2'][l]), w=['wa2a'], dma=True)
        S.op('sp', lambda e: e.dma_start(out=wa2[16:17, :], in_=dr['gla_b_a'][l:l + 1, :]), w=['wa2b'], dma=True)
        S.op('dve', lambda e: e.memset(aT[:], 1.0), w=['aT'])
        S.op('sp', lambda e: e.dma_start(out=aT[0:16, :], in_=aT_d[:, :]), w=['aT'], dma=True)
        S32 = sb("S32", [128, 4, 256], F32, st)
        Sb = sb("Sb", [128, 4, 256], BF16, st)
        S.op('dve', lambda e: e.memset(S32[:], 0.0), w=['S32'])
        S.op('pool', lambda e: e.memset(Sb[:], 0.0), w=['Sb'])
        qT = [sb("gqT%d" % i, [128, 4, 128], BF16, st) for i in range(2)]
        kT = [sb("gkT%d" % i, [128, 4, 128], BF16, st) for i in range(2)]
        kk = [sb("gk%d" % i, [128, 512], BF16, st) for i in range(2)]
        vv = [sb("gv%d" % i, [128, 1024], BF16, st) for i in range(2)]
        rs = [sb("grs%d" % i, [128, 1024], BF16, st) for i in range(2)]
        le = sb("le", [128, 512], F32, st)
        ll = sb("ll", [128, 512], F32, st)
        Eq = sb("Eq", [128, 512], F32, st)
        Ek = sb("Ek", [128, 512], F32, st)
        Ekh = sb("Ekh", [128, 512], F32, st)
        qs = sb("qs", [128, 512], BF16, st)
        ks = sb("ks", [128, 512], BF16, st)
        kh = sb("kh", [128, 512], BF16, st)
        att = sb("att", [128, 512], BF16, st)
        og = sb("og", [128, 1024], F32, st)
        ogb = sb("ogb", [128, 1024], BF16, st)
        grs = sb("grsf", [128, 1024], F32, st)
        stats = sb("gstats", [128, 4, 6], F32, st)
        mv = sb("gmv", [128, 4, 2], F32, st)
        rstd = sb("grstd", [128, 4], F32, st)
        for c in range(NT):
            b = c % 2
            cs = slice(c * 128, (c + 1) * 128)
            S.op('sp', lambda e: e.dma_start(out=qT[b][:], in_=hT_d[R_GQ:R_GQ + 512, cs].rearrange("(h d) t -> d h t", d=128)), w=[('qT', b)], dma=True)
            S.op('sp', lambda e: e.dma_start(out=kT[b][:], in_=hT_d[R_GK:R_GK + 512, cs].rearrange("(h d) t -> d h t", d=128)), w=[('kT', b)], dma=True)
            S.op('sp', lambda e: e.dma_start(out=kk[b][:], in_=h_d[cs, TC_GK:TC_GK + 512]), w=[('kk', b)], dma=True)
            S.op('sp', lambda e: e.dma_start(out=vv[b][:], in_=h_d[cs, TC_GV:TC_GV + 1024]), w=[('vv', b)], dma=True)
            S.op('sp', lambda e: e.dma_start(out=rs[b][:], in_=h_d[cs, TC_GR:TC_GR + 1024]), w=[('rs', b)], dma=True)
            S.op('pe', lambda e: e.matmul(PS[0][:, :], lhsT=aT[0:17, cs], rhs=wa2[0:17, :], start=True, stop=True), r=['aT', 'wa2a', 'wa2b'], w=[('ps', 0)])
            S.op('act', lambda e: e.activation(out=le[:], in_=PS[0][:, :], func=AF.Exp, scale=-1.0), r=[('ps', 0)], w=['le'])
            S.op('act', lambda e: e.activation(out=ll[:], in_=le[:], func=AF.Ln, bias=1.0, scale=1.0), r=['le'], w=['ll'])
            for h in range(4):
                S.op('pe', lambda e, h=h: e.matmul(PS[1][:, h * 128:(h + 1) * 128], lhsT=ll[:, h * 128:(h + 1) * 128], rhs=um[:], start=True, stop=True),
                     r=['ll', 'um'], w=[('ps', 1)], inc=(h == 3))
            S.op('pe', lambda e: e.matmul(PS[2][:, :], lhsT=um2[:], rhs=ll[:], start=True, stop=True), r=['ll', 'um2'], w=[('ps', 2)])
            S.op('act', lambda e: e.activation(out=Eq[:], in_=PS[1][:, :], func=AF.Exp), r=[('ps', 1)], w=['Eq'])
            S.op('act', lambda e: e.activation(out=Ek[:], in_=PS[1][:, :], func=AF.Exp, scale=-1.0), r=[('ps', 1)], w=['Ek'])
            S.op('act', lambda e: e.activation(out=Ekh[:], in_=PS[2][:, :], func=AF.Exp), r=[('ps', 2)], w=['Ekh'])
            S.op('dve', lambda e: e.scalar_tensor_tensor(out=qs[:], in0=qT[b][:].rearrange("p h t -> p (h t)"), scalar=float(128 ** -0.5), in1=Eq[:],
                                                         op0=ALU.mult, op1=ALU.mult), r=[('qT', b), 'Eq'], w=['qs'])
            S.op('dve', lambda e: e.tensor_tensor(out=ks[:], in0=kT[b][:].rearrange("p h t -> p (h t)"), in1=Ek[:], op=ALU.mult), r=[('kT', b), 'Ek'], w=['ks'])
            S.op('pool', lambda e: e.tensor_tensor(out=kh[:], in0=kk[b][:], in1=Ekh[:], op=ALU.mult), r=[('kk', b), 'Ekh'], w=['kh'])
            for h in range(4):
                hs = slice(h * 128, (h + 1) * 128)
                S.op('pe', lambda e, hs=hs: e.matmul(PS[3][:, hs], lhsT=ks[:, hs], rhs=qs[:, hs], start=True, stop=True), r=['ks', 'qs'], w=[('ps', 3)], inc=(h == 3))
            S.op('dve', lambda e: e.tensor_tensor(out=att[:], in0=PS[3][:, :], in1=caus4[:], op=ALU.mult), r=[('ps', 3), 'caus4'], w=['att'])
            for h in range(4):
                hs = slice(h * 128, (h + 1) * 128)
                pb = 4 + h // 2
                po = slice((h % 2) * 256, (h % 2) * 256 + 256)
                S.op('pe', lambda e, hs=hs, pb=pb, po=po, h=h: e.matmul(PS[pb][:, po], lhsT=att[:, hs], rhs=vv[b][:, h * 256:(h + 1) * 256], start=True, stop=False),
                     r=['att', ('vv', b)], w=[('ps', pb)], inc=False)
                S.op('pe', lambda e, hs=hs, pb=pb, po=po, h=h: e.matmul(PS[pb][:, po], lhsT=qs[:, hs], rhs=Sb[:, h, :], start=False, stop=True),
                     r=['qs', 'Sb'], w=[('ps', pb)], inc=True)
            for h in range(4):
                hs = slice(h * 128, (h + 1) * 128)
                pb = 6 + h // 2
                po = slice((h % 2) * 256, (h % 2) * 256 + 256)
                S.op('pe', lambda e, hs=hs, pb=pb, po=po, h=h: e.matmul(PS[pb][:, po], lhsT=kh[:, hs], rhs=vv[b][:, h * 256:(h + 1) * 256], start=True, stop=True),
                     r=['kh', ('vv', b)], w=[('ps', pb)])
                S.op('dve', lambda e, pb=pb, po=po, h=h: e.scalar_tensor_tensor(out=S32[:, h, :], in0=S32[:, h, :], scalar=Eq[:, h * 128 + 127:h * 128 + 128],
                                                                              in1=PS[pb][:, po], op0=ALU.mult, op1=ALU.add), r=[('ps', pb), 'Eq', 'S32'], w=['S32'])
            S.op('act', lambda e: e.activation(out=Sb[:], in_=S32[:], func=AF.Copy), r=['S32'], w=['Sb'])
            for h in range(4):
                pb = 4 + h // 2
                po = slice((h % 2) * 256, (h % 2) * 256 + 256)
                S.op('dve', lambda e, pb=pb, po=po, h=h: e.bn_stats(out=stats[:, h, :], in_=PS[pb][:, po]), r=[('ps', pb)], w=['gstats'])
                S.op('dve', lambda e, h=h: e.bn_aggr(out=mv[:, h, :], in_=stats[:, h, :]), r=['gstats'], w=['gmv'])
            S.op('act', lambda e: e.activation(out=rstd[:], in_=mv[:, :, 1], func=AF.Sqrt, bias=LN_EPS, scale=1.0), r=['gmv'], w=['grstd'])
            S.op('dve', lambda e: e.reciprocal(out=rstd[:], in_=rstd[:]), r=['grstd'], w=['grstd'])
            for h in range(4):
                pb = 4 + h // 2
                po = slice((h % 2) * 256, (h % 2) * 256 + 256)
                S.op('dve', lambda e, pb=pb, po=po, h=h: e.tensor_scalar(out=og[:, h * 256:(h + 1) * 256], in0=PS[pb][:, po], scalar1=mv[:, h, 0:1], scalar2=rstd[:, h:h + 1],
                                                                       op0=ALU.subtract, op1=ALU.mult), r=[('ps', pb), 'gmv', 'grstd'], w=['og'])
            S.op('pool', lambda e: e.tensor_tensor(out=grs[:], in0=rs[b][:], in1=gnbc[:], op=ALU.mult), r=[('rs', b), 'gnbc'], w=['grsf'])
            S.op('pool', lambda e: e.tensor_tensor(out=ogb[:], in0=og[:], in1=grs[:], op=ALU.mult), r=['og', 'grsf'], w=['ogb'])
            if 'o_gla' in dbg:
                S.op('pool', lambda e: e.tensor_tensor(out=og[:], in0=og[:], in1=grs[:], op=ALU.mult), r=['og', 'grsf'], w=['og'])
                S.op('sp', lambda e: e.dma_start(out=dbg['o_gla'][cs, :], in_=og[:]), r=['og'], w=[('dbg', c)], dma=True)
            transpose_to(ogb, 'ogb', 8, bufA, 'bufA', c, BF16)
        S.barrier()


def _resid_ln_phase(nc, S, sb, PS, st, l, Wres, KC, src, skeyf, alpha_src, gname, bname, dr, dst_dram, dstT, dstkeyf, transpose_to, ln_keys, nbuf=1):
    xt = [sb("rl_xt%d" % i, [128, D], F32, st) for i in range(nbuf)]
    g_bc = sb("rl_g", [128, D], F32, st)
    b_bc = sb("rl_b", [128, D], F32, st)
    stats = sb("rl_stats", [128, 4, 6], F32, st)
    mv = sb("rl_mv", [128, 2], F32, st)
    rstd = sb("rl_rstd", [128, 1], F32, st)
    S.op('sp', lambda e: e.dma_start(out=g_bc[:], in_=dr[gname][l, :].partition_broadcast(128)), w=['lng'], dma=True)
    S.op('sp', lambda e: e.dma_start(out=b_bc[:], in_=dr[bname][l, :].partition_broadcast(128)), w=['lnb'], dma=True)
    for tt in range(NT):
        b = tt % nbuf
        key = ('rlxt', b)
        S.op('sp', lambda e: e.dma_start(out=xt[b][:], in_=alpha_src[tt * 128:(tt + 1) * 128, :]), r=[('xres', tt)], w=[key], dma=True)
        for cb in range(4):
            if Wres is not None:
                for kc in range(KC):
                    S.op('pe', lambda e, kc=kc, cb=cb: e.matmul(PS[cb][:, :], lhsT=src[:, kc, tt * 128:(tt + 1) * 128], rhs=Wres[:, kc, cb * 512:(cb + 1) * 512],
                                                                start=(kc == 0), stop=(kc == KC - 1)), r=[skeyf(tt), 'Wres'], w=[('ps', cb)], inc=(kc == KC - 1))
                S.op('dve', lambda e, cb=cb: e.scalar_tensor_tensor(out=xt[b][:, cb * 512:(cb + 1) * 512], in0=xt[b][:, cb * 512:(cb + 1) * 512], scalar=DN_ALPHA,
                                                                   in1=PS[cb][:, :], op0=ALU.mult, op1=ALU.add), r=[('ps', cb), key], w=[key])
            else:
                if cb == 0:
                    ytile, ykey = src(tt)
                S.op('dve', lambda e, cb=cb: e.scalar_tensor_tensor(out=xt[b][:, cb * 512:(cb + 1) * 512], in0=xt[b][:, cb * 512:(cb + 1) * 512], scalar=DN_ALPHA,
                                                                   in1=ytile[:, cb * 512:(cb + 1) * 512], op0=ALU.mult, op1=ALU.add), r=[ykey, key], w=[key])
        for c in range(4):
            S.op('dve', lambda e, c=c: e.bn_stats(out=stats[:, c, :], in_=xt[b][:, c * 512:(c + 1) * 512]), r=[key], w=['stats'])
        S.op('dve', lambda e: e.bn_aggr(out=mv[:], in_=stats[:].rearrange("p a b -> p (a b)")), r=['stats'], w=['mv'])
        S.op('act', lambda e: e.activation(out=rstd[:], in_=mv[:, 1:2], func=AF.Sqrt, bias=LN_EPS, scale=1.0), r=['mv'], w=['rstd'])
        S.op('dve', lambda e: e.reciprocal(out=rstd[:], in_=rstd[:]), r=['rstd'], w=['rstd'])
        S.op('dve', lambda e: e.tensor_scalar(out=xt[b][:], in0=xt[b][:], scalar1=mv[:, 0:1], scalar2=rstd[:, 0:1], op0=ALU.subtract, op1=ALU.mult),
             r=[key, 'mv', 'rstd'], w=[key])
        S.op('pool', lambda e: e.tensor_tensor(out=xt[b][:], in0=xt[b][:], in1=g_bc[:], op=ALU.mult), r=[key, 'lng'], w=[key])
        S.op('pool', lambda e: e.tensor_tensor(out=xt[b][:], in0=xt[b][:], in1=b_bc[:], op=ALU.add), r=[key, 'lnb'], w=[key])
        S.op('sp', lambda e: e.dma_start(out=dst_dram[tt * 128:(tt + 1) * 128, :], in_=xt[b][:]), r=[key], w=[('xres', tt)], dma=True)
        transpose_to(xt[b], key, 16, dstT, dstkeyf(tt), tt, F32)


def _load_resident(S, dst, dkey, W2d, KC):
    for kc in range(KC):
        for cb in range(4):
            S.op('pool', lambda e, kc=kc, cb=cb: e.dma_start(out=dst[:, kc, cb * 512:(cb + 1) * 512], in_=W2d[kc * 128:(kc + 1) * 128, cb * 512:(cb + 1) * 512]),
                 w=[dkey], dma=True)


def _merge(nc, S, sb, PS, PSB, dr, cst, l, bufA, hT_d, xres, proj_fm, transpose_to, next_ps, st):
    bufB = sb("bufB", [128, 16, T], BF16, st)
    with ExitStack() as s2:
        _H['alloc_wt'](s2, 2)
        sg = [sb("sg%d" % i, [128, 512], BF16, s2) for i in range(2)]
        tmp = sb("mtmp", [128, 512], F32, s2)
        gi = [0]

        def epi(branch):
            def f(col, mm, tb, pi):
                i = gi[0] % 2
                gi[0] += 1
                r0 = R_MG + branch * 2048 + col
                S.op('sp', lambda e: e.dma_start(out=sg[i][:], in_=hT_d[r0:r0 + 128, tb * 512:(tb + 1) * 512]), w=[('sg', i)], dma=True)
                dsl = bufB[:, col // 128, tb * 512:(tb + 1) * 512]
                if branch == 0:
                    S.op('dve', lambda e: e.tensor_tensor(out=dsl, in0=PS[pi][:, :], in1=sg[i][:], op=ALU.mult), r=[('ps', pi), ('sg', i)], w=['bufB'])
                else:
                    S.op('dve', lambda e: e.tensor_tensor(out=tmp[:], in0=PS[pi][:, :], in1=sg[i][:], op=ALU.mult), r=[('ps', pi), ('sg', i)], w=['mtmp'])
                    S.op('pool', lambda e: e.tensor_tensor(out=dsl, in0=dsl, in1=tmp[:], op=ALU.add), r=['mtmp', 'bufB'], w=['bufB'])
            return f
        for j in range(4):
            proj_fm(bufA[:, 0:8, :], 'bufA', 8, dr["w_branch_gla"][l], 512 * j, 512, 128, epi(0))
        for j in range(4):
            proj_fm(bufA[:, 8:16, :], 'bufA', 8, dr["w_branch_nsa"][l], 512 * j, 512, 128, epi(1))
        S.barrier()
    with ExitStack() as s2:
        _load_resident(S, bufA, 'Wres', dr["w_out"][l], 16)
        _resid_ln_phase(nc, S, sb, PS, s2, l, bufA, 16, bufB, lambda tt: ('bufB', tt), xres, "ln_mix_g", "ln_mix_b", dr, xres, bufB,
                        lambda tt: ('bufB', tt), transpose_to, None)
        S.barrier()
    return bufB


def _xattn(nc, S, sb, PS, PSB, dr, cst, l, bufA, bufB, xres, proj_tm, proj_fm, transpose_to, ln_and_store, next_ps, ident, identb, dbg, load_w, Wt):
    with ExitStack() as st:
        _H['alloc_wt'](st, 2)
        memT = sb("memT", [128, 16, 256], BF16, st)
        mt_ = sb("memtile", [128, D], F32, st)
        kTs = sb("xkT", [128, 4, 256], BF16, st)
        vs = sb("xv", [128, 2, 4, 132], BF16, st)
        qx = bufB
        oT = sb("xoT", [128, 4, T], BF16, st)
        woT = sb("woT", [128, 4, D], BF16, st)
        eT = [sb("xeT%d" % i, [128, 512], BF16, st) for i in range(2)]
        oxa = sb("oxa", [128, 512], BF16, st)
        rden = sb("xrden", [128, 4], F32, st)
        for m in range(2):
            S.op('sp', lambda e: e.dma_start(out=mt_[:], in_=dr["mem"][m * 128:(m + 1) * 128, :]), w=['memtile'], dma=True)
            transpose_to(mt_, 'memtile', 16, memT, 'memT', m, F32)
        S.op('dve', lambda e: e.memset(vs[:], 1.0), w=['xv'])
        Wkv = dr["xa_wkv"][l]
        b = load_w(Wkv, 0, 16, 0, 512)
        for h in range(4):
            pi = next_ps()
            for kc in range(16):
                S.op('pe', lambda e, kc=kc: e.matmul(PS[pi][:, 0:256], lhsT=Wt[b][:, kc, h * 128:(h + 1) * 128], rhs=memT[:, kc, :], start=(kc == 0), stop=(kc == 15)),
                     r=[('Wt', b), 'memT'], w=[('ps', pi)], inc=(kc == 15))
            S.op('act', lambda e: e.activation(out=kTs[:, h, :], in_=PS[pi][:, 0:256], func=AF.Copy), r=[('ps', pi)], w=['xkT'])
        b = load_w(Wkv, 0, 16, 512, 512)
        for m in range(2):
            pi = next_ps()
            for kc in range(16):
                S.op('pe', lambda e, kc=kc: e.matmul(PS[pi][:, :], lhsT=memT[:, kc, m * 128:(m + 1) * 128], rhs=Wt[b][:, kc, :], start=(kc == 0), stop=(kc == 15)),
                     r=[('Wt', b), 'memT'], w=[('ps', pi)], inc=(kc == 15))
            S.op('act', lambda e: e.activation(out=vs[:, m, :, 0:128], in_=PS[pi][:, :].rearrange("p (h d) -> p h d", h=4), func=AF.Copy), r=[('ps', pi)], w=['xv'])

        for tt in range(NT):
            ts = slice(tt * 128, (tt + 1) * 128)
            for m in range(2):
                pi = next_ps()
                for h in range(4):
                    S.op('pe', lambda e, h=h: e.matmul(PS[pi][:, h * 128:(h + 1) * 128], lhsT=kTs[:, h, m * 128:(m + 1) * 128], rhs=qx[:, h, ts], start=True, stop=True),
                         r=['xkT', 'qxT'], w=[('ps', pi)], inc=(h == 3))
                S.op('act', lambda e: e.activation(out=eT[m][:], in_=PS[pi][:, :], func=AF.Exp), r=[('ps', pi)], w=[('xeT', m)])
            for h in range(4):
                pb = 4 + h // 2
                po = (h % 2) * 132
                for m in range(2):
                    S.op('pe', lambda e, m=m: e.matmul(PS[pb][:, po:po + 129], lhsT=eT[m][:, h * 128:(h + 1) * 128], rhs=vs[:, m, h, 0:129], start=(m == 0), stop=(m == 1)),
                         r=[('xeT', m), 'xv'], w=[('ps', pb)], inc=(m == 1))
                S.op('dve', lambda e: e.reciprocal(out=rden[:, h:h + 1], in_=PS[pb][:, po + 128:po + 129]), r=[('ps', pb)], w=['xrden'])
                S.op('dve', lambda e: e.tensor_scalar(out=oxa[:, h * 128:(h + 1) * 128], in0=PS[pb][:, po:po + 128], scalar1=rden[:, h:h + 1], scalar2=None, op0=ALU.mult),
                     r=[('ps', pb), 'xrden'], w=['oxa'])
            transpose_to(oxa, 'oxa', 4, oT, 'xoT', tt, BF16)
        S.barrier()
        for kc in range(4):
            for cb in range(4):
                S.op('pool', lambda e: e.dma_start(out=woT[:, kc, cb * 512:(cb + 1) * 512], in_=dr["xa_wo"][l][kc * 128:(kc + 1) * 128, cb * 512:(cb + 1) * 512]),
                     w=['Wres'], dma=True)
        _resid_ln_phase(nc, S, sb, PS, st, l, woT, 4, oT, lambda tt: 'xoT', xres, "ln_xa_g", "ln_xa_b", dr, xres, bufA, lambda tt: 'bufA', transpose_to, None, nbuf=2)
        S.barrier()


SKIP = set()
_H = {}
MOE_C = 384
MOE_BIG = 1.0e6


def _moe(nc, S, sb, PS, PSB, dr, cst, l, bufA, bufB, xres, y_d, dst, proj_tm, proj_fm, transpose_to, ln_and_store, next_ps, ident, identb, dbg, load_w, Wt):
    C = MOE_C
    CT = C // 128
    NSL = 16 * C
    xbuf = nc.dram_tensor("moe_xbuf%d" % l, [NSL, D], BF16).ap()
    ybuf = nc.dram_tensor("moe_ybuf%d" % l, [NSL, D], F32).ap()
    I32 = mybir.dt.int32
    breg = nc.gpsimd.to_reg(NSL - 1)
    with ExitStack() as st:
        gateA = sb("gateA", [128, NT], F32, st)
        slotAi = sb("slotAi", [128, NT], I32, st)
        slotBi = sb("slotBi", [128, NT], I32, st)
        with ExitStack() as s2:
            rw = sb("rw", [128, 16, 16], BF16, s2)
            rb = sb("rb", [128, 16], F32, s2)
            lg = sb("lg", [128, 16], F32, s2)
            lb = sb("lb", [128, 4, 4], F32, s2)
            eq = sb("eq", [128, 4, 4], F32, s2)
            lb2 = sb("lb2", [128, 4, 4], F32, s2)
            m1 = sb("m1", [128, 4], F32, s2)
            m2 = sb("m2", [128, 4], F32, s2)
            gs = sb("gs", [128, 4], F32, s2)
            gm = sb("gm", [128, 1], F32, s2)
            ex = sb("ex", [128, 16], F32, s2)
            den = sb("den", [128, 1], F32, s2)
            gate = sb("gate", [128, 16], F32, s2)
            maskall = sb("maskall", [128, NT, 16], BF16, s2)
            lstr = sb("lstr", [128, 128], BF16, s2)
            onesb = sb("onesb", [128, 128], BF16, s2)
            ebase = sb("ebase", [128, 16], F32, s2)
            smat = sb("smat", [128, 16], F32, s2)
            tA = sb("tA", [128, 16], F32, s2)
            tB = sb("tB", [128, 16], F32, s2)
            sA = sb("sA", [128, 1], F32, s2)
            sB = sb("sB", [128, 1], F32, s2)
            zt = sb("zt", [128, D], BF16, s2)
            xtf = [sb("mxtf%d" % i, [128, D], F32, s2) for i in range(2)]
            xtb = [sb("mxtb%d" % i, [128, D], BF16, s2) for i in range(2)]
            S.op('pool', lambda e: e.dma_start(out=rw[:], in_=dr["router_w"].rearrange("(kc p) e -> p kc e", p=128)), w=['rw'], dma=True)
            S.op('sp', lambda e: e.dma_start(out=rb[:], in_=dr["router_b"].partition_broadcast(128)), w=['rb'], dma=True)
            S.op('pool', lambda e: e.dma_start(out=lstr[:], in_=cst['lstr'][:, :]), w=['lstr'], dma=True)
            S.op('sp', lambda e: e.dma_start(out=ebase[:], in_=cst['ebase'][:, :]), w=['ebase'], dma=True)
            S.op('dve', lambda e: e.memset(onesb[:], 1.0), w=['onesb'])
            S.op('dve', lambda e: e.memset(zt[:], 0.0), w=['zt'])
            for ex_i in range(16):
                S.op('sp', lambda e: e.dma_start(out=xbuf[ex_i * C:(ex_i + 1) * C, :].rearrange("(a p) d -> p a d", p=128),
                                                 in_=zt[:].unsqueeze(1).to_broadcast([128, CT, D])), r=['zt'], w=[('xz', ex_i)], dma=True)
            for tt in range(NT):
                ts = slice(tt * 128, (tt + 1) * 128)
                b = tt % 2
                S.op('sp', lambda e: e.dma_start(out=xtf[b][:], in_=xres[ts, :]), r=[('xres', tt)], w=[('mxtf', b)], dma=True)
                S.op('act', lambda e: e.activation(out=xtb[b][:], in_=xtf[b][:], func=AF.Copy), r=[('mxtf', b)], w=[('mxtb', b)])
                pi = next_ps()
                for kc in range(16):
                    S.op('pe', lambda e, kc=kc: e.matmul(PS[pi][:, 0:16], lhsT=bufA[:, kc, ts], rhs=rw[:, kc, :], start=(kc == 0), stop=(kc == 15)),
                         r=['bufA', 'rw'], w=[('ps', pi)], inc=(kc == 15))
                lbf = lb[:].rearrange("p a b -> p (a b)")
                eqf = eq[:].rearrange("p a b -> p (a b)")
                S.op('dve', lambda e: e.tensor_copy(out=lg[:], in_=PS[pi][:, 0:16]), r=[('ps', pi)], w=['lg'])
                S.op('dve', lambda e: e.tensor_tensor(out=lbf, in0=lg[:], in1=rb[:], op=ALU.add), r=['lg', 'rb'], w=['lb'])
                S.op('dve', lambda e: e.tensor_reduce(out=m1[:], in_=lb[:], axis=AX.X, op=ALU.max), r=['lb'], w=['m1'])
                S.op('dve', lambda e: e.tensor_tensor(out=eq[:], in0=lb[:], in1=m1[:].unsqueeze(2).to_broadcast([128, 4, 4]), op=ALU.is_equal), r=['lb', 'm1'], w=['eq'])
                S.op('dve', lambda e: e.scalar_tensor_tensor(out=lb2[:], in0=eq[:], scalar=-1e30, in1=lb[:], op0=ALU.mult, op1=ALU.add), r=['eq', 'lb'], w=['lb2'])
                S.op('dve', lambda e: e.tensor_reduce(out=m2[:], in_=lb2[:], axis=AX.X, op=ALU.max), r=['lb2'], w=['m2'])
                S.op('dve', lambda e: e.tensor_tensor(out=gs[:], in0=m1[:], in1=m2[:], op=ALU.add), r=['m1', 'm2'], w=['gs'])
                S.op('dve', lambda e: e.tensor_reduce(out=gm[:], in_=gs[:], axis=AX.X, op=ALU.max), r=['gs'], w=['gm'])
                S.op('dve', lambda e: e.tensor_scalar(out=gs[:], in0=gs[:], scalar1=gm[:, 0:1], scalar2=None, op0=ALU.is_equal), r=['gs', 'gm'], w=['gs'])
                S.op('dve', lambda e: e.tensor_tensor(out=eq[:], in0=lb[:], in1=m2[:].unsqueeze(2).to_broadcast([128, 4, 4]), op=ALU.is_ge), r=['lb', 'm2'], w=['eq'])
                S.op('dve', lambda e: e.tensor_tensor(out=eq[:], in0=eq[:], in1=gs[:].unsqueeze(2).to_broadcast([128, 4, 4]), op=ALU.mult), r=['eq', 'gs'], w=['eq'])
                S.op('act', lambda e: e.activation(out=ex[:], in_=lg[:], func=AF.Exp), r=['lg'], w=['ex'])
                S.op('dve', lambda e: e.tensor_tensor(out=ex[:], in0=ex[:], in1=eqf, op=ALU.mult), r=['ex', 'eq'], w=['ex'])
                S.op('dve', lambda e: e.tensor_reduce(out=den[:], in_=ex[:], axis=AX.X, op=ALU.add), r=['ex'], w=['den'])
                S.op('dve', lambda e: e.reciprocal(out=den[:], in_=den[:]), r=['den'], w=['den'])
                S.op('dve', lambda e: e.tensor_scalar(out=gate[:], in0=ex[:], scalar1=den[:, 0:1], scalar2=None, op0=ALU.mult), r=['ex', 'den'], w=['gate'])
                S.op('act', lambda e: e.activation(out=maskall[:, tt, :], in_=eqf, func=AF.Copy), r=['eq'], w=[('maskall', tt)])
                pj = next_ps()
                for t2 in range(tt):
                    S.op('pe', lambda e, t2=t2: e.matmul(PS[pj][:, 0:16], lhsT=onesb[:], rhs=maskall[:, t2, :], start=(t2 == 0), stop=False),
                         r=['onesb', ('maskall', t2)], w=[('ps', pj)], inc=False)
                S.op('pe', lambda e: e.matmul(PS[pj][:, 0:16], lhsT=lstr[:], rhs=maskall[:, tt, :], start=(tt == 0), stop=True),
                     r=['lstr', ('maskall', tt)], w=[('ps', pj)], inc=True)
                S.op('dve', lambda e: e.tensor_tensor(out=smat[:], in0=PS[pj][:, 0:16], in1=ebase[:], op=ALU.add), r=[('ps', pj), 'ebase'], w=['smat'])
                S.op('dve', lambda e: e.scalar_tensor_tensor(out=tA[:], in0=eqf, scalar=-MOE_BIG, in1=smat[:], op0=ALU.mult, op1=ALU.add), r=['eq', 'smat'], w=['tA'])
                S.op('dve', lambda e: e.tensor_reduce(out=sA[:], in_=tA[:], axis=AX.X, op=ALU.min), r=['tA'], w=['sA'])
                S.op('dve', lambda e: e.tensor_tensor(out=tB[:], in0=tA[:], in1=eqf, op=ALU.mult), r=['tA', 'eq'], w=['tB'])
                S.op('dve', lambda e: e.tensor_reduce(out=sB[:], in_=tB[:], axis=AX.X, op=ALU.max), r=['tB'], w=['sB'])
                S.op('dve', lambda e: e.tensor_scalar(out=tB[:], in0=tA[:], scalar1=sA[:, 0:1], scalar2=None, op0=ALU.is_equal), r=['tA', 'sA'], w=['tB'])
                S.op('dve', lambda e: e.tensor_tensor(out=tB[:], in0=tB[:], in1=gate[:], op=ALU.mult), r=['tB', 'gate'], w=['tB'])
                S.op('dve', lambda e: e.tensor_reduce(out=gateA[:, tt:tt + 1], in_=tB[:], axis=AX.X, op=ALU.add), r=['tB'], w=['gateA'])
                S.op('dve', lambda e: e.tensor_copy(out=slotAi[:, tt:tt + 1], in_=sA[:]), r=['sA'], w=['slotAi'])
                S.op('dve', lambda e: e.tensor_copy(out=slotBi[:, tt:tt + 1], in_=sB[:]), r=['sB'], w=['slotBi'])
                for sl, sk in ((slotAi, 'slotAi'), (slotBi, 'slotBi')):
                    S.op('pool', lambda e: e.indirect_dma_start(out=xbuf[:, :], out_offset=bass.IndirectOffsetOnAxis(ap=sl[:, tt:tt + 1], axis=0),
                                                                in_=xtb[b][:, :], in_offset=None, bounds_check=breg, oob_is_err=False),
                         r=[('mxtb', b), sk] + [('xz', q) for q in range(16)], w=[('xsc', tt, sk)], dma=True)
            S.barrier()
        with ExitStack() as s2:
            _H['alloc_wt'](s2, 4)
            xe = [sb("xe%d" % i, [128, D], BF16, s2) for i in range(2)]
            xeT = sb("xeT", [128, 16, C], BF16, s2)
            actT = sb("actT", [128, 12, C], BF16, s2)
            ystg = [sb("ystg%d" % i, [128, 512], F32, s2) for i in range(3)]
            yi = [0]
            xi = [0]
            for ex_i in range(0 if 'moe2' in SKIP else 16):
                Wi = dr["moe_w_in"][l, ex_i]
                Wd = dr["moe_w_down"][l, ex_i]
                for sti in range(CT):
                    b = xi[0] % 2
                    xi[0] += 1
                    r0 = ex_i * C + sti * 128
                    S.op('sp', lambda e: e.dma_start(out=xe[b][:], in_=xbuf[r0:r0 + 128, :]), w=[('xe', b)], dma=True)
                    transpose_to(xe[b], ('xe', b), 16, xeT, 'xeT', sti, BF16)
                for j in range(6):
                    wb = load_w(Wi, 0, 16, 512 * j, 512)
                    for m in range(4):
                        pi = next_ps()
                        for kc in range(16):
                            S.op('pe', lambda e, kc=kc: e.matmul(PS[pi][:, 0:C], lhsT=Wt[wb][:, kc, m * 128:(m + 1) * 128], rhs=xeT[:, kc, :], start=(kc == 0), stop=(kc == 15)),
                                 r=[('Wt', wb), 'xeT'], w=[('ps', pi)], inc=(kc == 15))
                        fc = (j * 4 + m) % 12
                        if j < 3:
                            S.op('act', lambda e: e.activation(out=actT[:, fc, :], in_=PS[pi][:, 0:C], func=AF.Silu), r=[('ps', pi)], w=[('actT', fc)])
                        else:
                            S.op('dve', lambda e: e.tensor_tensor(out=actT[:, fc, :], in0=actT[:, fc, :], in1=PS[pi][:, 0:C], op=ALU.mult), r=[('ps', pi), ('actT', fc)], w=[('actT', fc)])
                for cb in range(4):
                    wb = load_w(Wd, 0, 12, 512 * cb, 512)
                    for sti in range(CT):
                        pi = next_ps()
                        for fc in range(12):
                            S.op('pe', lambda e, fc=fc: e.matmul(PS[pi][:, :], lhsT=actT[:, fc, sti * 128:(sti + 1) * 128], rhs=Wt[wb][:, fc, :], start=(fc == 0), stop=(fc == 11)),
                                 r=[('Wt', wb), ('actT', fc)], w=[('ps', pi)], inc=(fc == 11))
                        i = yi[0] % 3
                        yi[0] += 1
                        S.op('act', lambda e: e.activation(out=ystg[i][:], in_=PS[pi][:, :], func=AF.Copy), r=[('ps', pi)], w=[('ystg', i)])
                        r0 = ex_i * C + sti * 128
                        S.op('sp', lambda e: e.dma_start(out=ybuf[r0:r0 + 128, cb * 512:(cb + 1) * 512], in_=ystg[i][:]), r=[('ystg', i)], w=['ybuf'], dma=True)
            S.barrier()
        with ExitStack() as s2:
            yA = sb("yA", [128, D], F32, s2)
            yB = sb("yB", [128, D], F32, s2)

            def yfn(tt):
                S.op('dve', lambda e: e.memset(yA[:], 0.0), w=['yA'])
                S.op('dve', lambda e: e.memset(yB[:], 0.0), w=['yB'])
                S.op('pool', lambda e: e.indirect_dma_start(out=yA[:, :], out_offset=None, in_=ybuf[:, :],
                                                            in_offset=bass.IndirectOffsetOnAxis(ap=slotAi[:, tt:tt + 1], axis=0), bounds_check=breg, oob_is_err=False),
                     r=['slotAi'], w=['yA', 'gth'], dma=True)
                S.op('pool', lambda e: e.indirect_dma_start(out=yB[:, :], out_offset=None, in_=ybuf[:, :],
                                                            in_offset=bass.IndirectOffsetOnAxis(ap=slotBi[:, tt:tt + 1], axis=0), bounds_check=breg, oob_is_err=False),
                     r=['slotBi'], w=['yB', 'gth'], dma=True)
                S.op('dve', lambda e: e.tensor_tensor(out=yA[:], in0=yA[:], in1=yB[:], op=ALU.subtract), r=['yA', 'yB'], w=['yA'])
                S.op('dve', lambda e: e.scalar_tensor_tensor(out=yA[:], in0=yA[:], scalar=gateA[:, tt:tt + 1], in1=yB[:], op0=ALU.mult, op1=ALU.add),
                     r=['yA', 'yB', 'gateA'], w=['yA'])
                return yA, 'yA'
            _resid_ln_phase(nc, S, sb, PS, s2, l, None, 0, yfn, None, xres, "ln_ffn_g", "ln_ffn_b", dr, dst, bufA, lambda tt: 'bufA', transpose_to, None, nbuf=2)
            S.barrier()


def _nsa(nc, S, sb, PS, PSB, dr, cst, l, bufA, hT_d, h_d, transpose_to, next_ps, ident, identb, dbg):
    slopes = [2.0 ** (-8.0 * (h + 1) / 16.0) for h in range(16)]
    with ExitStack() as st:
        rel = sb("rel", [128, 2048], F32, st)
        cdiag = sb("cdiag", [128, 128], F32, st)
        cfar = sb("cfar", [128, 128], F32, st)
        dcon = sb("dcon", [128, 272], F32, st)
        cpb = sb("cpb", [128, 256], F32, st)
        cmask = sb("cmask", [128, 2048], BF16, st)
        selc = sb("selc", [128, NT, 32], F32, st)
        expd = sb("expd", [32, 2048], BF16, st)
        ng = sb("ng", [128, NT, 48], BF16, st)
        kcT = sb("kcT", [64, 4, 128], BF16, st)
        vca = sb("vca", [128, 4, 97], BF16, st)
        S.op('sp', lambda e: e.dma_start(out=rel[:], in_=cst['rel_mid'][:, :]), w=['rel'], dma=True)
        S.op('sp', lambda e: e.dma_start(out=cdiag[:], in_=cst['cdiag'][:, :]), w=['cdiag'], dma=True)
        S.op('sp', lambda e: e.dma_start(out=cfar[:], in_=cst['cfar'][:, :]), w=['cfar'], dma=True)
        S.op('sp', lambda e: e.dma_start(out=dcon[:], in_=cst['dconst'][:, :]), w=['dcon'], dma=True)
        S.op('sp', lambda e: e.dma_start(out=cpb[:], in_=cst['cmp_pb'][:, :]), w=['cpb'], dma=True)
        S.op('pool', lambda e: e.dma_start(out=cmask[:], in_=cst['cmp_mask'][:, :]), w=['cmask'], dma=True)
        S.op('sp', lambda e: e.dma_start(out=selc[:], in_=cst['selc'].rearrange("(tt p) j -> p tt j", p=128)), w=['selc'], dma=True)
        S.op('pool', lambda e: e.dma_start(out=expd[:], in_=cst['expand'][:, :]), w=['expd'], dma=True)
        S.op('sp', lambda e: e.dma_start(out=ng[:], in_=h_d[:, TC_NG:TC_NG + 48].rearrange("(tt p) j -> p tt j", p=128)), w=['ng'], dma=True)
        S.op('dve', lambda e: e.memset(kcT[:], 0.0), w=['kcT'])
        S.op('dve', lambda e: e.memset(vca[:], 0.0), w=['vca'])
        with ExitStack() as s2:
            w1 = sb("w1", [64, 2, 32, 256], BF16, s2)
            w2 = sb("w2", [128, 2, 2, 64], BF16, s2)
            pes = sb("pes", [32, 2, 64], F32, s2)
            peT = sb("peT", [64, 2, 32], BF16, s2)
            c1 = sb("c1", [128, 2, 2], F32, s2)
            srcT = sb("csrcT", [64, T], BF16, s2)
            u = sb("cu", [128, 128], F32, s2)
            t1 = sb("ct1", [128, 128], F32, s2)
            gel = sb("cgel", [128, 2, 128], BF16, s2)
            ovl = sb("ovl", [128, 32], F32, s2)
            for kv in range(2):
                S.op('pool', lambda e: e.dma_start(out=w1[:, kv, :, :], in_=dr["nsa_cmp_w1"][l, kv].rearrange("(l d) h -> d l h", d=64)), w=['w1'], dma=True)
                S.op('pool', lambda e: e.dma_start(out=w2[:, kv, :, :], in_=dr["nsa_cmp_w2"][l, kv].rearrange("(hc p) d -> p hc d", p=128)), w=['w2'], dma=True)
            S.op('sp', lambda e: e.dma_start(out=pes[:], in_=dr["nsa_cmp_pe"][l].rearrange("k l d -> l k d")), w=['pes'], dma=True)
            S.op('sp', lambda e: e.dma_start(out=ovl[:], in_=cst['overlap'][:, :]), w=['ovl'], dma=True)
            for g in range(4):
                S.op('dve', lambda e: e.memset(vca[:, g, 64:65], 1.0), r=[], w=['vca'])
                S.op('dve', lambda e: e.tensor_copy(out=vca[:, g, 65:97], in_=ovl[:]), r=['ovl'], w=['vca'])
            for kv in range(2):
                S.op('pe', lambda e: e.transpose(out=PS[0][0:64, 0:32], in_=pes[0:32, kv, :], identity=ident[0:32, 0:32]), r=['pes', 'ident'], w=[('ps', 0)])
                S.op('act', lambda e: e.activation(out=peT[:, kv, :], in_=PS[0][0:64, 0:32], func=AF.Copy), r=[('ps', 0)], w=['peT'])
                for hc in range(2):
                    for li in range(32):
                        S.op('pe', lambda e, li=li: e.matmul(PS[1][:, 0:1], lhsT=w1[:, kv, li, hc * 128:(hc + 1) * 128], rhs=peT[:, kv, li:li + 1], start=(li == 0), stop=(li == 31)),
                             r=['w1', 'peT'], w=[('ps', 1)], inc=(li == 31))
                    S.op('act', lambda e: e.activation(out=c1[:, kv, hc:hc + 1], in_=PS[1][:, 0:1], func=AF.Copy), r=[('ps', 1)], w=['c1'])
            for g in range(4):
                for kv in range(2):
                    r0 = (R_KC if kv == 0 else R_VC) + g * 64
                    S.op('sp', lambda e: e.dma_start(out=srcT[:], in_=hT_d[r0:r0 + 64, :]), w=['csrcT'], dma=True)
                    for hc in range(2):
                        pi = next_ps()
                        for li in range(32):
                            S.op('pe', lambda e, li=li: e.matmul(PS[pi][:, 0:127], lhsT=w1[:, kv, li, hc * 128:(hc + 1) * 128], rhs=srcT[:, li:li + 16 * 126 + 1:16],
                                                                 start=(li == 0), stop=(li == 31)), r=['w1', 'csrcT'], w=[('ps', pi)], inc=(li == 31))
                        S.op('act', lambda e: e.activation(out=u[:, 0:127], in_=PS[pi][:, 0:127], func=AF.Identity, bias=c1[:, kv, hc:hc + 1], scale=1.0), r=[('ps', pi), 'c1'], w=['cu'])
                        S.op('dve', lambda e: e.tensor_tensor(out=t1[:, 0:127], in0=u[:, 0:127], in1=u[:, 0:127], op=ALU.mult), r=['cu'], w=['ct1'])
                        S.op('dve', lambda e: e.tensor_scalar(out=t1[:, 0:127], in0=t1[:, 0:127], scalar1=0.044715, scalar2=1.0, op0=ALU.mult, op1=ALU.add), r=['ct1'], w=['ct1'])
                        S.op('dve', lambda e: e.tensor_tensor(out=t1[:, 0:127], in0=t1[:, 0:127], in1=u[:, 0:127], op=ALU.mult), r=['ct1', 'cu'], w=['ct1'])
                        S.op('act', lambda e: e.activation(out=t1[:, 0:127], in_=t1[:, 0:127], func=AF.Sigmoid, scale=2.0 * 0.7978845608028654), r=['ct1'], w=['ct1'])
                        S.op('dve', lambda e: e.tensor_tensor(out=gel[:, hc, 0:127], in0=t1[:, 0:127], in1=u[:, 0:127], op=ALU.mult), r=['ct1', 'cu'], w=['cgel'])
                    pi = next_ps()
                    if kv == 0:
                        for hc in range(2):
                            S.op('pe', lambda e, hc=hc: e.matmul(PS[pi][0:64, 0:127], lhsT=w2[:, 0, hc, :], rhs=gel[:, hc, 0:127], start=(hc == 0), stop=(hc == 1)),
                                 r=['w2', 'cgel'], w=[('ps', pi)], inc=(hc == 1))
                        S.op('act', lambda e: e.activation(out=kcT[:, g, 0:127], in_=PS[pi][0:64, 0:127], func=AF.Copy), r=[('ps', pi)], w=['kcT'])
                    else:
                        for hc in range(2):
                            S.op('pe', lambda e, hc=hc: e.matmul(PS[pi][0:127, 0:64], lhsT=gel[:, hc, 0:127], rhs=w2[:, 1, hc, :], start=(hc == 0), stop=(hc == 1)),
                                 r=['w2', 'cgel'], w=[('ps', pi)], inc=(hc == 1))
                        S.op('act', lambda e: e.activation(out=vca[0:127, g, 0:64], in_=PS[pi][0:127, 0:64], func=AF.Copy), r=[('ps', pi)], w=['vca'])
            S.barrier()
        dtmax = []
        for h in range(16):
            d = 1
            while d < 15 and slopes[h] * (128 * (d + 1) - 127) <= 40.0:
                d += 1
            dtmax.append(d)
        qT = sb("nqT", [64, 4, T], BF16, st)
        ksT = sb("nksT", [64, T], BF16, st)
        kwT = sb("nkwT", [64, T], BF16, st)
        vs = sb("nvs", [128, NT, 65], BF16, st)
        vw = sb("nvw", [128, NT, 65], BF16, st)
        NSC = 4
        sc = [sb("nsc%d" % i, [128, 512], F32, st) for i in range(NSC)]
        eTs = sb("neTs", [128, NT, 512], BF16, st)
        eTw = sb("neTw", [128, 5, 512], BF16, st)
        eTc = [sb("neTc%d" % i, [128, 512], BF16, st) for i in range(2)]
        imp = [sb("nimp%d" % i, [128, 32], F32, st) for i in range(2)]
        mx8 = [sb("nmx8%d" % i, [128, 8], F32, st) for i in range(2)]
        selb = [sb("nselb%d" % i, [128, 32], F32, st) for i in range(2)]
        selbT = sb("nselbT", [32, NT, 128], BF16, st)
        rd4 = [sb("nrd%d" % i, [128, 4], F32, st) for i in range(3)]
        oc = sb("noc", [128, NT, 256], F32, st)
        octmp = [sb("noctmp%d" % i, [128, 256], F32, st) for i in range(2)]
        ocb = [sb("nocb%d" % i, [128, 256], BF16, st) for i in range(2)]
        sci = [0]
        PB_SEL, PB_WIN, PB_CMP, PB_TR = 4, 5, 6, 7
        relgd = sb("relgd", [128, NT, 512], F32, st)

        for g in range(4):
            S.op('sp', lambda e: e.dma_start(out=qT[:], in_=hT_d[R_NQ + g * 256:R_NQ + (g + 1) * 256, :].rearrange("(h d) t -> d h t", d=64)), w=['nqT'], dma=True)
            S.op('sp', lambda e: e.dma_start(out=ksT[:], in_=hT_d[R_KS + g * 64:R_KS + (g + 1) * 64, :]), w=['nksT'], dma=True)
            S.op('sp', lambda e: e.dma_start(out=kwT[:], in_=hT_d[R_KW + g * 64:R_KW + (g + 1) * 64, :]), w=['nkwT'], dma=True)
            S.op('dve', lambda e: e.memset(vs[:], 1.0), w=['nvs'])
            S.op('dve', lambda e: e.memset(vw[:], 1.0), w=['nvw'])
            S.op('sp', lambda e: e.dma_start(out=vs[:, :, 0:64], in_=h_d[:, TC_VS + g * 64:TC_VS + (g + 1) * 64].rearrange("(kt p) d -> p kt d", p=128)), w=['nvs'], dma=True)
            S.op('sp', lambda e: e.dma_start(out=vw[:, :, 0:64], in_=h_d[:, TC_VW + g * 64:TC_VW + (g + 1) * 64].rearrange("(kt p) d -> p kt d", p=128)), w=['nvw'], dma=True)

            for dt_ in range(NT):
                for h in range(4):
                    hh = 4 * g + h
                    S.op('act', lambda e: e.activation(out=relgd[:, dt_, h * 128:(h + 1) * 128], in_=rel[:, hh * 128:(hh + 1) * 128], func=AF.Identity,
                                                       bias=dcon[:, hh * 17 + dt_:hh * 17 + dt_ + 1], scale=1.0), r=['rel', 'dcon'], w=[('relgd', dt_)])

            def scores(tt, lhs, kkey, kt, mode, use_sel, edst, ekey, extra_mask, heads):
                ts = slice(tt * 128, (tt + 1) * 128)
                pi = next_ps(0, 4)
                pv = PS[pi][:, :].rearrange("p (h j) -> p h j", h=4)
                S.op('pe', lambda e: e.matmul(pv, lhsT=lhs, rhs=qT[:, :, ts], start=True, stop=not use_sel), r=[kkey, 'nqT'], w=[('ps', pi)], inc=not use_sel)
                if use_sel:
                    S.op('pe', lambda e: e.matmul(pv, lhsT=expd[:, kt * 128:(kt + 1) * 128], rhs=selbT[:, tt, :].unsqueeze(1).to_broadcast([32, 4, 128]), start=False, stop=True),
                         r=['expd', ('nselbT', tt)], w=[('ps', pi)], inc=True)
                si = sci[0] % NSC
                sci[0] += 1
                if mode == 'cmp':
                    S.op('dve', lambda e: e.tensor_tensor(out=sc[si][:], in0=PS[pi][:, :], in1=rel[:, g * 512:(g + 1) * 512], op=ALU.add), r=[('ps', pi), 'rel'], w=[('nsc', si)])
                else:
                    S.op('dve', lambda e: e.tensor_tensor(out=sc[si][:], in0=PS[pi][:, :], in1=relgd[:, tt - kt, :], op=ALU.add), r=[('ps', pi), ('relgd', tt - kt)], w=[('nsc', si)])
                if extra_mask is not None:
                    mk, mkey = extra_mask
                    S.op('pool', lambda e: e.tensor_tensor(out=sc[si][:].rearrange("p (h j) -> p h j", h=4), in0=sc[si][:].rearrange("p (h j) -> p h j", h=4),
                                                           in1=mk.unsqueeze(1).to_broadcast([128, 4, 128]), op=ALU.add), r=[('nsc', si), mkey], w=[('nsc', si)])
                if mode == 'cmp':
                    for h in heads:
                        hh = 4 * g + h
                        bia = cpb[:, hh * 16 + tt:hh * 16 + tt + 1]
                        S.op('act', lambda e, h=h: e.activation(out=edst[:, h * 128:(h + 1) * 128], in_=sc[si][:, h * 128:(h + 1) * 128], func=AF.Exp, bias=bia, scale=1.0),
                             r=[('nsc', si), 'cpb'], w=[(ekey, h)], inc=True)
                else:
                    S.op('act', lambda e: e.activation(out=edst, in_=sc[si][:], func=AF.Exp), r=[('nsc', si)], w=[(ekey, h) for h in range(4)], inc=True)

            def pv_combine(tt, pb, per_head, ncol, br, first, ri):
                for h in range(4):
                    lst = per_head[h]
                    for i, (lt, rt, rk) in enumerate(lst):
                        S.op('pe', lambda e, lt=lt, rt=rt, i=i: e.matmul(PS[pb][:, h * 128:h * 128 + ncol], lhsT=lt, rhs=rt, start=(i == 0), stop=(i == len(lst) - 1)),
                             r=rk, w=[('ps', pb)], inc=(i == len(lst) - 1))
                rd = rd4[ri]
                rk_ = ('nrd', ri)
                S.op('dve', lambda e: e.tensor_scalar(out=rd[:], in0=PS[pb][:, 64::128], scalar1=1e-30, scalar2=None, op0=ALU.max), r=[('ps', pb)], w=[rk_])
                S.op('dve', lambda e: e.reciprocal(out=rd[:], in_=rd[:]), r=[rk_], w=[rk_])
                if br == 0:
                    ib = tt % 2
                    for h in range(4):
                        if h == 0:
                            S.op('dve', lambda e: e.tensor_scalar(out=imp[ib][:], in0=PS[pb][:, 65:97], scalar1=rd[:, 0:1], scalar2=None, op0=ALU.mult), r=[('ps', pb), rk_], w=[('nimp', ib)])
                        else:
                            S.op('dve', lambda e, h=h: e.scalar_tensor_tensor(out=imp[ib][:], in0=PS[pb][:, h * 128 + 65:h * 128 + 97], scalar=rd[:, h:h + 1], in1=imp[ib][:],
                                                                              op0=ALU.mult, op1=ALU.add), r=[('ps', pb), rk_, ('nimp', ib)], w=[('nimp', ib)])
                c0 = 12 * g + br
                S.op('dve', lambda e: e.tensor_tensor(out=rd[:], in0=rd[:], in1=ng[:, tt, c0:c0 + 10:3], op=ALU.mult), r=[rk_, 'ng'], w=[rk_])
                psv = PS[pb][:, :].rearrange("p (h c) -> p h c", h=4)[:, :, 0:64]
                wbc = rd[:].unsqueeze(2).to_broadcast([128, 4, 64])
                ocv = oc[:, tt, :].rearrange("p (h c) -> p h c", h=4)
                if first:
                    S.op('dve', lambda e: e.tensor_tensor(out=ocv, in0=psv, in1=wbc, op=ALU.mult), r=[('ps', pb), rk_], w=[('noc', tt)])
                else:
                    ti = (tt + br) % 2
                    S.op('dve', lambda e: e.tensor_tensor(out=octmp[ti][:].rearrange("p (h c) -> p h c", h=4), in0=psv, in1=wbc, op=ALU.mult), r=[('ps', pb), rk_], w=[('noctmp', ti)])
                    S.op('pool', lambda e: e.tensor_tensor(out=oc[:, tt, :], in0=oc[:, tt, :], in1=octmp[ti][:], op=ALU.add), r=[('noctmp', ti), ('noc', tt)], w=[('noc', tt)])

            def cmp_scores(tt):
                ts = slice(tt * 128, (tt + 1) * 128)
                cb_ = tt % 2
                scores(tt, kcT[:, g, :], 'kcT', 0, 'cmp', False, eTc[cb_], ('neTc', cb_), (cmask[:, ts], 'cmask'), range(4))

            def cmp_pv(tt):
                cb_ = tt % 2
                ph = [[(eTc[cb_][:, h * 128:(h + 1) * 128], vca[:, g, 0:97], [(('neTc', cb_), h), 'vca'])] for h in range(4)]
                pv_combine(tt, PB_CMP, ph, 97, 0, True, 0)
                ib = tt % 2
                S.op('dve', lambda e: e.tensor_tensor(out=imp[ib][:], in0=imp[ib][:], in1=selc[:, tt, :], op=ALU.add), r=[('nimp', ib), 'selc'], w=[('nimp', ib)])
                S.op('dve', lambda e: e.max(out=mx8[ib][:], in_=imp[ib][:]), r=[('nimp', ib)], w=[('nmx8', ib)])
                S.op('dve', lambda e: e.tensor_scalar(out=mx8[ib][:, 7:8], in0=mx8[ib][:, 7:8], scalar1=-5e29, scalar2=None, op0=ALU.max), r=[('nmx8', ib)], w=[('nmx8', ib)])
                S.op('dve', lambda e: e.tensor_scalar(out=selb[ib][:], in0=imp[ib][:], scalar1=mx8[ib][:, 7:8], scalar2=NEGB, op0=ALU.is_lt, op1=ALU.mult),
                     r=[('nimp', ib), ('nmx8', ib)], w=[('nselb', ib)])
                S.op('pe', lambda e: e.transpose(out=PS[PB_TR][0:32, 0:128], in_=selb[ib][:, :], identity=ident[:]), r=[('nselb', ib), 'ident'], w=[('ps', PB_TR)])
                S.op('act', lambda e: e.activation(out=selbT[:, tt, :], in_=PS[PB_TR][0:32, 0:128], func=AF.Copy), r=[('ps', PB_TR)], w=[('nselbT', tt)])

            cmp_scores(0)
            for tt in range(NT):
                if tt + 1 < NT:
                    cmp_scores(tt + 1)
                cmp_pv(tt)
            gd = max(dtmax[4 * g:4 * g + 4])

            def win_scores(tt):
                for kt in range(max(0, tt - 4), tt + 1):
                    em = (cdiag[:], 'cdiag') if kt == tt else ((cfar[:], 'cfar') if kt == tt - 4 else None)
                    hs = [h for h in range(4) if tt - kt <= dtmax[4 * g + h]]
                    scores(tt, kwT[:, kt * 128:(kt + 1) * 128], 'nkwT', kt, 'rel', False, eTw[:, tt - kt, :], ('neTw', tt - kt), em, hs)

            def win_pv(tt):
                kts = list(range(max(0, tt - 4), tt + 1))
                ph = [[(eTw[:, tt - kt, h * 128:(h + 1) * 128], vw[:, kt, :], [(('neTw', tt - kt), h), 'nvw']) for kt in kts if tt - kt <= dtmax[4 * g + h]] for h in range(4)]
                pv_combine(tt, PB_WIN, ph, 65, 2, False, 1)

            def sel_scores(tt):
                for kt in range(max(0, tt - gd), tt + 1):
                    hs = [h for h in range(4) if tt - kt <= dtmax[4 * g + h]]
                    scores(tt, ksT[:, kt * 128:(kt + 1) * 128], 'nksT', kt, 'rel', True, eTs[:, kt, :], ('neTs', kt), (cdiag[:], 'cdiag') if kt == tt else None, hs)

            def sel_pv(tt):
                kts = list(range(max(0, tt - gd), tt + 1))
                ph = [[(eTs[:, kt, h * 128:(h + 1) * 128], vs[:, kt, :], [(('neTs', kt), h), 'nvs']) for kt in kts if tt - kt <= dtmax[4 * g + h]] for h in range(4)]
                pv_combine(tt, PB_SEL, ph, 65, 1, False, 2)
                ob = tt % 2
                S.op('act', lambda e: e.activation(out=ocb[ob][:], in_=oc[:, tt, :], func=AF.Copy), r=[('noc', tt)], w=[('nocb', ob)])
                transpose_to(ocb[ob], ('nocb', ob), 2, bufA, 'bufA', tt, BF16, c_off=8 + 2 * g, banks=(PB_TR, PB_TR + 1))

            jobs = []
            for tt in range(NT):
                jobs.append((win_scores, win_pv, tt))
                jobs.append((sel_scores, sel_pv, tt))
            jobs[0][0](jobs[0][2])
            for ji, (fs, fp, tt) in enumerate(jobs):
                if ji + 1 < len(jobs):
                    jobs[ji + 1][0](jobs[ji + 1][2])
                fp(tt)
        S.barrier()


_NC_CACHE = {}


def kernel(**inputs):
    n = 8
    if "nc" not in _NC_CACHE:
        _NC_CACHE["nc"] = build()
    nc = _NC_CACHE["nc"]
    consts = make_consts()
    in_maps = []
    for c in range(n):
        m = {"x": np.ascontiguousarray(inputs["x"][c], dtype=np.float32), "mem": np.ascontiguousarray(inputs["mem"][c], dtype=np.float32)}
        for k in WNAMES:
            m[k] = np.ascontiguousarray(inputs[k], dtype=np.float32)
        for k, v in consts.items():
            m["c_" + k] = v
        in_maps.append(m)
    res = run_bass_kernel_spmd(nc, in_maps, core_ids=list(range(n)))
    return np.stack([res.results[c]["out"] for c in range(n)], axis=0).astype(np.float32)
```

```python
import numpy as np
from contextlib import ExitStack
import concourse.bass as bass
import concourse.mybir as mybir
from concourse.bass_utils import run_bass_kernel_spmd

F32 = mybir.dt.float32
BF16 = mybir.dt.bfloat16
AF = mybir.ActivationFunctionType
ALU = mybir.AluOpType
AX = mybir.AxisListType

T = 2048
D = 2048
NT = 16
DEPTH = 2
DN_ALPHA = float((2 * DEPTH) ** 0.25)
LN_EPS = 1e-5
D_IN = 9792
C_GQ, C_GK, C_GV, C_GR, C_GA, C_NQ, C_NKV, C_NG, C_MG = 0, 512, 1024, 2048, 3072, 3088, 4112, 5648, 5696
R_GQ, R_GK, R_GA, R_NQ, R_KC, R_VC, R_KS, R_KW, R_MG, NFM = 0, 512, 1024, 1056, 2080, 2336, 2592, 2848, 3104, 7200
TC_GK, TC_GV, TC_GR, TC_VS, TC_VW, TC_NG, NTM = 0, 512, 1536, 2560, 2816, 3072, 3120
NEGB = -30000.0


class Sch:
    def __init__(self, nc, es):
        self.nc = nc
        self.eng = {'pe': nc.tensor, 'act': nc.scalar, 'dve': nc.vector, 'pool': nc.gpsimd, 'sp': nc.sync}
        self.sem = {}
        self.cnt = {}
        for e in ('pe', 'act', 'dve', 'pool'):
            self.sem[e] = es.enter_context(nc.semaphore('s_' + e))
            self.cnt[e] = 0
        self.NS = 8
        for q in ('sp', 'pool'):
            for i in range(self.NS):
                k = ('dma', q, i)
                self.sem[k] = es.enter_context(nc.semaphore('d_%s%d' % (q, i)))
                self.cnt[k] = 0
        self.dma_i = {'sp': 0, 'pool': 0}
        self.waited = {e: {} for e in self.eng}
        self.lw = {}
        self.rd = {}
        self.nops = 0

    def _wait(self, e, tok):
        s, v = tok
        if self.waited[e].get(s, 0) >= v:
            return
        self.waited[e][s] = v
        self.eng[e].wait_ge(self.sem[s], v)

    def op(self, e, fn, r=(), w=(), dma=False, inc=True):
        deps = []
        for k in r:
            if k in self.lw:
                deps.append(self.lw[k])
        for k in w:
            if k in self.lw:
                deps.append(self.lw[k])
            deps.extend(self.rd.get(k, {}).values())
        if dma:
            i = self.dma_i[e]
            self.dma_i[e] += 1
            s = ('dma', e, i % self.NS)
            if self.cnt[s] > 0:
                deps.append((s, self.cnt[s]))
            self.cnt[s] += 16
            tok = (s, self.cnt[s])
        else:
            s = e
            if inc:
                self.cnt[s] += 1
                tok = (s, self.cnt[s])
            else:
                tok = (s, self.cnt[s] + 1)
        for d in deps:
            if d[0] == 'pe' and e == 'pe' and not dma:
                continue
            self._wait(e, d)
        ins = fn(self.eng[e])
        if dma:
            ins.then_inc(self.sem[s], 16)
        elif inc:
            ins.then_inc(self.sem[s], 1)
        for k in w:
            self.lw[k] = tok
            self.rd[k] = {}
        for k in r:
            self.rd.setdefault(k, {})[tok[0]] = tok
        self.nops += 1
        return tok

    def barrier(self):
        for e in self.eng:
            for s, c in self.cnt.items():
                if c > 0:
                    self._wait(e, (s, c))
        self.lw.clear()
        self.rd.clear()


def make_consts():
    c = {}
    i = np.arange(128)
    c['ident'] = np.eye(128, dtype=np.float32)
    c['um'] = (-(1.0 / 16.0) * (i[:, None] <= i[None, :])).astype(np.float32)
    c['um2'] = (-(1.0 / 16.0) * (i[:, None] > i[None, :])).astype(np.float32)
    c['caus4'] = np.tile((i[:, None] <= i[None, :]).astype(np.float32), (1, 4))
    slopes = 2.0 ** (-8.0 * np.arange(1, 17) / 16.0)
    rel = -(slopes[None, :, None]) * (i[None, None, :] - i[:, None, None]).astype(np.float64)
    c['rel_mid'] = rel.astype(np.float32).reshape(128, 16 * 128)
    dt = np.arange(17)
    c['dconst'] = np.broadcast_to((-slopes[:, None] * 128.0 * dt[None, :])[None], (128, 16, 17)).astype(np.float32).reshape(128, 16 * 17).copy()
    n = np.arange(128)
    cb = slopes[None, :, None] * (16.0 * n[:, None, None] + 15.5) - slopes[None, :, None] * 128.0 * np.arange(16)[None, None, :]
    cb = slopes[None, :, None] * (15.0 * n[:, None, None] + 15.5) - slopes[None, :, None] * 128.0 * np.arange(16)[None, None, :]
    c['cmp_pb'] = cb.astype(np.float32).reshape(128, 256)
    c['cdiag'] = np.where(i[None, :] >= i[:, None], 0.0, NEGB).astype(np.float32)
    c['cfar'] = np.where(i[None, :] < i[:, None], 0.0, NEGB).astype(np.float32)
    tq = (np.arange(16)[:, None] * 128 + i[None, :])
    valid = (16 * n[:, None, None] + 31) <= tq[None]
    valid[127] = False
    c['cmp_mask'] = np.where(valid, 0.0, NEGB).astype(np.float32).reshape(128, 2048)
    bs = 16 * n
    ss = 64 * np.arange(32)
    ov = ((bs[:, None] < ss[None, :] + 64) & (bs[:, None] + 32 > ss[None, :])).astype(np.float32)
    ov[127] = 0
    c['overlap'] = ov
    t = np.arange(T)
    cur = t // 64
    jj = np.arange(32)
    forced = (jj[None, :] == 0) | (jj[None, :] == cur[:, None]) | (jj[None, :] == cur[:, None] - 1)
    validb = (64 * jj[None, :]) <= t[:, None]
    c['selc'] = np.where(validb, np.where(forced, 1e6, 0.0), -1e30).astype(np.float32)
    ex = np.zeros((32, 16, 128), np.float32)
    for kt in range(16):
        ex[2 * kt, kt, :64] = 1
        ex[2 * kt + 1, kt, 64:] = 1
    c['expand'] = ex.reshape(32, 2048)
    c['lstr'] = (i[:, None] < i[None, :]).astype(np.float32)

    def bsplit(a):
        import ml_dtypes
        a = np.asarray(a, np.float64)
        h1 = a.astype(np.float32).astype(ml_dtypes.bfloat16).astype(np.float64)
        h2 = (a - h1).astype(np.float32).astype(ml_dtypes.bfloat16).astype(np.float64)
        h3 = (a - h1 - h2).astype(np.float32).astype(ml_dtypes.bfloat16).astype(np.float64)
        return [h1.astype(np.float32), h2.astype(np.float32), h3.astype(np.float32)]
    jt = -slopes[None, :, None] * (128.0 * np.arange(16)[:, None, None] + i[None, None, :])
    jp = bsplit(jt)
    sp = bsplit(np.broadcast_to(slopes[None, :, None], (16, 16, 128)))
    tab = np.stack(jp + sp, 0)
    tab = tab.reshape(6, 16, 4, 4, 128).transpose(0, 2, 1, 3, 4).reshape(6, 4, 16 * 512)
    c['btab'] = np.ascontiguousarray(tab.reshape(6, 4 * 16 * 512))
    l6 = np.ones((6, 128), np.float32)
    l6[3:6, :] = i[None, :]
    c['l6'] = l6
    c['ebase'] = np.broadcast_to((np.arange(16) * 384 + 1.0e6)[None, :], (128, 16)).astype(np.float32).copy()
    return c


CONST_SHAPES = {k: v.shape for k, v in make_consts().items()}

WNAMES = ["w_in", "gla_w_a2", "gla_b_a", "gla_norm_g", "nsa_cmp_pe", "nsa_cmp_w1", "nsa_cmp_w2",
          "w_branch_gla", "w_branch_nsa", "w_out", "ln_mix_g", "ln_mix_b", "xa_wq", "xa_wkv", "xa_wo",
          "ln_xa_g", "ln_xa_b", "router_w", "router_b", "moe_w_in", "moe_w_down", "ln_ffn_g", "ln_ffn_b"]
WSHAPES = {
    "w_in": [2, 2048, 9792], "gla_w_a2": [2, 16, 512], "gla_b_a": [2, 512], "gla_norm_g": [2, 1024],
    "nsa_cmp_pe": [2, 2, 32, 64], "nsa_cmp_w1": [2, 2, 2048, 256], "nsa_cmp_w2": [2, 2, 256, 64],
    "w_branch_gla": [2, 1024, 2048], "w_branch_nsa": [2, 1024, 2048], "w_out": [2, 2048, 2048],
    "ln_mix_g": [2, 2048], "ln_mix_b": [2, 2048], "xa_wq": [2, 2048, 512], "xa_wkv": [2, 2048, 1024],
    "xa_wo": [2, 512, 2048], "ln_xa_g": [2, 2048], "ln_xa_b": [2, 2048], "router_w": [2048, 16],
    "router_b": [16], "moe_w_in": [2, 16, 2048, 3072], "moe_w_down": [2, 16, 1536, 2048],
    "ln_ffn_g": [2, 2048], "ln_ffn_b": [2, 2048],
}


def build(n_layers=DEPTH, stages=("mix", "xa", "moe"), debug=(), wshapes=None):
    WS = dict(WSHAPES)
    WS.update(wshapes or {})
    nc = bass.Bass("TRN2", target_bir_lowering=False)
    dr = {}
    dr["x"] = nc.dram_tensor("x", [T, D], F32, kind="ExternalInput").ap()
    dr["mem"] = nc.dram_tensor("mem", [256, D], F32, kind="ExternalInput").ap()
    for k in WNAMES:
        dr[k] = nc.dram_tensor(k, WS[k], F32, kind="ExternalInput").ap()
    cst = {k: nc.dram_tensor("c_" + k, list(s), F32, kind="ExternalInput").ap() for k, s in CONST_SHAPES.items()}
    out = nc.dram_tensor("out", [T, D], F32, kind="ExternalOutput").ap()
    dbg = {k: nc.dram_tensor("dbg_" + k, list(s), F32, kind="ExternalOutput").ap() for k, s in debug}
    xres = nc.dram_tensor("xres", [T, D], F32).ap()
    hT_d = nc.dram_tensor("hT_d", [NFM, T], BF16).ap()
    h_d = nc.dram_tensor("h_d", [T, NTM], BF16).ap()
    y_d = nc.dram_tensor("y_d", [T, D], F32).ap()

    with ExitStack() as es:
        block = es.enter_context(nc.Block())

        @block.gpsimd
        def _(_g):
            _emit(nc, dr, cst, out, dbg, xres, hT_d, h_d, y_d, n_layers, stages)
    return nc


def _emit(nc, dr, cst, out, dbg, xres, hT_d, h_d, y_d, n_layers, stages):
    es = ExitStack()
    S = Sch(nc, es)

    uid = [0]

    def sb(name, shape, dt, st=es):
        uid[0] += 1
        return st.enter_context(nc.sbuf_tensor(name + '_%d' % uid[0], shape, dt))

    def ps(name, shape, dt=F32, st=es):
        return st.enter_context(nc.psum_tensor(name, shape, dt))

    bufA = sb("bufA", [128, 16, T], BF16)
    bufB = None
    wt_i = [0]
    Wt = []

    def alloc_wt(st, n):
        del Wt[:]
        for i in range(n):
            Wt.append(sb("Wt%d" % i, [128, 16, 512], BF16, st))
        wt_i[0] = 0
    ident = sb("ident", [128, 128], F32)
    identb = sb("identb", [128, 128], BF16)
    PS = [ps("ps%d" % i, [128, 512]) for i in range(8)]
    S.op('sp', lambda e: e.dma_start(out=ident[:], in_=cst['ident'][:, :]), w=['ident'], dma=True)
    S.op('dve', lambda e: e.tensor_copy(out=identb[:], in_=ident[:]), r=['ident'], w=['identb'])

    ps_i = [0]

    def next_ps(lo=0, hi=4):
        i = lo + ps_i[0] % (hi - lo)
        ps_i[0] += 1
        return i

    def load_w(W2d, k0, KC, c0, nb):
        b = wt_i[0] % len(Wt)
        wt_i[0] += 1
        src = W2d[k0:k0 + KC * 128, c0:c0 + nb].rearrange("(kc p) n -> p kc n", p=128)
        S.op('pool', lambda e: e.dma_start(out=Wt[b][:, 0:KC, 0:nb], in_=src), w=[('Wt', b)], dma=True)
        return b

    def proj_tm(src, skey, KC, W2d, c0, nb, epi, k0=0, tts=range(NT)):
        b = load_w(W2d, k0, KC, c0, nb)
        for tt in tts:
            pi = next_ps()
            for kc in range(KC):
                S.op('pe', lambda e, kc=kc, tt=tt, pi=pi: e.matmul(PS[pi][:, 0:nb], lhsT=src[:, kc, tt * 128:(tt + 1) * 128],
                                                                    rhs=Wt[b][:, kc, 0:nb], start=(kc == 0), stop=(kc == KC - 1)),
                     r=[('Wt', b), skey], w=[('ps', pi)], inc=(kc == KC - 1))
            epi(tt, pi)

    def proj_fm(src, skey, KC, W2d, c0, nb, M, epi, k0=0):
        b = load_w(W2d, k0, KC, c0, nb)
        for m0 in range(0, nb, M):
            mm = min(M, nb - m0)
            for tb in range(4):
                pi = next_ps()
                for kc in range(KC):
                    S.op('pe', lambda e, kc=kc, tb=tb, pi=pi, m0=m0, mm=mm: e.matmul(
                        PS[pi][0:mm, :], lhsT=Wt[b][:, kc, m0:m0 + mm], rhs=src[:, kc, tb * 512:(tb + 1) * 512],
                        start=(kc == 0), stop=(kc == KC - 1)),
                         r=[('Wt', b), skey], w=[('ps', pi)], inc=(kc == KC - 1))
                epi(c0 + m0, mm, tb, pi)

    def ln_and_store(l, st, xt_tile, key, tt, g_bc, b_bc, dst_dram, small):
        stats, mv, rstd = small
        for c in range(4):
            S.op('dve', lambda e, c=c: e.bn_stats(out=stats[:, c, :], in_=xt_tile[:, c * 512:(c + 1) * 512]), r=[key], w=['stats'])
        S.op('dve', lambda e: e.bn_aggr(out=mv[:], in_=stats[:].rearrange("p a b -> p (a b)")), r=['stats'], w=['mv'])
        S.op('act', lambda e: e.activation(out=rstd[:], in_=mv[:, 1:2], func=AF.Sqrt, bias=LN_EPS, scale=1.0), r=['mv'], w=['rstd'])
        S.op('dve', lambda e: e.reciprocal(out=rstd[:], in_=rstd[:]), r=['rstd'], w=['rstd'])
        S.op('dve', lambda e: e.tensor_scalar(out=xt_tile[:], in0=xt_tile[:], scalar1=mv[:, 0:1], scalar2=rstd[:, 0:1],
                                              op0=ALU.subtract, op1=ALU.mult), r=[key, 'mv', 'rstd'], w=[key])
        S.op('pool', lambda e: e.tensor_tensor(out=xt_tile[:], in0=xt_tile[:], in1=g_bc[:], op=ALU.mult), r=[key, 'lng'], w=[key])
        S.op('pool', lambda e: e.tensor_tensor(out=xt_tile[:], in0=xt_tile[:], in1=b_bc[:], op=ALU.add), r=[key, 'lnb'], w=[key])
        S.op('sp', lambda e: e.dma_start(out=dst_dram[tt * 128:(tt + 1) * 128, :], in_=xt_tile[:]), r=[key], w=[('xres', tt)], dma=True)
        transpose_to(xt_tile, key, 16, bufA, 'bufA', tt, F32)

    def transpose_to(tile, key, nchunks, dst, dkey, tt, dt, c_off=0, banks=(4, 8)):
        idm = ident if dt == F32 else identb
        for c4 in range(0, nchunks, 4):
            pi = next_ps(*banks)
            pst = PS[pi] if dt == F32 else PSB[pi - 4]
            for c in range(c4, min(c4 + 4, nchunks)):
                S.op('pe', lambda e, c=c, c4=c4, pst=pst: e.transpose(out=pst[:, (c - c4) * 128:(c - c4 + 1) * 128],
                                                                   in_=tile[:, c * 128:(c + 1) * 128], identity=idm[:]),
                     r=[key, 'ident', 'identb'], w=[('ps', pi)])
            n = min(4, nchunks - c4)
            S.op('act', lambda e, c4=c4, n=n, pst=pst: e.activation(
                out=dst[:, c_off + c4:c_off + c4 + n, tt * 128:(tt + 1) * 128],
                in_=pst[:, 0:n * 128].rearrange("p (a b) -> p a b", a=n), func=AF.Copy), r=[('ps', pi)], w=[dkey])

    PSB = [PS[i][:].bitcast(BF16)[:, 0:512] for i in range(4, 8)]

    with ExitStack() as st:
        xt = [sb("xt%d" % i, [128, D], F32, st) for i in range(2)]
        for tt in range(NT):
            b = tt % 2
            S.op('sp', lambda e, b=b, tt=tt: e.dma_start(out=xt[b][:], in_=dr["x"][tt * 128:(tt + 1) * 128, :]), w=[('xt', b)], dma=True)
            S.op('sp', lambda e, b=b, tt=tt: e.dma_start(out=xres[tt * 128:(tt + 1) * 128, :], in_=xt[b][:]), r=[('xt', b)], w=[('xres', tt)], dma=True)
            transpose_to(xt[b], ('xt', b), 16, bufA, 'bufA', tt, F32)
        S.barrier()

    _H['alloc_wt'] = alloc_wt
    for l in range(n_layers):
        _mixer(nc, S, sb, PS, PSB, dr, cst, l, bufA, bufB, hT_d, h_d, xres, proj_tm, proj_fm, transpose_to, ln_and_store, next_ps,
               ident, identb, dbg)
        if "mixonly" in stages:
            break
        with ExitStack() as lq:
            qx = sb("qxT", [128, 4, T], BF16, lq)
            with ExitStack() as lb:
                bB = _merge(nc, S, sb, PS, PSB, dr, cst, l, bufA, hT_d, xres, proj_fm, transpose_to, next_ps, lb)

                def epi_q(col, mm, tb, pi):
                    S.op('act', lambda e: e.activation(out=qx[:, col // 128, tb * 512:(tb + 1) * 512], in_=PS[pi][:, :], func=AF.Copy, scale=float(128 ** -0.5)),
                         r=[('ps', pi)], w=['qxT'])
                with ExitStack() as lw:
                    alloc_wt(lw, 2)
                    proj_fm(bB, 'bufBall', 16, dr["xa_wq"][l], 0, 512, 128, epi_q)
                    S.barrier()
            _xattn(nc, S, sb, PS, PSB, dr, cst, l, bufA, qx, xres, proj_tm, proj_fm, transpose_to, ln_and_store, next_ps, ident, identb, dbg, load_w, Wt)
        if 'moe' not in SKIP:
          _moe(nc, S, sb, PS, PSB, dr, cst, l, bufA, None, xres, y_d, out if l == n_layers - 1 else xres, proj_tm, proj_fm,
               transpose_to, ln_and_store, next_ps, ident, identb, dbg, load_w, Wt)
    S.barrier()
    es.close()


def _mixer(nc, S, sb, PS, PSB, dr, cst, l, bufA, bufB, hT_d, h_d, xres, proj_tm, proj_fm, transpose_to, ln_and_store, next_ps,
           ident, identb, dbg):
    W = dr["w_in"][l]
    aT_d = nc.dram_tensor("aT_d%d" % l, [16, T], F32).ap()
    with ExitStack() as st:
        _H['alloc_wt'](st, 3)
        stg = [sb("stg%d" % i, [128, 512], BF16, st) for i in range(4)]
        stgf = sb("stgf", [16, 512], F32, st)
        si = [0]

        def epi_fm(rbase, cbase, func, scale=1.0):
            def f(col, mm, tb, pi):
                i = si[0] % 4
                si[0] += 1
                S.op('act', lambda e: e.activation(out=stg[i][0:mm, :], in_=PS[pi][0:mm, :], func=func, scale=scale), r=[('ps', pi)], w=[('stg', i)])
                r0 = rbase + col - cbase
                S.op('sp', lambda e: e.dma_start(out=hT_d[r0:r0 + mm, tb * 512:(tb + 1) * 512], in_=stg[i][0:mm, :]), r=[('stg', i)],
                     w=[('hT', r0 // 64, tb)] + ([('hT', r0 // 64 + 1, tb)] if mm == 128 else []), dma=True)
            return f

        def epi_ga(col, mm, tb, pi):
            S.op('act', lambda e: e.activation(out=stgf[0:16, :], in_=PS[pi][0:16, :], func=AF.Copy), r=[('ps', pi)], w=['stgf'])
            S.op('sp', lambda e: e.dma_start(out=aT_d[:, tb * 512:(tb + 1) * 512], in_=stgf[0:16, :]), r=['stgf'], w=[('aT', tb)], dma=True)

        def epi_tm(tcbase, nb, func):
            def f(tt, pi):
                i = si[0] % 4
                si[0] += 1
                S.op('act', lambda e: e.activation(out=stg[i][:, 0:nb], in_=PS[pi][:, 0:nb], func=func), r=[('ps', pi)], w=[('stg', i)])
                S.op('sp', lambda e: e.dma_start(out=h_d[tt * 128:(tt + 1) * 128, tcbase:tcbase + nb], in_=stg[i][:, 0:nb]), r=[('stg', i)],
                     w=[('h', tt, tcbase)], dma=True)
            return f

        A = (bufA, 'bufA', 16, W)
        proj_fm(*A, C_GQ, 512, 128, epi_fm(R_GQ, C_GQ, AF.Copy))
        proj_fm(*A, C_GK, 512, 128, epi_fm(R_GK, C_GK, AF.Copy))
        proj_fm(*A, C_GA, 16, 16, epi_ga)
        proj_tm(*A, C_GK, 512, epi_tm(TC_GK, 512, AF.Copy))
        for j in range(2):
            proj_tm(*A, C_GV + 512 * j, 512, epi_tm(TC_GV + 512 * j, 512, AF.Copy))
            proj_tm(*A, C_GR + 512 * j, 512, epi_tm(TC_GR + 512 * j, 512, AF.Silu))
            proj_fm(*A, C_NQ + 512 * j, 512, 64, epi_fm(R_NQ + 512 * j, C_NQ + 512 * j, AF.Copy, 0.125))
        for (cc, rr) in ((0, R_KC), (256, R_VC), (512, R_KS), (1024, R_KW)):
            proj_fm(*A, C_NKV + cc, 256, 64, epi_fm(rr, C_NKV + cc, AF.Copy))
        proj_tm(*A, C_NKV + 768, 256, epi_tm(TC_VS, 256, AF.Copy))
        proj_tm(*A, C_NKV + 1280, 256, epi_tm(TC_VW, 256, AF.Copy))
        proj_tm(*A, C_NG, 48, epi_tm(TC_NG, 48, AF.Sigmoid))
        for j in range(8):
            proj_fm(*A, C_MG + 512 * j, 512, 128, epi_fm(R_MG + 512 * j, C_MG + 512 * j, AF.Sigmoid))
        S.barrier()

    if 'gla' not in SKIP:
        _gla(nc, S, sb, PS, PSB, dr, cst, l, bufA, hT_d, h_d, aT_d, transpose_to, ident, identb, dbg)
    if 'nsa' not in SKIP:
        _nsa(nc, S, sb, PS, PSB, dr, cst, l, bufA, hT_d, h_d, transpose_to, next_ps, ident, identb, dbg)


def _gla(nc, S, sb, PS, PSB, dr, cst, l, bufA, hT_d, h_d, aT_d, transpose_to, ident, identb, dbg):
    with ExitStack() as st:
        um = sb("um", [128, 128], F32, st)
        um2 = sb("um2", [128, 128], F32, st)
        caus4 = sb("caus4", [128, 512], F32, st)
        gnbc = sb("gnbc", [128, 1024], F32, st)
        wa2 = sb("wa2", [32, 512], F32, st)
        aT = sb("aT", [32, T], F32, st)
        S.op('sp', lambda e: e.dma_start(out=um[:], in_=cst['um'][:, :]), w=['um'], dma=True)
        S.op('sp', lambda e: e.dma_start(out=um2[:], in_=cst['um2'][:, :]), w=['um2'], dma=True)
        S.op('sp', lambda e: e.dma_start(out=caus4[:], in_=cst['caus4'][:, :]), w=['caus4'], dma=True)
        S.op('sp', lambda e: e.dma_start(out=gnbc[:], in_=dr['gla_norm_g'][l, :].partition_broadcast(128)), w=['gnbc'], dma=True)
        S.op('sp', lambda e: e.dma_start(out=wa2[0:16, :], in_=dr['gla_w_a2'][l]), w=['wa2a'], dma=True)
        S.op('sp', lambda e: e.dma_start(out=wa2[16:17, :], in_=dr['gla_b_a'][l:l + 1, :]), w=['wa2b'], dma=True)
        S.op('dve', lambda e: e.memset(aT[:], 1.0), w=['aT'])
        S.op('sp', lambda e: e.dma_start(out=aT[0:16, :], in_=aT_d[:, :]), w=['aT'], dma=True)
        S32 = sb("S32", [128, 4, 256], F32, st)
        Sb = sb("Sb", [128, 4, 256], BF16, st)
        S.op('dve', lambda e: e.memset(S32[:], 0.0), w=['S32'])
        S.op('pool', lambda e: e.memset(Sb[:], 0.0), w=['Sb'])
        qT = [sb("gqT%d" % i, [128, 4, 128], BF16, st) for i in range(2)]
        kT = [sb("gkT%d" % i, [128, 4, 128], BF16, st) for i in range(2)]
        kk = [sb("gk%d" % i, [128, 512], BF16, st) for i in range(2)]
        vv = [sb("gv%d" % i, [128, 1024], BF16, st) for i in range(2)]
        rs = [sb("grs%d" % i, [128, 1024], BF16, st) for i in range(2)]
        le = sb("le", [128, 512], F32, st)
        ll = sb("ll", [128, 512], F32, st)
        Eq = sb("Eq", [128, 512], F32, st)
        Ek = sb("Ek", [128, 512], F32, st)
        Ekh = sb("Ekh", [128, 512], F32, st)
        qs = sb("qs", [128, 512], BF16, st)
        ks = sb("ks", [128, 512], BF16, st)
        kh = sb("kh", [128, 512], BF16, st)
        att = sb("att", [128, 512], BF16, st)
        og = sb("og", [128, 1024], F32, st)
        ogb = sb("ogb", [128, 1024], BF16, st)
        grs = sb("grsf", [128, 1024], F32, st)
        stats = sb("gstats", [128, 4, 6], F32, st)
        mv = sb("gmv", [128, 4, 2], F32, st)
        rstd = sb("grstd", [128, 4], F32, st)
        for c in range(NT):
            b = c % 2
            cs = slice(c * 128, (c + 1) * 128)
            S.op('sp', lambda e: e.dma_start(out=qT[b][:], in_=hT_d[R_GQ:R_GQ + 512, cs].rearrange("(h d) t -> d h t", d=128)), w=[('qT', b)], dma=True)
            S.op('sp', lambda e: e.dma_start(out=kT[b][:], in_=hT_d[R_GK:R_GK + 512, cs].rearrange("(h d) t -> d h t", d=128)), w=[('kT', b)], dma=True)
            S.op('sp', lambda e: e.dma_start(out=kk[b][:], in_=h_d[cs, TC_GK:TC_GK + 512]), w=[('kk', b)], dma=True)
            S.op('sp', lambda e: e.dma_start(out=vv[b][:], in_=h_d[cs, TC_GV:TC_GV + 1024]), w=[('vv', b)], dma=True)
            S.op('sp', lambda e: e.dma_start(out=rs[b][:], in_=h_d[cs, TC_GR:TC_GR + 1024]), w=[('rs', b)], dma=True)
            S.op('pe', lambda e: e.matmul(PS[0][:, :], lhsT=aT[0:17, cs], rhs=wa2[0:17, :], start=True, stop=True), r=['aT', 'wa2a', 'wa2b'], w=[('ps', 0)])
            S.op('act', lambda e: e.activation(out=le[:], in_=PS[0][:, :], func=AF.Exp, scale=-1.0), r=[('ps', 0)], w=['le'])
            S.op('act', lambda e: e.activation(out=ll[:], in_=le[:], func=AF.Ln, bias=1.0, scale=1.0), r=['le'], w=['ll'])
            for h in range(4):
                S.op('pe', lambda e, h=h: e.matmul(PS[1][:, h * 128:(h + 1) * 128], lhsT=ll[:, h * 128:(h + 1) * 128], rhs=um[:], start=True, stop=True),
                     r=['ll', 'um'], w=[('ps', 1)], inc=(h == 3))
            S.op('pe', lambda e: e.matmul(PS[2][:, :], lhsT=um2[:], rhs=ll[:], start=True, stop=True), r=['ll', 'um2'], w=[('ps', 2)])
            S.op('act', lambda e: e.activation(out=Eq[:], in_=PS[1][:, :], func=AF.Exp), r=[('ps', 1)], w=['Eq'])
            S.op('act', lambda e: e.activation(out=Ek[:], in_=PS[1][:, :], func=AF.Exp, scale=-1.0), r=[('ps', 1)], w=['Ek'])
            S.op('act', lambda e: e.activation(out=Ekh[:], in_=PS[2][:, :], func=AF.Exp), r=[('ps', 2)], w=['Ekh'])
            S.op('dve', lambda e: e.scalar_tensor_tensor(out=qs[:], in0=qT[b][:].rearrange("p h t -> p (h t)"), scalar=float(128 ** -0.5), in1=Eq[:],
                                                         op0=ALU.mult, op1=ALU.mult), r=[('qT', b), 'Eq'], w=['qs'])
            S.op('dve', lambda e: e.tensor_tensor(out=ks[:], in0=kT[b][:].rearrange("p h t -> p (h t)"), in1=Ek[:], op=ALU.mult), r=[('kT', b), 'Ek'], w=['ks'])
            S.op('pool', lambda e: e.tensor_tensor(out=kh[:], in0=kk[b][:], in1=Ekh[:], op=ALU.mult), r=[('kk', b), 'Ekh'], w=['kh'])
            for h in range(4):
                hs = slice(h * 128, (h + 1) * 128)
                S.op('pe', lambda e, hs=hs: e.matmul(PS[3][:, hs], lhsT=ks[:, hs], rhs=qs[:, hs], start=True, stop=True), r=['ks', 'qs'], w=[('ps', 3)], inc=(h == 3))
            S.op('dve', lambda e: e.tensor_tensor(out=att[:], in0=PS[3][:, :], in1=caus4[:], op=ALU.mult), r=[('ps', 3), 'caus4'], w=['att'])
            for h in range(4):
                hs = slice(h * 128, (h + 1) * 128)
                pb = 4 + h // 2
                po = slice((h % 2) * 256, (h % 2) * 256 + 256)
                S.op('pe', lambda e, hs=hs, pb=pb, po=po, h=h: e.matmul(PS[pb][:, po], lhsT=att[:, hs], rhs=vv[b][:, h * 256:(h + 1) * 256], start=True, stop=False),
                     r=['att', ('vv', b)], w=[('ps', pb)], inc=False)
                S.op('pe', lambda e, hs=hs, pb=pb, po=po, h=h: e.matmul(PS[pb][:, po], lhsT=qs[:, hs], rhs=Sb[:, h, :], start=False, stop=True),
                     r=['qs', 'Sb'], w=[('ps', pb)], inc=True)
            for h in range(4):
                hs = slice(h * 128, (h + 1) * 128)
                pb = 6 + h // 2
                po = slice((h % 2) * 256, (h % 2) * 256 + 256)
                S.op('pe', lambda e, hs=hs, pb=pb, po=po, h=h: e.matmul(PS[pb][:, po], lhsT=kh[:, hs], rhs=vv[b][:, h * 256:(h + 1) * 256], start=True, stop=True),
                     r=['kh', ('vv', b)], w=[('ps', pb)])
                S.op('dve', lambda e, pb=pb, po=po, h=h: e.scalar_tensor_tensor(out=S32[:, h, :], in0=S32[:, h, :], scalar=Eq[:, h * 128 + 127:h * 128 + 128],
                                                                              in1=PS[pb][:, po], op0=ALU.mult, op1=ALU.add), r=[('ps', pb), 'Eq', 'S32'], w=['S32'])
            S.op('act', lambda e: e.activation(out=Sb[:], in_=S32[:], func=AF.Copy), r=['S32'], w=['Sb'])
            for h in range(4):
                pb = 4 + h // 2
                po = slice((h % 2) * 256, (h % 2) * 256 + 256)
                S.op('dve', lambda e, pb=pb, po=po, h=h: e.bn_stats(out=stats[:, h, :], in_=PS[pb][:, po]), r=[('ps', pb)], w=['gstats'])
                S.op('dve', lambda e, h=h: e.bn_aggr(out=mv[:, h, :], in_=stats[:, h, :]), r=['gstats'], w=['gmv'])
            S.op('act', lambda e: e.activation(out=rstd[:], in_=mv[:, :, 1], func=AF.Sqrt, bias=LN_EPS, scale=1.0), r=['gmv'], w=['grstd'])
            S.op('dve', lambda e: e.reciprocal(out=rstd[:], in_=rstd[:]), r=['grstd'], w=['grstd'])
            for h in range(4):
                pb = 4 + h // 2
                po = slice((h % 2) * 256, (h % 2) * 256 + 256)
                S.op('dve', lambda e, pb=pb, po=po, h=h: e.tensor_scalar(out=og[:, h * 256:(h + 1) * 256], in0=PS[pb][:, po], scalar1=mv[:, h, 0:1], scalar2=rstd[:, h:h + 1],
                                                                       op0=ALU.subtract, op1=ALU.mult), r=[('ps', pb), 'gmv', 'grstd'], w=['og'])
            S.op('pool', lambda e: e.tensor_tensor(out=grs[:], in0=rs[b][:], in1=gnbc[:], op=ALU.mult), r=[('rs', b), 'gnbc'], w=['grsf'])
            S.op('pool', lambda e: e.tensor_tensor(out=ogb[:], in0=og[:], in1=grs[:], op=ALU.mult), r=['og', 'grsf'], w=['ogb'])
            if 'o_gla' in dbg:
                S.op('pool', lambda e: e.tensor_tensor(out=og[:], in0=og[:], in1=grs[:], op=ALU.mult), r=['og', 'grsf'], w=['og'])
                S.op('sp', lambda e: e.dma_start(out=dbg['o_gla'][cs, :], in_=og[:]), r=['og'], w=[('dbg', c)], dma=True)
            transpose_to(ogb, 'ogb', 8, bufA, 'bufA', c, BF16)
        S.barrier()


def _resid_ln_phase(nc, S, sb, PS, st, l, Wres, KC, src, skeyf, alpha_src, gname, bname, dr, dst_dram, dstT, dstkeyf, transpose_to, ln_keys, nbuf=1):
    xt = [sb("rl_xt%d" % i, [128, D], F32, st) for i in range(nbuf)]
    g_bc = sb("rl_g", [128, D], F32, st)
    b_bc = sb("rl_b", [128, D], F32, st)
    stats = sb("rl_stats", [128, 4, 6], F32, st)
    mv = sb("rl_mv", [128, 2], F32, st)
    rstd = sb("rl_rstd", [128, 1], F32, st)
    S.op('sp', lambda e: e.dma_start(out=g_bc[:], in_=dr[gname][l, :].partition_broadcast(128)), w=['lng'], dma=True)
    S.op('sp', lambda e: e.dma_start(out=b_bc[:], in_=dr[bname][l, :].partition_broadcast(128)), w=['lnb'], dma=True)
    for tt in range(NT):
        b = tt % nbuf
        key = ('rlxt', b)
        S.op('sp', lambda e: e.dma_start(out=xt[b][:], in_=alpha_src[tt * 128:(tt + 1) * 128, :]), r=[('xres', tt)], w=[key], dma=True)
        for cb in range(4):
            if Wres is not None:
                for kc in range(KC):
                    S.op('pe', lambda e, kc=kc, cb=cb: e.matmul(PS[cb][:, :], lhsT=src[:, kc, tt * 128:(tt + 1) * 128], rhs=Wres[:, kc, cb * 512:(cb + 1) * 512],
                                                                start=(kc == 0), stop=(kc == KC - 1)), r=[skeyf(tt), ('Wres', kc, cb)], w=[('ps', cb)], inc=(kc == KC - 1))
                S.op('dve', lambda e, cb=cb: e.scalar_tensor_tensor(out=xt[b][:, cb * 512:(cb + 1) * 512], in0=xt[b][:, cb * 512:(cb + 1) * 512], scalar=DN_ALPHA,
                                                                   in1=PS[cb][:, :], op0=ALU.mult, op1=ALU.add), r=[('ps', cb), key], w=[key])
            else:
                if cb == 0:
                    ytile, ykey = src(tt)
                S.op('dve', lambda e, cb=cb: e.scalar_tensor_tensor(out=xt[b][:, cb * 512:(cb + 1) * 512], in0=xt[b][:, cb * 512:(cb + 1) * 512], scalar=DN_ALPHA,
                                                                   in1=ytile[:, cb * 512:(cb + 1) * 512], op0=ALU.mult, op1=ALU.add), r=[ykey, key], w=[key])
        for c in range(4):
            S.op('dve', lambda e, c=c: e.bn_stats(out=stats[:, c, :], in_=xt[b][:, c * 512:(c + 1) * 512]), r=[key], w=['stats'])
        S.op('dve', lambda e: e.bn_aggr(out=mv[:], in_=stats[:].rearrange("p a b -> p (a b)")), r=['stats'], w=['mv'])
        S.op('act', lambda e: e.activation(out=rstd[:], in_=mv[:, 1:2], func=AF.Sqrt, bias=LN_EPS, scale=1.0), r=['mv'], w=['rstd'])
        S.op('dve', lambda e: e.reciprocal(out=rstd[:], in_=rstd[:]), r=['rstd'], w=['rstd'])
        S.op('dve', lambda e: e.tensor_scalar(out=xt[b][:], in0=xt[b][:], scalar1=mv[:, 0:1], scalar2=rstd[:, 0:1], op0=ALU.subtract, op1=ALU.mult),
             r=[key, 'mv', 'rstd'], w=[key])
        S.op('pool', lambda e: e.tensor_tensor(out=xt[b][:], in0=xt[b][:], in1=g_bc[:], op=ALU.mult), r=[key, 'lng'], w=[key])
        S.op('pool', lambda e: e.tensor_tensor(out=xt[b][:], in0=xt[b][:], in1=b_bc[:], op=ALU.add), r=[key, 'lnb'], w=[key])
        S.op('sp', lambda e: e.dma_start(out=dst_dram[tt * 128:(tt + 1) * 128, :], in_=xt[b][:]), r=[key], w=[('xres', tt)], dma=True)
        transpose_to(xt[b], key, 16, dstT, dstkeyf(tt), tt, F32)


def _load_resident(S, dst, dkey, W2d, KC):
    for kc in range(KC):
        for cb in range(4):
            S.op('pool', lambda e, kc=kc, cb=cb: e.dma_start(out=dst[:, kc, cb * 512:(cb + 1) * 512], in_=W2d[kc * 128:(kc + 1) * 128, cb * 512:(cb + 1) * 512]),
                 w=[(dkey, kc, cb)], dma=True)


def _merge(nc, S, sb, PS, PSB, dr, cst, l, bufA, hT_d, xres, proj_fm, transpose_to, next_ps, st):
    bufB = sb("bufB", [128, 16, T], BF16, st)
    with ExitStack() as s2:
        _H['alloc_wt'](s2, 2)
        sg = [sb("sg%d" % i, [128, 512], BF16, s2) for i in range(2)]
        tmp = sb("mtmp", [128, 512], F32, s2)
        gi = [0]

        def epi(branch):
            def f(col, mm, tb, pi):
                i = gi[0] % 2
                gi[0] += 1
                r0 = R_MG + branch * 2048 + col
                S.op('sp', lambda e: e.dma_start(out=sg[i][:], in_=hT_d[r0:r0 + 128, tb * 512:(tb + 1) * 512]), w=[('sg', i)], dma=True)
                dsl = bufB[:, col // 128, tb * 512:(tb + 1) * 512]
                if branch == 0:
                    S.op('dve', lambda e: e.tensor_tensor(out=dsl, in0=PS[pi][:, :], in1=sg[i][:], op=ALU.mult), r=[('ps', pi), ('sg', i)], w=['bufB'])
                else:
                    S.op('dve', lambda e: e.tensor_tensor(out=tmp[:], in0=PS[pi][:, :], in1=sg[i][:], op=ALU.mult), r=[('ps', pi), ('sg', i)], w=['mtmp'])
                    S.op('pool', lambda e: e.tensor_tensor(out=dsl, in0=dsl, in1=tmp[:], op=ALU.add), r=['mtmp', 'bufB'], w=['bufB'])
            return f
        for j in range(4):
            proj_fm(bufA[:, 0:8, :], 'bufA', 8, dr["w_branch_gla"][l], 512 * j, 512, 128, epi(0))
        for j in range(4):
            proj_fm(bufA[:, 8:16, :], 'bufA', 8, dr["w_branch_nsa"][l], 512 * j, 512, 128, epi(1))
        S.barrier()
    with ExitStack() as s2:
        _load_resident(S, bufA, 'Wres', dr["w_out"][l], 16)
        _resid_ln_phase(nc, S, sb, PS, s2, l, bufA, 16, bufB, lambda tt: ('bufB', tt), xres, "ln_mix_g", "ln_mix_b", dr, xres, bufB,
                        lambda tt: ('bufB', tt), transpose_to, None, nbuf=2)
        S.barrier()
    return bufB


def _xattn(nc, S, sb, PS, PSB, dr, cst, l, bufA, bufB, xres, proj_tm, proj_fm, transpose_to, ln_and_store, next_ps, ident, identb, dbg, load_w, Wt):
    with ExitStack() as st:
        _H['alloc_wt'](st, 2)
        memT = sb("memT", [128, 16, 256], BF16, st)
        mt_ = sb("memtile", [128, D], F32, st)
        kTs = sb("xkT", [128, 4, 256], BF16, st)
        vs = sb("xv", [128, 2, 4, 132], BF16, st)
        qx = bufB
        oT = sb("xoT", [128, 4, T], BF16, st)
        woT = sb("woT", [128, 4, D], BF16, st)
        eT = [sb("xeT%d" % i, [128, 512], BF16, st) for i in range(2)]
        oxa = sb("oxa", [128, 512], BF16, st)
        rden = sb("xrden", [128, 4], F32, st)
        for m in range(2):
            S.op('sp', lambda e: e.dma_start(out=mt_[:], in_=dr["mem"][m * 128:(m + 1) * 128, :]), w=['memtile'], dma=True)
            transpose_to(mt_, 'memtile', 16, memT, 'memT', m, F32)
        S.op('dve', lambda e: e.memset(vs[:], 1.0), w=['xv'])
        Wkv = dr["xa_wkv"][l]
        b = load_w(Wkv, 0, 16, 0, 512)
        for h in range(4):
            pi = next_ps()
            for kc in range(16):
                S.op('pe', lambda e, kc=kc: e.matmul(PS[pi][:, 0:256], lhsT=Wt[b][:, kc, h * 128:(h + 1) * 128], rhs=memT[:, kc, :], start=(kc == 0), stop=(kc == 15)),
                     r=[('Wt', b), 'memT'], w=[('ps', pi)], inc=(kc == 15))
            S.op('act', lambda e: e.activation(out=kTs[:, h, :], in_=PS[pi][:, 0:256], func=AF.Copy), r=[('ps', pi)], w=['xkT'])
        b = load_w(Wkv, 0, 16, 512, 512)
        for m in range(2):
            pi = next_ps()
            for kc in range(16):
                S.op('pe', lambda e, kc=kc: e.matmul(PS[pi][:, :], lhsT=memT[:, kc, m * 128:(m + 1) * 128], rhs=Wt[b][:, kc, :], start=(kc == 0), stop=(kc == 15)),
                     r=[('Wt', b), 'memT'], w=[('ps', pi)], inc=(kc == 15))
            S.op('act', lambda e: e.activation(out=vs[:, m, :, 0:128], in_=PS[pi][:, :].rearrange("p (h d) -> p h d", h=4), func=AF.Copy), r=[('ps', pi)], w=['xv'])

        for tt in range(NT):
            ts = slice(tt * 128, (tt + 1) * 128)
            for m in range(2):
                pi = next_ps()
                for h in range(4):
                    S.op('pe', lambda e, h=h: e.matmul(PS[pi][:, h * 128:(h + 1) * 128], lhsT=kTs[:, h, m * 128:(m + 1) * 128], rhs=qx[:, h, ts], start=True, stop=True),
                         r=['xkT', 'qxT'], w=[('ps', pi)], inc=(h == 3))
                S.op('act', lambda e: e.activation(out=eT[m][:], in_=PS[pi][:, :], func=AF.Exp), r=[('ps', pi)], w=[('xeT', m)])
            for h in range(4):
                pb = 4 + h // 2
                po = (h % 2) * 132
                for m in range(2):
                    S.op('pe', lambda e, m=m: e.matmul(PS[pb][:, po:po + 129], lhsT=eT[m][:, h * 128:(h + 1) * 128], rhs=vs[:, m, h, 0:129], start=(m == 0), stop=(m == 1)),
                         r=[('xeT', m), 'xv'], w=[('ps', pb)], inc=(m == 1))
                S.op('dve', lambda e: e.reciprocal(out=rden[:, h:h + 1], in_=PS[pb][:, po + 128:po + 129]), r=[('ps', pb)], w=['xrden'])
                S.op('dve', lambda e: e.tensor_scalar(out=oxa[:, h * 128:(h + 1) * 128], in0=PS[pb][:, po:po + 128], scalar1=rden[:, h:h + 1], scalar2=None, op0=ALU.mult),
                     r=[('ps', pb), 'xrden'], w=['oxa'])
            transpose_to(oxa, 'oxa', 4, oT, 'xoT', tt, BF16)
        S.barrier()
        for kc in range(4):
            for cb in range(4):
                S.op('pool', lambda e: e.dma_start(out=woT[:, kc, cb * 512:(cb + 1) * 512], in_=dr["xa_wo"][l][kc * 128:(kc + 1) * 128, cb * 512:(cb + 1) * 512]),
                     w=[('Wres', kc, cb)], dma=True)
        _resid_ln_phase(nc, S, sb, PS, st, l, woT, 4, oT, lambda tt: 'xoT', xres, "ln_xa_g", "ln_xa_b", dr, xres, bufA, lambda tt: 'bufA', transpose_to, None, nbuf=2)
        S.barrier()


SKIP = set()
_H = {}
MOE_C = 384
MOE_BIG = 1.0e6


def _moe(nc, S, sb, PS, PSB, dr, cst, l, bufA, bufB, xres, y_d, dst, proj_tm, proj_fm, transpose_to, ln_and_store, next_ps, ident, identb, dbg, load_w, Wt):
    C = MOE_C
    CT = C // 128
    NSL = 16 * C
    xbuf = nc.dram_tensor("moe_xbuf%d" % l, [NSL, D], BF16).ap()
    ybuf = nc.dram_tensor("moe_ybuf%d" % l, [NSL, D], F32).ap()
    I32 = mybir.dt.int32
    breg = nc.gpsimd.to_reg(NSL - 1)
    with ExitStack() as st:
        gateA = sb("gateA", [128, NT], F32, st)
        slotAi = sb("slotAi", [128, NT], I32, st)
        slotBi = sb("slotBi", [128, NT], I32, st)
        with ExitStack() as s2:
            rw = sb("rw", [128, 16, 16], BF16, s2)
            rb = sb("rb", [128, 16], F32, s2)
            lg = sb("lg", [128, 16], F32, s2)
            lb = sb("lb", [128, 4, 4], F32, s2)
            eq = sb("eq", [128, 4, 4], F32, s2)
            lb2 = sb("lb2", [128, 4, 4], F32, s2)
            m1 = sb("m1", [128, 4], F32, s2)
            m2 = sb("m2", [128, 4], F32, s2)
            gs = sb("gs", [128, 4], F32, s2)
            gm = sb("gm", [128, 1], F32, s2)
            ex = sb("ex", [128, 16], F32, s2)
            den = sb("den", [128, 1], F32, s2)
            gate = sb("gate", [128, 16], F32, s2)
            maskall = sb("maskall", [128, NT, 16], BF16, s2)
            lstr = sb("lstr", [128, 128], BF16, s2)
            onesb = sb("onesb", [128, 128], BF16, s2)
            ebase = sb("ebase", [128, 16], F32, s2)
            smat = sb("smat", [128, 16], F32, s2)
            tA = sb("tA", [128, 16], F32, s2)
            tB = sb("tB", [128, 16], F32, s2)
            sA = sb("sA", [128, 1], F32, s2)
            sB = sb("sB", [128, 1], F32, s2)
            zt = sb("zt", [128, D], BF16, s2)
            xtf = [sb("mxtf%d" % i, [128, D], F32, s2) for i in range(2)]
            xtb = [sb("mxtb%d" % i, [128, D], BF16, s2) for i in range(2)]
            S.op('pool', lambda e: e.dma_start(out=rw[:], in_=dr["router_w"].rearrange("(kc p) e -> p kc e", p=128)), w=['rw'], dma=True)
            S.op('sp', lambda e: e.dma_start(out=rb[:], in_=dr["router_b"].partition_broadcast(128)), w=['rb'], dma=True)
            S.op('pool', lambda e: e.dma_start(out=lstr[:], in_=cst['lstr'][:, :]), w=['lstr'], dma=True)
            S.op('sp', lambda e: e.dma_start(out=ebase[:], in_=cst['ebase'][:, :]), w=['ebase'], dma=True)
            S.op('dve', lambda e: e.memset(onesb[:], 1.0), w=['onesb'])
            S.op('dve', lambda e: e.memset(zt[:], 0.0), w=['zt'])
            for ex_i in range(16):
                S.op('sp', lambda e: e.dma_start(out=xbuf[ex_i * C:(ex_i + 1) * C, :].rearrange("(a p) d -> p a d", p=128),
                                                 in_=zt[:].unsqueeze(1).to_broadcast([128, CT, D])), r=['zt'], w=[('xz', ex_i)], dma=True)
            for tt in range(NT):
                ts = slice(tt * 128, (tt + 1) * 128)
                b = tt % 2
                S.op('sp', lambda e: e.dma_start(out=xtf[b][:], in_=xres[ts, :]), r=[('xres', tt)], w=[('mxtf', b)], dma=True)
                S.op('act', lambda e: e.activation(out=xtb[b][:], in_=xtf[b][:], func=AF.Copy), r=[('mxtf', b)], w=[('mxtb', b)])
                pi = next_ps()
                for kc in range(16):
                    S.op('pe', lambda e, kc=kc: e.matmul(PS[pi][:, 0:16], lhsT=bufA[:, kc, ts], rhs=rw[:, kc, :], start=(kc == 0), stop=(kc == 15)),
                         r=['bufA', 'rw'], w=[('ps', pi)], inc=(kc == 15))
                lbf = lb[:].rearrange("p a b -> p (a b)")
                eqf = eq[:].rearrange("p a b -> p (a b)")
                S.op('dve', lambda e: e.tensor_copy(out=lg[:], in_=PS[pi][:, 0:16]), r=[('ps', pi)], w=['lg'])
                S.op('dve', lambda e: e.tensor_tensor(out=lbf, in0=lg[:], in1=rb[:], op=ALU.add), r=['lg', 'rb'], w=['lb'])
                S.op('dve', lambda e: e.tensor_reduce(out=m1[:], in_=lb[:], axis=AX.X, op=ALU.max), r=['lb'], w=['m1'])
                S.op('dve', lambda e: e.tensor_tensor(out=eq[:], in0=lb[:], in1=m1[:].unsqueeze(2).to_broadcast([128, 4, 4]), op=ALU.is_equal), r=['lb', 'm1'], w=['eq'])
                S.op('dve', lambda e: e.scalar_tensor_tensor(out=lb2[:], in0=eq[:], scalar=-1e30, in1=lb[:], op0=ALU.mult, op1=ALU.add), r=['eq', 'lb'], w=['lb2'])
                S.op('dve', lambda e: e.tensor_reduce(out=m2[:], in_=lb2[:], axis=AX.X, op=ALU.max), r=['lb2'], w=['m2'])
                S.op('dve', lambda e: e.tensor_tensor(out=gs[:], in0=m1[:], in1=m2[:], op=ALU.add), r=['m1', 'm2'], w=['gs'])
                S.op('dve', lambda e: e.tensor_reduce(out=gm[:], in_=gs[:], axis=AX.X, op=ALU.max), r=['gs'], w=['gm'])
                S.op('dve', lambda e: e.tensor_scalar(out=gs[:], in0=gs[:], scalar1=gm[:, 0:1], scalar2=None, op0=ALU.is_equal), r=['gs', 'gm'], w=['gs'])
                S.op('dve', lambda e: e.tensor_tensor(out=eq[:], in0=lb[:], in1=m2[:].unsqueeze(2).to_broadcast([128, 4, 4]), op=ALU.is_ge), r=['lb', 'm2'], w=['eq'])
                S.op('dve', lambda e: e.tensor_tensor(out=eq[:], in0=eq[:], in1=gs[:].unsqueeze(2).to_broadcast([128, 4, 4]), op=ALU.mult), r=['eq', 'gs'], w=['eq'])
                S.op('act', lambda e: e.activation(out=ex[:], in_=lg[:], func=AF.Exp), r=['lg'], w=['ex'])
                S.op('dve', lambda e: e.tensor_tensor(out=ex[:], in0=ex[:], in1=eqf, op=ALU.mult), r=['ex', 'eq'], w=['ex'])
                S.op('dve', lambda e: e.tensor_reduce(out=den[:], in_=ex[:], axis=AX.X, op=ALU.add), r=['ex'], w=['den'])
                S.op('dve', lambda e: e.reciprocal(out=den[:], in_=den[:]), r=['den'], w=['den'])
                S.op('dve', lambda e: e.tensor_scalar(out=gate[:], in0=ex[:], scalar1=den[:, 0:1], scalar2=None, op0=ALU.mult), r=['ex', 'den'], w=['gate'])
                S.op('act', lambda e: e.activation(out=maskall[:, tt, :], in_=eqf, func=AF.Copy), r=['eq'], w=[('maskall', tt)])
                pj = next_ps()
                for t2 in range(tt):
                    S.op('pe', lambda e, t2=t2: e.matmul(PS[pj][:, 0:16], lhsT=onesb[:], rhs=maskall[:, t2, :], start=(t2 == 0), stop=False),
                         r=['onesb', ('maskall', t2)], w=[('ps', pj)], inc=False)
                S.op('pe', lambda e: e.matmul(PS[pj][:, 0:16], lhsT=lstr[:], rhs=maskall[:, tt, :], start=(tt == 0), stop=True),
                     r=['lstr', ('maskall', tt)], w=[('ps', pj)], inc=True)
                S.op('dve', lambda e: e.tensor_tensor(out=smat[:], in0=PS[pj][:, 0:16], in1=ebase[:], op=ALU.add), r=[('ps', pj), 'ebase'], w=['smat'])
                S.op('dve', lambda e: e.scalar_tensor_tensor(out=tA[:], in0=eqf, scalar=-MOE_BIG, in1=smat[:], op0=ALU.mult, op1=ALU.add), r=['eq', 'smat'], w=['tA'])
                S.op('dve', lambda e: e.tensor_reduce(out=sA[:], in_=tA[:], axis=AX.X, op=ALU.min), r=['tA'], w=['sA'])
                S.op('dve', lambda e: e.tensor_tensor(out=tB[:], in0=tA[:], in1=eqf, op=ALU.mult), r=['tA', 'eq'], w=['tB'])
                S.op('dve', lambda e: e.tensor_reduce(out=sB[:], in_=tB[:], axis=AX.X, op=ALU.max), r=['tB'], w=['sB'])
                S.op('dve', lambda e: e.tensor_scalar(out=tB[:], in0=tA[:], scalar1=sA[:, 0:1], scalar2=None, op0=ALU.is_equal), r=['tA', 'sA'], w=['tB'])
                S.op('dve', lambda e: e.tensor_tensor(out=tB[:], in0=tB[:], in1=gate[:], op=ALU.mult), r=['tB', 'gate'], w=['tB'])
                S.op('dve', lambda e: e.tensor_reduce(out=gateA[:, tt:tt + 1], in_=tB[:], axis=AX.X, op=ALU.add), r=['tB'], w=['gateA'])
                S.op('dve', lambda e: e.tensor_copy(out=slotAi[:, tt:tt + 1], in_=sA[:]), r=['sA'], w=['slotAi'])
                S.op('dve', lambda e: e.tensor_copy(out=slotBi[:, tt:tt + 1], in_=sB[:]), r=['sB'], w=['slotBi'])
                for sl, sk in ((slotAi, 'slotAi'), (slotBi, 'slotBi')):
                    S.op('pool', lambda e: e.indirect_dma_start(out=xbuf[:, :], out_offset=bass.IndirectOffsetOnAxis(ap=sl[:, tt:tt + 1], axis=0),
                                                                in_=xtb[b][:, :], in_offset=None, bounds_check=breg, oob_is_err=False),
                         r=[('mxtb', b), sk] + [('xz', q) for q in range(16)], w=[('xsc', tt, sk)], dma=True)
            S.barrier()
        with ExitStack() as s2:
            _H['alloc_wt'](s2, 4)
            xe = [sb("xe%d" % i, [128, D], BF16, s2) for i in range(2)]
            xeT = sb("xeT", [128, 16, C], BF16, s2)
            actT = sb("actT", [128, 12, C], BF16, s2)
            ystg = [sb("ystg%d" % i, [128, 512], F32, s2) for i in range(3)]
            yi = [0]
            xi = [0]
            for ex_i in range(0 if 'moe2' in SKIP else 16):
                Wi = dr["moe_w_in"][l, ex_i]
                Wd = dr["moe_w_down"][l, ex_i]
                for sti in range(CT):
                    b = xi[0] % 2
                    xi[0] += 1
                    r0 = ex_i * C + sti * 128
                    S.op('sp', lambda e: e.dma_start(out=xe[b][:], in_=xbuf[r0:r0 + 128, :]), w=[('xe', b)], dma=True)
                    transpose_to(xe[b], ('xe', b), 16, xeT, 'xeT', sti, BF16)
                for j in range(6):
                    wb = load_w(Wi, 0, 16, 512 * j, 512)
                    for m in range(4):
                        pi = next_ps()
                        for kc in range(16):
                            S.op('pe', lambda e, kc=kc: e.matmul(PS[pi][:, 0:C], lhsT=Wt[wb][:, kc, m * 128:(m + 1) * 128], rhs=xeT[:, kc, :], start=(kc == 0), stop=(kc == 15)),
                                 r=[('Wt', wb), 'xeT'], w=[('ps', pi)], inc=(kc == 15))
                        fc = (j * 4 + m) % 12
                        if j < 3:
                            S.op('act', lambda e: e.activation(out=actT[:, fc, :], in_=PS[pi][:, 0:C], func=AF.Silu), r=[('ps', pi)], w=[('actT', fc)])
                        else:
                            S.op('dve', lambda e: e.tensor_tensor(out=actT[:, fc, :], in0=actT[:, fc, :], in1=PS[pi][:, 0:C], op=ALU.mult), r=[('ps', pi), ('actT', fc)], w=[('actT', fc)])
                for cb in range(4):
                    wb = load_w(Wd, 0, 12, 512 * cb, 512)
                    for sti in range(CT):
                        pi = next_ps()
                        for fc in range(12):
                            S.op('pe', lambda e, fc=fc: e.matmul(PS[pi][:, :], lhsT=actT[:, fc, sti * 128:(sti + 1) * 128], rhs=Wt[wb][:, fc, :], start=(fc == 0), stop=(fc == 11)),
                                 r=[('Wt', wb), ('actT', fc)], w=[('ps', pi)], inc=(fc == 11))
                        i = yi[0] % 3
                        yi[0] += 1
                        S.op('act', lambda e: e.activation(out=ystg[i][:], in_=PS[pi][:, :], func=AF.Copy), r=[('ps', pi)], w=[('ystg', i)])
                        r0 = ex_i * C + sti * 128
                        S.op('sp', lambda e: e.dma_start(out=ybuf[r0:r0 + 128, cb * 512:(cb + 1) * 512], in_=ystg[i][:]), r=[('ystg', i)], w=['ybuf'], dma=True)
            S.barrier()
        with ExitStack() as s2:
            yA = sb("yA", [128, D], F32, s2)
            yB = sb("yB", [128, D], F32, s2)

            def yfn(tt):
                S.op('pool', lambda e: e.indirect_dma_start(out=yA[:, :], out_offset=None, in_=ybuf[:, :],
                                                            in_offset=bass.IndirectOffsetOnAxis(ap=slotAi[:, tt:tt + 1], axis=0), bounds_check=breg, oob_is_err=False),
                     r=['slotAi'], w=['yA'], dma=True)
                S.op('pool', lambda e: e.indirect_dma_start(out=yB[:, :], out_offset=None, in_=ybuf[:, :],
                                                            in_offset=bass.IndirectOffsetOnAxis(ap=slotBi[:, tt:tt + 1], axis=0), bounds_check=breg, oob_is_err=False),
                     r=['slotBi'], w=['yB'], dma=True)
                S.op('dve', lambda e: e.tensor_tensor(out=yA[:], in0=yA[:], in1=yB[:], op=ALU.subtract), r=['yA', 'yB'], w=['yA'])
                S.op('dve', lambda e: e.scalar_tensor_tensor(out=yA[:], in0=yA[:], scalar=gateA[:, tt:tt + 1], in1=yB[:], op0=ALU.mult, op1=ALU.add),
                     r=['yA', 'yB', 'gateA'], w=['yA'])
                return yA, 'yA'
            _resid_ln_phase(nc, S, sb, PS, s2, l, None, 0, yfn, None, xres, "ln_ffn_g", "ln_ffn_b", dr, dst, bufA, lambda tt: 'bufA', transpose_to, None, nbuf=2)
            S.barrier()


def _nsa(nc, S, sb, PS, PSB, dr, cst, l, bufA, hT_d, h_d, transpose_to, next_ps, ident, identb, dbg):
    slopes = [2.0 ** (-8.0 * (h + 1) / 16.0) for h in range(16)]
    with ExitStack() as st:
        rel = sb("rel", [128, 2048], F32, st)
        cdiag = sb("cdiag", [128, 128], F32, st)
        cfar = sb("cfar", [128, 128], F32, st)
        dcon = sb("dcon", [128, 272], F32, st)
        cpb = sb("cpb", [128, 256], F32, st)
        cmask = sb("cmask", [128, 2048], BF16, st)
        selc = sb("selc", [128, NT, 32], F32, st)
        expd = sb("expd", [32, 2048], BF16, st)
        ng = sb("ng", [128, NT, 48], BF16, st)
        kcT = sb("kcT", [64, 4, 128], BF16, st)
        vca = sb("vca", [128, 4, 97], BF16, st)
        S.op('sp', lambda e: e.dma_start(out=rel[:], in_=cst['rel_mid'][:, :]), w=['rel'], dma=True)
        S.op('sp', lambda e: e.dma_start(out=cdiag[:], in_=cst['cdiag'][:, :]), w=['cdiag'], dma=True)
        S.op('sp', lambda e: e.dma_start(out=cfar[:], in_=cst['cfar'][:, :]), w=['cfar'], dma=True)
        S.op('sp', lambda e: e.dma_start(out=dcon[:], in_=cst['dconst'][:, :]), w=['dcon'], dma=True)
        S.op('sp', lambda e: e.dma_start(out=cpb[:], in_=cst['cmp_pb'][:, :]), w=['cpb'], dma=True)
        S.op('pool', lambda e: e.dma_start(out=cmask[:], in_=cst['cmp_mask'][:, :]), w=['cmask'], dma=True)
        S.op('sp', lambda e: e.dma_start(out=selc[:], in_=cst['selc'].rearrange("(tt p) j -> p tt j", p=128)), w=['selc'], dma=True)
        S.op('pool', lambda e: e.dma_start(out=expd[:], in_=cst['expand'][:, :]), w=['expd'], dma=True)
        S.op('sp', lambda e: e.dma_start(out=ng[:], in_=h_d[:, TC_NG:TC_NG + 48].rearrange("(tt p) j -> p tt j", p=128)), w=['ng'], dma=True)
        S.op('dve', lambda e: e.memset(kcT[:], 0.0), w=['kcT'])
        S.op('dve', lambda e: e.memset(vca[:], 0.0), w=['vca'])
        with ExitStack() as s2:
            w1 = sb("w1", [64, 2, 32, 256], BF16, s2)
            w2 = sb("w2", [128, 2, 2, 64], BF16, s2)
            pes = sb("pes", [32, 2, 64], F32, s2)
            peT = sb("peT", [64, 2, 32], BF16, s2)
            c1 = sb("c1", [128, 2, 2], F32, s2)
            srcT = sb("csrcT", [64, T], BF16, s2)
            u = sb("cu", [128, 128], F32, s2)
            t1 = sb("ct1", [128, 128], F32, s2)
            gel = sb("cgel", [128, 2, 128], BF16, s2)
            ovl = sb("ovl", [128, 32], F32, s2)
            for kv in range(2):
                S.op('pool', lambda e: e.dma_start(out=w1[:, kv, :, :], in_=dr["nsa_cmp_w1"][l, kv].rearrange("(l d) h -> d l h", d=64)), w=['w1'], dma=True)
                S.op('pool', lambda e: e.dma_start(out=w2[:, kv, :, :], in_=dr["nsa_cmp_w2"][l, kv].rearrange("(hc p) d -> p hc d", p=128)), w=['w2'], dma=True)
            S.op('sp', lambda e: e.dma_start(out=pes[:], in_=dr["nsa_cmp_pe"][l].rearrange("k l d -> l k d")), w=['pes'], dma=True)
            S.op('sp', lambda e: e.dma_start(out=ovl[:], in_=cst['overlap'][:, :]), w=['ovl'], dma=True)
            for g in range(4):
                S.op('dve', lambda e: e.memset(vca[:, g, 64:65], 1.0), r=[], w=['vca'])
                S.op('dve', lambda e: e.tensor_copy(out=vca[:, g, 65:97], in_=ovl[:]), r=['ovl'], w=['vca'])
            for kv in range(2):
                S.op('pe', lambda e: e.transpose(out=PS[0][0:64, 0:32], in_=pes[0:32, kv, :], identity=ident[0:32, 0:32]), r=['pes', 'ident'], w=[('ps', 0)])
                S.op('act', lambda e: e.activation(out=peT[:, kv, :], in_=PS[0][0:64, 0:32], func=AF.Copy), r=[('ps', 0)], w=['peT'])
                for hc in range(2):
                    for li in range(32):
                        S.op('pe', lambda e, li=li: e.matmul(PS[1][:, 0:1], lhsT=w1[:, kv, li, hc * 128:(hc + 1) * 128], rhs=peT[:, kv, li:li + 1], start=(li == 0), stop=(li == 31)),
                             r=['w1', 'peT'], w=[('ps', 1)], inc=(li == 31))
                    S.op('act', lambda e: e.activation(out=c1[:, kv, hc:hc + 1], in_=PS[1][:, 0:1], func=AF.Copy), r=[('ps', 1)], w=['c1'])
            for g in range(4):
                for kv in range(2):
                    r0 = (R_KC if kv == 0 else R_VC) + g * 64
                    S.op('sp', lambda e: e.dma_start(out=srcT[:], in_=hT_d[r0:r0 + 64, :]), w=['csrcT'], dma=True)
                    for hc in range(2):
                        pi = next_ps()
                        for li in range(32):
                            S.op('pe', lambda e, li=li: e.matmul(PS[pi][:, 0:127], lhsT=w1[:, kv, li, hc * 128:(hc + 1) * 128], rhs=srcT[:, li:li + 16 * 126 + 1:16],
                                                                 start=(li == 0), stop=(li == 31)), r=['w1', 'csrcT'], w=[('ps', pi)], inc=(li == 31))
                        S.op('act', lambda e: e.activation(out=u[:, 0:127], in_=PS[pi][:, 0:127], func=AF.Identity, bias=c1[:, kv, hc:hc + 1], scale=1.0), r=[('ps', pi), 'c1'], w=['cu'])
                        S.op('dve', lambda e: e.tensor_tensor(out=t1[:, 0:127], in0=u[:, 0:127], in1=u[:, 0:127], op=ALU.mult), r=['cu'], w=['ct1'])
                        S.op('dve', lambda e: e.tensor_scalar(out=t1[:, 0:127], in0=t1[:, 0:127], scalar1=0.044715, scalar2=1.0, op0=ALU.mult, op1=ALU.add), r=['ct1'], w=['ct1'])
                        S.op('dve', lambda e: e.tensor_tensor(out=t1[:, 0:127], in0=t1[:, 0:127], in1=u[:, 0:127], op=ALU.mult), r=['ct1', 'cu'], w=['ct1'])
                        S.op('act', lambda e: e.activation(out=t1[:, 0:127], in_=t1[:, 0:127], func=AF.Sigmoid, scale=2.0 * 0.7978845608028654), r=['ct1'], w=['ct1'])
                        S.op('dve', lambda e: e.tensor_tensor(out=gel[:, hc, 0:127], in0=t1[:, 0:127], in1=u[:, 0:127], op=ALU.mult), r=['ct1', 'cu'], w=['cgel'])
                    pi = next_ps()
                    if kv == 0:
                        for hc in range(2):
                            S.op('pe', lambda e, hc=hc: e.matmul(PS[pi][0:64, 0:127], lhsT=w2[:, 0, hc, :], rhs=gel[:, hc, 0:127], start=(hc == 0), stop=(hc == 1)),
                                 r=['w2', 'cgel'], w=[('ps', pi)], inc=(hc == 1))
                        S.op('act', lambda e: e.activation(out=kcT[:, g, 0:127], in_=PS[pi][0:64, 0:127], func=AF.Copy), r=[('ps', pi)], w=['kcT'])
                    else:
                        for hc in range(2):
                            S.op('pe', lambda e, hc=hc: e.matmul(PS[pi][0:127, 0:64], lhsT=gel[:, hc, 0:127], rhs=w2[:, 1, hc, :], start=(hc == 0), stop=(hc == 1)),
                                 r=['w2', 'cgel'], w=[('ps', pi)], inc=(hc == 1))
                        S.op('act', lambda e: e.activation(out=vca[0:127, g, 0:64], in_=PS[pi][0:127, 0:64], func=AF.Copy), r=[('ps', pi)], w=['vca'])
            S.barrier()
        dtmax = []
        for h in range(16):
            d = 1
            while d < 15 and slopes[h] * (128 * (d + 1) - 127) <= 40.0:
                d += 1
            dtmax.append(d)
        qT = sb("nqT", [64, 4, T], BF16, st)
        ksT = sb("nksT", [64, T], BF16, st)
        kwT = sb("nkwT", [64, T], BF16, st)
        vs = sb("nvs", [128, NT, 65], BF16, st)
        vw = sb("nvw", [128, NT, 65], BF16, st)
        NSC = 4
        sc = [sb("nsc%d" % i, [128, 512], F32, st) for i in range(NSC)]
        eTs = sb("neTs", [128, NT, 512], BF16, st)
        eTw = sb("neTw", [128, 5, 512], BF16, st)
        eTc = [sb("neTc%d" % i, [128, 512], BF16, st) for i in range(2)]
        imp = [sb("nimp%d" % i, [128, 32], F32, st) for i in range(2)]
        mx8 = [sb("nmx8%d" % i, [128, 8], F32, st) for i in range(2)]
        selb = [sb("nselb%d" % i, [128, 32], F32, st) for i in range(2)]
        selbT = sb("nselbT", [32, NT, 128], BF16, st)
        rd4 = [sb("nrd%d" % i, [128, 4], F32, st) for i in range(3)]
        oc = sb("noc", [128, NT, 256], F32, st)
        octmp = [sb("noctmp%d" % i, [128, 256], F32, st) for i in range(2)]
        ocb = [sb("nocb%d" % i, [128, 256], BF16, st) for i in range(2)]
        sci = [0]
        PB_SEL, PB_WIN, PB_CMP, PB_TR = 4, 5, 6, 7
        relgd = sb("relgd", [128, 2, 512], F32, st)
        RGI = {0: 0, 4: 1}
        btab = sb("btab", [6, NT * 512], BF16, st)
        l6 = sb("l6", [6, 128], BF16, st)
        S.op('pool', lambda e: e.dma_start(out=l6[:], in_=cst['l6'][:, :]), w=['l6'], dma=True)

        for g in range(4):
            S.op('sp', lambda e: e.dma_start(out=qT[:], in_=hT_d[R_NQ + g * 256:R_NQ + (g + 1) * 256, :].rearrange("(h d) t -> d h t", d=64)), w=['nqT'], dma=True)
            S.op('sp', lambda e: e.dma_start(out=ksT[:], in_=hT_d[R_KS + g * 64:R_KS + (g + 1) * 64, :]), w=['nksT'], dma=True)
            S.op('sp', lambda e: e.dma_start(out=kwT[:], in_=hT_d[R_KW + g * 64:R_KW + (g + 1) * 64, :]), w=['nkwT'], dma=True)
            S.op('dve', lambda e: e.memset(vs[:], 1.0), w=['nvs'])
            S.op('dve', lambda e: e.memset(vw[:], 1.0), w=['nvw'])
            S.op('sp', lambda e: e.dma_start(out=vs[:, :, 0:64], in_=h_d[:, TC_VS + g * 64:TC_VS + (g + 1) * 64].rearrange("(kt p) d -> p kt d", p=128)), w=['nvs'], dma=True)
            S.op('sp', lambda e: e.dma_start(out=vw[:, :, 0:64], in_=h_d[:, TC_VW + g * 64:TC_VW + (g + 1) * 64].rearrange("(kt p) d -> p kt d", p=128)), w=['nvw'], dma=True)

            for q4 in range(4):
                S.op('pool', lambda e: e.dma_start(out=btab[:, q4 * 2048:(q4 + 1) * 2048], in_=cst['btab'][:, g * 8192 + q4 * 2048:g * 8192 + (q4 + 1) * 2048]), w=['btab'], dma=True)
            for dt_ in (0, 4):
                for h in range(4):
                    hh = 4 * g + h
                    S.op('act', lambda e: e.activation(out=relgd[:, RGI[dt_], h * 128:(h + 1) * 128], in_=rel[:, hh * 128:(hh + 1) * 128], func=AF.Identity,
                                                       bias=dcon[:, hh * 17 + dt_:hh * 17 + dt_ + 1], scale=1.0), r=['rel', 'dcon'], w=[('relgd', dt_)])

            def scores(tt, lhs, kkey, kt, mode, use_sel, edst, ekey, extra_mask, heads):
                ts = slice(tt * 128, (tt + 1) * 128)
                pi = next_ps(0, 4)
                pv = PS[pi][:, :].rearrange("p (h j) -> p h j", h=4)
                pebias = (mode != 'cmp') and (extra_mask is None)
                S.op('pe', lambda e: e.matmul(pv, lhsT=lhs, rhs=qT[:, :, ts], start=True, stop=not (use_sel or pebias)), r=[kkey, 'nqT'], w=[('ps', pi)], inc=not (use_sel or pebias))
                if use_sel:
                    S.op('pe', lambda e: e.matmul(pv, lhsT=expd[:, kt * 128:(kt + 1) * 128], rhs=selbT[:, tt, :].unsqueeze(1).to_broadcast([32, 4, 128]), start=False, stop=not pebias),
                         r=['expd', ('nselbT', tt)], w=[('ps', pi)], inc=not pebias)
                if pebias:
                    d_ = tt - kt
                    S.op('pe', lambda e: e.matmul(PS[pi][:, :], lhsT=l6[:, :], rhs=btab[:, d_ * 512:(d_ + 1) * 512], start=False, stop=True), r=['l6', 'btab'], w=[('ps', pi)], inc=True)
                    S.op('act', lambda e: e.activation(out=edst, in_=PS[pi][:, :], func=AF.Exp), r=[('ps', pi)], w=[(ekey, h) for h in range(4)], inc=True)
                    return
                si = sci[0] % NSC
                sci[0] += 1
                if mode == 'cmp':
                    S.op('dve', lambda e: e.tensor_tensor(out=sc[si][:], in0=PS[pi][:, :], in1=rel[:, g * 512:(g + 1) * 512], op=ALU.add), r=[('ps', pi), 'rel'], w=[('nsc', si)])
                else:
                    S.op('dve', lambda e: e.tensor_tensor(out=sc[si][:], in0=PS[pi][:, :], in1=relgd[:, RGI[tt - kt], :], op=ALU.add), r=[('ps', pi), ('relgd', tt - kt)], w=[('nsc', si)])
                if extra_mask is not None:
                    mk, mkey = extra_mask
                    S.op('pool', lambda e: e.tensor_tensor(out=sc[si][:].rearrange("p (h j) -> p h j", h=4), in0=sc[si][:].rearrange("p (h j) -> p h j", h=4),
                                                           in1=mk.unsqueeze(1).to_broadcast([128, 4, 128]), op=ALU.add), r=[('nsc', si), mkey], w=[('nsc', si)])
                if mode == 'cmp':
                    for h in heads:
                        hh = 4 * g + h
                        bia = cpb[:, hh * 16 + tt:hh * 16 + tt + 1]
                        S.op('act', lambda e, h=h: e.activation(out=edst[:, h * 128:(h + 1) * 128], in_=sc[si][:, h * 128:(h + 1) * 128], func=AF.Exp, bias=bia, scale=1.0),
                             r=[('nsc', si), 'cpb'], w=[(ekey, h)], inc=True)
                else:
                    S.op('act', lambda e: e.activation(out=edst, in_=sc[si][:], func=AF.Exp), r=[('nsc', si)], w=[(ekey, h) for h in range(4)], inc=True)

            def pv_combine(tt, pb, per_head, ncol, br, first, ri):
                for h in range(4):
                    lst = per_head[h]
                    for i, (lt, rt, rk) in enumerate(lst):
                        S.op('pe', lambda e, lt=lt, rt=rt, i=i: e.matmul(PS[pb][:, h * 128:h * 128 + ncol], lhsT=lt, rhs=rt, start=(i == 0), stop=(i == len(lst) - 1)),
                             r=rk, w=[('ps', pb)], inc=(i == len(lst) - 1))
                rd = rd4[ri]
                rk_ = ('nrd', ri)
                S.op('dve', lambda e: e.tensor_scalar(out=rd[:], in0=PS[pb][:, 64::128], scalar1=1e-30, scalar2=None, op0=ALU.max), r=[('ps', pb)], w=[rk_])
                S.op('dve', lambda e: e.reciprocal(out=rd[:], in_=rd[:]), r=[rk_], w=[rk_])
                if br == 0:
                    ib = tt % 2
                    for h in range(4):
                        if h == 0:
                            S.op('dve', lambda e: e.tensor_scalar(out=imp[ib][:], in0=PS[pb][:, 65:97], scalar1=rd[:, 0:1], scalar2=None, op0=ALU.mult), r=[('ps', pb), rk_], w=[('nimp', ib)])
                        else:
                            S.op('dve', lambda e, h=h: e.scalar_tensor_tensor(out=imp[ib][:], in0=PS[pb][:, h * 128 + 65:h * 128 + 97], scalar=rd[:, h:h + 1], in1=imp[ib][:],
                                                                              op0=ALU.mult, op1=ALU.add), r=[('ps', pb), rk_, ('nimp', ib)], w=[('nimp', ib)])
                c0 = 12 * g + br
                S.op('dve', lambda e: e.tensor_tensor(out=rd[:], in0=rd[:], in1=ng[:, tt, c0:c0 + 10:3], op=ALU.mult), r=[rk_, 'ng'], w=[rk_])
                psv = PS[pb][:, :].rearrange("p (h c) -> p h c", h=4)[:, :, 0:64]
                wbc = rd[:].unsqueeze(2).to_broadcast([128, 4, 64])
                ocv = oc[:, tt, :].rearrange("p (h c) -> p h c", h=4)
                if first:
                    S.op('dve', lambda e: e.tensor_tensor(out=ocv, in0=psv, in1=wbc, op=ALU.mult), r=[('ps', pb), rk_], w=[('noc', tt)])
                else:
                    ti = (tt + br) % 2
                    S.op('dve', lambda e: e.tensor_tensor(out=octmp[ti][:].rearrange("p (h c) -> p h c", h=4), in0=psv, in1=wbc, op=ALU.mult), r=[('ps', pb), rk_], w=[('noctmp', ti)])
                    S.op('pool', lambda e: e.tensor_tensor(out=oc[:, tt, :], in0=oc[:, tt, :], in1=octmp[ti][:], op=ALU.add), r=[('noctmp', ti), ('noc', tt)], w=[('noc', tt)])

            def cmp_scores(tt):
                ts = slice(tt * 128, (tt + 1) * 128)
                cb_ = tt % 2
                scores(tt, kcT[:, g, :], 'kcT', 0, 'cmp', False, eTc[cb_], ('neTc', cb_), (cmask[:, ts], 'cmask'), range(4))

            def cmp_pv(tt):
                cb_ = tt % 2
                ph = [[(eTc[cb_][:, h * 128:(h + 1) * 128], vca[:, g, 0:97], [(('neTc', cb_), h), 'vca'])] for h in range(4)]
                pv_combine(tt, PB_CMP, ph, 97, 0, True, 0)
                ib = tt % 2
                S.op('dve', lambda e: e.tensor_tensor(out=imp[ib][:], in0=imp[ib][:], in1=selc[:, tt, :], op=ALU.add), r=[('nimp', ib), 'selc'], w=[('nimp', ib)])
                S.op('dve', lambda e: e.max(out=mx8[ib][:], in_=imp[ib][:]), r=[('nimp', ib)], w=[('nmx8', ib)])
                S.op('dve', lambda e: e.tensor_scalar(out=mx8[ib][:, 7:8], in0=mx8[ib][:, 7:8], scalar1=-5e29, scalar2=None, op0=ALU.max), r=[('nmx8', ib)], w=[('nmx8', ib)])
                S.op('dve', lambda e: e.tensor_scalar(out=selb[ib][:], in0=imp[ib][:], scalar1=mx8[ib][:, 7:8], scalar2=NEGB, op0=ALU.is_lt, op1=ALU.mult),
                     r=[('nimp', ib), ('nmx8', ib)], w=[('nselb', ib)])
                S.op('pe', lambda e: e.transpose(out=PS[PB_TR][0:32, 0:128], in_=selb[ib][:, :], identity=ident[:]), r=[('nselb', ib), 'ident'], w=[('ps', PB_TR)])
                S.op('act', lambda e: e.activation(out=selbT[:, tt, :], in_=PS[PB_TR][0:32, 0:128], func=AF.Copy), r=[('ps', PB_TR)], w=[('nselbT', tt)])

            cmp_scores(0)
            for tt in range(NT):
                if tt + 1 < NT:
                    cmp_scores(tt + 1)
                cmp_pv(tt)
            gd = max(dtmax[4 * g:4 * g + 4])

            def win_scores(tt):
                for kt in range(max(0, tt - 4), tt + 1):
                    em = (cdiag[:], 'cdiag') if kt == tt else ((cfar[:], 'cfar') if kt == tt - 4 else None)
                    hs = [h for h in range(4) if tt - kt <= dtmax[4 * g + h]]
                    scores(tt, kwT[:, kt * 128:(kt + 1) * 128], 'nkwT', kt, 'rel', False, eTw[:, tt - kt, :], ('neTw', tt - kt), em, hs)

            def win_pv(tt):
                kts = list(range(max(0, tt - 4), tt + 1))
                ph = [[(eTw[:, tt - kt, h * 128:(h + 1) * 128], vw[:, kt, :], [(('neTw', tt - kt), h), 'nvw']) for kt in kts if tt - kt <= dtmax[4 * g + h]] for h in range(4)]
                pv_combine(tt, PB_WIN, ph, 65, 2, False, 1)

            def sel_scores(tt):
                for kt in range(max(0, tt - gd), tt + 1):
                    hs = [h for h in range(4) if tt - kt <= dtmax[4 * g + h]]
                    scores(tt, ksT[:, kt * 128:(kt + 1) * 128], 'nksT', kt, 'rel', True, eTs[:, kt, :], ('neTs', kt), (cdiag[:], 'cdiag') if kt == tt else None, hs)

            def sel_pv(tt):
                kts = list(range(max(0, tt - gd), tt + 1))
                ph = [[(eTs[:, kt, h * 128:(h + 1) * 128], vs[:, kt, :], [(('neTs', kt), h), 'nvs']) for kt in kts if tt - kt <= dtmax[4 * g + h]] for h in range(4)]
                pv_combine(tt, PB_SEL, ph, 65, 1, False, 2)
                ob = tt % 2
                S.op('act', lambda e: e.activation(out=ocb[ob][:], in_=oc[:, tt, :], func=AF.Copy), r=[('noc', tt)], w=[('nocb', ob)])
                transpose_to(ocb[ob], ('nocb', ob), 2, bufA, 'bufA', tt, BF16, c_off=8 + 2 * g, banks=(PB_TR, PB_TR + 1))

            jobs = []
            for tt in range(NT):
                jobs.append((win_scores, win_pv, tt))
                jobs.append((sel_scores, sel_pv, tt))
            jobs[0][0](jobs[0][2])
            for ji, (fs, fp, tt) in enumerate(jobs):
                if ji + 1 < len(jobs):
                    jobs[ji + 1][0](jobs[ji + 1][2])
                fp(tt)
        S.barrier()


_NC_CACHE = {}


def kernel(**inputs):
    n = 8
    if "nc" not in _NC_CACHE:
        _NC_CACHE["nc"] = build()
    nc = _NC_CACHE["nc"]
    consts = make_consts()
    in_maps = []
    for c in range(n):
        m = {"x": np.ascontiguousarray(inputs["x"][c], dtype=np.float32), "mem": np.ascontiguousarray(inputs["mem"][c], dtype=np.float32)}
        for k in WNAMES:
            m[k] = np.ascontiguousarray(inputs[k], dtype=np.float32)
        for k, v in consts.items():
            m["c_" + k] = v
        in_maps.append(m)
    res = run_bass_kernel_spmd(nc, in_maps, core_ids=list(range(n)))
    return np.stack([res.results[c]["out"] for c in range(n)], axis=0).astype(np.float32)
```

```python
import numpy as np
from contextlib import ExitStack
import concourse.bass as bass
import concourse.mybir as mybir
from concourse.bass_utils import run_bass_kernel_spmd

F32 = mybir.dt.float32
BF16 = mybir.dt.bfloat16
AF = mybir.ActivationFunctionType
ALU = mybir.AluOpType
AX = mybir.AxisListType

T = 2048
D = 2048
NT = 16
DEPTH = 2
DN_ALPHA = float((2 * DEPTH) ** 0.25)
LN_EPS = 1e-5
D_IN = 9792
C_GQ, C_GK, C_GV, C_GR, C_GA, C_NQ, C_NKV, C_NG, C_MG = 0, 512, 1024, 2048, 3072, 3088, 4112, 5648, 5696
R_GQ, R_GK, R_GA, R_NQ, R_KC, R_VC, R_KS, R_KW, R_MG, NFM = 0, 512, 1024, 1056, 2080, 2336, 2592, 2848, 3104, 7200
TC_GK, TC_GV, TC_GR, TC_VS, TC_VW, TC_NG, NTM = 0, 512, 1536, 2560, 2816, 3072, 3120
NEGB = -30000.0


class Sch:
    def __init__(self, nc, es):
        self.nc = nc
        self.eng = {'pe': nc.tensor, 'act': nc.scalar, 'dve': nc.vector, 'pool': nc.gpsimd, 'sp': nc.sync}
        self.sem = {}
        self.cnt = {}
        for e in ('pe', 'act', 'dve', 'pool'):
            self.sem[e] = es.enter_context(nc.semaphore('s_' + e))
            self.cnt[e] = 0
        self.NS = 8
        for q in ('sp', 'pool'):
            for i in range(self.NS):
                k = ('dma', q, i)
                self.sem[k] = es.enter_context(nc.semaphore('d_%s%d' % (q, i)))
                self.cnt[k] = 0
        self.dma_i = {'sp': 0, 'pool': 0}
        self.waited = {e: {} for e in self.eng}
        self.lw = {}
        self.rd = {}
        self.nops = 0

    def _wait(self, e, tok):
        s, v = tok
        if self.waited[e].get(s, 0) >= v:
            return
        self.waited[e][s] = v
        self.eng[e].wait_ge(self.sem[s], v)

    def op(self, e, fn, r=(), w=(), dma=False, inc=True):
        deps = []
        for k in r:
            if k in self.lw:
                deps.append(self.lw[k])
        for k in w:
            if k in self.lw:
                deps.append(self.lw[k])
            deps.extend(self.rd.get(k, {}).values())
        if dma:
            i = self.dma_i[e]
            self.dma_i[e] += 1
            s = ('dma', e, i % self.NS)
            if self.cnt[s] > 0:
                deps.append((s, self.cnt[s]))
            self.cnt[s] += 16
            tok = (s, self.cnt[s])
        else:
            s = e
            if inc:
                self.cnt[s] += 1
                tok = (s, self.cnt[s])
            else:
                tok = (s, self.cnt[s] + 1)
        for d in deps:
            if d[0] == 'pe' and e == 'pe' and not dma:
                continue
            self._wait(e, d)
        ins = fn(self.eng[e])
        if dma:
            ins.then_inc(self.sem[s], 16)
        elif inc:
            ins.then_inc(self.sem[s], 1)
        for k in w:
            self.lw[k] = tok
            self.rd[k] = {}
        for k in r:
            self.rd.setdefault(k, {})[tok[0]] = tok
        self.nops += 1
        return tok

    def barrier(self):
        for e in self.eng:
            for s, c in self.cnt.items():
                if c > 0:
                    self._wait(e, (s, c))
        self.lw.clear()
        self.rd.clear()


def make_consts():
    c = {}
    i = np.arange(128)
    c['ident'] = np.eye(128, dtype=np.float32)
    c['um'] = (-(1.0 / 16.0) * (i[:, None] <= i[None, :])).astype(np.float32)
    c['um2'] = (-(1.0 / 16.0) * (i[:, None] > i[None, :])).astype(np.float32)
    c['caus4'] = np.tile((i[:, None] <= i[None, :]).astype(np.float32), (1, 4))
    slopes = 2.0 ** (-8.0 * np.arange(1, 17) / 16.0)
    rel = -(slopes[None, :, None]) * (i[None, None, :] - i[:, None, None]).astype(np.float64)
    c['rel_mid'] = rel.astype(np.float32).reshape(128, 16 * 128)
    dt = np.arange(17)
    c['dconst'] = np.broadcast_to((-slopes[:, None] * 128.0 * dt[None, :])[None], (128, 16, 17)).astype(np.float32).reshape(128, 16 * 17).copy()
    n = np.arange(128)
    cb = slopes[None, :, None] * (16.0 * n[:, None, None] + 15.5) - slopes[None, :, None] * 128.0 * np.arange(16)[None, None, :]
    cb = slopes[None, :, None] * (15.0 * n[:, None, None] + 15.5) - slopes[None, :, None] * 128.0 * np.arange(16)[None, None, :]
    c['cmp_pb'] = cb.astype(np.float32).reshape(128, 256)
    c['cdiag'] = np.where(i[None, :] >= i[:, None], 0.0, NEGB).astype(np.float32)
    c['cfar'] = np.where(i[None, :] < i[:, None], 0.0, NEGB).astype(np.float32)
    tq = (np.arange(16)[:, None] * 128 + i[None, :])
    valid = (16 * n[:, None, None] + 31) <= tq[None]
    valid[127] = False
    c['cmp_mask'] = np.where(valid, 0.0, NEGB).astype(np.float32).reshape(128, 2048)
    bs = 16 * n
    ss = 64 * np.arange(32)
    ov = ((bs[:, None] < ss[None, :] + 64) & (bs[:, None] + 32 > ss[None, :])).astype(np.float32)
    ov[127] = 0
    c['overlap'] = ov
    t = np.arange(T)
    cur = t // 64
    jj = np.arange(32)
    forced = (jj[None, :] == 0) | (jj[None, :] == cur[:, None]) | (jj[None, :] == cur[:, None] - 1)
    validb = (64 * jj[None, :]) <= t[:, None]
    c['selc'] = np.where(validb, np.where(forced, 1e6, 0.0), -1e30).astype(np.float32)
    ex = np.zeros((32, 16, 128), np.float32)
    for kt in range(16):
        ex[2 * kt, kt, :64] = 1
        ex[2 * kt + 1, kt, 64:] = 1
    c['expand'] = ex.reshape(32, 2048)
    c['lstr'] = (i[:, None] < i[None, :]).astype(np.float32)

    def bsplit(a):
        import ml_dtypes
        a = np.asarray(a, np.float64)
        h1 = a.astype(np.float32).astype(ml_dtypes.bfloat16).astype(np.float64)
        h2 = (a - h1).astype(np.float32).astype(ml_dtypes.bfloat16).astype(np.float64)
        h3 = (a - h1 - h2).astype(np.float32).astype(ml_dtypes.bfloat16).astype(np.float64)
        return [h1.astype(np.float32), h2.astype(np.float32), h3.astype(np.float32)]
    jt = -slopes[None, :, None] * (128.0 * np.arange(16)[:, None, None] + i[None, None, :])
    jp = bsplit(jt)
    sp = bsplit(np.broadcast_to(slopes[None, :, None], (16, 16, 128)))
    tab = np.stack(jp + sp, 0)
    tab = tab.reshape(6, 16, 4, 4, 128).transpose(0, 2, 1, 3, 4).reshape(6, 4, 16 * 512)
    c['btab'] = np.ascontiguousarray(tab.reshape(6, 4 * 16 * 512))
    l6 = np.ones((6, 128), np.float32)
    l6[3:6, :] = i[None, :]
    c['l6'] = l6
    c['ebase'] = np.broadcast_to((np.arange(16) * 384 + 1.0e6)[None, :], (128, 16)).astype(np.float32).copy()
    return c


CONST_SHAPES = {k: v.shape for k, v in make_consts().items()}

WNAMES = ["w_in", "gla_w_a2", "gla_b_a", "gla_norm_g", "nsa_cmp_pe", "nsa_cmp_w1", "nsa_cmp_w2",
          "w_branch_gla", "w_branch_nsa", "w_out", "ln_mix_g", "ln_mix_b", "xa_wq", "xa_wkv", "xa_wo",
          "ln_xa_g", "ln_xa_b", "router_w", "router_b", "moe_w_in", "moe_w_down", "ln_ffn_g", "ln_ffn_b"]
WSHAPES = {
    "w_in": [2, 2048, 9792], "gla_w_a2": [2, 16, 512], "gla_b_a": [2, 512], "gla_norm_g": [2, 1024],
    "nsa_cmp_pe": [2, 2, 32, 64], "nsa_cmp_w1": [2, 2, 2048, 256], "nsa_cmp_w2": [2, 2, 256, 64],
    "w_branch_gla": [2, 1024, 2048], "w_branch_nsa": [2, 1024, 2048], "w_out": [2, 2048, 2048],
    "ln_mix_g": [2, 2048], "ln_mix_b": [2, 2048], "xa_wq": [2, 2048, 512], "xa_wkv": [2, 2048, 1024],
    "xa_wo": [2, 512, 2048], "ln_xa_g": [2, 2048], "ln_xa_b": [2, 2048], "router_w": [2048, 16],
    "router_b": [16], "moe_w_in": [2, 16, 2048, 3072], "moe_w_down": [2, 16, 1536, 2048],
    "ln_ffn_g": [2, 2048], "ln_ffn_b": [2, 2048],
}


def build(n_layers=DEPTH, stages=("mix", "xa", "moe"), debug=(), wshapes=None):
    WS = dict(WSHAPES)
    WS.update(wshapes or {})
    nc = bass.Bass("TRN2", target_bir_lowering=False)
    dr = {}
    dr["x"] = nc.dram_tensor("x", [T, D], F32, kind="ExternalInput").ap()
    dr["mem"] = nc.dram_tensor("mem", [256, D], F32, kind="ExternalInput").ap()
    for k in WNAMES:
        dr[k] = nc.dram_tensor(k, WS[k], F32, kind="ExternalInput").ap()
    cst = {k: nc.dram_tensor("c_" + k, list(s), F32, kind="ExternalInput").ap() for k, s in CONST_SHAPES.items()}
    out = nc.dram_tensor("out", [T, D], F32, kind="ExternalOutput").ap()
    dbg = {k: nc.dram_tensor("dbg_" + k, list(s), F32, kind="ExternalOutput").ap() for k, s in debug}
    xres = nc.dram_tensor("xres", [T, D], F32).ap()
    hT_d = nc.dram_tensor("hT_d", [NFM, T], BF16).ap()
    h_d = nc.dram_tensor("h_d", [T, NTM], BF16).ap()
    y_d = nc.dram_tensor("y_d", [T, D], F32).ap()

    with ExitStack() as es:
        block = es.enter_context(nc.Block())

        @block.gpsimd
        def _(_g):
            _emit(nc, dr, cst, out, dbg, xres, hT_d, h_d, y_d, n_layers, stages)
    return nc


def _emit(nc, dr, cst, out, dbg, xres, hT_d, h_d, y_d, n_layers, stages):
    es = ExitStack()
    S = Sch(nc, es)

    uid = [0]

    def sb(name, shape, dt, st=es):
        uid[0] += 1
        return st.enter_context(nc.sbuf_tensor(name + '_%d' % uid[0], shape, dt))

    def ps(name, shape, dt=F32, st=es):
        return st.enter_context(nc.psum_tensor(name, shape, dt))

    bufA = sb("bufA", [128, 16, T], BF16)
    bufB = None
    wt_i = [0]
    Wt = []

    def alloc_wt(st, n):
        del Wt[:]
        for i in range(n):
            Wt.append(sb("Wt%d" % i, [128, 16, 512], BF16, st))
        wt_i[0] = 0
    ident = sb("ident", [128, 128], F32)
    identb = sb("identb", [128, 128], BF16)
    PS = [ps("ps%d" % i, [128, 512]) for i in range(8)]
    S.op('sp', lambda e: e.dma_start(out=ident[:], in_=cst['ident'][:, :]), w=['ident'], dma=True)
    S.op('dve', lambda e: e.tensor_copy(out=identb[:], in_=ident[:]), r=['ident'], w=['identb'])

    ps_i = [0]

    def next_ps(lo=0, hi=4):
        i = lo + ps_i[0] % (hi - lo)
        ps_i[0] += 1
        return i

    def load_w(W2d, k0, KC, c0, nb):
        b = wt_i[0] % len(Wt)
        wt_i[0] += 1
        src = W2d[k0:k0 + KC * 128, c0:c0 + nb].rearrange("(kc p) n -> p kc n", p=128)
        S.op('pool', lambda e: e.dma_start(out=Wt[b][:, 0:KC, 0:nb], in_=src), w=[('Wt', b)], dma=True)
        return b

    def proj_tm(src, skey, KC, W2d, c0, nb, epi, k0=0, tts=range(NT)):
        b = load_w(W2d, k0, KC, c0, nb)
        for tt in tts:
            pi = next_ps()
            for kc in range(KC):
                S.op('pe', lambda e, kc=kc, tt=tt, pi=pi: e.matmul(PS[pi][:, 0:nb], lhsT=src[:, kc, tt * 128:(tt + 1) * 128],
                                                                    rhs=Wt[b][:, kc, 0:nb], start=(kc == 0), stop=(kc == KC - 1)),
                     r=[('Wt', b), skey], w=[('ps', pi)], inc=(kc == KC - 1))
            epi(tt, pi)

    def proj_fm(src, skey, KC, W2d, c0, nb, M, epi, k0=0):
        b = load_w(W2d, k0, KC, c0, nb)
        for m0 in range(0, nb, M):
            mm = min(M, nb - m0)
            for tb in range(4):
                pi = next_ps()
                for kc in range(KC):
                    S.op('pe', lambda e, kc=kc, tb=tb, pi=pi, m0=m0, mm=mm: e.matmul(
                        PS[pi][0:mm, :], lhsT=Wt[b][:, kc, m0:m0 + mm], rhs=src[:, kc, tb * 512:(tb + 1) * 512],
                        start=(kc == 0), stop=(kc == KC - 1)),
                         r=[('Wt', b), skey], w=[('ps', pi)], inc=(kc == KC - 1))
                epi(c0 + m0, mm, tb, pi)

    def ln_and_store(l, st, xt_tile, key, tt, g_bc, b_bc, dst_dram, small):
        stats, mv, rstd = small
        for c in range(4):
            S.op('dve', lambda e, c=c: e.bn_stats(out=stats[:, c, :], in_=xt_tile[:, c * 512:(c + 1) * 512]), r=[key], w=['stats'])
        S.op('dve', lambda e: e.bn_aggr(out=mv[:], in_=stats[:].rearrange("p a b -> p (a b)")), r=['stats'], w=['mv'])
        S.op('act', lambda e: e.activation(out=rstd[:], in_=mv[:, 1:2], func=AF.Sqrt, bias=LN_EPS, scale=1.0), r=['mv'], w=['rstd'])
        S.op('dve', lambda e: e.reciprocal(out=rstd[:], in_=rstd[:]), r=['rstd'], w=['rstd'])
        S.op('dve', lambda e: e.tensor_scalar(out=xt_tile[:], in0=xt_tile[:], scalar1=mv[:, 0:1], scalar2=rstd[:, 0:1],
                                              op0=ALU.subtract, op1=ALU.mult), r=[key, 'mv', 'rstd'], w=[key])
        S.op('dve', lambda e: e.tensor_tensor(out=xt_tile[:], in0=xt_tile[:], in1=g_bc[:], op=ALU.mult), r=[key, 'lng'], w=[key])
        S.op('dve', lambda e: e.tensor_tensor(out=xt_tile[:], in0=xt_tile[:], in1=b_bc[:], op=ALU.add), r=[key, 'lnb'], w=[key])
        S.op('sp', lambda e: e.dma_start(out=dst_dram[tt * 128:(tt + 1) * 128, :], in_=xt_tile[:]), r=[key], w=[('xres', tt)], dma=True)
        transpose_to(xt_tile, key, 16, bufA, 'bufA', tt, F32)

    def transpose_to(tile, key, nchunks, dst, dkey, tt, dt, c_off=0, banks=(4, 8)):
        idm = ident if dt == F32 else identb
        for c4 in range(0, nchunks, 4):
            pi = next_ps(*banks)
            pst = PS[pi] if dt == F32 else PSB[pi - 4]
            for c in range(c4, min(c4 + 4, nchunks)):
                S.op('pe', lambda e, c=c, c4=c4, pst=pst: e.transpose(out=pst[:, (c - c4) * 128:(c - c4 + 1) * 128],
                                                                   in_=tile[:, c * 128:(c + 1) * 128], identity=idm[:]),
                     r=[key, 'ident', 'identb'], w=[('ps', pi)])
            n = min(4, nchunks - c4)
            S.op('act', lambda e, c4=c4, n=n, pst=pst: e.activation(
                out=dst[:, c_off + c4:c_off + c4 + n, tt * 128:(tt + 1) * 128],
                in_=pst[:, 0:n * 128].rearrange("p (a b) -> p a b", a=n), func=AF.Copy), r=[('ps', pi)], w=[dkey])

    PSB = [PS[i][:].bitcast(BF16)[:, 0:512] for i in range(4, 8)]

    with ExitStack() as st:
        xt = [sb("xt%d" % i, [128, D], F32, st) for i in range(2)]
        for tt in range(NT):
            b = tt % 2
            S.op('sp', lambda e, b=b, tt=tt: e.dma_start(out=xt[b][:], in_=dr["x"][tt * 128:(tt + 1) * 128, :]), w=[('xt', b)], dma=True)
            S.op('sp', lambda e, b=b, tt=tt: e.dma_start(out=xres[tt * 128:(tt + 1) * 128, :], in_=xt[b][:]), r=[('xt', b)], w=[('xres', tt)], dma=True)
            transpose_to(xt[b], ('xt', b), 16, bufA, 'bufA', tt, F32)
        S.barrier()

    _H['alloc_wt'] = alloc_wt
    for l in range(n_layers):
        _mixer(nc, S, sb, PS, PSB, dr, cst, l, bufA, bufB, hT_d, h_d, xres, proj_tm, proj_fm, transpose_to, ln_and_store, next_ps,
               ident, identb, dbg)
        if "mixonly" in stages:
            break
        with ExitStack() as lq:
            qx = sb("qxT", [128, 4, T], BF16, lq)
            with ExitStack() as lb:
                bB = _merge(nc, S, sb, PS, PSB, dr, cst, l, bufA, hT_d, xres, proj_fm, transpose_to, next_ps, lb)

                def epi_q(col, mm, tb, pi):
                    S.op('act', lambda e: e.activation(out=qx[:, col // 128, tb * 512:(tb + 1) * 512], in_=PS[pi][:, :], func=AF.Copy, scale=float(128 ** -0.5)),
                         r=[('ps', pi)], w=['qxT'])
                with ExitStack() as lw:
                    alloc_wt(lw, 2)
                    proj_fm(bB, 'bufBall', 16, dr["xa_wq"][l], 0, 512, 128, epi_q)
                    S.barrier()
            _xattn(nc, S, sb, PS, PSB, dr, cst, l, bufA, qx, xres, proj_tm, proj_fm, transpose_to, ln_and_store, next_ps, ident, identb, dbg, load_w, Wt)
        if 'moe' not in SKIP:
          _moe(nc, S, sb, PS, PSB, dr, cst, l, bufA, None, xres, y_d, out if l == n_layers - 1 else xres, proj_tm, proj_fm,
               transpose_to, ln_and_store, next_ps, ident, identb, dbg, load_w, Wt)
    S.barrier()
    es.close()


def _mixer(nc, S, sb, PS, PSB, dr, cst, l, bufA, bufB, hT_d, h_d, xres, proj_tm, proj_fm, transpose_to, ln_and_store, next_ps,
           ident, identb, dbg):
    W = dr["w_in"][l]
    aT_d = nc.dram_tensor("aT_d%d" % l, [16, T], F32).ap()
    with ExitStack() as st:
        _H['alloc_wt'](st, 3)
        stg = [sb("stg%d" % i, [128, 512], BF16, st) for i in range(4)]
        stgf = sb("stgf", [16, 512], F32, st)
        si = [0]

        def epi_fm(rbase, cbase, func, scale=1.0):
            def f(col, mm, tb, pi):
                i = si[0] % 4
                si[0] += 1
                S.op('act', lambda e: e.activation(out=stg[i][0:mm, :], in_=PS[pi][0:mm, :], func=func, scale=scale), r=[('ps', pi)], w=[('stg', i)])
                r0 = rbase + col - cbase
                S.op('sp', lambda e: e.dma_start(out=hT_d[r0:r0 + mm, tb * 512:(tb + 1) * 512], in_=stg[i][0:mm, :]), r=[('stg', i)],
                     w=[('hT', r0 // 64, tb)] + ([('hT', r0 // 64 + 1, tb)] if mm == 128 else []), dma=True)
            return f

        def epi_ga(col, mm, tb, pi):
            S.op('act', lambda e: e.activation(out=stgf[0:16, :], in_=PS[pi][0:16, :], func=AF.Copy), r=[('ps', pi)], w=['stgf'])
            S.op('sp', lambda e: e.dma_start(out=aT_d[:, tb * 512:(tb + 1) * 512], in_=stgf[0:16, :]), r=['stgf'], w=[('aT', tb)], dma=True)

        def epi_tm(tcbase, nb, func):
            def f(tt, pi):
                i = si[0] % 4
                si[0] += 1
                S.op('act', lambda e: e.activation(out=stg[i][:, 0:nb], in_=PS[pi][:, 0:nb], func=func), r=[('ps', pi)], w=[('stg', i)])
                S.op('sp', lambda e: e.dma_start(out=h_d[tt * 128:(tt + 1) * 128, tcbase:tcbase + nb], in_=stg[i][:, 0:nb]), r=[('stg', i)],
                     w=[('h', tt, tcbase)], dma=True)
            return f

        A = (bufA, 'bufA', 16, W)
        proj_fm(*A, C_GQ, 512, 128, epi_fm(R_GQ, C_GQ, AF.Copy))
        proj_fm(*A, C_GK, 512, 128, epi_fm(R_GK, C_GK, AF.Copy))
        proj_fm(*A, C_GA, 16, 16, epi_ga)
        proj_tm(*A, C_GK, 512, epi_tm(TC_GK, 512, AF.Copy))
        for j in range(2):
            proj_tm(*A, C_GV + 512 * j, 512, epi_tm(TC_GV + 512 * j, 512, AF.Copy))
            proj_tm(*A, C_GR + 512 * j, 512, epi_tm(TC_GR + 512 * j, 512, AF.Silu))
            proj_fm(*A, C_NQ + 512 * j, 512, 64, epi_fm(R_NQ + 512 * j, C_NQ + 512 * j, AF.Copy, 0.125))
        for (cc, rr) in ((0, R_KC), (256, R_VC), (512, R_KS), (1024, R_KW)):
            proj_fm(*A, C_NKV + cc, 256, 64, epi_fm(rr, C_NKV + cc, AF.Copy))
        proj_tm(*A, C_NKV + 768, 256, epi_tm(TC_VS, 256, AF.Copy))
        proj_tm(*A, C_NKV + 1280, 256, epi_tm(TC_VW, 256, AF.Copy))
        proj_tm(*A, C_NG, 48, epi_tm(TC_NG, 48, AF.Sigmoid))
        for j in range(8):
            proj_fm(*A, C_MG + 512 * j, 512, 128, epi_fm(R_MG + 512 * j, C_MG + 512 * j, AF.Sigmoid))
        S.barrier()

    if 'gla' not in SKIP:
        _gla(nc, S, sb, PS, PSB, dr, cst, l, bufA, hT_d, h_d, aT_d, transpose_to, ident, identb, dbg)
    if 'nsa' not in SKIP:
        _nsa(nc, S, sb, PS, PSB, dr, cst, l, bufA, hT_d, h_d, transpose_to, next_ps, ident, identb, dbg)


def _gla(nc, S, sb, PS, PSB, dr, cst, l, bufA, hT_d, h_d, aT_d, transpose_to, ident, identb, dbg):
    with ExitStack() as st:
        um = sb("um", [128, 128], F32, st)
        um2 = sb("um2", [128, 128], F32, st)
        caus4 = sb("caus4", [128, 512], F32, st)
        gnbc = sb("gnbc", [128, 1024], F32, st)
        wa2 = sb("wa2", [32, 512], F32, st)
        aT = sb("aT", [32, T], F32, st)
        S.op('sp', lambda e: e.dma_start(out=um[:], in_=cst['um'][:, :]), w=['um'], dma=True)
        S.op('sp', lambda e: e.dma_start(out=um2[:], in_=cst['um2'][:, :]), w=['um2'], dma=True)
        S.op('sp', lambda e: e.dma_start(out=caus4[:], in_=cst['caus4'][:, :]), w=['caus4'], dma=True)
        S.op('sp', lambda e: e.dma_start(out=gnbc[:], in_=dr['gla_norm_g'][l, :].partition_broadcast(128)), w=['gnbc'], dma=True)
        S.op('sp', lambda e: e.dma_start(out=wa2[0:16, :], in_=dr['gla_w_a2'][l]), w=['wa2a'], dma=True)
        S.op('sp', lambda e: e.dma_start(out=wa2[16:17, :], in_=dr['gla_b_a'][l:l + 1, :]), w=['wa2b'], dma=True)
        S.op('dve', lambda e: e.memset(aT[:], 1.0), w=['aT'])
        S.op('sp', lambda e: e.dma_start(out=aT[0:16, :], in_=aT_d[:, :]), w=['aT'], dma=True)
        S32 = sb("S32", [128, 4, 256], F32, st)
        Sb = sb("Sb", [128, 4, 256], BF16, st)
        S.op('dve', lambda e: e.memset(S32[:], 0.0), w=['S32'])
        S.op('dve', lambda e: e.memset(Sb[:], 0.0), w=['Sb'])
        qT = [sb("gqT%d" % i, [128, 4, 128], BF16, st) for i in range(2)]
        kT = [sb("gkT%d" % i, [128, 4, 128], BF16, st) for i in range(2)]
        kk = [sb("gk%d" % i, [128, 512], BF16, st) for i in range(2)]
        vv = [sb("gv%d" % i, [128, 1024], BF16, st) for i in range(2)]
        rs = [sb("grs%d" % i, [128, 1024], BF16, st) for i in range(2)]
        le = sb("le", [128, 512], F32, st)
        ll = sb("ll", [128, 512], F32, st)
        Eq = sb("Eq", [128, 512], F32, st)
        Ek = sb("Ek", [128, 512], F32, st)
        Ekh = sb("Ekh", [128, 512], F32, st)
        qs = sb("qs", [128, 512], BF16, st)
        ks = sb("ks", [128, 512], BF16, st)
        kh = sb("kh", [128, 512], BF16, st)
        att = sb("att", [128, 512], BF16, st)
        og = sb("og", [128, 1024], F32, st)
        ogb = sb("ogb", [128, 1024], BF16, st)
        grs = sb("grsf", [128, 1024], F32, st)
        stats = sb("gstats", [128, 4, 6], F32, st)
        mv = sb("gmv", [128, 4, 2], F32, st)
        rstd = sb("grstd", [128, 4], F32, st)
        for c in range(NT):
            b = c % 2
            cs = slice(c * 128, (c + 1) * 128)
            S.op('sp', lambda e: e.dma_start(out=qT[b][:], in_=hT_d[R_GQ:R_GQ + 512, cs].rearrange("(h d) t -> d h t", d=128)), w=[('qT', b)], dma=True)
            S.op('sp', lambda e: e.dma_start(out=kT[b][:], in_=hT_d[R_GK:R_GK + 512, cs].rearrange("(h d) t -> d h t", d=128)), w=[('kT', b)], dma=True)
            S.op('sp', lambda e: e.dma_start(out=kk[b][:], in_=h_d[cs, TC_GK:TC_GK + 512]), w=[('kk', b)], dma=True)
            S.op('sp', lambda e: e.dma_start(out=vv[b][:], in_=h_d[cs, TC_GV:TC_GV + 1024]), w=[('vv', b)], dma=True)
            S.op('sp', lambda e: e.dma_start(out=rs[b][:], in_=h_d[cs, TC_GR:TC_GR + 1024]), w=[('rs', b)], dma=True)
            S.op('pe', lambda e: e.matmul(PS[0][:, :], lhsT=aT[0:17, cs], rhs=wa2[0:17, :], start=True, stop=True), r=['aT', 'wa2a', 'wa2b'], w=[('ps', 0)])
            S.op('act', lambda e: e.activation(out=le[:], in_=PS[0][:, :], func=AF.Exp, scale=-1.0), r=[('ps', 0)], w=['le'])
            S.op('act', lambda e: e.activation(out=ll[:], in_=le[:], func=AF.Ln, bias=1.0, scale=1.0), r=['le'], w=['ll'])
            for h in range(4):
                S.op('pe', lambda e, h=h: e.matmul(PS[1][:, h * 128:(h + 1) * 128], lhsT=ll[:, h * 128:(h + 1) * 128], rhs=um[:], start=True, stop=True),
                     r=['ll', 'um'], w=[('ps', 1)], inc=(h == 3))
            S.op('pe', lambda e: e.matmul(PS[2][:, :], lhsT=um2[:], rhs=ll[:], start=True, stop=True), r=['ll', 'um2'], w=[('ps', 2)])
            S.op('act', lambda e: e.activation(out=Eq[:], in_=PS[1][:, :], func=AF.Exp), r=[('ps', 1)], w=['Eq'])
            S.op('act', lambda e: e.activation(out=Ek[:], in_=PS[1][:, :], func=AF.Exp, scale=-1.0), r=[('ps', 1)], w=['Ek'])
            S.op('act', lambda e: e.activation(out=Ekh[:], in_=PS[2][:, :], func=AF.Exp), r=[('ps', 2)], w=['Ekh'])
            S.op('dve', lambda e: e.scalar_tensor_tensor(out=qs[:], in0=qT[b][:].rearrange("p h t -> p (h t)"), scalar=float(128 ** -0.5), in1=Eq[:],
                                                         op0=ALU.mult, op1=ALU.mult), r=[('qT', b), 'Eq'], w=['qs'])
            S.op('dve', lambda e: e.tensor_tensor(out=ks[:], in0=kT[b][:].rearrange("p h t -> p (h t)"), in1=Ek[:], op=ALU.mult), r=[('kT', b), 'Ek'], w=['ks'])
            S.op('dve', lambda e: e.tensor_tensor(out=kh[:], in0=kk[b][:], in1=Ekh[:], op=ALU.mult), r=[('kk', b), 'Ekh'], w=['kh'])
            for h in range(4):
                hs = slice(h * 128, (h + 1) * 128)
                S.op('pe', lambda e, hs=hs: e.matmul(PS[3][:, hs], lhsT=ks[:, hs], rhs=qs[:, hs], start=True, stop=True), r=['ks', 'qs'], w=[('ps', 3)], inc=(h == 3))
            S.op('dve', lambda e: e.tensor_tensor(out=att[:], in0=PS[3][:, :], in1=caus4[:], op=ALU.mult), r=[('ps', 3), 'caus4'], w=['att'])
            for h in range(4):
                hs = slice(h * 128, (h + 1) * 128)
                pb = 4 + h // 2
                po = slice((h % 2) * 256, (h % 2) * 256 + 256)
                S.op('pe', lambda e, hs=hs, pb=pb, po=po, h=h: e.matmul(PS[pb][:, po], lhsT=att[:, hs], rhs=vv[b][:, h * 256:(h + 1) * 256], start=True, stop=False),
                     r=['att', ('vv', b)], w=[('ps', pb)], inc=False)
                S.op('pe', lambda e, hs=hs, pb=pb, po=po, h=h: e.matmul(PS[pb][:, po], lhsT=qs[:, hs], rhs=Sb[:, h, :], start=False, stop=True),
                     r=['qs', 'Sb'], w=[('ps', pb)], inc=True)
            for h in range(4):
                hs = slice(h * 128, (h + 1) * 128)
                pb = 6 + h // 2
                po = slice((h % 2) * 256, (h % 2) * 256 + 256)
                S.op('pe', lambda e, hs=hs, pb=pb, po=po, h=h: e.matmul(PS[pb][:, po], lhsT=kh[:, hs], rhs=vv[b][:, h * 256:(h + 1) * 256], start=True, stop=True),
                     r=['kh', ('vv', b)], w=[('ps', pb)])
                S.op('dve', lambda e, pb=pb, po=po, h=h: e.scalar_tensor_tensor(out=S32[:, h, :], in0=S32[:, h, :], scalar=Eq[:, h * 128 + 127:h * 128 + 128],
                                                                              in1=PS[pb][:, po], op0=ALU.mult, op1=ALU.add), r=[('ps', pb), 'Eq', 'S32'], w=['S32'])
            S.op('act', lambda e: e.activation(out=Sb[:], in_=S32[:], func=AF.Copy), r=['S32'], w=['Sb'])
            for h in range(4):
                pb = 4 + h // 2
                po = slice((h % 2) * 256, (h % 2) * 256 + 256)
                S.op('dve', lambda e, pb=pb, po=po, h=h: e.bn_stats(out=stats[:, h, :], in_=PS[pb][:, po]), r=[('ps', pb)], w=['gstats'])
                S.op('dve', lambda e, h=h: e.bn_aggr(out=mv[:, h, :], in_=stats[:, h, :]), r=['gstats'], w=['gmv'])
            S.op('act', lambda e: e.activation(out=rstd[:], in_=mv[:, :, 1], func=AF.Sqrt, bias=LN_EPS, scale=1.0), r=['gmv'], w=['grstd'])
            S.op('dve', lambda e: e.reciprocal(out=rstd[:], in_=rstd[:]), r=['grstd'], w=['grstd'])
            for h in range(4):
                pb = 4 + h // 2
                po = slice((h % 2) * 256, (h % 2) * 256 + 256)
                S.op('dve', lambda e, pb=pb, po=po, h=h: e.tensor_scalar(out=og[:, h * 256:(h + 1) * 256], in0=PS[pb][:, po], scalar1=mv[:, h, 0:1], scalar2=rstd[:, h:h + 1],
                                                                       op0=ALU.subtract, op1=ALU.mult), r=[('ps', pb), 'gmv', 'grstd'], w=['og'])
            S.op('dve', lambda e: e.tensor_tensor(out=grs[:], in0=rs[b][:], in1=gnbc[:], op=ALU.mult), r=[('rs', b), 'gnbc'], w=['grsf'])
            S.op('dve', lambda e: e.tensor_tensor(out=ogb[:], in0=og[:], in1=grs[:], op=ALU.mult), r=['og', 'grsf'], w=['ogb'])
            if 'o_gla' in dbg:
                S.op('dve', lambda e: e.tensor_tensor(out=og[:], in0=og[:], in1=grs[:], op=ALU.mult), r=['og', 'grsf'], w=['og'])
                S.op('sp', lambda e: e.dma_start(out=dbg['o_gla'][cs, :], in_=og[:]), r=['og'], w=[('dbg', c)], dma=True)
            transpose_to(ogb, 'ogb', 8, bufA, 'bufA', c, BF16)
        S.barrier()


def _resid_ln_phase(nc, S, sb, PS, st, l, Wres, KC, src, skeyf, alpha_src, gname, bname, dr, dst_dram, dstT, dstkeyf, transpose_to, ln_keys, nbuf=1):
    xt = [sb("rl_xt%d" % i, [128, D], F32, st) for i in range(nbuf)]
    g_bc = sb("rl_g", [128, D], F32, st)
    b_bc = sb("rl_b", [128, D], F32, st)
    stats = sb("rl_stats", [128, 4, 6], F32, st)
    mv = sb("rl_mv", [128, 2], F32, st)
    rstd = sb("rl_rstd", [128, 1], F32, st)
    S.op('sp', lambda e: e.dma_start(out=g_bc[:], in_=dr[gname][l, :].partition_broadcast(128)), w=['lng'], dma=True)
    S.op('sp', lambda e: e.dma_start(out=b_bc[:], in_=dr[bname][l, :].partition_broadcast(128)), w=['lnb'], dma=True)
    for tt in range(NT):
        b = tt % nbuf
        key = ('rlxt', b)
        S.op('sp', lambda e: e.dma_start(out=xt[b][:], in_=alpha_src[tt * 128:(tt + 1) * 128, :]), r=[('xres', tt)], w=[key], dma=True)
        for cb in range(4):
            if Wres is not None:
                for kc in range(KC):
                    S.op('pe', lambda e, kc=kc, cb=cb: e.matmul(PS[cb][:, :], lhsT=src[:, kc, tt * 128:(tt + 1) * 128], rhs=Wres[:, kc, cb * 512:(cb + 1) * 512],
                                                                start=(kc == 0), stop=(kc == KC - 1)), r=[skeyf(tt), ('Wres', kc, cb)], w=[('ps', cb)], inc=(kc == KC - 1))
                S.op('dve', lambda e, cb=cb: e.scalar_tensor_tensor(out=xt[b][:, cb * 512:(cb + 1) * 512], in0=xt[b][:, cb * 512:(cb + 1) * 512], scalar=DN_ALPHA,
                                                                   in1=PS[cb][:, :], op0=ALU.mult, op1=ALU.add), r=[('ps', cb), key], w=[key])
            else:
                if cb == 0:
                    ytile, ykey = src(tt)
                S.op('dve', lambda e, cb=cb: e.scalar_tensor_tensor(out=xt[b][:, cb * 512:(cb + 1) * 512], in0=xt[b][:, cb * 512:(cb + 1) * 512], scalar=DN_ALPHA,
                                                                   in1=ytile[:, cb * 512:(cb + 1) * 512], op0=ALU.mult, op1=ALU.add), r=[ykey, key], w=[key])
        for c in range(4):
            S.op('dve', lambda e, c=c: e.bn_stats(out=stats[:, c, :], in_=xt[b][:, c * 512:(c + 1) * 512]), r=[key], w=['stats'])
        S.op('dve', lambda e: e.bn_aggr(out=mv[:], in_=stats[:].rearrange("p a b -> p (a b)")), r=['stats'], w=['mv'])
        S.op('act', lambda e: e.activation(out=rstd[:], in_=mv[:, 1:2], func=AF.Sqrt, bias=LN_EPS, scale=1.0), r=['mv'], w=['rstd'])
        S.op('dve', lambda e: e.reciprocal(out=rstd[:], in_=rstd[:]), r=['rstd'], w=['rstd'])
        S.op('dve', lambda e: e.scalar_tensor_tensor(out=xt[b][:], in0=xt[b][:], scalar=mv[:, 0:1], in1=g_bc[:], op0=ALU.subtract, op1=ALU.mult),
             r=[key, 'mv', 'lng'], w=[key])
        S.op('dve', lambda e: e.scalar_tensor_tensor(out=xt[b][:], in0=xt[b][:], scalar=rstd[:, 0:1], in1=b_bc[:], op0=ALU.mult, op1=ALU.add),
             r=[key, 'rstd', 'lnb'], w=[key])
        S.op('sp', lambda e: e.dma_start(out=dst_dram[tt * 128:(tt + 1) * 128, :], in_=xt[b][:]), r=[key], w=[('xres', tt)], dma=True)
        transpose_to(xt[b], key, 16, dstT, dstkeyf(tt), tt, F32)


def _load_resident(S, dst, dkey, W2d, KC):
    for kc in range(KC):
        for cb in range(4):
            S.op('pool', lambda e, kc=kc, cb=cb: e.dma_start(out=dst[:, kc, cb * 512:(cb + 1) * 512], in_=W2d[kc * 128:(kc + 1) * 128, cb * 512:(cb + 1) * 512]),
                 w=[(dkey, kc, cb)], dma=True)


def _merge(nc, S, sb, PS, PSB, dr, cst, l, bufA, hT_d, xres, proj_fm, transpose_to, next_ps, st):
    bufB = sb("bufB", [128, 16, T], BF16, st)
    with ExitStack() as s2:
        _H['alloc_wt'](s2, 2)
        sg = [sb("sg%d" % i, [128, 512], BF16, s2) for i in range(2)]
        tmp = sb("mtmp", [128, 512], F32, s2)
        gi = [0]

        def epi(branch):
            def f(col, mm, tb, pi):
                i = gi[0] % 2
                gi[0] += 1
                r0 = R_MG + branch * 2048 + col
                S.op('sp', lambda e: e.dma_start(out=sg[i][:], in_=hT_d[r0:r0 + 128, tb * 512:(tb + 1) * 512]), w=[('sg', i)], dma=True)
                dsl = bufB[:, col // 128, tb * 512:(tb + 1) * 512]
                if branch == 0:
                    S.op('dve', lambda e: e.tensor_tensor(out=dsl, in0=PS[pi][:, :], in1=sg[i][:], op=ALU.mult), r=[('ps', pi), ('sg', i)], w=['bufB'])
                else:
                    S.op('dve', lambda e: e.tensor_tensor(out=tmp[:], in0=PS[pi][:, :], in1=sg[i][:], op=ALU.mult), r=[('ps', pi), ('sg', i)], w=['mtmp'])
                    S.op('dve', lambda e: e.tensor_tensor(out=dsl, in0=dsl, in1=tmp[:], op=ALU.add), r=['mtmp', 'bufB'], w=['bufB'])
            return f
        for j in range(4):
            proj_fm(bufA[:, 0:8, :], 'bufA', 8, dr["w_branch_gla"][l], 512 * j, 512, 128, epi(0))
        for j in range(4):
            proj_fm(bufA[:, 8:16, :], 'bufA', 8, dr["w_branch_nsa"][l], 512 * j, 512, 128, epi(1))
        S.barrier()
    with ExitStack() as s2:
        _load_resident(S, bufA, 'Wres', dr["w_out"][l], 16)
        _resid_ln_phase(nc, S, sb, PS, s2, l, bufA, 16, bufB, lambda tt: ('bufB', tt), xres, "ln_mix_g", "ln_mix_b", dr, xres, bufB,
                        lambda tt: ('bufB', tt), transpose_to, None, nbuf=2)
        S.barrier()
    return bufB


def _xattn(nc, S, sb, PS, PSB, dr, cst, l, bufA, bufB, xres, proj_tm, proj_fm, transpose_to, ln_and_store, next_ps, ident, identb, dbg, load_w, Wt):
    with ExitStack() as st:
        _H['alloc_wt'](st, 2)
        memT = sb("memT", [128, 16, 256], BF16, st)
        mt_ = sb("memtile", [128, D], F32, st)
        kTs = sb("xkT", [128, 4, 256], BF16, st)
        vs = sb("xv", [128, 2, 4, 132], BF16, st)
        qx = bufB
        oT = sb("xoT", [128, 4, T], BF16, st)
        woT = sb("woT", [128, 4, D], BF16, st)
        eT = [sb("xeT%d" % i, [128, 512], BF16, st) for i in range(2)]
        oxa = sb("oxa", [128, 512], BF16, st)
        rden = sb("xrden", [128, 4], F32, st)
        for m in range(2):
            S.op('sp', lambda e: e.dma_start(out=mt_[:], in_=dr["mem"][m * 128:(m + 1) * 128, :]), w=['memtile'], dma=True)
            transpose_to(mt_, 'memtile', 16, memT, 'memT', m, F32)
        S.op('dve', lambda e: e.memset(vs[:], 1.0), w=['xv'])
        Wkv = dr["xa_wkv"][l]
        b = load_w(Wkv, 0, 16, 0, 512)
        for h in range(4):
            pi = next_ps()
            for kc in range(16):
                S.op('pe', lambda e, kc=kc: e.matmul(PS[pi][:, 0:256], lhsT=Wt[b][:, kc, h * 128:(h + 1) * 128], rhs=memT[:, kc, :], start=(kc == 0), stop=(kc == 15)),
                     r=[('Wt', b), 'memT'], w=[('ps', pi)], inc=(kc == 15))
            S.op('act', lambda e: e.activation(out=kTs[:, h, :], in_=PS[pi][:, 0:256], func=AF.Copy), r=[('ps', pi)], w=['xkT'])
        b = load_w(Wkv, 0, 16, 512, 512)
        for m in range(2):
            pi = next_ps()
            for kc in range(16):
                S.op('pe', lambda e, kc=kc: e.matmul(PS[pi][:, :], lhsT=memT[:, kc, m * 128:(m + 1) * 128], rhs=Wt[b][:, kc, :], start=(kc == 0), stop=(kc == 15)),
                     r=[('Wt', b), 'memT'], w=[('ps', pi)], inc=(kc == 15))
            S.op('act', lambda e: e.activation(out=vs[:, m, :, 0:128], in_=PS[pi][:, :].rearrange("p (h d) -> p h d", h=4), func=AF.Copy), r=[('ps', pi)], w=['xv'])

        for tt in range(NT):
            ts = slice(tt * 128, (tt + 1) * 128)
            for m in range(2):
                pi = next_ps()
                for h in range(4):
                    S.op('pe', lambda e, h=h: e.matmul(PS[pi][:, h * 128:(h + 1) * 128], lhsT=kTs[:, h, m * 128:(m + 1) * 128], rhs=qx[:, h, ts], start=True, stop=True),
                         r=['xkT', 'qxT'], w=[('ps', pi)], inc=(h == 3))
                S.op('act', lambda e: e.activation(out=eT[m][:], in_=PS[pi][:, :], func=AF.Exp), r=[('ps', pi)], w=[('xeT', m)])
            for h in range(4):
                pb = 4 + h // 2
                po = (h % 2) * 132
                for m in range(2):
                    S.op('pe', lambda e, m=m: e.matmul(PS[pb][:, po:po + 129], lhsT=eT[m][:, h * 128:(h + 1) * 128], rhs=vs[:, m, h, 0:129], start=(m == 0), stop=(m == 1)),
                         r=[('xeT', m), 'xv'], w=[('ps', pb)], inc=(m == 1))
                S.op('dve', lambda e: e.reciprocal(out=rden[:, h:h + 1], in_=PS[pb][:, po + 128:po + 129]), r=[('ps', pb)], w=['xrden'])
                S.op('dve', lambda e: e.tensor_scalar(out=oxa[:, h * 128:(h + 1) * 128], in0=PS[pb][:, po:po + 128], scalar1=rden[:, h:h + 1], scalar2=None, op0=ALU.mult),
                     r=[('ps', pb), 'xrden'], w=['oxa'])
            transpose_to(oxa, 'oxa', 4, oT, 'xoT', tt, BF16)
        S.barrier()
        for kc in range(4):
            for cb in range(4):
                S.op('pool', lambda e: e.dma_start(out=woT[:, kc, cb * 512:(cb + 1) * 512], in_=dr["xa_wo"][l][kc * 128:(kc + 1) * 128, cb * 512:(cb + 1) * 512]),
                     w=[('Wres', kc, cb)], dma=True)
        _resid_ln_phase(nc, S, sb, PS, st, l, woT, 4, oT, lambda tt: 'xoT', xres, "ln_xa_g", "ln_xa_b", dr, xres, bufA, lambda tt: 'bufA', transpose_to, None, nbuf=2)
        S.barrier()


SKIP = set()
_H = {}
MOE_C = 384
MOE_BIG = 1.0e6


def _moe(nc, S, sb, PS, PSB, dr, cst, l, bufA, bufB, xres, y_d, dst, proj_tm, proj_fm, transpose_to, ln_and_store, next_ps, ident, identb, dbg, load_w, Wt):
    C = MOE_C
    CT = C // 128
    NSL = 16 * C
    xbuf = nc.dram_tensor("moe_xbuf%d" % l, [NSL, D], BF16).ap()
    ybuf = nc.dram_tensor("moe_ybuf%d" % l, [NSL, D], F32).ap()
    I32 = mybir.dt.int32
    breg = nc.gpsimd.to_reg(NSL - 1)
    with ExitStack() as st:
        gateA = sb("gateA", [128, NT], F32, st)
        slotAi = sb("slotAi", [128, NT], I32, st)
        slotBi = sb("slotBi", [128, NT], I32, st)
        with ExitStack() as s2:
            rw = sb("rw", [128, 16, 16], BF16, s2)
            rb = sb("rb", [128, 16], F32, s2)
            lg = sb("lg", [128, 16], F32, s2)
            lb = sb("lb", [128, 4, 4], F32, s2)
            eq = sb("eq", [128, 4, 4], F32, s2)
            lb2 = sb("lb2", [128, 4, 4], F32, s2)
            m1 = sb("m1", [128, 4], F32, s2)
            m2 = sb("m2", [128, 4], F32, s2)
            gs = sb("gs", [128, 4], F32, s2)
            gm = sb("gm", [128, 1], F32, s2)
            ex = sb("ex", [128, 16], F32, s2)
            den = sb("den", [128, 1], F32, s2)
            gate = sb("gate", [128, 16], F32, s2)
            maskall = sb("maskall", [128, NT, 16], BF16, s2)
            lstr = sb("lstr", [128, 128], BF16, s2)
            onesb = sb("onesb", [128, 128], BF16, s2)
            ebase = sb("ebase", [128, 16], F32, s2)
            smat = sb("smat", [128, 16], F32, s2)
            tA = sb("tA", [128, 16], F32, s2)
            tB = sb("tB", [128, 16], F32, s2)
            sA = sb("sA", [128, 1], F32, s2)
            sB = sb("sB", [128, 1], F32, s2)
            zt = sb("zt", [128, D], BF16, s2)
            xtf = [sb("mxtf%d" % i, [128, D], F32, s2) for i in range(2)]
            xtb = [sb("mxtb%d" % i, [128, D], BF16, s2) for i in range(2)]
            S.op('pool', lambda e: e.dma_start(out=rw[:], in_=dr["router_w"].rearrange("(kc p) e -> p kc e", p=128)), w=['rw'], dma=True)
            S.op('sp', lambda e: e.dma_start(out=rb[:], in_=dr["router_b"].partition_broadcast(128)), w=['rb'], dma=True)
            S.op('pool', lambda e: e.dma_start(out=lstr[:], in_=cst['lstr'][:, :]), w=['lstr'], dma=True)
            S.op('sp', lambda e: e.dma_start(out=ebase[:], in_=cst['ebase'][:, :]), w=['ebase'], dma=True)
            S.op('dve', lambda e: e.memset(onesb[:], 1.0), w=['onesb'])
            S.op('dve', lambda e: e.memset(zt[:], 0.0), w=['zt'])
            for ex_i in range(16):
                S.op('sp', lambda e: e.dma_start(out=xbuf[ex_i * C:(ex_i + 1) * C, :].rearrange("(a p) d -> p a d", p=128),
                                                 in_=zt[:].unsqueeze(1).to_broadcast([128, CT, D])), r=['zt'], w=[('xz', ex_i)], dma=True)
            lgA = sb("lgA", [128, NT, 16], F32, s2)
            lbA = sb("lbA", [128, NT, 16], F32, s2)
            eqA = sb("eqA", [128, NT, 16], F32, s2)
            lb2A = sb("lb2A", [128, NT, 16], F32, s2)
            m1A = sb("m1A", [128, NT, 4], F32, s2)
            m2A = sb("m2A", [128, NT, 4], F32, s2)
            gsA = sb("gsA", [128, NT, 4], F32, s2)
            gmA = sb("gmA", [128, NT], F32, s2)
            exA = sb("exA", [128, NT, 16], F32, s2)
            denA = sb("denA", [128, NT], F32, s2)
            gateAll = sb("gateAll", [128, NT, 16], F32, s2)
            smA = sb("smA", [128, NT, 16], F32, s2)
            tAA = sb("tAA", [128, NT, 16], F32, s2)
            tBA = sb("tBA", [128, NT, 16], F32, s2)
            sAA = sb("sAA", [128, NT], F32, s2)
            sBA = sb("sBA", [128, NT], F32, s2)

            def v4(t):
                return t[:].rearrange("p t (g e) -> p t g e", g=4)

            def b3(t, n):
                return t[:].unsqueeze(2).to_broadcast([128, NT, n])

            def b4(t):
                return t[:].unsqueeze(3).to_broadcast([128, NT, 4, 4])
            pR = 0
            for tt in range(NT):
                ts = slice(tt * 128, (tt + 1) * 128)
                for kc in range(16):
                    S.op('pe', lambda e, kc=kc: e.matmul(PS[pR][:, tt * 16:(tt + 1) * 16], lhsT=bufA[:, kc, ts], rhs=rw[:, kc, :], start=(kc == 0), stop=(kc == 15)),
                         r=['bufA', 'rw'], w=[('ps', pR)], inc=(kc == 15))
            fl = lambda t: t[:].rearrange("p t e -> p (t e)")
            S.op('dve', lambda e: e.tensor_copy(out=fl(lgA), in_=PS[pR][:, 0:256]), r=[('ps', pR)], w=['lgA'])
            S.op('dve', lambda e: e.tensor_tensor(out=lbA[:], in0=lgA[:], in1=rb[:].unsqueeze(1).to_broadcast([128, NT, 16]), op=ALU.add), r=['lgA', 'rb'], w=['lbA'])
            S.op('dve', lambda e: e.tensor_reduce(out=m1A[:], in_=v4(lbA), axis=AX.X, op=ALU.max), r=['lbA'], w=['m1A'])
            S.op('dve', lambda e: e.tensor_tensor(out=v4(eqA), in0=v4(lbA), in1=b4(m1A), op=ALU.is_equal), r=['lbA', 'm1A'], w=['eqA'])
            S.op('dve', lambda e: e.scalar_tensor_tensor(out=lb2A[:], in0=eqA[:], scalar=-1e30, in1=lbA[:], op0=ALU.mult, op1=ALU.add), r=['eqA', 'lbA'], w=['lb2A'])
            S.op('dve', lambda e: e.tensor_reduce(out=m2A[:], in_=v4(lb2A), axis=AX.X, op=ALU.max), r=['lb2A'], w=['m2A'])
            S.op('dve', lambda e: e.tensor_tensor(out=gsA[:], in0=m1A[:], in1=m2A[:], op=ALU.add), r=['m1A', 'm2A'], w=['gsA'])
            S.op('dve', lambda e: e.tensor_reduce(out=gmA[:], in_=gsA[:], axis=AX.X, op=ALU.max), r=['gsA'], w=['gmA'])
            S.op('dve', lambda e: e.tensor_tensor(out=gsA[:], in0=gsA[:], in1=b3(gmA, 4), op=ALU.is_equal), r=['gsA', 'gmA'], w=['gsA'])
            S.op('dve', lambda e: e.tensor_tensor(out=v4(eqA), in0=v4(lbA), in1=b4(m2A), op=ALU.is_ge), r=['lbA', 'm2A'], w=['eqA'])
            S.op('dve', lambda e: e.tensor_tensor(out=v4(eqA), in0=v4(eqA), in1=b4(gsA), op=ALU.mult), r=['eqA', 'gsA'], w=['eqA'])
            S.op('act', lambda e: e.activation(out=exA[:], in_=lgA[:], func=AF.Exp), r=['lgA'], w=['exA'])
            S.op('dve', lambda e: e.tensor_tensor(out=exA[:], in0=exA[:], in1=eqA[:], op=ALU.mult), r=['exA', 'eqA'], w=['exA'])
            S.op('dve', lambda e: e.tensor_reduce(out=denA[:], in_=exA[:], axis=AX.X, op=ALU.add), r=['exA'], w=['denA'])
            S.op('dve', lambda e: e.reciprocal(out=denA[:], in_=denA[:]), r=['denA'], w=['denA'])
            S.op('dve', lambda e: e.tensor_tensor(out=gateAll[:], in0=exA[:], in1=b3(denA, 16), op=ALU.mult), r=['exA', 'denA'], w=['gateAll'])
            S.op('act', lambda e: e.activation(out=maskall[:], in_=eqA[:], func=AF.Copy), r=['eqA'], w=['maskall'])
            pP = 1
            for tt in range(NT):
                for t2 in range(tt):
                    S.op('pe', lambda e, t2=t2: e.matmul(PS[pP][:, tt * 16:(tt + 1) * 16], lhsT=onesb[:], rhs=maskall[:, t2, :], start=(t2 == 0), stop=False),
                         r=['onesb', 'maskall'], w=[('ps', pP)], inc=False)
                S.op('pe', lambda e: e.matmul(PS[pP][:, tt * 16:(tt + 1) * 16], lhsT=lstr[:], rhs=maskall[:, tt, :], start=(tt == 0), stop=True),
                     r=['lstr', 'maskall'], w=[('ps', pP)], inc=True)
            S.op('dve', lambda e: e.tensor_tensor(out=smA[:], in0=PS[pP][:, 0:256].rearrange("p (t e) -> p t e", t=NT), in1=ebase[:].unsqueeze(1).to_broadcast([128, NT, 16]), op=ALU.add),
                 r=[('ps', pP), 'ebase'], w=['smA'])
            S.op('dve', lambda e: e.scalar_tensor_tensor(out=tAA[:], in0=eqA[:], scalar=-MOE_BIG, in1=smA[:], op0=ALU.mult, op1=ALU.add), r=['eqA', 'smA'], w=['tAA'])
            S.op('dve', lambda e: e.tensor_reduce(out=sAA[:], in_=tAA[:], axis=AX.X, op=ALU.min), r=['tAA'], w=['sAA'])
            S.op('dve', lambda e: e.tensor_tensor(out=tBA[:], in0=tAA[:], in1=eqA[:], op=ALU.mult), r=['tAA', 'eqA'], w=['tBA'])
            S.op('dve', lambda e: e.tensor_reduce(out=sBA[:], in_=tBA[:], axis=AX.X, op=ALU.max), r=['tBA'], w=['sBA'])
            S.op('dve', lambda e: e.tensor_tensor(out=tBA[:], in0=tAA[:], in1=b3(sAA, 16), op=ALU.is_equal), r=['tAA', 'sAA'], w=['tBA'])
            S.op('dve', lambda e: e.tensor_tensor(out=tBA[:], in0=tBA[:], in1=gateAll[:], op=ALU.mult), r=['tBA', 'gateAll'], w=['tBA'])
            S.op('dve', lambda e: e.tensor_reduce(out=gateA[:], in_=tBA[:], axis=AX.X, op=ALU.add), r=['tBA'], w=['gateA'])
            S.op('dve', lambda e: e.tensor_copy(out=slotAi[:], in_=sAA[:]), r=['sAA'], w=['slotAi'])
            S.op('dve', lambda e: e.tensor_copy(out=slotBi[:], in_=sBA[:]), r=['sBA'], w=['slotBi'])
            for tt in range(NT):
                ts = slice(tt * 128, (tt + 1) * 128)
                b = tt % 2
                S.op('sp', lambda e: e.dma_start(out=xtf[b][:], in_=xres[ts, :]), r=[('xres', tt)], w=[('mxtf', b)], dma=True)
                S.op('act', lambda e: e.activation(out=xtb[b][:], in_=xtf[b][:], func=AF.Copy), r=[('mxtf', b)], w=[('mxtb', b)])
                for sl, sk in ((slotAi, 'slotAi'), (slotBi, 'slotBi')):
                    S.op('pool', lambda e: e.indirect_dma_start(out=xbuf[:, :], out_offset=bass.IndirectOffsetOnAxis(ap=sl[:, tt:tt + 1], axis=0),
                                                                in_=xtb[b][:, :], in_offset=None, bounds_check=breg, oob_is_err=False),
                         r=[('mxtb', b), sk] + [('xz', q) for q in range(16)], w=[('xsc', tt, sk)], dma=True)
            S.barrier()
        with ExitStack() as s2:
            _H['alloc_wt'](s2, 4)
            xe = [sb("xe%d" % i, [128, D], BF16, s2) for i in range(2)]
            xeT = sb("xeT", [128, 16, C], BF16, s2)
            actT = sb("actT", [128, 12, C], BF16, s2)
            ystg = [sb("ystg%d" % i, [128, 512], F32, s2) for i in range(3)]
            yi = [0]
            xi = [0]
            for ex_i in range(0 if 'moe2' in SKIP else 16):
                Wi = dr["moe_w_in"][l, ex_i]
                Wd = dr["moe_w_down"][l, ex_i]
                for sti in range(CT):
                    b = xi[0] % 2
                    xi[0] += 1
                    r0 = ex_i * C + sti * 128
                    S.op('sp', lambda e: e.dma_start(out=xe[b][:], in_=xbuf[r0:r0 + 128, :]), w=[('xe', b)], dma=True)
                    transpose_to(xe[b], ('xe', b), 16, xeT, 'xeT', sti, BF16)
                for j in range(6):
                    wb = load_w(Wi, 0, 16, 512 * j, 512)
                    for m in range(4):
                        pi = next_ps()
                        for kc in range(16):
                            S.op('pe', lambda e, kc=kc: e.matmul(PS[pi][:, 0:C], lhsT=Wt[wb][:, kc, m * 128:(m + 1) * 128], rhs=xeT[:, kc, :], start=(kc == 0), stop=(kc == 15)),
                                 r=[('Wt', wb), 'xeT'], w=[('ps', pi)], inc=(kc == 15))
                        fc = (j * 4 + m) % 12
                        if j < 3:
                            S.op('act', lambda e: e.activation(out=actT[:, fc, :], in_=PS[pi][:, 0:C], func=AF.Silu), r=[('ps', pi)], w=[('actT', fc)])
                        else:
                            S.op('dve', lambda e: e.tensor_tensor(out=actT[:, fc, :], in0=actT[:, fc, :], in1=PS[pi][:, 0:C], op=ALU.mult), r=[('ps', pi), ('actT', fc)], w=[('actT', fc)])
                for cb in range(4):
                    wb = load_w(Wd, 0, 12, 512 * cb, 512)
                    for sti in range(CT):
                        pi = next_ps()
                        for fc in range(12):
                            S.op('pe', lambda e, fc=fc: e.matmul(PS[pi][:, :], lhsT=actT[:, fc, sti * 128:(sti + 1) * 128], rhs=Wt[wb][:, fc, :], start=(fc == 0), stop=(fc == 11)),
                                 r=[('Wt', wb), ('actT', fc)], w=[('ps', pi)], inc=(fc == 11))
                        i = yi[0] % 3
                        yi[0] += 1
                        S.op('act', lambda e: e.activation(out=ystg[i][:], in_=PS[pi][:, :], func=AF.Copy), r=[('ps', pi)], w=[('ystg', i)])
                        r0 = ex_i * C + sti * 128
                        S.op('sp', lambda e: e.dma_start(out=ybuf[r0:r0 + 128, cb * 512:(cb + 1) * 512], in_=ystg[i][:]), r=[('ystg', i)], w=['ybuf'], dma=True)
            S.barrier()
        with ExitStack() as s2:
            yA = sb("yA", [128, D], F32, s2)
            yB = sb("yB", [128, D], F32, s2)

            def yfn(tt):
                S.op('pool', lambda e: e.indirect_dma_start(out=yA[:, :], out_offset=None, in_=ybuf[:, :],
                                                            in_offset=bass.IndirectOffsetOnAxis(ap=slotAi[:, tt:tt + 1], axis=0), bounds_check=breg, oob_is_err=False),
                     r=['slotAi'], w=['yA'], dma=True)
                S.op('pool', lambda e: e.indirect_dma_start(out=yB[:, :], out_offset=None, in_=ybuf[:, :],
                                                            in_offset=bass.IndirectOffsetOnAxis(ap=slotBi[:, tt:tt + 1], axis=0), bounds_check=breg, oob_is_err=False),
                     r=['slotBi'], w=['yB'], dma=True)
                S.op('dve', lambda e: e.tensor_tensor(out=yA[:], in0=yA[:], in1=yB[:], op=ALU.subtract), r=['yA', 'yB'], w=['yA'])
                S.op('dve', lambda e: e.scalar_tensor_tensor(out=yA[:], in0=yA[:], scalar=gateA[:, tt:tt + 1], in1=yB[:], op0=ALU.mult, op1=ALU.add),
                     r=['yA', 'yB', 'gateA'], w=['yA'])
                return yA, 'yA'
            _resid_ln_phase(nc, S, sb, PS, s2, l, None, 0, yfn, None, xres, "ln_ffn_g", "ln_ffn_b", dr, dst, bufA, lambda tt: 'bufA', transpose_to, None, nbuf=2)
            S.barrier()


def _nsa(nc, S, sb, PS, PSB, dr, cst, l, bufA, hT_d, h_d, transpose_to, next_ps, ident, identb, dbg):
    slopes = [2.0 ** (-8.0 * (h + 1) / 16.0) for h in range(16)]
    with ExitStack() as st:
        rel = sb("rel", [128, 2048], F32, st)
        cdiag = sb("cdiag", [128, 128], F32, st)
        cfar = sb("cfar", [128, 128], F32, st)
        dcon = sb("dcon", [128, 272], F32, st)
        cpb = sb("cpb", [128, 256], F32, st)
        cmask = sb("cmask", [128, 2048], BF16, st)
        selc = sb("selc", [128, NT, 32], F32, st)
        expd = sb("expd", [32, 2048], BF16, st)
        ng = sb("ng", [128, NT, 48], BF16, st)
        kcT = sb("kcT", [64, 4, 128], BF16, st)
        vca = sb("vca", [128, 4, 97], BF16, st)
        S.op('sp', lambda e: e.dma_start(out=rel[:], in_=cst['rel_mid'][:, :]), w=['rel'], dma=True)
        S.op('sp', lambda e: e.dma_start(out=cdiag[:], in_=cst['cdiag'][:, :]), w=['cdiag'], dma=True)
        S.op('sp', lambda e: e.dma_start(out=cfar[:], in_=cst['cfar'][:, :]), w=['cfar'], dma=True)
        S.op('sp', lambda e: e.dma_start(out=dcon[:], in_=cst['dconst'][:, :]), w=['dcon'], dma=True)
        S.op('sp', lambda e: e.dma_start(out=cpb[:], in_=cst['cmp_pb'][:, :]), w=['cpb'], dma=True)
        S.op('pool', lambda e: e.dma_start(out=cmask[:], in_=cst['cmp_mask'][:, :]), w=['cmask'], dma=True)
        S.op('sp', lambda e: e.dma_start(out=selc[:], in_=cst['selc'].rearrange("(tt p) j -> p tt j", p=128)), w=['selc'], dma=True)
        S.op('pool', lambda e: e.dma_start(out=expd[:], in_=cst['expand'][:, :]), w=['expd'], dma=True)
        S.op('sp', lambda e: e.dma_start(out=ng[:], in_=h_d[:, TC_NG:TC_NG + 48].rearrange("(tt p) j -> p tt j", p=128)), w=['ng'], dma=True)
        S.op('dve', lambda e: e.memset(kcT[:], 0.0), w=['kcT'])
        S.op('dve', lambda e: e.memset(vca[:], 0.0), w=['vca'])
        with ExitStack() as s2:
            w1 = sb("w1", [64, 2, 32, 256], BF16, s2)
            w2 = sb("w2", [128, 2, 2, 64], BF16, s2)
            pes = sb("pes", [32, 2, 64], F32, s2)
            peT = sb("peT", [64, 2, 32], BF16, s2)
            c1 = sb("c1", [128, 2, 2], F32, s2)
            srcT = sb("csrcT", [64, T], BF16, s2)
            u = sb("cu", [128, 128], F32, s2)
            t1 = sb("ct1", [128, 128], F32, s2)
            gel = sb("cgel", [128, 2, 128], BF16, s2)
            ovl = sb("ovl", [128, 32], F32, s2)
            for kv in range(2):
                S.op('pool', lambda e: e.dma_start(out=w1[:, kv, :, :], in_=dr["nsa_cmp_w1"][l, kv].rearrange("(l d) h -> d l h", d=64)), w=['w1'], dma=True)
                S.op('pool', lambda e: e.dma_start(out=w2[:, kv, :, :], in_=dr["nsa_cmp_w2"][l, kv].rearrange("(hc p) d -> p hc d", p=128)), w=['w2'], dma=True)
            S.op('sp', lambda e: e.dma_start(out=pes[:], in_=dr["nsa_cmp_pe"][l].rearrange("k l d -> l k d")), w=['pes'], dma=True)
            S.op('sp', lambda e: e.dma_start(out=ovl[:], in_=cst['overlap'][:, :]), w=['ovl'], dma=True)
            for g in range(4):
                S.op('dve', lambda e: e.memset(vca[:, g, 64:65], 1.0), r=[], w=['vca'])
                S.op('dve', lambda e: e.tensor_copy(out=vca[:, g, 65:97], in_=ovl[:]), r=['ovl'], w=['vca'])
            for kv in range(2):
                S.op('pe', lambda e: e.transpose(out=PS[0][0:64, 0:32], in_=pes[0:32, kv, :], identity=ident[0:32, 0:32]), r=['pes', 'ident'], w=[('ps', 0)])
                S.op('act', lambda e: e.activation(out=peT[:, kv, :], in_=PS[0][0:64, 0:32], func=AF.Copy), r=[('ps', 0)], w=['peT'])
                for hc in range(2):
                    for li in range(32):
                        S.op('pe', lambda e, li=li: e.matmul(PS[1][:, 0:1], lhsT=w1[:, kv, li, hc * 128:(hc + 1) * 128], rhs=peT[:, kv, li:li + 1], start=(li == 0), stop=(li == 31)),
                             r=['w1', 'peT'], w=[('ps', 1)], inc=(li == 31))
                    S.op('act', lambda e: e.activation(out=c1[:, kv, hc:hc + 1], in_=PS[1][:, 0:1], func=AF.Copy), r=[('ps', 1)], w=['c1'])
            for g in range(4):
                for kv in range(2):
                    r0 = (R_KC if kv == 0 else R_VC) + g * 64
                    S.op('sp', lambda e: e.dma_start(out=srcT[:], in_=hT_d[r0:r0 + 64, :]), w=['csrcT'], dma=True)
                    for hc in range(2):
                        pi = next_ps()
                        for li in range(32):
                            S.op('pe', lambda e, li=li: e.matmul(PS[pi][:, 0:127], lhsT=w1[:, kv, li, hc * 128:(hc + 1) * 128], rhs=srcT[:, li:li + 16 * 126 + 1:16],
                                                                 start=(li == 0), stop=(li == 31)), r=['w1', 'csrcT'], w=[('ps', pi)], inc=(li == 31))
                        S.op('act', lambda e: e.activation(out=u[:, 0:127], in_=PS[pi][:, 0:127], func=AF.Identity, bias=c1[:, kv, hc:hc + 1], scale=1.0), r=[('ps', pi), 'c1'], w=['cu'])
                        S.op('dve', lambda e: e.tensor_tensor(out=t1[:, 0:127], in0=u[:, 0:127], in1=u[:, 0:127], op=ALU.mult), r=['cu'], w=['ct1'])
                        S.op('dve', lambda e: e.tensor_scalar(out=t1[:, 0:127], in0=t1[:, 0:127], scalar1=0.044715, scalar2=1.0, op0=ALU.mult, op1=ALU.add), r=['ct1'], w=['ct1'])
                        S.op('dve', lambda e: e.tensor_tensor(out=t1[:, 0:127], in0=t1[:, 0:127], in1=u[:, 0:127], op=ALU.mult), r=['ct1', 'cu'], w=['ct1'])
                        S.op('act', lambda e: e.activation(out=t1[:, 0:127], in_=t1[:, 0:127], func=AF.Sigmoid, scale=2.0 * 0.7978845608028654), r=['ct1'], w=['ct1'])
                        S.op('dve', lambda e: e.tensor_tensor(out=gel[:, hc, 0:127], in0=t1[:, 0:127], in1=u[:, 0:127], op=ALU.mult), r=['ct1', 'cu'], w=['cgel'])
                    pi = next_ps()
                    if kv == 0:
                        for hc in range(2):
                            S.op('pe', lambda e, hc=hc: e.matmul(PS[pi][0:64, 0:127], lhsT=w2[:, 0, hc, :], rhs=gel[:, hc, 0:127], start=(hc == 0), stop=(hc == 1)),
                                 r=['w2', 'cgel'], w=[('ps', pi)], inc=(hc == 1))
                        S.op('act', lambda e: e.activation(out=kcT[:, g, 0:127], in_=PS[pi][0:64, 0:127], func=AF.Copy), r=[('ps', pi)], w=['kcT'])
                    else:
                        for hc in range(2):
                            S.op('pe', lambda e, hc=hc: e.matmul(PS[pi][0:127, 0:64], lhsT=gel[:, hc, 0:127], rhs=w2[:, 1, hc, :], start=(hc == 0), stop=(hc == 1)),
                                 r=['w2', 'cgel'], w=[('ps', pi)], inc=(hc == 1))
                        S.op('act', lambda e: e.activation(out=vca[0:127, g, 0:64], in_=PS[pi][0:127, 0:64], func=AF.Copy), r=[('ps', pi)], w=['vca'])
            S.barrier()
        dtmax = []
        for h in range(16):
            d = 1
            while d < 15 and slopes[h] * (128 * (d + 1) - 127) <= 40.0:
                d += 1
            dtmax.append(d)
        qT = sb("nqT", [64, 4, T], BF16, st)
        ksT = sb("nksT", [64, T], BF16, st)
        kwT = sb("nkwT", [64, T], BF16, st)
        vs = sb("nvs", [128, NT, 65], BF16, st)
        vw = sb("nvw", [128, NT, 65], BF16, st)
        NSC = 4
        sc = [sb("nsc%d" % i, [128, 512], F32, st) for i in range(NSC)]
        eTs = sb("neTs", [128, NT, 512], BF16, st)
        eTw = sb("neTw", [128, 5, 512], BF16, st)
        eTc = [sb("neTc%d" % i, [128, 512], BF16, st) for i in range(2)]
        imp = [sb("nimp%d" % i, [128, 32], F32, st) for i in range(2)]
        mx8 = [sb("nmx8%d" % i, [128, 8], F32, st) for i in range(2)]
        selb = [sb("nselb%d" % i, [128, 32], F32, st) for i in range(2)]
        selbT = sb("nselbT", [32, NT, 128], BF16, st)
        rd4 = [sb("nrd%d" % i, [128, 4], F32, st) for i in range(3)]
        oc = sb("noc", [128, NT, 256], F32, st)
        octmp = [sb("noctmp%d" % i, [128, 256], F32, st) for i in range(2)]
        ocb = [sb("nocb%d" % i, [128, 256], BF16, st) for i in range(2)]
        sci = [0]
        PB_SEL, PB_WIN, PB_CMP, PB_TR = 4, 5, 6, 7
        relgd = sb("relgd", [128, 2, 512], F32, st)
        RGI = {0: 0, 4: 1}
        btab = sb("btab", [6, NT * 512], BF16, st)
        l6 = sb("l6", [6, 128], BF16, st)
        S.op('pool', lambda e: e.dma_start(out=l6[:], in_=cst['l6'][:, :]), w=['l6'], dma=True)

        for g in range(4):
            S.op('sp', lambda e: e.dma_start(out=qT[:], in_=hT_d[R_NQ + g * 256:R_NQ + (g + 1) * 256, :].rearrange("(h d) t -> d h t", d=64)), w=['nqT'], dma=True)
            S.op('sp', lambda e: e.dma_start(out=ksT[:], in_=hT_d[R_KS + g * 64:R_KS + (g + 1) * 64, :]), w=['nksT'], dma=True)
            S.op('sp', lambda e: e.dma_start(out=kwT[:], in_=hT_d[R_KW + g * 64:R_KW + (g + 1) * 64, :]), w=['nkwT'], dma=True)
            S.op('dve', lambda e: e.memset(vs[:], 1.0), w=['nvs'])
            S.op('dve', lambda e: e.memset(vw[:], 1.0), w=['nvw'])
            S.op('sp', lambda e: e.dma_start(out=vs[:, :, 0:64], in_=h_d[:, TC_VS + g * 64:TC_VS + (g + 1) * 64].rearrange("(kt p) d -> p kt d", p=128)), w=['nvs'], dma=True)
            S.op('sp', lambda e: e.dma_start(out=vw[:, :, 0:64], in_=h_d[:, TC_VW + g * 64:TC_VW + (g + 1) * 64].rearrange("(kt p) d -> p kt d", p=128)), w=['nvw'], dma=True)

            for q4 in range(4):
                S.op('pool', lambda e: e.dma_start(out=btab[:, q4 * 2048:(q4 + 1) * 2048], in_=cst['btab'][:, g * 8192 + q4 * 2048:g * 8192 + (q4 + 1) * 2048]), w=['btab'], dma=True)
            for dt_ in (0, 4):
                for h in range(4):
                    hh = 4 * g + h
                    S.op('act', lambda e: e.activation(out=relgd[:, RGI[dt_], h * 128:(h + 1) * 128], in_=rel[:, hh * 128:(hh + 1) * 128], func=AF.Identity,
                                                       bias=dcon[:, hh * 17 + dt_:hh * 17 + dt_ + 1], scale=1.0), r=['rel', 'dcon'], w=[('relgd', dt_)])

            def scores(tt, lhs, kkey, kt, mode, use_sel, edst, ekey, extra_mask, heads):
                ts = slice(tt * 128, (tt + 1) * 128)
                pi = next_ps(0, 4)
                pv = PS[pi][:, :].rearrange("p (h j) -> p h j", h=4)
                pebias = (mode != 'cmp') and (extra_mask is None)
                S.op('pe', lambda e: e.matmul(pv, lhsT=lhs, rhs=qT[:, :, ts], start=True, stop=not (use_sel or pebias)), r=[kkey, 'nqT'], w=[('ps', pi)], inc=not (use_sel or pebias))
                if use_sel:
                    S.op('pe', lambda e: e.matmul(pv, lhsT=expd[:, kt * 128:(kt + 1) * 128], rhs=selbT[:, tt, :].unsqueeze(1).to_broadcast([32, 4, 128]), start=False, stop=not pebias),
                         r=['expd', ('nselbT', tt)], w=[('ps', pi)], inc=not pebias)
                if pebias:
                    d_ = tt - kt
                    S.op('pe', lambda e: e.matmul(PS[pi][:, :], lhsT=l6[:, :], rhs=btab[:, d_ * 512:(d_ + 1) * 512], start=False, stop=True), r=['l6', 'btab'], w=[('ps', pi)], inc=True)
                    S.op('act', lambda e: e.activation(out=edst, in_=PS[pi][:, :], func=AF.Exp), r=[('ps', pi)], w=[(ekey, h) for h in range(4)], inc=True)
                    return
                si = sci[0] % NSC
                sci[0] += 1
                if mode == 'cmp':
                    S.op('dve', lambda e: e.tensor_tensor(out=sc[si][:], in0=PS[pi][:, :], in1=rel[:, g * 512:(g + 1) * 512], op=ALU.add), r=[('ps', pi), 'rel'], w=[('nsc', si)])
                else:
                    S.op('dve', lambda e: e.tensor_tensor(out=sc[si][:], in0=PS[pi][:, :], in1=relgd[:, RGI[tt - kt], :], op=ALU.add), r=[('ps', pi), ('relgd', tt - kt)], w=[('nsc', si)])
                if extra_mask is not None:
                    mk, mkey = extra_mask
                    S.op('dve', lambda e: e.tensor_tensor(out=sc[si][:].rearrange("p (h j) -> p h j", h=4), in0=sc[si][:].rearrange("p (h j) -> p h j", h=4),
                                                           in1=mk.unsqueeze(1).to_broadcast([128, 4, 128]), op=ALU.add), r=[('nsc', si), mkey], w=[('nsc', si)])
                if mode == 'cmp':
                    for h in heads:
                        hh = 4 * g + h
                        bia = cpb[:, hh * 16 + tt:hh * 16 + tt + 1]
                        S.op('act', lambda e, h=h: e.activation(out=edst[:, h * 128:(h + 1) * 128], in_=sc[si][:, h * 128:(h + 1) * 128], func=AF.Exp, bias=bia, scale=1.0),
                             r=[('nsc', si), 'cpb'], w=[(ekey, h)], inc=True)
                else:
                    S.op('act', lambda e: e.activation(out=edst, in_=sc[si][:], func=AF.Exp), r=[('nsc', si)], w=[(ekey, h) for h in range(4)], inc=True)

            def pv_combine(tt, pb, per_head, ncol, br, first, ri):
                for h in range(4):
                    lst = per_head[h]
                    for i, (lt, rt, rk) in enumerate(lst):
                        S.op('pe', lambda e, lt=lt, rt=rt, i=i: e.matmul(PS[pb][:, h * 128:h * 128 + ncol], lhsT=lt, rhs=rt, start=(i == 0), stop=(i == len(lst) - 1)),
                             r=rk, w=[('ps', pb)], inc=(i == len(lst) - 1))
                rd = rd4[ri]
                rk_ = ('nrd', ri)
                S.op('dve', lambda e: e.tensor_scalar(out=rd[:], in0=PS[pb][:, 64::128], scalar1=1e-30, scalar2=None, op0=ALU.max), r=[('ps', pb)], w=[rk_])
                S.op('dve', lambda e: e.reciprocal(out=rd[:], in_=rd[:]), r=[rk_], w=[rk_])
                if br == 0:
                    ib = tt % 2
                    for h in range(4):
                        if h == 0:
                            S.op('dve', lambda e: e.tensor_scalar(out=imp[ib][:], in0=PS[pb][:, 65:97], scalar1=rd[:, 0:1], scalar2=None, op0=ALU.mult), r=[('ps', pb), rk_], w=[('nimp', ib)])
                        else:
                            S.op('dve', lambda e, h=h: e.scalar_tensor_tensor(out=imp[ib][:], in0=PS[pb][:, h * 128 + 65:h * 128 + 97], scalar=rd[:, h:h + 1], in1=imp[ib][:],
                                                                              op0=ALU.mult, op1=ALU.add), r=[('ps', pb), rk_, ('nimp', ib)], w=[('nimp', ib)])
                c0 = 12 * g + br
                S.op('dve', lambda e: e.tensor_tensor(out=rd[:], in0=rd[:], in1=ng[:, tt, c0:c0 + 10:3], op=ALU.mult), r=[rk_, 'ng'], w=[rk_])
                psv = PS[pb][:, :].rearrange("p (h c) -> p h c", h=4)[:, :, 0:64]
                wbc = rd[:].unsqueeze(2).to_broadcast([128, 4, 64])
                ocv = oc[:, tt, :].rearrange("p (h c) -> p h c", h=4)
                if first:
                    S.op('dve', lambda e: e.tensor_tensor(out=ocv, in0=psv, in1=wbc, op=ALU.mult), r=[('ps', pb), rk_], w=[('noc', tt)])
                else:
                    ti = (tt + br) % 2
                    S.op('dve', lambda e: e.tensor_tensor(out=octmp[ti][:].rearrange("p (h c) -> p h c", h=4), in0=psv, in1=wbc, op=ALU.mult), r=[('ps', pb), rk_], w=[('noctmp', ti)])
                    S.op('dve', lambda e: e.tensor_tensor(out=oc[:, tt, :], in0=oc[:, tt, :], in1=octmp[ti][:], op=ALU.add), r=[('noctmp', ti), ('noc', tt)], w=[('noc', tt)])

            def cmp_scores(tt):
                ts = slice(tt * 128, (tt + 1) * 128)
                cb_ = tt % 2
                scores(tt, kcT[:, g, :], 'kcT', 0, 'cmp', False, eTc[cb_], ('neTc', cb_), (cmask[:, ts], 'cmask'), range(4))

            def cmp_pv(tt):
                cb_ = tt % 2
                ph = [[(eTc[cb_][:, h * 128:(h + 1) * 128], vca[:, g, 0:97], [(('neTc', cb_), h), 'vca'])] for h in range(4)]
                pv_combine(tt, PB_CMP, ph, 97, 0, True, 0)
                ib = tt % 2
                S.op('dve', lambda e: e.tensor_tensor(out=imp[ib][:], in0=imp[ib][:], in1=selc[:, tt, :], op=ALU.add), r=[('nimp', ib), 'selc'], w=[('nimp', ib)])
                S.op('dve', lambda e: e.max(out=mx8[ib][:], in_=imp[ib][:]), r=[('nimp', ib)], w=[('nmx8', ib)])
                S.op('dve', lambda e: e.tensor_scalar(out=mx8[ib][:, 7:8], in0=mx8[ib][:, 7:8], scalar1=-5e29, scalar2=None, op0=ALU.max), r=[('nmx8', ib)], w=[('nmx8', ib)])
                S.op('dve', lambda e: e.tensor_scalar(out=selb[ib][:], in0=imp[ib][:], scalar1=mx8[ib][:, 7:8], scalar2=NEGB, op0=ALU.is_lt, op1=ALU.mult),
                     r=[('nimp', ib), ('nmx8', ib)], w=[('nselb', ib)])
                S.op('pe', lambda e: e.transpose(out=PS[PB_TR][0:32, 0:128], in_=selb[ib][:, :], identity=ident[:]), r=[('nselb', ib), 'ident'], w=[('ps', PB_TR)])
                S.op('act', lambda e: e.activation(out=selbT[:, tt, :], in_=PS[PB_TR][0:32, 0:128], func=AF.Copy), r=[('ps', PB_TR)], w=[('nselbT', tt)])

            cmp_scores(0)
            for tt in range(NT):
                if tt + 1 < NT:
                    cmp_scores(tt + 1)
                cmp_pv(tt)
            gd = max(dtmax[4 * g:4 * g + 4])

            def win_scores(tt):
                for kt in range(max(0, tt - 4), tt + 1):
                    em = (cdiag[:], 'cdiag') if kt == tt else ((cfar[:], 'cfar') if kt == tt - 4 else None)
                    hs = [h for h in range(4) if tt - kt <= dtmax[4 * g + h]]
                    scores(tt, kwT[:, kt * 128:(kt + 1) * 128], 'nkwT', kt, 'rel', False, eTw[:, tt - kt, :], ('neTw', tt - kt), em, hs)

            def win_pv(tt):
                kts = list(range(max(0, tt - 4), tt + 1))
                ph = [[(eTw[:, tt - kt, h * 128:(h + 1) * 128], vw[:, kt, :], [(('neTw', tt - kt), h), 'nvw']) for kt in kts if tt - kt <= dtmax[4 * g + h]] for h in range(4)]
                pv_combine(tt, PB_WIN, ph, 65, 2, False, 1)

            def sel_scores(tt):
                for kt in range(max(0, tt - gd), tt + 1):
                    hs = [h for h in range(4) if tt - kt <= dtmax[4 * g + h]]
                    scores(tt, ksT[:, kt * 128:(kt + 1) * 128], 'nksT', kt, 'rel', True, eTs[:, kt, :], ('neTs', kt), (cdiag[:], 'cdiag') if kt == tt else None, hs)

            def sel_pv(tt):
                kts = list(range(max(0, tt - gd), tt + 1))
                ph = [[(eTs[:, kt, h * 128:(h + 1) * 128], vs[:, kt, :], [(('neTs', kt), h), 'nvs']) for kt in kts if tt - kt <= dtmax[4 * g + h]] for h in range(4)]
                pv_combine(tt, PB_SEL, ph, 65, 1, False, 2)
                ob = tt % 2
                S.op('act', lambda e: e.activation(out=ocb[ob][:], in_=oc[:, tt, :], func=AF.Copy), r=[('noc', tt)], w=[('nocb', ob)])
                transpose_to(ocb[ob], ('nocb', ob), 2, bufA, 'bufA', tt, BF16, c_off=8 + 2 * g, banks=(PB_TR, PB_TR + 1))

            jobs = []
            for tt in range(NT):
                jobs.append((win_scores, win_pv, tt))
                jobs.append((sel_scores, sel_pv, tt))
            jobs[0][0](jobs[0][2])
            for ji, (fs, fp, tt) in enumerate(jobs):
                if ji + 1 < len(jobs):
                    jobs[ji + 1][0](jobs[ji + 1][2])
                fp(tt)
        S.barrier()


_NC_CACHE = {}


def kernel(**inputs):
    n = 8
    if "nc" not in _NC_CACHE:
        _NC_CACHE["nc"] = build()
    nc = _NC_CACHE["nc"]
    consts = make_consts()
    in_maps = []
    for c in range(n):
        m = {"x": np.ascontiguousarray(inputs["x"][c], dtype=np.float32), "mem": np.ascontiguousarray(inputs["mem"][c], dtype=np.float32)}
        for k in WNAMES:
            m[k] = np.ascontiguousarray(inputs[k], dtype=np.float32)
        for k, v in consts.items():
            m["c_" + k] = v
        in_maps.append(m)
    res = run_bass_kernel_spmd(nc, in_maps, core_ids=list(range(n)))
    return np.stack([res.results[c]["out"] for c in range(n)], axis=0).astype(np.float32)
```
